# Optimizing a Trainium2 kernel written in Bass

```python
import jax, jax.numpy as jnp
from jax import lax
import numpy as np

D_MODEL = 2048
BATCH = 8
SEQ = 4096
DEPTH = 2

D_MIX = D_MODEL
W_ATTN = D_MIX // 4
W_CONV = D_MIX // 4
W_HGRN = D_MIX // 4
W_RET = D_MIX - W_ATTN - W_CONV - W_HGRN

HEAD_DIM = 64
N_ATTN_HEADS = W_ATTN // HEAD_DIM
N_KV_HEADS = 2
WINDOW = 128
ATTN_BLOCK = 128

CONV_WIDTH = 3

HGRN_HEADS = 4
HGRN_DK = W_HGRN // HGRN_HEADS
HGRN_DV = W_HGRN // HGRN_HEADS
HGRN_CHUNK = 64

RET_HEADS = 4
RET_DK = W_RET // RET_HEADS
RET_DV = W_RET // RET_HEADS
RET_CHUNK = 128

N_EXPERTS = 16
N_GROUPS = 4
EXPERTS_PER_GROUP = N_EXPERTS // N_GROUPS
TOP_K = 2
D_EXPERT = 1024

DEEPNORM_ALPHA = (2.0 * DEPTH) ** 0.25
DEEPNORM_BETA = (8.0 * DEPTH) ** -0.25
LN_EPS = 1e-5
HEAD_NORM_EPS = 1e-6

SPLIT_SIZES = (
    N_ATTN_HEADS * HEAD_DIM,
    N_KV_HEADS * HEAD_DIM,
    N_KV_HEADS * HEAD_DIM,
    W_CONV,
    W_CONV,
    W_CONV,
    HGRN_HEADS * HGRN_DK,
    HGRN_HEADS * HGRN_DK,
    HGRN_HEADS * HGRN_DK,
    HGRN_HEADS * HGRN_DV,
    W_HGRN,
    RET_HEADS * RET_DK,
    RET_HEADS * RET_DK,
    RET_HEADS * RET_DV,
    W_RET,
)
D_IN_PROJ = sum(SPLIT_SIZES)
SPLIT_POINTS = tuple(sum(SPLIT_SIZES[:i + 1]) for i in range(len(SPLIT_SIZES) - 1))

kernel_name = "hybrid_parallel_groups_deepnorm_grouped_moe"


def layer_norm(x, g, b):
    xf = x.astype(jnp.float32)
    mu = xf.mean(-1, keepdims=True)
    var = jnp.square(xf - mu).mean(-1, keepdims=True)
    return ((xf - mu) * lax.rsqrt(var + LN_EPS)).astype(x.dtype) * g + b


def rms_norm_heads(o, g):
    o = o * lax.rsqrt(jnp.square(o).mean(-1, keepdims=True) + HEAD_NORM_EPS)
    return o.reshape(o.shape[0], o.shape[1], -1) * g.astype(jnp.float32)


def group_norm_heads(o, g):
    o = o - o.mean(-1, keepdims=True)
    o = o * lax.rsqrt(jnp.square(o).mean(-1, keepdims=True) + HEAD_NORM_EPS)
    return o.reshape(o.shape[0], o.shape[1], -1) * g.astype(jnp.float32)


def flip_seq(t):
    return jnp.flip(t, axis=1)


def windowed_gqa_attention(q, k, v, sink):
    B, S = q.shape[0], q.shape[1]
    L = ATTN_BLOCK
    nb = S // L
    G = N_ATTN_HEADS // N_KV_HEADS
    qb = q.reshape(B, nb, L, N_KV_HEADS, G, HEAD_DIM)

    def neighbours(t):
        tb = t.reshape(B, nb, L, N_KV_HEADS, HEAD_DIM)
        tp = jnp.pad(tb, ((0, 0), (1, 1), (0, 0), (0, 0), (0, 0)))
        return jnp.concatenate([tp[:, :-2], tp[:, 1:-1], tp[:, 2:]], axis=2)

    kb, vb = neighbours(k), neighbours(v)
    scores = jnp.einsum('bnqhgd,bnkhd->bnhgqk', qb, kb,
                        preferred_element_type=jnp.float32) * (HEAD_DIM ** -0.5)

    k_rel = jnp.arange(3 * L) - L
    dist = k_rel[None, :] - jnp.arange(L)[:, None]
    k_abs = jnp.arange(nb)[:, None] * L + k_rel[None, :]
    valid = (k_abs >= 0) & (k_abs < S)
    mask = (jnp.abs(dist) <= WINDOW)[None] & valid[:, None, :]

    slopes = 2.0 ** (-8.0 * jnp.arange(1, N_ATTN_HEADS + 1, dtype=jnp.float32) / N_ATTN_HEADS)
    alibi = -slopes.reshape(N_KV_HEADS, G)[:, :, None, None] * jnp.abs(dist).astype(jnp.float32)
    scores = jnp.where(mask[None, :, None, None], scores + alibi, -jnp.inf)

    sink_l = sink.astype(jnp.float32).reshape(N_KV_HEADS, G)[None, None, :, :, None, None]
    m = jnp.maximum(scores.max(-1, keepdims=True), sink_l)
    p = jnp.exp(scores - m)
    probs = p / (p.sum(-1, keepdims=True) + jnp.exp(sink_l - m))
    out = jnp.einsum('bnhgqk,bnkhd->bnqhgd', probs.astype(vb.dtype), vb)
    return out.reshape(B, S, N_ATTN_HEADS * HEAD_DIM)


def short_conv_mixer(b_gate, c_gate, h, conv_w):
    u = c_gate * h
    up = jnp.pad(u, ((0, 0), (1, 1), (0, 0)))
    y = conv_w[0] * up[:, :-2] + conv_w[1] * up[:, 1:-1] + conv_w[2] * up[:, 2:]
    return b_gate * y


def hgrn2_chunk_scan(q, k, log_f, v):
    B, S, H, dk = q.shape
    dv = v.shape[-1]
    L = HGRN_CHUNK
    nc = S // L

    def chunks(t):
        return t.reshape(B, nc, L, H, t.shape[-1]).transpose(1, 0, 3, 2, 4)

    causal = jnp.tril(jnp.ones((L, L), dtype=bool))[..., None]

    def step(state, inp):
        qi, ki, gi, vi = inp
        b = jnp.cumsum(gi.astype(jnp.float32), axis=2)
        diff = b[:, :, :, None, :] - b[:, :, None, :, :]
        decay = jnp.exp(jnp.where(causal, diff, -jnp.inf))
        a = jnp.einsum('bhtd,bhsd,bhtsd->bhts', qi, ki, decay)
        o_intra = jnp.einsum('bhts,bhsv->bhtv', a, vi)
        o_inter = jnp.einsum('bhtd,bhdv->bhtv', qi * jnp.exp(b), state)
        b_last = b[:, :, -1:, :]
        k_dec = ki * jnp.exp(b_last - b)
        new_state = jnp.exp(b_last[:, :, 0, :])[..., None] * state + \
            jnp.einsum('bhsd,bhsv->bhdv', k_dec, vi)
        return new_state, o_intra + o_inter

    state0 = jnp.zeros((B, H, dk, dv), jnp.float32)
    _, o = lax.scan(step, state0, (chunks(q), chunks(k), chunks(log_f), chunks(v)))
    return o.transpose(1, 0, 3, 2, 4).reshape(B, S, H, dv)


def hgrn2_mixer(q, z_fwd, z_bwd, i, g, lb, norm_g):
    lb = lb.reshape(HGRN_HEADS, HGRN_DK)

    def gates(z):
        z = z.astype(jnp.float32)
        log_f = jnp.logaddexp(jnp.log(lb), jnp.log1p(-lb) + jax.nn.log_sigmoid(z))
        k = (1.0 - lb) * jax.nn.sigmoid(-z)
        return k, log_f

    k_f, lf_f = gates(z_fwd)
    k_b, lf_b = gates(z_bwd)
    o = hgrn2_chunk_scan(q, k_f, lf_f, i) + flip_seq(
        hgrn2_chunk_scan(flip_seq(q), flip_seq(k_b), flip_seq(lf_b), flip_seq(i)))
    return rms_norm_heads(o, norm_g) * jax.nn.silu(g.astype(jnp.float32))


def retention_chunkwise(q, k, v, log_gamma):
    B, S, H, dk = q.shape
    dv = v.shape[-1]
    L = RET_CHUNK
    nc = S // L
    qc = q.reshape(B, nc, L, H, dk)
    kc = k.reshape(B, nc, L, H, dk)
    vc = v.reshape(B, nc, L, H, dv)
    pos = jnp.arange(L, dtype=jnp.float32)
    rel = pos[:, None] - pos[None, :]
    D = jnp.exp(jnp.where(rel[None] >= 0, log_gamma[:, None, None] * rel[None], -jnp.inf))
    a = jnp.einsum('bnthd,bnshd->bnhts', qc, kc) * D
    o_intra = jnp.einsum('bnhts,bnshv->bnthv', a, vc)
    k_dec = kc * jnp.exp(log_gamma[None, :] * (L - 1 - pos)[:, None])[:, :, None]
    kv = jnp.einsum('bnshd,bnshv->nbhdv', k_dec, vc)
    chunk_decay = jnp.exp(log_gamma * L)[None, :, None, None]

    def step(state, kv_c):
        return chunk_decay * state + kv_c, state

    _, prev = lax.scan(step, jnp.zeros((B, H, dk, dv), jnp.float32), kv)
    q_dec = qc * jnp.exp(log_gamma[None, :] * (pos + 1.0)[:, None])[:, :, None]
    o_inter = jnp.einsum('bnthd,nbhdv->bnthv', q_dec, prev)
    return (o_intra + o_inter).reshape(B, S, H, dv)


def retention_mixer(q, k, v, g, decay_logit, norm_g):
    log_gamma = jax.nn.log_sigmoid(decay_logit.astype(jnp.float32))
    k = k * (RET_DK ** -0.5)
    o = retention_chunkwise(q, k, v, log_gamma[0]) + flip_seq(
        retention_chunkwise(flip_seq(q), flip_seq(k), flip_seq(v), log_gamma[1]))
    return group_norm_heads(o, norm_g) * jax.nn.silu(g.astype(jnp.float32))


def token_mixing(h, w_in, w_out, attn_sink, conv_w, lb, hgrn_norm_g, ret_decay_logit, ret_norm_g):
    B, S, _ = h.shape
    proj = h @ w_in
    (a_q, a_k, a_v, c_b, c_c, c_h, g_q, g_zf, g_zb, g_i, g_o,
     r_q, r_k, r_v, r_g) = jnp.split(proj, SPLIT_POINTS, axis=-1)

    def heads(t, n):
        return t.reshape(B, S, n, -1)

    y_attn = windowed_gqa_attention(heads(a_q, N_ATTN_HEADS), heads(a_k, N_KV_HEADS),
                                    heads(a_v, N_KV_HEADS), attn_sink)
    y_conv = short_conv_mixer(c_b, c_c, c_h, conv_w)
    y_hgrn = hgrn2_mixer(heads(g_q, HGRN_HEADS), heads(g_zf, HGRN_HEADS), heads(g_zb, HGRN_HEADS),
                         heads(g_i, HGRN_HEADS), g_o, lb, hgrn_norm_g)
    y_ret = retention_mixer(heads(r_q, RET_HEADS), heads(r_k, RET_HEADS), heads(r_v, RET_HEADS),
                            r_g, ret_decay_logit, ret_norm_g)
    y = jnp.concatenate([y_attn.astype(h.dtype), y_conv.astype(h.dtype),
                         y_hgrn.astype(h.dtype), y_ret.astype(h.dtype)], axis=-1)
    return y @ w_out


def grouped_moe(h, router_w, router_b, w_gate, w_up, w_down):
    B, S, D = h.shape
    t = h.reshape(-1, D)
    logits = (t @ router_w).astype(jnp.float32) + router_b.astype(jnp.float32)
    probs = jax.nn.softmax(logits, axis=-1)
    grouped = probs.reshape(-1, N_GROUPS, EXPERTS_PER_GROUP)
    group_score = lax.top_k(grouped, TOP_K)[0].sum(-1)
    g_sel = jnp.argmax(group_score, axis=-1)
    in_group = jnp.take_along_axis(grouped, g_sel[:, None, None], axis=1)[:, 0]
    top_p, top_i = lax.top_k(in_group, TOP_K)
    weights = top_p / top_p.sum(-1, keepdims=True)
    expert_ids = g_sel[:, None] * EXPERTS_PER_GROUP + top_i
    combine = (jax.nn.one_hot(expert_ids, N_EXPERTS, dtype=jnp.float32)
               * weights[..., None]).sum(1).astype(t.dtype)
    out = jnp.zeros_like(t)
    for e in range(N_EXPERTS):
        hid = jax.nn.silu(t @ w_gate[e]) * (t @ w_up[e])
        out = out + combine[:, e:e + 1] * (hid @ w_down[e])
    return out.reshape(B, S, D)


def setup_inputs(seed: int = 0) -> dict:
    key = jax.random.key(seed)
    ks = jax.random.split(key, 21)
    f32 = jnp.float32

    def nrm(k, shape, scale):
        return jax.random.normal(k, shape, f32) * scale

    gamma0 = 1.0 - 2.0 ** (-5.0 - np.arange(RET_HEADS, dtype=np.float32))
    ret_logit0 = jnp.asarray(np.log(gamma0) - np.log1p(-gamma0), dtype=f32)
    return {
        "x": nrm(ks[0], (BATCH, SEQ, D_MODEL), 1.0),
        "emb_ln_g": 1.0 + nrm(ks[1], (D_MODEL,), 0.02),
        "emb_ln_b": nrm(ks[2], (D_MODEL,), 0.02),
        "w_in": nrm(ks[3], (DEPTH, D_MODEL, D_IN_PROJ), D_MODEL ** -0.5),
        "attn_sink": nrm(ks[4], (DEPTH, N_ATTN_HEADS), 0.5),
        "conv_w": nrm(ks[5], (DEPTH, CONV_WIDTH, W_CONV), CONV_WIDTH ** -0.5),
        "hgrn_lb": nrm(ks[6], (DEPTH, HGRN_HEADS * HGRN_DK), 1.0),
        "hgrn_norm_g": 1.0 + nrm(ks[7], (DEPTH, W_HGRN), 0.02),
        "ret_decay_logit": ret_logit0[None, None, :] + nrm(ks[8], (DEPTH, 2, RET_HEADS), 0.01),
        "ret_norm_g": 1.0 + nrm(ks[9], (DEPTH, W_RET), 0.02),
        "w_out": nrm(ks[10], (DEPTH, D_MIX, D_MODEL), D_MIX ** -0.5 * DEEPNORM_BETA),
        "ln1_g": 1.0 + nrm(ks[11], (DEPTH, D_MODEL), 0.02),
        "ln1_b": nrm(ks[12], (DEPTH, D_MODEL), 0.02),
        "router_w": nrm(ks[13], (D_MODEL, N_EXPERTS), D_MODEL ** -0.5),
        "router_b": nrm(ks[14], (N_EXPERTS,), 0.01),
        "w_gate": nrm(ks[15], (DEPTH, N_EXPERTS, D_MODEL, D_EXPERT), D_MODEL ** -0.5),
        "w_up": nrm(ks[16], (DEPTH, N_EXPERTS, D_MODEL, D_EXPERT), D_MODEL ** -0.5),
        "w_down": nrm(ks[17], (DEPTH, N_EXPERTS, D_EXPERT, D_MODEL), D_EXPERT ** -0.5 * DEEPNORM_BETA),
        "ln2_g": 1.0 + nrm(ks[18], (DEPTH, D_MODEL), 0.02),
        "ln2_b": nrm(ks[19], (DEPTH, D_MODEL), 0.02),
    }


def reference(x, emb_ln_g, emb_ln_b, w_in, attn_sink, conv_w, hgrn_lb, hgrn_norm_g,
              ret_decay_logit, ret_norm_g, w_out, ln1_g, ln1_b, router_w, router_b,
              w_gate, w_up, w_down, ln2_g, ln2_b):
    lb_all = jnp.cumsum(jax.nn.softmax(hgrn_lb.astype(jnp.float32), axis=0), axis=0)
    lb_all = lb_all - lb_all[0:1]
    h = layer_norm(x, emb_ln_g, emb_ln_b)
    for l in range(DEPTH):
        mix = token_mixing(h, w_in[l], w_out[l], attn_sink[l], conv_w[l], lb_all[l],
                           hgrn_norm_g[l], ret_decay_logit[l], ret_norm_g[l])
        h = layer_norm(DEEPNORM_ALPHA * h + mix, ln1_g[l], ln1_b[l])
        ffn = grouped_moe(h, router_w, router_b, w_gate[l], w_up[l], w_down[l])
        h = layer_norm(DEEPNORM_ALPHA * h + ffn, ln2_g[l], ln2_b[l])
    return h
```

```python
import numpy as np
import ml_dtypes
from contextlib import ExitStack
import concourse.bass as bass
import concourse.mybir as mybir
from concourse.bass_utils import run_bass_kernel_spmd

F32 = mybir.dt.float32
BF16 = mybir.dt.bfloat16
AF = mybir.ActivationFunctionType
ALU = mybir.AluOpType
AX = mybir.AxisListType

P = 128
S = 4096
D = 2048
NT = S // P
KC = D // P
DEPTH = 2
D_IN = 6912
NE = 16
DE = 1024
ALPHA = (2.0 * DEPTH) ** 0.25
LN_EPS = 1e-5
HN_EPS = 1e-6
NEG = -30000.0

C_AQ, C_AK, C_AV = 0, 512, 640
C_CB, C_CC, C_CH = 768, 1280, 1792
C_GQ, C_GZF, C_GZB, C_GI, C_GO = 2304, 2816, 3328, 3840, 4352
C_RQ, C_RK, C_RV, C_RG = 4864, 5376, 5888, 6400


class Prog:
    EPOCH = 30000

    def __init__(self, nc, es, n_dma_sems=12):
        self.nc = nc
        self.es = es
        self.E = {'pe': nc.tensor, 'act': nc.scalar, 'dve': nc.vector,
                  'pool': nc.gpsimd, 'sp': nc.sync}
        self.nsem = 0
        self.sem = {e: self._new_sem(e) for e in self.E}
        self.cnt = {e: 0 for e in self.E}
        self.known = {e: {} for e in self.E}
        self.semobj = {}
        self.tokw = {}
        self.tokr = {}
        self.dq = {}
        for q in ('sp', 'pool', 'act'):
            self.dq[q] = {'sems': [self._new_sem('d' + q) for _ in range(n_dma_sems)],
                          'rr': 0}
        self.dtarget = {}
        self.all_sems = {}
        self.n_ops = 0
        self.n_waits = 0

    def _new_sem(self, tag):
        self.nsem += 1
        s = self.es.enter_context(self.nc.semaphore(f"s_{tag}_{self.nsem}"))
        return s

    def _key(self, s):
        return id(s)

    def _collect(self, eng, reads, writes):
        need = {}
        def addh(h):
            k = self._key(h[0])
            if k not in need or need[k][1] < h[1]:
                need[k] = h
        for t in reads:
            for h in self.tokw.get(t, ()):
                addh(h)
        for t in writes:
            for h in self.tokw.get(t, ()):
                addh(h)
            for h in self.tokr.get(t, ()):
                addh(h)
        return need

    def _emit_waits(self, eng, need, skip_own_pe=True):
        E = self.E[eng]
        kn = self.known[eng]
        for k, (s, v, src) in need.items():
            if eng == 'pe' and src == 'pe':
                continue
            if kn.get(k, 0) >= v:
                continue
            E.wait_ge(s, v)
            kn[k] = v
            self.n_waits += 1

    def _update(self, h, reads, writes):
        k = self._key(h[0])
        for t in writes:
            self.tokw[t] = [h]
            self.tokr[t] = []
        for t in reads:
            if t in writes:
                continue
            lst = self.tokr.get(t)
            if lst is None:
                self.tokr[t] = [h]
            else:
                self.tokr[t] = [x for x in lst if self._key(x[0]) != k] + [h]

    def op(self, eng, fn, r=(), w=()):
        need = self._collect(eng, r, w)
        self._emit_waits(eng, need)
        ins = fn(self.E[eng])
        if self.cnt[eng] >= self.EPOCH:
            self.sem[eng] = self._new_sem(eng)
            self.cnt[eng] = 0
        self.cnt[eng] += 1
        ins.then_inc(self.sem[eng], 1)
        h = (self.sem[eng], self.cnt[eng], eng)
        self.known[eng][self._key(h[0])] = max(self.known[eng].get(self._key(h[0]), 0), 0)
        self._update(h, r, w)
        self.n_ops += 1
        return h

    def dma(self, q, out, in_, r=(), w=(), **kw):
        dq = self.dq[q]
        s = dq['sems'][dq['rr'] % len(dq['sems'])]
        dq['rr'] += 1
        prev = self.dtarget.get(self._key(s), 0)
        need = self._collect(q, r, w)
        if prev > 0:
            k = self._key(s)
            if k not in need or need[k][1] < prev:
                need[k] = (s, prev, 'dma')
        self._emit_waits(q, need)
        tgt = prev + 16
        self.dtarget[self._key(s)] = tgt
        self.all_sems[self._key(s)] = s
        self.E[q].dma_start(out=out, in_=in_, **kw).then_inc(s, 16)
        h = (s, tgt, 'dma')
        self._update(h, r, w)
        self.n_ops += 1
        return h

    def barrier(self, engines=None, keep=()):
        engines = engines or list(self.E)
        need = {}
        for e in self.E:
            if self.cnt[e] > 0:
                need[self._key(self.sem[e])] = (self.sem[e], self.cnt[e], e)
        for k, s in self.all_sems.items():
            need[k] = (s, self.dtarget[k], 'dma')
        for e in engines:
            E = self.E[e]
            kn = self.known[e]
            for k, (s, v, src) in need.items():
                if src == e and e != 'sp':
                    pass
                if kn.get(k, 0) >= v:
                    continue
                E.wait_ge(s, v)
                kn[k] = v
        self.tokw.clear()
        self.tokr.clear()


def host_consts():
    c = {}
    c['ident_f'] = np.eye(P, dtype=np.float32)
    c['ident_b'] = np.eye(P, dtype=np.float32).astype(ml_dtypes.bfloat16)
    ab = np.zeros((P, 8, 3, P), np.float32)
    s_i = np.arange(P)[:, None]
    t_i = np.arange(P)[None, :]
    for h in range(8):
        slope = 2.0 ** (-(h + 1))
        for j in range(3):
            dist = (j - 1) * P + s_i - t_i
            ab[:, h, j, :] = np.where(np.abs(dist) <= 128, -slope * np.abs(dist), NEG)
    c['attn_bias'] = ab.reshape(P, 8 * 3 * P)
    rc = np.zeros((P, 5, P), np.float32)
    rc[:, 0] = np.maximum(t_i - s_i, 0)
    rc[:, 1] = np.maximum(s_i - t_i, 0)
    rc[:, 2] = (t_i > s_i)
    rc[:, 3] = (s_i > t_i)
    rc[:, 4] = 2.0 * np.eye(P)
    c['ret_c'] = rc.reshape(P, 5 * P)
    rv = np.zeros((P, 2, P), np.float32)
    rv[:, 0] = t_i + 1.0
    rv[:, 1] = 128.0 - t_i
    c['ret_vec'] = rv.reshape(P, 2 * P)
    cmk = np.ones((P, S), np.float32)
    cmk[:, ::64] = 0.0
    c['hg_cm'] = cmk.astype(ml_dtypes.bfloat16)
    s6 = np.arange(64)[:, None]
    t6 = np.arange(64)[None, :]
    hm = np.zeros((64, 2, 64), np.float32)
    hm[:, 0] = (s6 <= t6)
    hm[:, 1] = (s6 >= t6)
    c['hg_mask'] = hm.reshape(64, 128)
    c['ret_pcol'] = np.stack([127.0 - np.arange(P), np.arange(P) * 1.0], 1).astype(np.float32)
    return c


class Builder:
    def __init__(self, n_layers=DEPTH, stop=None, taps=(), skip=()):
        self.skip = set(skip)
        self.n_layers = n_layers
        self.stop = stop
        self.taps = set(taps)
        self.nc = bass.Bass("TRN2", target_bir_lowering=False)
        self.es = ExitStack()
        self.pg = Prog(self.nc, self.es)
        self.dr = {}
        self.consts = host_consts()

    def din(self, name, shape, dtype=F32):
        t = self.nc.dram_tensor(name, list(shape), dtype, kind="ExternalInput")
        self.dr[name] = t
        return t

    def dscr(self, name, shape, dtype):
        kind = "ExternalOutput" if name in self.taps else "Internal"
        t = self.nc.dram_tensor(name, list(shape), dtype, kind=kind)
        self.dr[name] = t
        return t

    def sb(self, st, name, shape, dtype):
        self._uid = getattr(self, '_uid', 0) + 1
        return st.enter_context(self.nc.sbuf_tensor(f"{name}_u{self._uid}", list(shape), dtype))

    def psum(self, st, name, shape, dtype=F32):
        self._uid = getattr(self, '_uid', 0) + 1
        return st.enter_context(self.nc.psum_tensor(f"{name}_u{self._uid}", list(shape), dtype))

    IN_SHAPES = {
        'x': [S, D], 'emb_ln_g': [D], 'emb_ln_b': [D], 'w_in': [DEPTH, D, D_IN],
        'attn_sink': [DEPTH, 8], 'conv_w': [DEPTH, 3, 512], 'hgrn_lb': [DEPTH, 512],
        'hgrn_norm_g': [DEPTH, 512], 'ret_decay_logit': [DEPTH, 2, 4], 'ret_norm_g': [DEPTH, 512],
        'w_out': [DEPTH, D, D], 'ln1_g': [DEPTH, D], 'ln1_b': [DEPTH, D],
        'router_w': [D, NE], 'router_b': [NE], 'w_gate': [DEPTH, NE, D, DE],
        'w_up': [DEPTH, NE, D, DE], 'w_down': [DEPTH, NE, DE, D],
        'ln2_g': [DEPTH, D], 'ln2_b': [DEPTH, D],
    }

    def I(self, name):
        if name not in self.dr:
            if name.startswith('c_'):
                v = self.consts[name[2:]]
                self.din(name, v.shape, BF16 if v.dtype == ml_dtypes.bfloat16 else F32)
            else:
                self.din(name, self.IN_SHAPES[name])
        return self.dr[name].ap()

    def declare(self):
        nc = self.nc
        self.out = nc.dram_tensor('out', [S, D], F32, kind="ExternalOutput")
        self.dscr('h_dram', [S, D], F32)
        self.dscr('hT_dram', [D, S], BF16)
        self.declare_proj()
        self.dscr('yT_dram', [D, S], BF16)
        self.dscr('res_dram', [S, D], F32)
        for d_ in range(2):
            self.dscr(f'hg_q{d_}', [512, S], BF16)
            self.dscr(f'hg_k{d_}', [512, S], BF16)
            self.dscr(f'hg_S{d_}', [S // 64, P, 512], BF16)

    def build(self):
        self.declare()
        nc, pg = self.nc, self.pg
        with ExitStack() as g:
            self.g = g
            self.ident_f = self.sb(g, 'ident_f', [P, P], F32)
            self.ident_b = self.sb(g, 'ident_b', [P, P], BF16)
            self.comb = self.sb(g, 'comb', [P, NT, NE], F32)
            self.comb_toks = [('comb', t) for t in range(NT)]
            pg.dma('sp', self.ident_f[:], self.I('c_ident_f'), w=['ident_f'])
            pg.dma('sp', self.ident_b[:], self.I('c_ident_b'), w=['ident_b'])
            self.ln_phase(src='x', g_ap=self.I('emb_ln_g'), b_ap=self.I('emb_ln_b'),
                          mode='x')
            pg.barrier()
            for l in range(self.n_layers):
                if self.stop == 'ln0':
                    break
                self.in_proj_phase(l)
                pg.barrier()
                if self.stop == 'inproj':
                    break
                if 'attn' not in self.skip:
                    self.attn_phase(l)
                    pg.barrier()
                if self.stop == 'attn':
                    break
                if 'conv' not in self.skip:
                    self.conv_phase(l)
                    pg.barrier()
                if 'ret' not in self.skip:
                    self.ret_phase(l)
                    pg.barrier()
                if self.stop in ('conv', 'ret'):
                    break
                if 'hgrn' not in self.skip:
                    self.hgrn_phase(l)
                    pg.barrier()
                if self.stop in ('hgrn', 'mix'):
                    break
                self.outproj_phase(l)
                pg.barrier()
                if self.stop == 'outproj':
                    break
                self.ln_phase(None, self.I('ln1_g')[l], self.I('ln1_b')[l], 'res', router=('norouter' not in self.skip))
                pg.barrier(keep=self.comb_toks)
                if self.stop == 'ln1':
                    break
                self.moe_phase(l)
                pg.barrier()
                last = (l == self.n_layers - 1)
                self.ln_phase(None, self.I('ln2_g')[l], self.I('ln2_b')[l], 'res', final=last)
                pg.barrier()
        self.es.close()
        return nc

    def ln_phase(self, src, g_ap, b_ap, mode, final=False, router=False):
        nc, pg = self.nc, self.pg
        with ExitStack() as st:
            gt = self.sb(st, 'ln_g', [P, D], F32)
            bt = self.sb(st, 'ln_b', [P, D], F32)
            pg.dma('sp', gt[:], g_ap.partition_broadcast(P), w=['ln_g'])
            pg.dma('sp', bt[:], b_ap.partition_broadcast(P), w=['ln_b'])
            NB = 2
            xt = [self.sb(st, f'ln_x{i}', [P, D], F32) for i in range(NB)]
            x2 = [self.sb(st, f'ln_r{i}', [P, D], F32) for i in range(NB)] if mode == 'res' else None
            hn = [self.sb(st, f'ln_h{i}', [P, D], F32) for i in range(NB)]
            stt = [self.sb(st, f'ln_st{i}', [P, 4, 6], F32) for i in range(NB)]
            mv = [self.sb(st, f'ln_mv{i}', [P, 4], F32) for i in range(NB)]
            hst = [self.sb(st, f'ln_hst{i}', [P, KC, 512], BF16) for i in range(2)]
            ps = self.psum(st, 'ln_ps', [P, 4 * 512], F32)
            if router:
                hTf = self.sb(st, 'ln_loT', [P, KC, P], BF16)
                Hb = self.sb(st, 'ln_Hb', [P, D], BF16)
                Lo = self.sb(st, 'ln_Lo', [P, D], F32)
                rw = self.sb(st, 'ln_rw', [P, KC, NE], F32)
                rwh = self.sb(st, 'ln_rwh', [P, KC, NE], BF16)
                rwl = self.sb(st, 'ln_rwl', [P, KC, NE], BF16)
                rb = self.sb(st, 'ln_rb', [P, NE], F32)
                rs = self.sb(st, 'ln_rs', [P, 8, NE], F32)
                ps_r = self.psum(st, 'ln_ps_r', [P, 512], F32)
                pg.dma('sp', rw[:], self.I('router_w').rearrange("(k p) e -> p k e", p=P), w=['ln_rw'])
                pg.dma('sp', rb[:], self.I('router_b').partition_broadcast(P), w=['ln_rb'])
                pg.op('act', lambda E: E.copy(rwh[:], rw[:]), r=['ln_rw'], w=['ln_rwh'])
                pg.op('dve', lambda E: E.tensor_tensor(rwl[:], rw[:], rwh[:], ALU.subtract), r=['ln_rw', 'ln_rwh'], w=['ln_rwl'])
            if mode == 'x':
                srcv = self.I(src).rearrange("(n p) d -> n p d", p=P)
            else:
                resv = self.dr['res_dram'].ap().rearrange("(n p) d -> n p d", p=P)
            hdv = self.dr['h_dram'].ap().rearrange("(n p) d -> n p d", p=P)
            outv = self.out.ap().rearrange("(n p) d -> n p d", p=P)
            hTv = self.dr['hT_dram'].ap().rearrange("(k p) t -> p k t", p=P)
            for t in range(NT):
                i = t % NB
                X, H, ST, MV = xt[i], hn[i], stt[i], mv[i]
                tx, th = f'ln_x{i}', f'ln_h{i}'
                if mode == 'x':
                    pg.dma('sp', X[:], srcv[t], w=[tx])
                else:
                    pg.dma('sp', X[:], hdv[t], r=[('h_dram', t)], w=[tx])
                    pg.dma('sp', x2[i][:], resv[t], r=[('res_dram', t)], w=[f'ln_r{i}'])
                    pg.op('dve', lambda E, X=X, R=x2[i]: E.scalar_tensor_tensor(X[:], X[:], ALPHA, R[:], ALU.mult, ALU.add),
                          r=[tx, f'ln_r{i}'], w=[tx])
                self.ln_tile(X, H, ST, MV, gt, bt, tx, th, f'ln_s{i}')
                if final:
                    pg.dma('sp', outv[t], H[:], r=[th], w=[('out', t)])
                    continue
                pg.dma('sp', hdv[t], H[:], r=[th], w=[('h_dram', t)])
                slot = t % 4
                hb = (t // 4) % 2
                HS = hst[hb]
                for half in range(4):
                    def tr(E, half=half, H=H):
                        ins = None
                        for j in range(4):
                            k = half * 4 + j
                            ins = E.transpose(ps[:, half * 512 + j * P: half * 512 + (j + 1) * P],
                                              H[:, k * P:(k + 1) * P], self.ident_f[:])
                        return ins
                    pg.op('pe', tr, r=[th, 'ident_f'], w=[('ln_ps', half)])
                    o_ap = HS[:, half * 4:(half + 1) * 4, slot * P:(slot + 1) * P]
                    i_ap = ps[:, half * 512:(half + 1) * 512].rearrange("p (a b) -> p a b", a=4)
                    if half % 2 == 0:
                        pg.op('act', lambda E, o=o_ap, a=i_ap: E.copy(o, a),
                              r=[('ln_ps', half)], w=[('ln_hst', hb, half, slot)])
                    else:
                        pg.op('dve', lambda E, o=o_ap, a=i_ap: E.tensor_copy(o, a),
                              r=[('ln_ps', half)], w=[('ln_hst', hb, half, slot)])
                if slot == 3:
                    tb = t // 4
                    pg.dma('sp', hTv[:, :, tb * 512:(tb + 1) * 512], HS[:],
                           r=[('ln_hst', hb, hf, sl) for hf in range(4) for sl in range(4)],
                           w=[('hT_dram', tb)])
                if router:
                    pg.op('act', lambda E, H=H: E.copy(Hb[:], H[:]), r=[th], w=['ln_Hb'])
                    pg.op('dve', lambda E, H=H: E.tensor_tensor(Lo[:], H[:], Hb[:], ALU.subtract), r=[th, 'ln_Hb'], w=['ln_Lo'])
                    for half in range(4):
                        def tr2(E, half=half):
                            ins = None
                            for j in range(4):
                                k = half * 4 + j
                                ins = E.transpose(ps[:, half * 512 + j * P: half * 512 + (j + 1) * P],
                                                  Lo[:, k * P:(k + 1) * P], self.ident_f[:])
                            return ins
                        pg.op('pe', tr2, r=['ln_Lo', 'ident_f'], w=[('ln_ps', half)])
                        o2 = hTf[:, half * 4:(half + 1) * 4, :]
                        i_ap = ps[:, half * 512:(half + 1) * 512].rearrange("p (a b) -> p a b", a=4)
                        if half % 2 == 1:
                            pg.op('act', lambda E, o=o2, a=i_ap: E.copy(o, a), r=[('ln_ps', half)], w=[('ln_hTf', half)])
                        else:
                            pg.op('dve', lambda E, o=o2, a=i_ap: E.tensor_copy(o, a), r=[('ln_ps', half)], w=[('ln_hTf', half)])
                    hiT = HS[:, :, slot * P:(slot + 1) * P]
                    hit = [('ln_hst', hb, hf, slot) for hf in range(4)]
                    self.route_tile(t, hTf, hiT, hit, rwh, rwl, rb, rs, ps_r)

    def route_tile(self, t, loT, hiT, hit, rwh, rwl, rb, rs, ps_r):
        pg = self.pg
        def mm(E):
            ins = None
            for k in range(KC):
                E.matmul(ps_r[:, 0:NE], hiT[:, k, :], rwh[:, k, :], start=(k == 0), stop=False)
                E.matmul(ps_r[:, 0:NE], loT[:, k, :], rwh[:, k, :], start=False, stop=False)
                ins = E.matmul(ps_r[:, 0:NE], hiT[:, k, :], rwl[:, k, :], start=False, stop=(k == KC - 1))
            return ins
        pg.op('pe', mm, r=[('ln_hTf', hf) for hf in range(4)] + hit + ['ln_rwh', 'ln_rwl'], w=['ln_ps_r'])
        lg, ex, eq, ex2, sel = [rs[:, i, :] for i in range(5)]
        sm = rs[:, 5, :]
        gmk = rs[:, 6, 0:4]
        g3 = lambda ap: ap.rearrange("p (g e) -> p g e", e=4)
        bc = lambda ap: ap.unsqueeze(2).to_broadcast([P, 4, 4])
        T = 'rt'
        pg.op('dve', lambda E: E.tensor_tensor(lg, ps_r[:, 0:NE], rb[:], ALU.add), r=['ln_ps_r', 'ln_rb'], w=[T])
        pg.op('dve', lambda E: E.tensor_reduce(sm[:, 12:13], lg, AX.X, ALU.max), r=[T], w=[T])
        pg.op('dve', lambda E: E.tensor_scalar(sm[:, 12:13], sm[:, 12:13], -1.0, None, ALU.mult), r=[T], w=[T])
        pg.op('act', lambda E: E.activation(ex, lg, AF.Exp, bias=sm[:, 12:13], scale=1.0), r=[T], w=[T])
        pg.op('dve', lambda E: E.tensor_reduce(sm[:, 0:4], g3(ex), AX.X, ALU.max), r=[T], w=[T])
        pg.op('dve', lambda E: E.tensor_tensor(g3(eq), g3(ex), bc(sm[:, 0:4]), ALU.is_equal), r=[T], w=[T])
        pg.op('dve', lambda E: E.scalar_tensor_tensor(ex2, eq, -4.0, ex, ALU.mult, ALU.add), r=[T], w=[T])
        pg.op('dve', lambda E: E.tensor_reduce(sm[:, 4:8], g3(ex2), AX.X, ALU.max), r=[T], w=[T])
        pg.op('dve', lambda E: E.tensor_tensor(sm[:, 8:12], sm[:, 0:4], sm[:, 4:8], ALU.add), r=[T], w=[T])
        pg.op('dve', lambda E: E.tensor_reduce(sm[:, 13:14], sm[:, 8:12], AX.X, ALU.max), r=[T], w=[T])
        pg.op('dve', lambda E: E.tensor_scalar(gmk, sm[:, 8:12], sm[:, 13:14], None, ALU.is_equal), r=[T], w=[T])
        pg.op('dve', lambda E: E.tensor_tensor(g3(sel), g3(ex), bc(sm[:, 4:8]), ALU.is_ge), r=[T], w=[T])
        pg.op('dve', lambda E: E.tensor_tensor(g3(sel), g3(sel), bc(gmk), ALU.mult), r=[T], w=[T])
        pg.op('dve', lambda E: E.tensor_tensor(sel, sel, ex, ALU.mult), r=[T], w=[T])
        pg.op('dve', lambda E: E.reciprocal(sm[:, 14:15], sm[:, 13:14]), r=[T], w=[T])
        pg.op('dve', lambda E: E.tensor_scalar(self.comb[:, t, :], sel, sm[:, 14:15], None, ALU.mult), r=[T], w=[('comb', t)])

    def ln_tile(self, X, H, ST, MV, gt, bt, tx, th, ts):
        pg = self.pg
        for j in range(4):
            pg.op('dve', lambda E, j=j: E.bn_stats(ST[:, j, :], X[:, j * 512:(j + 1) * 512]),
                  r=[tx], w=[(ts, 'st', j)])
        pg.op('dve', lambda E: E.bn_aggr(MV[:, 0:2], ST[:].rearrange("p a b -> p (a b)")),
              r=[(ts, 'st', j) for j in range(4)], w=[(ts, 'mv')])
        pg.op('act', lambda E: E.activation(MV[:, 2:3], MV[:, 1:2], AF.Ln, bias=LN_EPS, scale=1.0),
              r=[(ts, 'mv')], w=[(ts, 'lnv')])
        pg.op('act', lambda E: E.activation(MV[:, 3:4], MV[:, 2:3], AF.Exp, scale=-0.5),
              r=[(ts, 'lnv')], w=[(ts, 'rstd')])
        pg.op('dve', lambda E: E.tensor_scalar(H[:], X[:], MV[:, 0:1], MV[:, 3:4],
                                               ALU.subtract, ALU.mult),
              r=[tx, (ts, 'mv'), (ts, 'rstd')], w=[th])
        pg.op('pool', lambda E: E.tensor_tensor(H[:], H[:], gt[:], ALU.mult),
              r=[th, 'ln_g'], w=[th])
        pg.op('pool', lambda E: E.tensor_tensor(H[:], H[:], bt[:], ALU.add),
              r=[th, 'ln_b'], w=[th])


    PF_SPECS = [('aq', C_AQ, 512), ('akd', None, 256), ('cb', C_CB, 512), ('cc', C_CC, 512),
                ('ch', C_CH, 512), ('gq', C_GQ, 512), ('gzf', C_GZF, 512), ('gzb', C_GZB, 512),
                ('rq', C_RQ, 512), ('rk', C_RK, 512)]
    PT_SPECS = [('av', C_AV, 128), ('gi', C_GI, 512), ('go', C_GO, 512), ('rkt', C_RK, 512),
                ('rv', C_RV, 512), ('rg', C_RG, 512)]

    def declare_proj(self):
        for n, _, w in self.PF_SPECS:
            self.dscr('pf_' + n, [w, S], BF16)
        for n, _, w in self.PT_SPECS:
            self.dscr('pt_' + n, [S, w], BF16)

    def load_hT(self, st):
        hT = self.sb(st, 'hT_bf', [P, KC, S], BF16)
        hTv = self.dr['hT_dram'].ap().rearrange("(k p) t -> p k t", p=P)
        for k in range(KC):
            self.pg.dma('sp', hT[:, k, :], hTv[:, k, :], r=[('hT_dram', tb) for tb in range(8)],
                        w=[('hT_bf', k)])
        return hT

    def in_proj_phase(self, l):
        nc, pg = self.nc, self.pg
        with ExitStack() as st:
            hT = self.load_hT(st)
            hT_toks = [('hT_bf', k) for k in range(KC)]
            wt = [self.sb(st, f'ip_w{i}', [P, KC, 512], BF16) for i in range(2)]
            stF = [self.sb(st, f'ip_sf{i}', [P, S], BF16) for i in range(2)]
            stT = [self.sb(st, f'ip_st{i}', [P, 4, 512], BF16) for i in range(2)]
            ps = self.psum(st, 'ip_ps', [P, 8 * 512], F32)
            w_in = self.I('w_in')[l].rearrange("(k p) n -> p k n", p=P)
            gi = 0
            bank = 0
            ev = 0
            nsf = 0
            nst = 0
            for (name, c0, width) in self.PF_SPECS:
                W = wt[gi % 2]; wtok = f'ip_w{gi % 2}'; gi += 1
                if name == 'akd':
                    for j, cc in enumerate([C_AK, C_AK, C_AK + 64, C_AK + 64]):
                        pg.dma('pool', W[:, :, j * 64:(j + 1) * 64], w_in[:, :, cc:cc + 64],
                               w=[(wtok, j)])
                    wtoks = [(wtok, j) for j in range(4)]
                else:
                    pg.dma('pool', W[:, :, 0:width], w_in[:, :, c0:c0 + width], w=[(wtok, 0)])
                    wtoks = [(wtok, 0)]
                dst = self.dr['pf_' + name].ap()
                for j in range(width // P):
                    SF = stF[nsf % 2]; sftok = f'ip_sf{nsf % 2}'; nsf += 1
                    for tb in range(8):
                        b = bank % 8; bank += 1
                        def mm(E, W=W, j=j, tb=tb, b=b):
                            ins = None
                            for k in range(KC):
                                ins = E.matmul(ps[:, b * 512:(b + 1) * 512], W[:, k, j * P:(j + 1) * P],
                                               hT[:, k, tb * 512:(tb + 1) * 512],
                                               start=(k == 0), stop=(k == KC - 1))
                            return ins
                        pg.op('pe', mm, r=wtoks + hT_toks, w=[('ip_ps', b)])
                        o_ap = SF[:, tb * 512:(tb + 1) * 512]
                        i_ap = ps[:, b * 512:(b + 1) * 512]
                        if ev % 2 == 0:
                            pg.op('act', lambda E, o=o_ap, a=i_ap: E.copy(o, a),
                                  r=[('ip_ps', b)], w=[(sftok, tb)])
                        else:
                            pg.op('dve', lambda E, o=o_ap, a=i_ap: E.tensor_copy(o, a),
                                  r=[('ip_ps', b)], w=[(sftok, tb)])
                        ev += 1
                    pg.dma('sp', dst[j * P:(j + 1) * P, :], SF[:],
                           r=[(sftok, tb) for tb in range(8)], w=[('pf_' + name, j)])
            for (name, c0, width) in self.PT_SPECS:
                W = wt[gi % 2]; wtok = f'ip_w{gi % 2}'; gi += 1
                pg.dma('pool', W[:, :, 0:width], w_in[:, :, c0:c0 + width], w=[(wtok, 0)])
                wtoks = [(wtok, 0)]
                dst = self.dr['pt_' + name].ap().rearrange("(n j p) c -> n p j c", p=P, j=4)
                for t in range(NT):
                    b = bank % 8; bank += 1
                    slot = t % 4
                    if slot == 0:
                        ST = stT[nst % 2]; sttok = f'ip_st{nst % 2}'; nst += 1
                    def mm(E, W=W, t=t, b=b, width=width):
                        ins = None
                        for k in range(KC):
                            ins = E.matmul(ps[:, b * 512:b * 512 + width], hT[:, k, t * P:(t + 1) * P],
                                           W[:, k, 0:width], start=(k == 0), stop=(k == KC - 1))
                        return ins
                    pg.op('pe', mm, r=wtoks + hT_toks, w=[('ip_ps', b)])
                    o_ap = ST[:, slot, 0:width]
                    i_ap = ps[:, b * 512:b * 512 + width]
                    if ev % 2 == 0:
                        pg.op('act', lambda E, o=o_ap, a=i_ap: E.copy(o, a),
                              r=[('ip_ps', b)], w=[(sttok, slot)])
                    else:
                        pg.op('dve', lambda E, o=o_ap, a=i_ap: E.tensor_copy(o, a),
                              r=[('ip_ps', b)], w=[(sttok, slot)])
                    ev += 1
                    if slot == 3:
                        pg.dma('sp', dst[t // 4][:, :, 0:width], ST[:, :, 0:width],
                               r=[(sttok, s_) for s_ in range(4)],
                               w=[('pt_' + name, t // 4)])


    def attn_phase(self, l):
        nc, pg = self.nc, self.pg
        with ExitStack() as st:
            q = self.sb(st, 'at_q', [P, 4, S], BF16)
            kd = self.sb(st, 'at_k', [P, 2, S], BF16)
            va = self.sb(st, 'at_v', [P, NT, 2, 65], BF16)
            bias = self.sb(st, 'at_bias', [P, 8, 3, P], F32)
            esink = self.sb(st, 'at_esink', [P, 8], F32)
            yst = self.sb(st, 'at_yst', [P, 4, S], BF16)
            tmp = [self.sb(st, f'at_tmp{i}', [P, 3 * P], F32) for i in range(2)]
            pT = [self.sb(st, f'at_pT{i}', [P, 3 * P], BF16) for i in range(2)]
            den = [self.sb(st, f'at_den{i}', [P, 8], F32) for i in range(2)]
            y = [self.sb(st, f'at_y{i}', [P, 8, 64], BF16) for i in range(2)]
            ps_s = self.psum(st, 'at_ps_s', [P, 2, 512], F32)
            ps_o = self.psum(st, 'at_ps_o', [P, 2, 2, 512], F32)
            ps_t = self.psum(st, 'at_ps_t', [P, 2, 512], BF16)
            pg.dma('sp', q[:], self.dr['pf_aq'].ap().rearrange("(c p) t -> p c t", p=P),
                   r=[('pf_aq', j) for j in range(4)], w=['at_q'])
            pg.dma('sp', kd[:], self.dr['pf_akd'].ap().rearrange("(c p) t -> p c t", p=P),
                   r=[('pf_akd', j) for j in range(2)], w=['at_k'])
            pg.op('pool', lambda E: E.memset(va[:], 1.0), w=['at_v'])
            avv = self.dr['pt_av'].ap().rearrange("(n p) c -> p n c", p=P)
            for kv in range(2):
                pg.dma('sp', va[:, :, kv, 0:64], avv[:, :, kv * 64:(kv + 1) * 64],
                       r=[('pt_av', j) for j in range(8)], w=['at_v'])
            pg.dma('sp', bias[:].rearrange("p a b c -> p (a b c)"), self.I('c_attn_bias'), w=['at_bias'])
            pg.dma('sp', esink[:], self.I('attn_sink')[l].partition_broadcast(P), w=['at_esink'])
            pg.op('act', lambda E: E.activation(esink[:], esink[:], AF.Exp), r=['at_esink'], w=['at_esink'])
            it = 0
            for n in range(NT):
                ob = n % 2
                js = [j for j in range(3) if 0 <= n + j - 1 < NT]
                c0, c1 = js[0] * P, (js[-1] + 1) * P
                for h in range(8):
                    kv = h // 4
                    r0 = (h % 2) * 64
                    sb_ = it % 2; it += 1
                    def mm(E, h=h, kv=kv, r0=r0, sb_=sb_, n=n, js=js):
                        ins = None
                        for j in js:
                            kb = n + j - 1
                            ins = E.matmul(ps_s[:, sb_, j * P:(j + 1) * P],
                                           kd[r0:r0 + 64, kv, kb * P:(kb + 1) * P],
                                           q[r0:r0 + 64, h // 2, n * P:(n + 1) * P],
                                           start=True, stop=True)
                        return ins
                    pg.op('pe', mm, r=['at_q', 'at_k'], w=[('at_ps_s', sb_)])
                    T, PT = tmp[sb_], pT[sb_]
                    pg.op('dve', lambda E, T=T, sb_=sb_, h=h, c0=c0, c1=c1: E.scalar_tensor_tensor(
                        T[:, c0:c1], ps_s[:, sb_, c0:c1], 0.125,
                        bias[:, h, :, :].rearrange("p a b -> p (a b)")[:, c0:c1], ALU.mult, ALU.add),
                        r=[('at_ps_s', sb_), 'at_bias'], w=[('at_tmp', sb_)])
                    pg.op('act', lambda E, T=T, PT=PT, c0=c0, c1=c1: E.activation(PT[:, c0:c1], T[:, c0:c1], AF.Exp),
                          r=[('at_tmp', sb_)], w=[('at_pT', sb_)])
                    def pv(E, h=h, kv=kv, PT=PT, n=n, js=js, ob=ob):
                        ins = None
                        for idx, j in enumerate(js):
                            kb = n + j - 1
                            ins = E.matmul(ps_o[:, ob, h // 4, (h % 4) * 65:(h % 4) * 65 + 65],
                                           PT[:, j * P:(j + 1) * P], va[:, kb, kv, :],
                                           start=(idx == 0), stop=(idx == len(js) - 1))
                        return ins
                    pg.op('pe', pv, r=[('at_pT', sb_), 'at_v'], w=[('at_ps_o', ob, h)])
                DEN, Y = den[ob], y[ob]
                po = ps_o[:, ob, :, 0:260].rearrange("p b (h e) -> p b h e", e=65)
                pg.op('dve', lambda E, DEN=DEN, po=po: E.tensor_tensor(
                    DEN[:].rearrange("p (b h) -> p b h", b=2), po[:, :, :, 64],
                    esink[:].rearrange("p (b h) -> p b h", b=2), ALU.add),
                    r=[('at_ps_o', ob, h) for h in range(8)] + ['at_esink'], w=[('at_den', ob)])
                pg.op('dve', lambda E, DEN=DEN: E.reciprocal(DEN[:], DEN[:]),
                      r=[('at_den', ob)], w=[('at_den', ob)])
                pg.op('dve', lambda E, DEN=DEN, Y=Y, po=po: E.tensor_tensor(
                    Y[:].rearrange("p (b h) d -> p b h d", b=2), po[:, :, :, 0:64],
                    DEN[:].rearrange("p (b h) -> p b h", b=2).unsqueeze(3).to_broadcast([P, 2, 4, 64]),
                    ALU.mult),
                    r=[('at_ps_o', ob, h) for h in range(8)] + [('at_den', ob)], w=[('at_y', ob)])
                self.transpose_out(Y[:].rearrange("p h d -> p (h d)"), ('at_y', ob), ps_t, 'at_ps_t',
                                   yst, 'at_yst', n, ob)
            self.store_yT(yst, 'at_yst', 0)

    def transpose_out(self, Yflat, ytok, ps_t, pstok, yst, ysttok, n, ob, rows=P):
        pg = self.pg
        def tr(E):
            ins = None
            for c in range(4):
                ins = E.transpose(ps_t[:, ob, c * P:(c + 1) * P], Yflat[:, c * P:(c + 1) * P], self.ident_b[:])
            return ins
        pg.op('pe', tr, r=[ytok, 'ident_b'], w=[(pstok, ob)])
        pg.op('act', lambda E: E.copy(yst[:, :, n * P:(n + 1) * P],
                                      ps_t[:, ob, :].rearrange("p (c t) -> p c t", c=4)),
              r=[(pstok, ob)], w=[(ysttok, n)])

    def store_yT(self, yst, ysttok, row0):
        dst = self.dr['yT_dram'].ap()
        for c in range(4):
            self.pg.dma('sp', dst[row0 + c * P: row0 + (c + 1) * P, :], yst[:, c, :],
                        r=[(ysttok, n) for n in range(NT)], w=[('yT_dram', row0 // P + c)])


    def load_chan(self, dst2d, src1d, wtok):
        self.pg.dma('sp', dst2d, src1d.rearrange("(c p) -> p c", p=P), w=[wtok],
                    allow_slow_non_contiguous=True)

    def conv_phase(self, l):
        nc, pg = self.nc, self.pg
        with ExitStack() as st:
            cw = self.sb(st, 'cv_w', [P, 3, 4], F32)
            for wi in range(3):
                self.load_chan(cw[:, wi, :], self.I('conv_w')[l, wi], ('cv_w', wi))
            cwt = [('cv_w', wi) for wi in range(3)]
            U = [self.sb(st, f'cv_u{i}', [P, S + 2], F32) for i in range(2)]
            A = [self.sb(st, f'cv_a{i}', [P, S], F32) for i in range(2)]
            cb = [self.sb(st, f'cv_b{i}', [P, S], BF16) for i in range(2)]
            cc = [self.sb(st, f'cv_c{i}', [P, S], BF16) for i in range(2)]
            ch = [self.sb(st, f'cv_h{i}', [P, S], BF16) for i in range(2)]
            yo = [self.sb(st, f'cv_y{i}', [P, S], BF16) for i in range(2)]
            for i in range(2):
                pg.op('pool', lambda E, i=i: E.memset(U[i][:], 0.0), w=[('cv_u', i)])
            for c in range(4):
                i = c % 2
                rows = slice(c * P, (c + 1) * P)
                pg.dma('sp', cb[i][:], self.dr['pf_cb'].ap()[rows, :], r=[('pf_cb', c)], w=[('cv_b', i)])
                pg.dma('sp', cc[i][:], self.dr['pf_cc'].ap()[rows, :], r=[('pf_cc', c)], w=[('cv_c', i)])
                pg.dma('sp', ch[i][:], self.dr['pf_ch'].ap()[rows, :], r=[('pf_ch', c)], w=[('cv_h', i)])
                pg.op('pool', lambda E, i=i: E.tensor_tensor(U[i][:, 1:S + 1], cc[i][:], ch[i][:], ALU.mult),
                      r=[('cv_c', i), ('cv_h', i)], w=[('cv_u', i)])
                pg.op('dve', lambda E, i=i, c=c: E.tensor_scalar(A[i][:], U[i][:, 1:S + 1], cw[:, 1, c:c + 1], None, ALU.mult),
                      r=[('cv_u', i)] + cwt, w=[('cv_a', i)])
                pg.op('dve', lambda E, i=i, c=c: E.scalar_tensor_tensor(A[i][:], U[i][:, 0:S], cw[:, 0, c:c + 1], A[i][:], ALU.mult, ALU.add),
                      r=[('cv_u', i), ('cv_a', i)] + cwt, w=[('cv_a', i)])
                pg.op('dve', lambda E, i=i, c=c: E.scalar_tensor_tensor(A[i][:], U[i][:, 2:S + 2], cw[:, 2, c:c + 1], A[i][:], ALU.mult, ALU.add),
                      r=[('cv_u', i), ('cv_a', i)] + cwt, w=[('cv_a', i)])
                pg.op('pool', lambda E, i=i: E.tensor_tensor(yo[i][:], A[i][:], cb[i][:], ALU.mult),
                      r=[('cv_a', i), ('cv_b', i)], w=[('cv_y', i)])
                pg.dma('sp', self.dr['yT_dram'].ap()[512 + c * P: 512 + (c + 1) * P, :], yo[i][:],
                       r=[('cv_y', i)], w=[('yT_dram', 4 + c)])

    def alloc_norm(self, st, pfx, rows, nslot=1):
        d = {}
        d['sq'] = self.sb(st, pfx + '_sq', [rows, nslot * 512], F32)
        d['on'] = self.sb(st, pfx + '_on', [rows, nslot * 512], F32)
        d['e'] = self.sb(st, pfx + '_e', [rows, nslot * 512], F32)
        d['st'] = self.sb(st, pfx + '_st', [rows, 6, nslot * 4], F32)
        d['y'] = [self.sb(st, pfx + f'_y{i}', [rows, nslot * 512], BF16) for i in range(2)]
        d['pfx'] = pfx
        return d

    def norm_gate(self, d, po, potoks, G, gtok, NG, ngtok, mode, yi, rows, nh):
        pg = self.pg
        pfx = d['pfx']
        W = nh * 128
        sq, on, e, stt = d['sq'][:, 0:W], d['on'][:, 0:W], d['e'][:, 0:W], d['st']
        Y = d['y'][yi][:, 0:W]
        v3 = lambda ap: ap.rearrange("p (h v) -> p h v", v=128)
        tk = lambda s: (pfx, s)
        ss, sm, mean, var, rstd, msq = [stt[:, i, 0:nh] for i in range(6)]
        pg.op('act', lambda E: E.activation(sq, po, AF.Square), r=potoks, w=[tk('sq')])
        pg.op('dve', lambda E: E.tensor_reduce(ss, v3(sq), AX.X, ALU.add), r=[tk('sq')], w=[tk('ss')])
        if mode == 'gn':
            pg.op('dve', lambda E: E.tensor_reduce(sm, v3(po), AX.X, ALU.add), r=potoks, w=[tk('sm')])
            pg.op('dve', lambda E: E.tensor_scalar(mean, sm, 1.0 / 128, None, ALU.mult), r=[tk('sm')], w=[tk('mean')])
            pg.op('dve', lambda E: E.tensor_tensor(msq, mean, mean, ALU.mult), r=[tk('mean')], w=[tk('msq')])
            pg.op('dve', lambda E: E.scalar_tensor_tensor(var, ss, 1.0 / 128, msq, ALU.mult, ALU.subtract),
                  r=[tk('ss'), tk('msq')], w=[tk('var')])
        else:
            pg.op('dve', lambda E: E.tensor_scalar(var, ss, 1.0 / 128, None, ALU.mult), r=[tk('ss')], w=[tk('var')])
        pg.op('act', lambda E: E.activation(rstd, var, AF.Ln, bias=HN_EPS, scale=1.0), r=[tk('var')], w=[tk('rstd')])
        pg.op('act', lambda E: E.activation(rstd, rstd, AF.Exp, scale=-0.5), r=[tk('rstd')], w=[tk('rstd')])
        bc = lambda ap: ap.unsqueeze(2).to_broadcast([rows, nh, 128])
        if mode == 'gn':
            pg.op('dve', lambda E: E.tensor_tensor(v3(on), v3(po), bc(mean), ALU.subtract),
                  r=potoks + [tk('mean')], w=[tk('on')])
            pg.op('dve', lambda E: E.tensor_tensor(v3(on), v3(on), bc(rstd), ALU.mult),
                  r=[tk('on'), tk('rstd')], w=[tk('on')])
        else:
            pg.op('dve', lambda E: E.tensor_tensor(v3(on), v3(po), bc(rstd), ALU.mult),
                  r=potoks + [tk('rstd')], w=[tk('on')])
        pg.op('pool', lambda E: E.tensor_tensor(on, on, NG, ALU.mult), r=[tk('on'), ngtok], w=[tk('on')])
        pg.op('act', lambda E: E.activation(e, G, AF.Exp, scale=-1.0), r=[gtok], w=[tk('e')])
        pg.op('pool', lambda E: E.tensor_scalar(e, e, 1.0, None, ALU.add), r=[tk('e')], w=[tk('e')])
        pg.op('dve', lambda E: E.reciprocal(e, e), r=[tk('e')], w=[tk('e')])
        pg.op('pool', lambda E: E.tensor_tensor(e, e, G, ALU.mult), r=[tk('e'), gtok], w=[tk('e')])
        pg.op('pool', lambda E: E.tensor_tensor(Y, on, e, ALU.mult), r=[tk('on'), tk('e')], w=[(pfx + '_y', yi)])
        return d['y'][yi]

    def ret_phase(self, l):
        nc, pg = self.nc, self.pg
        SC = 128.0 ** -0.5
        with ExitStack() as st:
            cst = self.sb(st, 'rt_c', [P, 5, P], F32)
            vec = self.sb(st, 'rt_vec', [P, 2, P], F32)
            pcol = self.sb(st, 'rt_pcol', [P, 2], F32)
            lg = self.sb(st, 'rt_lg', [P, 8], F32)
            GL = self.sb(st, 'rt_GL', [P, 8], F32)
            vd = self.sb(st, 'rt_vd', [P, 8], F32)
            DT = self.sb(st, 'rt_DT', [P, 4, P], F32)
            tmpD = self.sb(st, 'rt_tmpD', [P, P], F32)
            dec = self.sb(st, 'rt_dec', [P, 2, 4, P], F32)
            NG = self.sb(st, 'rt_ng', [P, 512], F32)
            prevF = self.sb(st, 'rt_prevF', [P, NT, 512], BF16)
            yst = self.sb(st, 'rt_yst', [P, 4, S], BF16)
            Fs = self.sb(st, 'rt_F', [P, 512], F32)
            Bs = self.sb(st, 'rt_B', [P, 512], F32)
            tmpS = self.sb(st, 'rt_tmpS', [P, 512], F32)
            pB = [self.sb(st, f'rt_pB{i}', [P, 512], BF16) for i in range(2)]
            Kt = [self.sb(st, f'rt_Kt{i}', [P, 512], BF16) for i in range(2)]
            Vt = [self.sb(st, f'rt_Vt{i}', [P, 512], BF16) for i in range(2)]
            Gt = [self.sb(st, f'rt_Gt{i}', [P, 512], BF16) for i in range(2)]
            Qf = [self.sb(st, f'rt_Qf{i}', [P, 4, P], BF16) for i in range(2)]
            Kf = [self.sb(st, f'rt_Kf{i}', [P, 4, P], BF16) for i in range(2)]
            vS = [self.sb(st, f'rt_vS{i}', [P, 512], BF16) for i in range(2)]
            aTm = [self.sb(st, f'rt_aTm{i}', [P, 512], BF16) for i in range(2)]
            qF = [self.sb(st, f'rt_qF{i}', [P, 4, P], BF16) for i in range(2)]
            qB = [self.sb(st, f'rt_qB{i}', [P, 4, P], BF16) for i in range(2)]
            nd = self.alloc_norm(st, 'rt_n', P)
            ps_a = self.psum(st, 'rt_ps_a', [P, 2, 512], F32)
            ps_o = self.psum(st, 'rt_ps_o', [P, 2, 512], F32)
            ps_kv = self.psum(st, 'rt_ps_kv', [P, 2, 512], F32)
            ps_t = self.psum(st, 'rt_ps_t', [P, 2, 512], BF16)
            pg.dma('sp', cst[:].rearrange("p a b -> p (a b)"), self.I('c_ret_c'), w=['rt_c'])
            pg.dma('sp', vec[:].rearrange("p a b -> p (a b)"), self.I('c_ret_vec'), w=['rt_vec'])
            pg.dma('sp', pcol[:], self.I('c_ret_pcol'), w=['rt_pcol'])
            pg.dma('sp', NG[:], self.I('ret_norm_g')[l].partition_broadcast(P), w=['rt_ng'])
            pg.dma('sp', lg[:], self.I('ret_decay_logit')[l].rearrange("a b -> (a b)").partition_broadcast(P), w=['rt_lg'])
            pg.op('act', lambda E: E.activation(lg[:], lg[:], AF.Exp, scale=-1.0), r=['rt_lg'], w=['rt_lg'])
            pg.op('dve', lambda E: E.tensor_scalar(lg[:], lg[:], 1.0, None, ALU.add), r=['rt_lg'], w=['rt_lg'])
            pg.op('act', lambda E: E.activation(lg[:], lg[:], AF.Ln), r=['rt_lg'], w=['rt_lg'])
            pg.op('dve', lambda E: E.tensor_scalar(lg[:], lg[:], -1.0, None, ALU.mult), r=['rt_lg'], w=['rt_lg'])
            pg.op('act', lambda E: E.activation(GL[:], lg[:], AF.Exp, scale=128.0), r=['rt_lg'], w=['rt_GL'])
            lnsc = float(np.log(SC))
            for h in range(4):
                pg.op('act', lambda E, h=h: E.activation(vd[:, h:h + 1], pcol[:, 0:1], AF.Exp, scale=lg[:, h:h + 1], bias=lnsc),
                      r=['rt_lg', 'rt_pcol'], w=[('rt_vd', h)])
                pg.op('act', lambda E, h=h: E.activation(vd[:, 4 + h:5 + h], pcol[:, 1:2], AF.Exp, scale=lg[:, 4 + h:5 + h], bias=lnsc),
                      r=['rt_lg', 'rt_pcol'], w=[('rt_vd', 4 + h)])
                pg.op('act', lambda E, h=h: E.activation(dec[:, 0, h, :], vec[:, 0, :], AF.Exp, scale=lg[:, h:h + 1]),
                      r=['rt_lg', 'rt_vec'], w=[('rt_dec', 0, h)])
                pg.op('act', lambda E, h=h: E.activation(dec[:, 1, h, :], vec[:, 1, :], AF.Exp, scale=lg[:, 4 + h:5 + h]),
                      r=['rt_lg', 'rt_vec'], w=[('rt_dec', 1, h)])
                pg.op('act', lambda E, h=h: E.activation(DT[:, h, :], cst[:, 0, :], AF.Exp, scale=lg[:, h:h + 1]),
                      r=['rt_lg', 'rt_c'], w=[('rt_DT', h)])
                pg.op('dve', lambda E, h=h: E.tensor_tensor(DT[:, h, :], DT[:, h, :], cst[:, 2, :], ALU.mult),
                      r=[('rt_DT', h), 'rt_c'], w=[('rt_DT', h)])
                pg.op('act', lambda E, h=h: E.activation(tmpD[:], cst[:, 1, :], AF.Exp, scale=lg[:, 4 + h:5 + h]),
                      r=['rt_lg', 'rt_c'], w=['rt_tmpD'])
                pg.op('dve', lambda E: E.tensor_tensor(tmpD[:], tmpD[:], cst[:, 3, :], ALU.mult),
                      r=['rt_tmpD', 'rt_c'], w=['rt_tmpD'])
                pg.op('dve', lambda E, h=h: E.tensor_tensor(DT[:, h, :], DT[:, h, :], tmpD[:], ALU.add),
                      r=[('rt_DT', h), 'rt_tmpD'], w=[('rt_DT', h)])
                pg.op('dve', lambda E, h=h: E.tensor_tensor(DT[:, h, :], DT[:, h, :], cst[:, 4, :], ALU.add),
                      r=[('rt_DT', h), 'rt_c'], w=[('rt_DT', h)])
                pg.op('dve', lambda E, h=h: E.tensor_scalar(DT[:, h, :], DT[:, h, :], SC, None, ALU.mult),
                      r=[('rt_DT', h)], w=[('rt_DT', h)])
            DTt = [('rt_DT', h) for h in range(4)]
            vdt = [('rt_vd', h) for h in range(8)]
            dect = [('rt_dec', a, h) for a in range(2) for h in range(4)]
            pg.op('pool', lambda E: E.memset(Fs[:], 0.0), w=['rt_F'])
            pg.op('pool', lambda E: E.memset(Bs[:], 0.0), w=['rt_B'])
            ktv = self.dr['pt_rkt'].ap().rearrange("(n p) c -> n p c", p=P)
            vtv = self.dr['pt_rv'].ap().rearrange("(n p) c -> n p c", p=P)
            gtv = self.dr['pt_rg'].ap().rearrange("(n p) c -> n p c", p=P)
            qfv = self.dr['pf_rq'].ap().rearrange("(h p) t -> p h t", p=P)
            kfv = self.dr['pf_rk'].ap().rearrange("(h p) t -> p h t", p=P)
            v3 = lambda ap: ap.rearrange("p (h v) -> p h v", v=128)
            bc4 = lambda ap: ap.unsqueeze(2).to_broadcast([P, 4, 128])
            it = 0

            def kv_step(n, i, K, V, vdcols, state, stok, GLcols):
                VS = vS[i]
                pg.op('pool', lambda E: E.tensor_tensor(v3(VS[:]), v3(V[:]), bc4(vdcols), ALU.mult),
                      r=[('rt_Vt', i)] + vdt, w=[('rt_vS', i)])
                def mm(E):
                    ins = None
                    for h in range(4):
                        ins = E.matmul(ps_kv[:, i, h * P:(h + 1) * P], K[:, h * P:(h + 1) * P], VS[:, h * P:(h + 1) * P],
                                       start=True, stop=True)
                    return ins
                pg.op('pe', mm, r=[('rt_Kt', i), ('rt_vS', i)], w=[('rt_ps_kv', i)])
                pg.op('pool', lambda E: E.tensor_tensor(v3(tmpS[:]), v3(state[:]), bc4(GLcols), ALU.mult),
                      r=[stok, 'rt_GL'], w=['rt_tmpS'])
                pg.op('dve', lambda E: E.tensor_tensor(state[:], tmpS[:], ps_kv[:, i, :], ALU.add),
                      r=['rt_tmpS', ('rt_ps_kv', i)], w=[stok])

            for n in range(NT):
                i = it % 2; it += 1
                pg.dma('sp', Kt[i][:], ktv[n], r=[('pt_rkt', n // 4)], w=[('rt_Kt', i)])
                pg.dma('sp', Vt[i][:], vtv[n], r=[('pt_rv', n // 4)], w=[('rt_Vt', i)])
                pg.op('act', lambda E, n=n: E.copy(prevF[:, n, :], Fs[:]), r=['rt_F'], w=[('rt_prevF', n)])
                kv_step(n, i, Kt[i], Vt[i], vd[:, 0:4], Fs, 'rt_F', GL[:, 0:4])
            for n in range(NT - 1, -1, -1):
                i = it % 2; it += 1
                tsl = slice(n * P, (n + 1) * P)
                pg.dma('sp', Kt[i][:], ktv[n], r=[('pt_rkt', n // 4)], w=[('rt_Kt', i)])
                pg.dma('sp', Vt[i][:], vtv[n], r=[('pt_rv', n // 4)], w=[('rt_Vt', i)])
                pg.dma('sp', Gt[i][:], gtv[n], r=[('pt_rg', n // 4)], w=[('rt_Gt', i)])
                pg.dma('sp', Qf[i][:], qfv[:, :, tsl], r=[('pf_rq', h) for h in range(4)], w=[('rt_Qf', i)])
                pg.dma('sp', Kf[i][:], kfv[:, :, tsl], r=[('pf_rk', h) for h in range(4)], w=[('rt_Kf', i)])
                def mma(E, i=i):
                    ins = None
                    for h in range(4):
                        ins = E.matmul(ps_a[:, i, h * P:(h + 1) * P], Kf[i][:, h, :], Qf[i][:, h, :], start=True, stop=True)
                    return ins
                pg.op('pe', mma, r=[('rt_Qf', i), ('rt_Kf', i)], w=[('rt_ps_a', i)])
                pg.op('dve', lambda E, i=i: E.tensor_tensor(aTm[i][:], ps_a[:, i, :], DT[:].rearrange("p h t -> p (h t)"), ALU.mult),
                      r=[('rt_ps_a', i)] + DTt, w=[('rt_aTm', i)])
                pg.op('pool', lambda E, i=i: E.tensor_tensor(qF[i][:], Qf[i][:], dec[:, 0, :, :], ALU.mult),
                      r=[('rt_Qf', i)] + dect, w=[('rt_qF', i)])
                pg.op('pool', lambda E, i=i: E.tensor_tensor(qB[i][:], Qf[i][:], dec[:, 1, :, :], ALU.mult),
                      r=[('rt_Qf', i)] + dect, w=[('rt_qB', i)])
                pg.op('act', lambda E, i=i: E.copy(pB[i][:], Bs[:]), r=['rt_B'], w=[('rt_pB', i)])
                def mmo(E, i=i, n=n):
                    ins = None
                    for h in range(4):
                        hs = slice(h * P, (h + 1) * P)
                        E.matmul(ps_o[:, i, hs], aTm[i][:, hs], Vt[i][:, hs], start=True, stop=False)
                        E.matmul(ps_o[:, i, hs], qF[i][:, h, :], prevF[:, n, hs], start=False, stop=False)
                        ins = E.matmul(ps_o[:, i, hs], qB[i][:, h, :], pB[i][:, hs], start=False, stop=True)
                    return ins
                pg.op('pe', mmo, r=[('rt_aTm', i), ('rt_Vt', i), ('rt_qF', i), ('rt_qB', i), ('rt_prevF', n), ('rt_pB', i)],
                      w=[('rt_ps_o', i)])
                kv_step(n, i, Kt[i], Vt[i], vd[:, 4:8], Bs, 'rt_B', GL[:, 4:8])
                Y = self.norm_gate(nd, ps_o[:, i, :], [('rt_ps_o', i)], Gt[i][:], ('rt_Gt', i), NG[:], 'rt_ng',
                                   'gn', i, P, 4)
                self.transpose_out(Y[:], ('rt_n_y', i), ps_t, 'rt_ps_t', yst, 'rt_yst', n, i)
            self.store_yT(yst, 'rt_yst', 1536)


    def hgrn_phase(self, l):
        nc, pg = self.nc, self.pg
        NCH = S // 64
        with ExitStack() as outer:
            DEC = self.sb(outer, 'hg_DEC', [P, 2, 3, 4, NCH], F32)
            dect = [('hg_DEC', d_, hd) for d_ in range(2) for hd in range(4)]
            with ExitStack() as st:
                lb = self.sb(st, 'hg_lb', [P, 4], F32)
                oml = self.sb(st, 'hg_oml', [P, 4], F32)
                a0 = self.sb(st, 'hg_a0', [P, 4], F32)
                cm = self.sb(st, 'hg_cm', [P, S], BF16)
                T = [self.sb(st, f'hg_T{i}', [P, S], F32) for i in range(4)]
                tmpd = self.sb(st, 'hg_tmpd', [P, NCH], F32)
                zb = [self.sb(st, f'hg_z{i}', [P, S], BF16) for i in range(2)]
                qb = [self.sb(st, f'hg_qin{i}', [P, S], BF16) for i in range(2)]
                qo = [self.sb(st, f'hg_qo{i}', [P, S], BF16) for i in range(2)]
                ko = [self.sb(st, f'hg_ko{i}', [P, S], BF16) for i in range(2)]
                pg.dma('sp', cm[:], self.I('c_hg_cm'), w=['hg_cm'])
                if l == 0:
                    pg.op('pool', lambda E: E.memset(lb[:], 0.0), w=['hg_lb'])
                else:
                    self.load_chan(a0[:], self.I('hgrn_lb')[0], 'hg_a0')
                    self.load_chan(lb[:], self.I('hgrn_lb')[1], 'hg_lb')
                    pg.op('dve', lambda E: E.tensor_tensor(lb[:], a0[:], lb[:], ALU.subtract), r=['hg_a0', 'hg_lb'], w=['hg_lb'])
                    pg.op('act', lambda E: E.activation(lb[:], lb[:], AF.Exp), r=['hg_lb'], w=['hg_lb'])
                    pg.op('dve', lambda E: E.tensor_scalar(lb[:], lb[:], 1.0, None, ALU.add), r=['hg_lb'], w=['hg_lb'])
                    pg.op('dve', lambda E: E.reciprocal(lb[:], lb[:]), r=['hg_lb'], w=['hg_lb'])
                pg.op('dve', lambda E: E.tensor_scalar(oml[:], lb[:], -1.0, 1.0, ALU.mult, ALU.add), r=['hg_lb'], w=['hg_oml'])
                it = 0
                for d_ in range(2):
                    zname = 'pf_gzf' if d_ == 0 else 'pf_gzb'
                    mid, last = (31, 63) if d_ == 0 else (32, 0)
                    for hd in range(4):
                        i = it % 2; it += 1
                        rows = slice(hd * P, (hd + 1) * P)
                        T1, T2, T3, T4 = T
                        pg.dma('sp', zb[i][:], self.dr[zname].ap()[rows, :], r=[(zname, hd)], w=[('hg_z', i)])
                        pg.dma('sp', qb[i][:], self.dr['pf_gq'].ap()[rows, :], r=[('pf_gq', hd)], w=[('hg_qin', i)])
                        pg.op('act', lambda E, i=i: E.activation(T1[:], zb[i][:], AF.Exp, scale=-1.0), r=[('hg_z', i)], w=['hg_T1'])
                        pg.op('pool', lambda E: E.tensor_scalar(T1[:], T1[:], 1.0, None, ALU.add), r=['hg_T1'], w=['hg_T1'])
                        pg.op('dve', lambda E: E.reciprocal(T1[:], T1[:]), r=['hg_T1'], w=['hg_T1'])
                        pg.op('dve', lambda E, hd=hd: E.tensor_scalar(T1[:], T1[:], oml[:, hd:hd + 1], lb[:, hd:hd + 1], ALU.mult, ALU.add),
                              r=['hg_T1', 'hg_lb', 'hg_oml'], w=['hg_T1'])
                        pg.op('act', lambda E: E.activation(T2[:], T1[:], AF.Ln), r=['hg_T1'], w=['hg_T2'])
                        pg.op('dve', lambda E: E.tensor_tensor_scan(T3[:], cm[:], T2[:], 0.0, ALU.mult, ALU.add),
                              r=['hg_cm', 'hg_T2'], w=['hg_T3'])
                        c3 = lambda ap: ap.rearrange("p (n c) -> p n c", c=64)
                        if d_ == 0:
                            Bt, Btok = T3, 'hg_T3'
                        else:
                            pg.op('dve', lambda E: E.tensor_tensor(c3(T4[:]), c3(T3[:])[:, :, 63:64].to_broadcast([P, NCH, 64]),
                                                                   c3(T3[:]), ALU.subtract), r=['hg_T3'], w=['hg_T4'])
                            pg.op('pool', lambda E: E.tensor_tensor(T4[:], T4[:], T2[:], ALU.add), r=['hg_T4', 'hg_T2'], w=['hg_T4'])
                            Bt, Btok = T4, 'hg_T4'
                        B3 = c3(Bt[:])
                        dk = ('hg_DEC', d_, hd)
                        pg.op('act', lambda E, B3=B3, d_=d_, hd=hd, last=last: E.activation(DEC[:, d_, 0, hd, :], B3[:, :, last], AF.Exp),
                              r=[Btok], w=[dk])
                        pg.op('act', lambda E, B3=B3, d_=d_, hd=hd, mid=mid: E.activation(DEC[:, d_, 1, hd, :], B3[:, :, mid], AF.Exp),
                              r=[Btok], w=[dk])
                        pg.op('dve', lambda E, B3=B3, mid=mid, last=last: E.tensor_tensor(tmpd[:], B3[:, :, last], B3[:, :, mid], ALU.subtract),
                              r=[Btok], w=['hg_tmpd'])
                        pg.op('act', lambda E, d_=d_, hd=hd: E.activation(DEC[:, d_, 2, hd, :], tmpd[:], AF.Exp),
                              r=['hg_tmpd'], w=[dk])
                        pg.op('dve', lambda E, B3=B3, mid=mid: E.tensor_tensor(c3(T2[:]), B3, B3[:, :, mid:mid + 1].to_broadcast([P, NCH, 64]),
                                                                               ALU.subtract), r=[Btok, 'hg_T2'], w=['hg_T2'])
                        EP, EPtok = (T4, 'hg_T4') if d_ == 0 else (T3, 'hg_T3')
                        pg.op('act', lambda E, EP=EP: E.activation(EP[:], T2[:], AF.Exp), r=['hg_T2', Btok], w=[EPtok])
                        pg.op('pool', lambda E, i=i, EP=EP: E.tensor_tensor(qo[i][:], qb[i][:], EP[:], ALU.mult),
                              r=[('hg_qin', i), EPtok], w=[('hg_qo', i)])
                        pg.dma('sp', self.dr[f'hg_q{d_}'].ap()[rows, :], qo[i][:], r=[('hg_qo', i)], w=[(f'hg_q{d_}', hd)])
                        EM, EMtok = (T3, 'hg_T3') if d_ == 0 else (T4, 'hg_T4')
                        pg.op('act', lambda E, EM=EM: E.activation(EM[:], T2[:], AF.Exp, scale=-1.0), r=['hg_T2', EPtok, ('hg_qo', i)], w=[EMtok])
                        pg.op('pool', lambda E: E.tensor_scalar(T1[:], T1[:], -1.0, 1.0, ALU.mult, ALU.add), r=['hg_T1'], w=['hg_T1'])
                        pg.op('dve', lambda E, i=i, EM=EM: E.tensor_tensor(ko[i][:], T1[:], EM[:], ALU.mult),
                              r=['hg_T1', EMtok], w=[('hg_ko', i)])
                        pg.dma('sp', self.dr[f'hg_k{d_}'].ap()[rows, :], ko[i][:], r=[('hg_ko', i)], w=[(f'hg_k{d_}', hd)])
            pg.barrier()
            v3 = lambda ap: ap.rearrange("p (h v) -> p h v", v=128)
            with ExitStack() as st:
                Sst = self.sb(st, 'hs_S', [P, 512], F32)
                t2 = self.sb(st, 'hs_t2', [P, 512], F32)
                Sbf = [self.sb(st, f'hs_Sbf{i}', [P, 512], BF16) for i in range(2)]
                kblk = [self.sb(st, f'hs_kb{i}', [P, 4, P], BF16) for i in range(2)]
                ktok = [self.sb(st, f'hs_kt{i}', [P, 512], BF16) for i in range(2)]
                gi = [self.sb(st, f'hs_gi{i}', [P, 512], BF16) for i in range(2)]
                ps_t = self.psum(st, 'hs_ps_t', [P, 2, 512], BF16)
                ps_kv = self.psum(st, 'hs_ps_kv', [P, 2, 512], F32)
                giv = self.dr['pt_gi'].ap().rearrange("(n p) c -> n p c", p=P)
                it = 0
                ic = 0
                for d_ in range(2):
                    kv_ = self.dr[f'hg_k{d_}'].ap().rearrange("(h p) t -> p h t", p=P)
                    Sd = self.dr[f'hg_S{d_}'].ap()
                    pg.op('pool', lambda E: E.memset(Sst[:], 0.0), r=[], w=['hs_S'])
                    order = range(NT) if d_ == 0 else range(NT - 1, -1, -1)
                    for tt in order:
                        i = it % 2; it += 1
                        pg.dma('sp', kblk[i][:], kv_[:, :, tt * P:(tt + 1) * P], r=[(f'hg_k{d_}', hd) for hd in range(4)], w=[('hs_kb', i)])
                        pg.dma('sp', gi[i][:], giv[tt], r=[('pt_gi', tt // 4)], w=[('hs_gi', i)])
                        def tr(E, i=i):
                            ins = None
                            for h in range(4):
                                ins = E.transpose(ps_t[:, i, h * P:(h + 1) * P], kblk[i][:, h, :], self.ident_b[:])
                            return ins
                        pg.op('pe', tr, r=[('hs_kb', i), 'ident_b'], w=[('hs_ps_t', i)])
                        pg.op('act', lambda E, i=i: E.copy(ktok[i][:], ps_t[:, i, :]), r=[('hs_ps_t', i)], w=[('hs_kt', i)])
                        for c in ((0, 1) if d_ == 0 else (1, 0)):
                            n = tt * 2 + c
                            j = ic % 2; ic += 1
                            r0 = c * 64
                            def mm(E, i=i, j=j, r0=r0):
                                ins = None
                                for h in range(4):
                                    hs = slice(h * P, (h + 1) * P)
                                    ins = E.matmul(ps_kv[:, j, hs], ktok[i][r0:r0 + 64, hs], gi[i][r0:r0 + 64, hs], start=True, stop=True)
                                return ins
                            pg.op('pe', mm, r=[('hs_kt', i), ('hs_gi', i)], w=[('hs_ps_kv', j)])
                            dbc = lambda kind, n=n, d_=d_: DEC[:, d_, kind, :, n].unsqueeze(2).to_broadcast([P, 4, 128])
                            pg.op('pool', lambda E, j=j, dbc=dbc: E.tensor_tensor(v3(Sbf[j][:]), v3(Sst[:]), dbc(1), ALU.mult),
                                  r=['hs_S'] + dect, w=[('hs_Sbf', j)])
                            pg.dma('sp', Sd[n], Sbf[j][:], r=[('hs_Sbf', j)], w=[(f'hg_S{d_}', n)])
                            pg.op('dve', lambda E, j=j, dbc=dbc: E.tensor_tensor(v3(t2[:]), v3(ps_kv[:, j, :]), dbc(2), ALU.mult),
                                  r=[('hs_ps_kv', j)] + dect, w=['hs_t2'])
                            pg.op('pool', lambda E, dbc=dbc: E.tensor_tensor(v3(Sst[:]), v3(Sst[:]), dbc(0), ALU.mult),
                                  r=['hs_S'] + dect, w=['hs_S'])
                            pg.op('dve', lambda E: E.tensor_tensor(Sst[:], Sst[:], t2[:], ALU.add), r=['hs_S', 'hs_t2'], w=['hs_S'])
            pg.barrier()
            with ExitStack() as st:
                H = 64
                mask = self.sb(st, 'ho_mask', [H, 2, 64], F32)
                NG = self.sb(st, 'ho_ng', [H, 2, 512], F32)
                yst = self.sb(st, 'ho_yst', [P, 4, S], BF16)
                blk = {}
                for nm in ('q0', 'k0', 'q1', 'k1'):
                    blk[nm] = [self.sb(st, f'ho_{nm}_{i}', [P, 4, P], BF16) for i in range(2)]
                gi = [self.sb(st, f'ho_gi{i}', [H, 2, 512], BF16) for i in range(2)]
                go = [self.sb(st, f'ho_go{i}', [H, 2, 512], BF16) for i in range(2)]
                Sb = [[self.sb(st, f'ho_S{d_}_{i}', [P, 2, 512], BF16) for i in range(2)] for d_ in range(2)]
                aTm = [self.sb(st, f'ho_aTm{i}', [H, 2, 2, 4, 64], BF16) for i in range(2)]
                nd = self.alloc_norm(st, 'ho_n', H, nslot=2)
                ps_a = self.psum(st, 'ho_ps_a', [H, 2, 2, 4, 64], F32)
                ps_o = self.psum(st, 'ho_ps_o', [H, 2, 2, 512], F32)
                ps_t = self.psum(st, 'ho_ps_t', [P, 2, 512], BF16)
                pg.dma('sp', mask[:].rearrange("p a b -> p (a b)"), self.I('c_hg_mask'), w=['ho_mask'])
                for c in range(2):
                    pg.dma('sp', NG[:, c, :], self.I('hgrn_norm_g')[l].partition_broadcast(H), w=[('ho_ng', c)])
                ngt = [('ho_ng', c) for c in range(2)]
                giv = self.dr['pt_gi'].ap().rearrange("(n c p) v -> n p c v", p=H, c=2)
                gov = self.dr['pt_go'].ap().rearrange("(n c p) v -> n p c v", p=H, c=2)
                fm = {nm: self.dr['hg_' + nm].ap().rearrange("(h p) t -> p h t", p=P) for nm in blk}
                Sv = [self.dr[f'hg_S{d_}'].ap().rearrange("(n c) p v -> n p c v", c=2) for d_ in range(2)]
                for tt in range(NT):
                    i = tt % 2
                    tsl = slice(tt * P, (tt + 1) * P)
                    for nm in blk:
                        pg.dma('sp', blk[nm][i][:], fm[nm][:, :, tsl], r=[('hg_' + nm, hd) for hd in range(4)], w=[('ho_' + nm, i)])
                    pg.dma('sp', gi[i][:], giv[tt], r=[('pt_gi', tt // 4)], w=[('ho_gi', i)])
                    pg.dma('sp', go[i][:], gov[tt], r=[('pt_go', tt // 4)], w=[('ho_go', i)])
                    for d_ in range(2):
                        pg.dma('sp', Sb[d_][i][:], Sv[d_][tt], r=[(f'hg_S{d_}', 2 * tt), (f'hg_S{d_}', 2 * tt + 1)], w=[('ho_S', d_, i)])
                    def mma(E, i=i):
                        ins = None
                        for c in range(2):
                            cs = slice(c * 64, (c + 1) * 64)
                            for d_ in range(2):
                                for h in range(4):
                                    ins = E.matmul(ps_a[:, c, d_, h, :], blk[f'k{d_}'][i][:, h, cs], blk[f'q{d_}'][i][:, h, cs],
                                                   start=True, stop=True)
                        return ins
                    pg.op('pe', mma, r=[('ho_' + nm, i) for nm in blk], w=['ho_ps_a'])
                    for c in range(2):
                        pg.op('dve', lambda E, i=i, c=c: E.tensor_tensor(
                            aTm[i][:, c].rearrange("p d h t -> p d h t"), ps_a[:, c],
                            mask[:].unsqueeze(2).to_broadcast([H, 2, 4, 64]), ALU.mult),
                            r=['ho_ps_a', 'ho_mask'], w=[('ho_aTm', i, c)])
                    def mmo(E, i=i):
                        ins = None
                        for c in range(2):
                            cs = slice(c * 64, (c + 1) * 64)
                            for h in range(4):
                                hs = slice(h * P, (h + 1) * P)
                                o = ps_o[:, i, c, hs]
                                E.matmul(o, aTm[i][:, c, 0, h, :], gi[i][:, c, hs], start=True, stop=False)
                                E.matmul(o, aTm[i][:, c, 1, h, :], gi[i][:, c, hs], start=False, stop=False)
                                E.matmul(o, blk['q0'][i][:, h, cs], Sb[0][i][:, c, hs], start=False, stop=False)
                                ins = E.matmul(o, blk['q1'][i][:, h, cs], Sb[1][i][:, c, hs], start=False, stop=True)
                        return ins
                    pg.op('pe', mmo, r=[('ho_aTm', i, 0), ('ho_aTm', i, 1), ('ho_gi', i), ('ho_q0', i), ('ho_q1', i),
                                        ('ho_S', 0, i), ('ho_S', 1, i)], w=[('ho_ps_o', i)])
                    Y = self.norm_gate(nd, ps_o[:, i].rearrange("p c v -> p (c v)"), [('ho_ps_o', i)],
                                       go[i][:].rearrange("p c v -> p (c v)"), ('ho_go', i),
                                       NG[:].rearrange("p c v -> p (c v)"), ngt[0], 'rms', i, H, 8)
                    def tr(E, i=i, Y=Y):
                        ins = None
                        for c in range(2):
                            for cc in range(4):
                                ins = E.transpose(ps_t[:, i, cc * P + c * 64: cc * P + (c + 1) * 64],
                                                  Y[:, c * 512 + cc * P: c * 512 + (cc + 1) * P], self.ident_b[0:H, 0:H])
                        return ins
                    pg.op('pe', tr, r=[('ho_n_y', i), 'ident_b'], w=[('ho_ps_t', i)])
                    pg.op('act', lambda E, i=i, tsl=tsl: E.copy(yst[:, :, tsl], ps_t[:, i, :].rearrange("p (c t) -> p c t", c=4)),
                          r=[('ho_ps_t', i)], w=[('ho_yst', tt)])
                self.store_yT(yst, 'ho_yst', 1024)


    def outproj_phase(self, l):
        nc, pg = self.nc, self.pg
        with ExitStack() as st:
            W = self.sb(st, 'op_w', [P, KC, D], BF16)
            wv = self.I('w_out')[l].rearrange("(k p) n -> p k n", p=P)
            for j in range(4):
                pg.dma('pool', W[:, :, j * 512:(j + 1) * 512], wv[:, :, j * 512:(j + 1) * 512], w=[('op_w', j)])
            wt = [('op_w', j) for j in range(4)]
            yT = [self.sb(st, f'op_y{i}', [P, KC, P], BF16) for i in range(2)]
            so = [self.sb(st, f'op_o{i}', [P, D], F32) for i in range(2)]
            ps = self.psum(st, 'op_ps', [P, 2, 4, 512], F32)
            yv = self.dr['yT_dram'].ap().rearrange("(k p) t -> p k t", p=P)
            rv = self.dr['res_dram'].ap().rearrange("(n p) d -> n p d", p=P)
            for t in range(NT):
                i = t % 2
                pg.dma('sp', yT[i][:], yv[:, :, t * P:(t + 1) * P], r=[('yT_dram', c) for c in range(KC)], w=[('op_y', i)])
                for j in range(4):
                    def mm(E, i=i, j=j):
                        ins = None
                        for k in range(KC):
                            ins = E.matmul(ps[:, i, j, :], yT[i][:, k, :], W[:, k, j * 512:(j + 1) * 512],
                                           start=(k == 0), stop=(k == KC - 1))
                        return ins
                    pg.op('pe', mm, r=[('op_y', i)] + wt, w=[('op_ps', i, j)])
                    o_ap = so[i][:, j * 512:(j + 1) * 512]
                    if j % 2 == 0:
                        pg.op('act', lambda E, o=o_ap, a=ps[:, i, j, :]: E.copy(o, a), r=[('op_ps', i, j)], w=[('op_o', i, j)])
                    else:
                        pg.op('dve', lambda E, o=o_ap, a=ps[:, i, j, :]: E.tensor_copy(o, a), r=[('op_ps', i, j)], w=[('op_o', i, j)])
                pg.dma('sp', rv[t], so[i][:], r=[('op_o', i, j) for j in range(4)], w=[('res_dram', t)])

    def moe_phase(self, l):
        nc, pg = self.nc, self.pg
        TBS = 1024
        NTB = S // TBS
        TPB = TBS // P
        with ExitStack() as st:
            hTb = self.sb(st, 'mo_h', [P, KC, TBS], BF16)
            acc = self.sb(st, 'mo_acc', [P, TPB, D], F32)
            hid = self.sb(st, 'mo_hid', [P, 8, TBS], BF16)
            wg = [self.sb(st, f'mo_wg{i}', [P, KC, 256], BF16) for i in range(2)]
            wu = [self.sb(st, f'mo_wu{i}', [P, KC, 256], BF16) for i in range(2)]
            wd = [self.sb(st, f'mo_wd{i}', [P, 8, 512], BF16) for i in range(2)]
            sg = [self.sb(st, f'mo_sg{i}', [P, 512], F32) for i in range(2)]
            ps_gu = self.psum(st, 'mo_ps_gu', [P, 2, 2, 512], F32)
            ps_d = self.psum(st, 'mo_ps_d', [P, 3, 512], F32)
            hTv = self.dr['hT_dram'].ap().rearrange("(k p) t -> p k t", p=P)
            rv = self.dr['res_dram'].ap().rearrange("(n j p) d -> n p j d", p=P, j=TPB)
            iw = 0; idw = 0; igu = 0; ipd = 0
            for tb in range(NTB):
                for k in range(KC):
                    pg.dma('sp', hTb[:, k, :], hTv[:, k, tb * TBS:(tb + 1) * TBS],
                           r=[('hT_dram', tb * 2), ('hT_dram', tb * 2 + 1)], w=[('mo_h', k)])
                ht = [('mo_h', k) for k in range(KC)]
                for e in range(NE):
                    wgv = self.I('w_gate')[l, e].rearrange("(k p) f -> p k f", p=P)
                    wuv = self.I('w_up')[l, e].rearrange("(k p) f -> p k f", p=P)
                    wdv = self.I('w_down')[l, e].rearrange("(c p) n -> p c n", p=P)
                    for hf in range(4):
                        wi = iw % 2; iw += 1
                        pg.dma('pool', wg[wi][:], wgv[:, :, hf * 256:(hf + 1) * 256], w=[('mo_wg', wi)])
                        pg.dma('pool', wu[wi][:], wuv[:, :, hf * 256:(hf + 1) * 256], w=[('mo_wu', wi)])
                        for f2 in range(2):
                            fc = hf * 2 + f2
                            for th in range(TBS // 512):
                                b = igu % 2; igu += 1
                                def mm(E, wi=wi, f2=f2, th=th, b=b):
                                    ins = None
                                    for k in range(KC):
                                        E.matmul(ps_gu[:, b, 0, :], wg[wi][:, k, f2 * P:(f2 + 1) * P], hTb[:, k, th * 512:(th + 1) * 512],
                                                 start=(k == 0), stop=(k == KC - 1))
                                    for k in range(KC):
                                        ins = E.matmul(ps_gu[:, b, 1, :], wu[wi][:, k, f2 * P:(f2 + 1) * P], hTb[:, k, th * 512:(th + 1) * 512],
                                                       start=(k == 0), stop=(k == KC - 1))
                                    return ins
                                pg.op('pe', mm, r=[('mo_wg', wi), ('mo_wu', wi)] + ht, w=[('mo_ps_gu', b)])
                                pg.op('act', lambda E, b=b: E.activation(sg[b][:], ps_gu[:, b, 0, :], AF.Silu),
                                      r=[('mo_ps_gu', b)], w=[('mo_sg', b)])
                                pg.op('dve', lambda E, b=b, fc=fc, th=th: E.tensor_tensor(hid[:, fc, th * 512:(th + 1) * 512], sg[b][:], ps_gu[:, b, 1, :], ALU.mult),
                                      r=[('mo_sg', b), ('mo_ps_gu', b)], w=[('mo_hid', fc, th)])
                    hidt = [('mo_hid', fc, th) for fc in range(8) for th in range(TBS // 512)]
                    for nch in range(4):
                        di = idw % 2; idw += 1
                        pg.dma('pool', wd[di][:], wdv[:, :, nch * 512:(nch + 1) * 512], w=[('mo_wd', di)])
                        for tl in range(TPB):
                            pb = ipd % 3; ipd += 1
                            tg = tb * TPB + tl
                            def mmd(E, di=di, tl=tl, pb=pb):
                                ins = None
                                for fc in range(8):
                                    ins = E.matmul(ps_d[:, pb, :], hid[:, fc, tl * P:(tl + 1) * P], wd[di][:, fc, :],
                                                   start=(fc == 0), stop=(fc == 7))
                                return ins
                            pg.op('pe', mmd, r=hidt + [('mo_wd', di)], w=[('mo_ps_d', pb)])
                            a_ap = acc[:, tl, nch * 512:(nch + 1) * 512]
                            if e == 0:
                                pg.op('dve', lambda E, a=a_ap, pb=pb, tg=tg, e=e: E.tensor_scalar(a, ps_d[:, pb, :], self.comb[:, tg, e:e + 1], None, ALU.mult),
                                      r=[('mo_ps_d', pb), ('comb', tg)], w=[('mo_acc', tl, nch)])
                            else:
                                pg.op('dve', lambda E, a=a_ap, pb=pb, tg=tg, e=e: E.scalar_tensor_tensor(a, ps_d[:, pb, :], self.comb[:, tg, e:e + 1], a, ALU.mult, ALU.add),
                                      r=[('mo_ps_d', pb), ('comb', tg)], w=[('mo_acc', tl, nch)])
                pg.dma('sp', rv[tb], acc[:], r=[('mo_acc', tl, nch) for tl in range(TPB) for nch in range(4)],
                       w=[('res_dram', tb * TPB + tl) for tl in range(TPB)])


_CACHE = {}


def make_in_map(inputs, core, b):
    m = {}
    for k in b.dr:
        if k.startswith('c_'):
            m[k] = b.consts[k[2:]]
        elif k in inputs:
            v = np.asarray(inputs[k])
            m[k] = np.ascontiguousarray(v[core]) if k == 'x' else np.ascontiguousarray(v)
    return m


def kernel(**inputs):
    b = Builder()
    nc = b.build()
    in_maps = [make_in_map(inputs, c, b) for c in range(8)]
    res = run_bass_kernel_spmd(nc, in_maps, core_ids=list(range(8)))
    return np.stack([np.asarray(r['out']) for r in res.results], axis=0).astype(np.float32)
```

```python
import numpy as np
import ml_dtypes
from contextlib import ExitStack
import concourse.bass as bass
import concourse.mybir as mybir
from concourse.bass_utils import run_bass_kernel_spmd

F32 = mybir.dt.float32
BF16 = mybir.dt.bfloat16
AF = mybir.ActivationFunctionType
ALU = mybir.AluOpType
AX = mybir.AxisListType

P = 128
S = 4096
D = 2048
NT = S // P
KC = D // P
DEPTH = 2
D_IN = 6912
NE = 16
DE = 1024
ALPHA = (2.0 * DEPTH) ** 0.25
LN_EPS = 1e-5
HN_EPS = 1e-6
NEG = -30000.0
CAP = 4096

C_AQ, C_AK, C_AV = 0, 512, 640
C_CB, C_CC, C_CH = 768, 1280, 1792
C_GQ, C_GZF, C_GZB, C_GI, C_GO = 2304, 2816, 3328, 3840, 4352
C_RQ, C_RK, C_RV, C_RG = 4864, 5376, 5888, 6400


class Prog:
    EPOCH = 30000

    def __init__(self, nc, es, n_dma_sems=12):
        self.nc = nc
        self.es = es
        self.E = {'pe': nc.tensor, 'act': nc.scalar, 'dve': nc.vector,
                  'pool': nc.gpsimd, 'sp': nc.sync}
        self.nsem = 0
        self.sem = {e: self._new_sem(e) for e in self.E}
        self.cnt = {e: 0 for e in self.E}
        self.known = {e: {} for e in self.E}
        self.semobj = {}
        self.tokw = {}
        self.tokr = {}
        self.dq = {}
        self._sem_owner = {}
        self._cstack = []
        for q in ('sp', 'pool', 'act'):
            self.dq[q] = {'sems': [self._new_sem('d' + q) for _ in range(n_dma_sems)],
                          'rr': 0}
            for s_ in self.dq[q]['sems']:
                self._sem_owner[id(s_)] = q
        self.dtarget = {}
        self.all_sems = {}
        self.n_ops = 0
        self.n_waits = 0

    def _new_sem(self, tag):
        self.nsem += 1
        s = self.es.enter_context(self.nc.semaphore(f"s_{tag}_{self.nsem}"))
        return s

    def _key(self, s):
        return id(s)

    def _collect(self, eng, reads, writes):
        need = {}
        def addh(h):
            k = self._key(h[0])
            if k not in need or need[k][1] < h[1]:
                need[k] = h
        for t in reads:
            for h in self.tokw.get(t, ()):
                addh(h)
        for t in writes:
            for h in self.tokw.get(t, ()):
                addh(h)
            for h in self.tokr.get(t, ()):
                addh(h)
        return need

    def _emit_waits(self, eng, need, skip_own_pe=True):
        kn = self.known[eng]
        acts = []
        for k, (s, v, src) in need.items():
            if eng == 'pe' and src == 'pe':
                continue
            if kn.get(k, 0) >= v:
                continue
            acts.append(('wait', s, v))
            kn[k] = v
            self.n_waits += 1
        return acts

    def _run(self, eng, acts):
        if self._cstack:
            assert eng in self._cstack[-1]['engines'], eng
            self._cstack[-1]['buf'][eng].extend(acts)
            return
        self._do(eng, acts)

    def _do(self, eng, acts):
        E = self.E[eng]
        for a in acts:
            if a[0] == 'wait':
                E.wait_ge(a[1], a[2])
            elif a[0] == 'ins':
                a[1](E).then_inc(a[2], a[3])
            elif a[0] == 'seminc':
                E.sem_inc(a[1], a[2])
            else:
                with E.If_cmp(a[1], a[2], "IS_GT"):
                    self._do(eng, a[3])
                with E.Else():
                    self._do(eng, a[4])

    def _update(self, h, reads, writes):
        k = self._key(h[0])
        for t in writes:
            self.tokw[t] = [h]
            self.tokr[t] = []
        for t in reads:
            if t in writes:
                continue
            lst = self.tokr.get(t)
            if lst is None:
                self.tokr[t] = [h]
            else:
                self.tokr[t] = [x for x in lst if self._key(x[0]) != k] + [h]

    def op(self, eng, fn, r=(), w=()):
        need = self._collect(eng, r, w)
        acts = self._emit_waits(eng, need)
        if self.cnt[eng] >= self.EPOCH:
            assert not self._cstack
            self.sem[eng] = self._new_sem(eng)
            self.cnt[eng] = 0
        self.cnt[eng] += 1
        acts.append(('ins', fn, self.sem[eng], 1))
        self._run(eng, acts)
        h = (self.sem[eng], self.cnt[eng], eng)
        self._update(h, r, w)
        self.n_ops += 1
        return h

    def dma(self, q, out, in_, r=(), w=(), fn=None, **kw):
        dq = self.dq[q]
        s = dq['sems'][dq['rr'] % len(dq['sems'])]
        dq['rr'] += 1
        prev = self.dtarget.get(self._key(s), 0)
        need = self._collect(q, r, w)
        if prev > 0:
            k = self._key(s)
            if k not in need or need[k][1] < prev:
                need[k] = (s, prev, 'dma')
        acts = self._emit_waits(q, need)
        tgt = prev + 16
        self.dtarget[self._key(s)] = tgt
        self.all_sems[self._key(s)] = s
        if fn is None:
            fn = lambda E, out=out, in_=in_, kw=kw: E.dma_start(out=out, in_=in_, **kw)
        acts.append(('ins', fn, s, 16))
        self._run(q, acts)
        h = (s, tgt, 'dma')
        self._update(h, r, w)
        self.n_ops += 1
        return h

    def cond_begin(self, regs, thresh, engines):
        for e in engines:
            assert self.cnt[e] < self.EPOCH - 4000 or self._cstack
        self._cstack.append({'engines': engines, 'regs': regs, 'thresh': thresh,
                             'cnt0': {e: (self.sem[e], self.cnt[e]) for e in engines},
                             'dt0': dict(self.dtarget),
                             'known0': {e: dict(self.known[e]) for e in self.E},
                             'buf': {e: [] for e in engines}})

    def cond_end(self, dma_issuers=('sp',)):
        c = self._cstack.pop()
        for e in c['engines']:
            s0, c0 = c['cnt0'][e]
            assert s0 is self.sem[e]
            n = self.cnt[e] - c0
            els = []
            if n > 0:
                if c0 > 0:
                    els.append(('wait', s0, c0))
                els.append(('seminc', s0, n))
            if e in dma_issuers:
                for k, tgt in self.dtarget.items():
                    t0 = c['dt0'].get(k, 0)
                    if tgt > t0 and self._sem_owner.get(k) == e:
                        if t0 > 0:
                            els.append(('wait', self.all_sems[k], t0))
                        els.append(('seminc', self.all_sems[k], tgt - t0))
            if not c['buf'][e] and not els:
                continue
            act = ('cond', c['regs'][e], c['thresh'], c['buf'][e], els)
            if self._cstack:
                self._cstack[-1]['buf'][e].append(act)
            else:
                self._do(e, [act])
        for e in self.E:
            self.known[e] = c['known0'][e]

    def barrier(self, engines=None, keep=()):
        engines = engines or list(self.E)
        need = {}
        for e in self.E:
            if self.cnt[e] > 0:
                need[self._key(self.sem[e])] = (self.sem[e], self.cnt[e], e)
        for k, s in self.all_sems.items():
            need[k] = (s, self.dtarget[k], 'dma')
        for e in engines:
            E = self.E[e]
            kn = self.known[e]
            for k, (s, v, src) in need.items():
                if src == e and e != 'sp':
                    pass
                if kn.get(k, 0) >= v:
                    continue
                E.wait_ge(s, v)
                kn[k] = v
        self.tokw.clear()
        self.tokr.clear()


def host_consts():
    c = {}
    c['ident_f'] = np.eye(P, dtype=np.float32)
    c['ident_b'] = np.eye(P, dtype=np.float32).astype(ml_dtypes.bfloat16)
    ab = np.zeros((P, 8, 3, P), np.float32)
    s_i = np.arange(P)[:, None]
    t_i = np.arange(P)[None, :]
    for h in range(8):
        slope = 2.0 ** (-(h + 1))
        for j in range(3):
            dist = (j - 1) * P + s_i - t_i
            ab[:, h, j, :] = np.where(np.abs(dist) <= 128, -slope * np.abs(dist), NEG)
    c['attn_bias'] = ab.reshape(P, 8 * 3 * P)
    rc = np.zeros((P, 5, P), np.float32)
    rc[:, 0] = np.maximum(t_i - s_i, 0)
    rc[:, 1] = np.maximum(s_i - t_i, 0)
    rc[:, 2] = (t_i > s_i)
    rc[:, 3] = (s_i > t_i)
    rc[:, 4] = 2.0 * np.eye(P)
    c['ret_c'] = rc.reshape(P, 5 * P)
    rv = np.zeros((P, 2, P), np.float32)
    rv[:, 0] = t_i + 1.0
    rv[:, 1] = 128.0 - t_i
    c['ret_vec'] = rv.reshape(P, 2 * P)
    cmk = np.ones((P, S), np.float32)
    cmk[:, ::64] = 0.0
    c['hg_cm'] = cmk.astype(ml_dtypes.bfloat16)
    s6 = np.arange(64)[:, None]
    t6 = np.arange(64)[None, :]
    hm = np.zeros((64, 2, 64), np.float32)
    hm[:, 0] = (s6 <= t6)
    hm[:, 1] = (s6 >= t6)
    c['hg_mask'] = hm.reshape(64, 128)
    c['ones_b'] = np.ones((P, P), np.float32).astype(ml_dtypes.bfloat16)
    c['ustrict_b'] = (s_i < t_i).astype(np.float32).astype(ml_dtypes.bfloat16)
    mE = np.ones((P, NE, NT), np.float32); mE[:, :, 0] = 0.0
    c['maskE'] = mE.reshape(P, NE * NT).astype(ml_dtypes.bfloat16)
    mS = np.ones((P, NT, NE), np.float32); mS[:, :, 0] = 0.0
    c['maskS'] = mS.reshape(P, NT * NE).astype(ml_dtypes.bfloat16)
    ec = np.zeros((P, NT, NE), np.float32); ec[:, :, :] = (np.arange(NE) * CAP)[None, None, :]
    c['ecap'] = ec.reshape(P, NT * NE)
    th_ = np.zeros((P, NE, NT), np.float32); th_[:, :, :] = (np.arange(NT) * 128.0)[None, None, :]
    c['thr'] = th_.reshape(P, NE * NT)
    c['ret_pcol'] = np.stack([127.0 - np.arange(P), np.arange(P) * 1.0], 1).astype(np.float32)
    return c


class Builder:
    def __init__(self, n_layers=DEPTH, stop=None, taps=(), skip=()):
        self.skip = set(skip)
        self.dense_moe = 'dense' in self.skip
        self.wlim = 2 if 'wlim' in self.skip else 99
        self.qlim = 8 if 'qlim' in self.skip else NT
        self.n_layers = n_layers
        self.stop = stop
        self.taps = set(taps)
        self.nc = bass.Bass("TRN2", target_bir_lowering=False)
        self.es = ExitStack()
        self.pg = Prog(self.nc, self.es)
        self.dr = {}
        self.consts = host_consts()

    def din(self, name, shape, dtype=F32):
        t = self.nc.dram_tensor(name, list(shape), dtype, kind="ExternalInput")
        self.dr[name] = t
        return t

    def dscr(self, name, shape, dtype):
        kind = "ExternalOutput" if name in self.taps else "Internal"
        t = self.nc.dram_tensor(name, list(shape), dtype, kind=kind)
        self.dr[name] = t
        return t

    def sb(self, st, name, shape, dtype):
        self._uid = getattr(self, '_uid', 0) + 1
        return st.enter_context(self.nc.sbuf_tensor(f"{name}_u{self._uid}", list(shape), dtype))

    def psum(self, st, name, shape, dtype=F32):
        self._uid = getattr(self, '_uid', 0) + 1
        return st.enter_context(self.nc.psum_tensor(f"{name}_u{self._uid}", list(shape), dtype))

    IN_SHAPES = {
        'x': [S, D], 'emb_ln_g': [D], 'emb_ln_b': [D], 'w_in': [DEPTH, D, D_IN],
        'attn_sink': [DEPTH, 8], 'conv_w': [DEPTH, 3, 512], 'hgrn_lb': [DEPTH, 512],
        'hgrn_norm_g': [DEPTH, 512], 'ret_decay_logit': [DEPTH, 2, 4], 'ret_norm_g': [DEPTH, 512],
        'w_out': [DEPTH, D, D], 'ln1_g': [DEPTH, D], 'ln1_b': [DEPTH, D],
        'router_w': [D, NE], 'router_b': [NE], 'w_gate': [DEPTH, NE, D, DE],
        'w_up': [DEPTH, NE, D, DE], 'w_down': [DEPTH, NE, DE, D],
        'ln2_g': [DEPTH, D], 'ln2_b': [DEPTH, D],
    }

    def I(self, name):
        if name not in self.dr:
            if name.startswith('c_'):
                v = self.consts[name[2:]]
                self.din(name, v.shape, BF16 if v.dtype == ml_dtypes.bfloat16 else F32)
            else:
                self.din(name, self.IN_SHAPES[name])
        return self.dr[name].ap()

    def declare(self):
        nc = self.nc
        self.out = nc.dram_tensor('out', [S, D], F32, kind="ExternalOutput")
        self.dscr('h_dram', [S, D], F32)
        self.dscr('hT_dram', [D, S], BF16)
        self.declare_proj()
        self.dscr('yT_dram', [D, S], BF16)
        self.dscr('res_dram', [S, D], F32)
        self.dscr('hb_dram', [S, D], BF16)
        self.dscr('bucket', [NE * CAP, D], BF16)
        self.dscr('ybucket', [NE * CAP, D], BF16)
        for d_ in range(2):
            self.dscr(f'hg_q{d_}', [512, S], BF16)
            self.dscr(f'hg_k{d_}', [512, S], BF16)
            self.dscr(f'hg_S{d_}', [S // 64, P, 512], BF16)

    def build(self):
        self.declare()
        nc, pg = self.nc, self.pg
        with ExitStack() as g:
            self.g = g
            self.ident_f = self.sb(g, 'ident_f', [P, P], F32)
            self.ident_b = self.sb(g, 'ident_b', [P, P], BF16)
            self.comb = self.sb(g, 'comb', [P, NT, NE], F32)
            self.comb_toks = [('comb', t) for t in range(NT)]
            I32 = mybir.dt.int32
            self.selm = self.sb(g, 'selm', [P, NT, NE], BF16)
            self.D0i = self.sb(g, 'D0i', [P, NT], I32)
            self.D1i = self.sb(g, 'D1i', [P, NT], I32)
            self.W0 = self.sb(g, 'W0', [P, NT], F32)
            self.W1 = self.sb(g, 'W1', [P, NT], F32)
            self.nti = self.sb(g, 'nti', [P, NE], I32)
            self.regs = {e: pg.E[e].alloc_register('r_nt_' + e) for e in ('pe', 'act', 'dve', 'sp')}
            pg.dma('sp', self.ident_f[:], self.I('c_ident_f'), w=['ident_f'])
            pg.dma('sp', self.ident_b[:], self.I('c_ident_b'), w=['ident_b'])
            self.ln_phase(src='x', g_ap=self.I('emb_ln_g'), b_ap=self.I('emb_ln_b'),
                          mode='x')
            pg.barrier()
            for l in range(self.n_layers):
                if self.stop == 'ln0':
                    break
                self.in_proj_phase(l)
                pg.barrier()
                if self.stop == 'inproj':
                    break
                if 'attn' not in self.skip:
                    self.attn_phase(l)
                    pg.barrier()
                if self.stop == 'attn':
                    break
                if 'conv' not in self.skip:
                    self.conv_phase(l)
                    pg.barrier()
                if 'ret' not in self.skip:
                    self.ret_phase(l)
                    pg.barrier()
                if self.stop in ('conv', 'ret'):
                    break
                if 'hgrn' not in self.skip:
                    self.hgrn_phase(l)
                    pg.barrier()
                if self.stop in ('hgrn', 'mix'):
                    break
                self.outproj_phase(l)
                pg.barrier()
                if self.stop == 'outproj':
                    break
                self.ln_phase(None, self.I('ln1_g')[l], self.I('ln1_b')[l], 'res', router=('norouter' not in self.skip))
                pg.barrier(keep=self.comb_toks)
                if self.stop == 'ln1':
                    break
                if self.dense_moe:
                    self.moe_phase(l)
                    pg.barrier()
                else:
                    self.route_finalize()
                    pg.barrier()
                    if self.stop == 'route':
                        break
                    self.scatter_phase()
                    pg.barrier()
                    if self.stop == 'scatter':
                        break
                    self.expert_phase(l)
                    pg.barrier()
                    if self.stop == 'expert':
                        break
                    self.gather_phase()
                    pg.barrier()
                    if self.stop == 'gather':
                        break
                last = (l == self.n_layers - 1)
                self.ln_phase(None, self.I('ln2_g')[l], self.I('ln2_b')[l], 'res', final=last)
                pg.barrier()
        self.es.close()
        return nc

    def ln_phase(self, src, g_ap, b_ap, mode, final=False, router=False):
        nc, pg = self.nc, self.pg
        with ExitStack() as st:
            gt = self.sb(st, 'ln_g', [P, D], F32)
            bt = self.sb(st, 'ln_b', [P, D], F32)
            pg.dma('sp', gt[:], g_ap.partition_broadcast(P), w=['ln_g'])
            pg.dma('sp', bt[:], b_ap.partition_broadcast(P), w=['ln_b'])
            NB = 2
            xt = [self.sb(st, f'ln_x{i}', [P, D], F32) for i in range(NB)]
            x2 = [self.sb(st, f'ln_r{i}', [P, D], F32) for i in range(NB)] if mode == 'res' else None
            hn = [self.sb(st, f'ln_h{i}', [P, D], F32) for i in range(NB)]
            stt = [self.sb(st, f'ln_st{i}', [P, 4, 6], F32) for i in range(NB)]
            mv = [self.sb(st, f'ln_mv{i}', [P, 4], F32) for i in range(NB)]
            hst = [self.sb(st, f'ln_hst{i}', [P, KC, 512], BF16) for i in range(2)]
            ps = self.psum(st, 'ln_ps', [P, 4 * 512], F32)
            if router:
                hTf = self.sb(st, 'ln_loT', [P, KC, P], BF16)
                Hb = self.sb(st, 'ln_Hb', [P, D], BF16)
                Lo = self.sb(st, 'ln_Lo', [P, D], F32)
                rw = self.sb(st, 'ln_rw', [P, KC, NE], F32)
                rwh = self.sb(st, 'ln_rwh', [P, KC, NE], BF16)
                rwl = self.sb(st, 'ln_rwl', [P, KC, NE], BF16)
                rb = self.sb(st, 'ln_rb', [P, NE], F32)
                rs = self.sb(st, 'ln_rs', [P, 8, NE], F32)
                ps_r = self.psum(st, 'ln_ps_r', [P, 512], F32)
                pg.dma('sp', rw[:], self.I('router_w').rearrange("(k p) e -> p k e", p=P), w=['ln_rw'])
                pg.dma('sp', rb[:], self.I('router_b').partition_broadcast(P), w=['ln_rb'])
                pg.op('act', lambda E: E.copy(rwh[:], rw[:]), r=['ln_rw'], w=['ln_rwh'])
                pg.op('dve', lambda E: E.tensor_tensor(rwl[:], rw[:], rwh[:], ALU.subtract), r=['ln_rw', 'ln_rwh'], w=['ln_rwl'])
            if mode == 'x':
                srcv = self.I(src).rearrange("(n p) d -> n p d", p=P)
            else:
                resv = self.dr['res_dram'].ap().rearrange("(n p) d -> n p d", p=P)
            hdv = self.dr['h_dram'].ap().rearrange("(n p) d -> n p d", p=P)
            outv = self.out.ap().rearrange("(n p) d -> n p d", p=P)
            hTv = self.dr['hT_dram'].ap().rearrange("(k p) t -> p k t", p=P)
            for t in range(NT):
                i = t % NB
                X, H, ST, MV = xt[i], hn[i], stt[i], mv[i]
                tx, th = f'ln_x{i}', f'ln_h{i}'
                if mode == 'x':
                    pg.dma('sp', X[:], srcv[t], w=[tx])
                else:
                    pg.dma('sp', X[:], hdv[t], r=[('h_dram', t)], w=[tx])
                    pg.dma('sp', x2[i][:], resv[t], r=[('res_dram', t)], w=[f'ln_r{i}'])
                    pg.op('dve', lambda E, X=X, R=x2[i]: E.scalar_tensor_tensor(X[:], X[:], ALPHA, R[:], ALU.mult, ALU.add),
                          r=[tx, f'ln_r{i}'], w=[tx])
                self.ln_tile(X, H, ST, MV, gt, bt, tx, th, f'ln_s{i}')
                if final:
                    pg.dma('sp', outv[t], H[:], r=[th], w=[('out', t)])
                    continue
                pg.dma('sp', hdv[t], H[:], r=[th], w=[('h_dram', t)])
                slot = t % 4
                hb = (t // 4) % 2
                HS = hst[hb]
                for half in range(4):
                    def tr(E, half=half, H=H):
                        ins = None
                        for j in range(4):
                            k = half * 4 + j
                            ins = E.transpose(ps[:, half * 512 + j * P: half * 512 + (j + 1) * P],
                                              H[:, k * P:(k + 1) * P], self.ident_f[:])
                        return ins
                    pg.op('pe', tr, r=[th, 'ident_f'], w=[('ln_ps', half)])
                    o_ap = HS[:, half * 4:(half + 1) * 4, slot * P:(slot + 1) * P]
                    i_ap = ps[:, half * 512:(half + 1) * 512].rearrange("p (a b) -> p a b", a=4)
                    if half % 2 == 0:
                        pg.op('act', lambda E, o=o_ap, a=i_ap: E.copy(o, a),
                              r=[('ln_ps', half)], w=[('ln_hst', hb, half, slot)])
                    else:
                        pg.op('dve', lambda E, o=o_ap, a=i_ap: E.tensor_copy(o, a),
                              r=[('ln_ps', half)], w=[('ln_hst', hb, half, slot)])
                if slot == 3:
                    tb = t // 4
                    pg.dma('sp', hTv[:, :, tb * 512:(tb + 1) * 512], HS[:],
                           r=[('ln_hst', hb, hf, sl) for hf in range(4) for sl in range(4)],
                           w=[('hT_dram', tb)])
                if router:
                    pg.op('act', lambda E, H=H: E.copy(Hb[:], H[:]), r=[th], w=['ln_Hb'])
                    pg.dma('sp', self.dr['hb_dram'].ap().rearrange("(n p) d -> n p d", p=P)[t], Hb[:], r=['ln_Hb'], w=[('hb_dram', t)])
                    pg.op('dve', lambda E, H=H: E.tensor_tensor(Lo[:], H[:], Hb[:], ALU.subtract), r=[th, 'ln_Hb'], w=['ln_Lo'])
                    for half in range(4):
                        def tr2(E, half=half):
                            ins = None
                            for j in range(4):
                                k = half * 4 + j
                                ins = E.transpose(ps[:, half * 512 + j * P: half * 512 + (j + 1) * P],
                                                  Lo[:, k * P:(k + 1) * P], self.ident_f[:])
                            return ins
                        pg.op('pe', tr2, r=['ln_Lo', 'ident_f'], w=[('ln_ps', half)])
                        o2 = hTf[:, half * 4:(half + 1) * 4, :]
                        i_ap = ps[:, half * 512:(half + 1) * 512].rearrange("p (a b) -> p a b", a=4)
                        if half % 2 == 1:
                            pg.op('act', lambda E, o=o2, a=i_ap: E.copy(o, a), r=[('ln_ps', half)], w=[('ln_hTf', half)])
                        else:
                            pg.op('dve', lambda E, o=o2, a=i_ap: E.tensor_copy(o, a), r=[('ln_ps', half)], w=[('ln_hTf', half)])
                    hiT = HS[:, :, slot * P:(slot + 1) * P]
                    hit = [('ln_hst', hb, hf, slot) for hf in range(4)]
                    self.route_tile(t, hTf, hiT, hit, rwh, rwl, rb, rs, ps_r)

    def route_tile(self, t, loT, hiT, hit, rwh, rwl, rb, rs, ps_r):
        pg = self.pg
        def mm(E):
            ins = None
            for k in range(KC):
                E.matmul(ps_r[:, 0:NE], hiT[:, k, :], rwh[:, k, :], start=(k == 0), stop=False)
                E.matmul(ps_r[:, 0:NE], loT[:, k, :], rwh[:, k, :], start=False, stop=False)
                ins = E.matmul(ps_r[:, 0:NE], hiT[:, k, :], rwl[:, k, :], start=False, stop=(k == KC - 1))
            return ins
        pg.op('pe', mm, r=[('ln_hTf', hf) for hf in range(4)] + hit + ['ln_rwh', 'ln_rwl'], w=['ln_ps_r'])
        lg, ex, eq, ex2, sel = [rs[:, i, :] for i in range(5)]
        sm = rs[:, 5, :]
        gmk = rs[:, 6, 0:4]
        g3 = lambda ap: ap.rearrange("p (g e) -> p g e", e=4)
        bc = lambda ap: ap.unsqueeze(2).to_broadcast([P, 4, 4])
        T = 'rt'
        pg.op('dve', lambda E: E.tensor_tensor(lg, ps_r[:, 0:NE], rb[:], ALU.add), r=['ln_ps_r', 'ln_rb'], w=[T])
        pg.op('dve', lambda E: E.tensor_reduce(sm[:, 12:13], lg, AX.X, ALU.max), r=[T], w=[T])
        pg.op('dve', lambda E: E.tensor_scalar(sm[:, 12:13], sm[:, 12:13], -1.0, None, ALU.mult), r=[T], w=[T])
        pg.op('act', lambda E: E.activation(ex, lg, AF.Exp, bias=sm[:, 12:13], scale=1.0), r=[T], w=[T])
        pg.op('dve', lambda E: E.tensor_reduce(sm[:, 0:4], g3(ex), AX.X, ALU.max), r=[T], w=[T])
        pg.op('dve', lambda E: E.tensor_tensor(g3(eq), g3(ex), bc(sm[:, 0:4]), ALU.is_equal), r=[T], w=[T])
        pg.op('dve', lambda E: E.scalar_tensor_tensor(ex2, eq, -4.0, ex, ALU.mult, ALU.add), r=[T], w=[T])
        pg.op('dve', lambda E: E.tensor_reduce(sm[:, 4:8], g3(ex2), AX.X, ALU.max), r=[T], w=[T])
        pg.op('dve', lambda E: E.tensor_tensor(sm[:, 8:12], sm[:, 0:4], sm[:, 4:8], ALU.add), r=[T], w=[T])
        pg.op('dve', lambda E: E.tensor_reduce(sm[:, 13:14], sm[:, 8:12], AX.X, ALU.max), r=[T], w=[T])
        pg.op('dve', lambda E: E.tensor_scalar(gmk, sm[:, 8:12], sm[:, 13:14], None, ALU.is_equal), r=[T], w=[T])
        pg.op('dve', lambda E: E.tensor_tensor(g3(sel), g3(ex), bc(sm[:, 4:8]), ALU.is_ge), r=[T], w=[T])
        pg.op('dve', lambda E: E.tensor_tensor(g3(sel), g3(sel), bc(gmk), ALU.mult), r=[T], w=[T])
        pg.op('dve', lambda E: E.tensor_copy(self.selm[:, t, :], sel), r=[T], w=[('selm', t)])
        pg.op('dve', lambda E: E.tensor_tensor(sel, sel, ex, ALU.mult), r=[T], w=[T])
        pg.op('dve', lambda E: E.reciprocal(sm[:, 14:15], sm[:, 13:14]), r=[T], w=[T])
        pg.op('dve', lambda E: E.tensor_scalar(self.comb[:, t, :], sel, sm[:, 14:15], None, ALU.mult), r=[T], w=[('comb', t)])

    def ln_tile(self, X, H, ST, MV, gt, bt, tx, th, ts):
        pg = self.pg
        for j in range(4):
            pg.op('dve', lambda E, j=j: E.bn_stats(ST[:, j, :], X[:, j * 512:(j + 1) * 512]),
                  r=[tx], w=[(ts, 'st', j)])
        pg.op('dve', lambda E: E.bn_aggr(MV[:, 0:2], ST[:].rearrange("p a b -> p (a b)")),
              r=[(ts, 'st', j) for j in range(4)], w=[(ts, 'mv')])
        pg.op('act', lambda E: E.activation(MV[:, 2:3], MV[:, 1:2], AF.Ln, bias=LN_EPS, scale=1.0),
              r=[(ts, 'mv')], w=[(ts, 'lnv')])
        pg.op('act', lambda E: E.activation(MV[:, 3:4], MV[:, 2:3], AF.Exp, scale=-0.5),
              r=[(ts, 'lnv')], w=[(ts, 'rstd')])
        pg.op('dve', lambda E: E.tensor_scalar(H[:], X[:], MV[:, 0:1], MV[:, 3:4],
                                               ALU.subtract, ALU.mult),
              r=[tx, (ts, 'mv'), (ts, 'rstd')], w=[th])
        pg.op('pool', lambda E: E.tensor_tensor(H[:], H[:], gt[:], ALU.mult),
              r=[th, 'ln_g'], w=[th])
        pg.op('pool', lambda E: E.tensor_tensor(H[:], H[:], bt[:], ALU.add),
              r=[th, 'ln_b'], w=[th])


    PF_SPECS = [('aq', C_AQ, 512), ('akd', None, 256), ('cb', C_CB, 512), ('cc', C_CC, 512),
                ('ch', C_CH, 512), ('gq', C_GQ, 512), ('gzf', C_GZF, 512), ('gzb', C_GZB, 512),
                ('rq', C_RQ, 512), ('rk', C_RK, 512)]
    PT_SPECS = [('av', C_AV, 128), ('gi', C_GI, 512), ('go', C_GO, 512), ('rkt', C_RK, 512),
                ('rv', C_RV, 512), ('rg', C_RG, 512)]

    def declare_proj(self):
        for n, _, w in self.PF_SPECS:
            self.dscr('pf_' + n, [w, S], BF16)
        for n, _, w in self.PT_SPECS:
            self.dscr('pt_' + n, [S, w], BF16)

    def load_hT(self, st):
        hT = self.sb(st, 'hT_bf', [P, KC, S], BF16)
        hTv = self.dr['hT_dram'].ap().rearrange("(k p) t -> p k t", p=P)
        for k in range(KC):
            self.pg.dma('sp', hT[:, k, :], hTv[:, k, :], r=[('hT_dram', tb) for tb in range(8)],
                        w=[('hT_bf', k)])
        return hT

    def in_proj_phase(self, l):
        nc, pg = self.nc, self.pg
        with ExitStack() as st:
            hT = self.load_hT(st)
            hT_toks = [('hT_bf', k) for k in range(KC)]
            wt = [self.sb(st, f'ip_w{i}', [P, KC, 512], BF16) for i in range(2)]
            stF = [self.sb(st, f'ip_sf{i}', [P, S], BF16) for i in range(2)]
            stT = [self.sb(st, f'ip_st{i}', [P, 4, 512], BF16) for i in range(2)]
            ps = self.psum(st, 'ip_ps', [P, 8 * 512], F32)
            w_in = self.I('w_in')[l].rearrange("(k p) n -> p k n", p=P)
            gi = 0
            bank = 0
            ev = 0
            nsf = 0
            nst = 0
            for (name, c0, width) in self.PF_SPECS:
                W = wt[gi % 2]; wtok = f'ip_w{gi % 2}'; gi += 1
                if name == 'akd':
                    for j, cc in enumerate([C_AK, C_AK, C_AK + 64, C_AK + 64]):
                        pg.dma('pool', W[:, :, j * 64:(j + 1) * 64], w_in[:, :, cc:cc + 64],
                               w=[(wtok, j)])
                    wtoks = [(wtok, j) for j in range(4)]
                else:
                    pg.dma('pool', W[:, :, 0:width], w_in[:, :, c0:c0 + width], w=[(wtok, 0)])
                    wtoks = [(wtok, 0)]
                dst = self.dr['pf_' + name].ap()
                for j in range(width // P):
                    SF = stF[nsf % 2]; sftok = f'ip_sf{nsf % 2}'; nsf += 1
                    for tb in range(8):
                        b = bank % 8; bank += 1
                        def mm(E, W=W, j=j, tb=tb, b=b):
                            ins = None
                            for k in range(KC):
                                ins = E.matmul(ps[:, b * 512:(b + 1) * 512], W[:, k, j * P:(j + 1) * P],
                                               hT[:, k, tb * 512:(tb + 1) * 512],
                                               start=(k == 0), stop=(k == KC - 1))
                            return ins
                        pg.op('pe', mm, r=wtoks + hT_toks, w=[('ip_ps', b)])
                        o_ap = SF[:, tb * 512:(tb + 1) * 512]
                        i_ap = ps[:, b * 512:(b + 1) * 512]
                        if ev % 2 == 0:
                            pg.op('act', lambda E, o=o_ap, a=i_ap: E.copy(o, a),
                                  r=[('ip_ps', b)], w=[(sftok, tb)])
                        else:
                            pg.op('dve', lambda E, o=o_ap, a=i_ap: E.tensor_copy(o, a),
                                  r=[('ip_ps', b)], w=[(sftok, tb)])
                        ev += 1
                    pg.dma('sp', dst[j * P:(j + 1) * P, :], SF[:],
                           r=[(sftok, tb) for tb in range(8)], w=[('pf_' + name, j)])
            for (name, c0, width) in self.PT_SPECS:
                W = wt[gi % 2]; wtok = f'ip_w{gi % 2}'; gi += 1
                pg.dma('pool', W[:, :, 0:width], w_in[:, :, c0:c0 + width], w=[(wtok, 0)])
                wtoks = [(wtok, 0)]
                dst = self.dr['pt_' + name].ap().rearrange("(n j p) c -> n p j c", p=P, j=4)
                for t in range(NT):
                    b = bank % 8; bank += 1
                    slot = t % 4
                    if slot == 0:
                        ST = stT[nst % 2]; sttok = f'ip_st{nst % 2}'; nst += 1
                    def mm(E, W=W, t=t, b=b, width=width):
                        ins = None
                        for k in range(KC):
                            ins = E.matmul(ps[:, b * 512:b * 512 + width], hT[:, k, t * P:(t + 1) * P],
                                           W[:, k, 0:width], start=(k == 0), stop=(k == KC - 1))
                        return ins
                    pg.op('pe', mm, r=wtoks + hT_toks, w=[('ip_ps', b)])
                    o_ap = ST[:, slot, 0:width]
                    i_ap = ps[:, b * 512:b * 512 + width]
                    if ev % 2 == 0:
                        pg.op('act', lambda E, o=o_ap, a=i_ap: E.copy(o, a),
                              r=[('ip_ps', b)], w=[(sttok, slot)])
                    else:
                        pg.op('dve', lambda E, o=o_ap, a=i_ap: E.tensor_copy(o, a),
                              r=[('ip_ps', b)], w=[(sttok, slot)])
                    ev += 1
                    if slot == 3:
                        pg.dma('sp', dst[t // 4][:, :, 0:width], ST[:, :, 0:width],
                               r=[(sttok, s_) for s_ in range(4)],
                               w=[('pt_' + name, t // 4)])


    def attn_phase(self, l):
        nc, pg = self.nc, self.pg
        with ExitStack() as st:
            q = self.sb(st, 'at_q', [P, 4, S], BF16)
            kd = self.sb(st, 'at_k', [P, 2, S], BF16)
            va = self.sb(st, 'at_v', [P, NT, 2, 65], BF16)
            bias = self.sb(st, 'at_bias', [P, 8, 3, P], F32)
            esink = self.sb(st, 'at_esink', [P, 8], F32)
            yst = self.sb(st, 'at_yst', [P, 4, S], BF16)
            tmp = [self.sb(st, f'at_tmp{i}', [P, 3 * P], F32) for i in range(2)]
            pT = [self.sb(st, f'at_pT{i}', [P, 3 * P], BF16) for i in range(2)]
            den = [self.sb(st, f'at_den{i}', [P, 8], F32) for i in range(2)]
            y = [self.sb(st, f'at_y{i}', [P, 8, 64], BF16) for i in range(2)]
            ps_s = self.psum(st, 'at_ps_s', [P, 2, 512], F32)
            ps_o = self.psum(st, 'at_ps_o', [P, 2, 2, 512], F32)
            ps_t = self.psum(st, 'at_ps_t', [P, 2, 512], BF16)
            pg.dma('sp', q[:], self.dr['pf_aq'].ap().rearrange("(c p) t -> p c t", p=P),
                   r=[('pf_aq', j) for j in range(4)], w=['at_q'])
            pg.dma('sp', kd[:], self.dr['pf_akd'].ap().rearrange("(c p) t -> p c t", p=P),
                   r=[('pf_akd', j) for j in range(2)], w=['at_k'])
            pg.op('pool', lambda E: E.memset(va[:], 1.0), w=['at_v'])
            avv = self.dr['pt_av'].ap().rearrange("(n p) c -> p n c", p=P)
            for kv in range(2):
                pg.dma('sp', va[:, :, kv, 0:64], avv[:, :, kv * 64:(kv + 1) * 64],
                       r=[('pt_av', j) for j in range(8)], w=['at_v'])
            pg.dma('sp', bias[:].rearrange("p a b c -> p (a b c)"), self.I('c_attn_bias'), w=['at_bias'])
            pg.dma('sp', esink[:], self.I('attn_sink')[l].partition_broadcast(P), w=['at_esink'])
            pg.op('act', lambda E: E.activation(esink[:], esink[:], AF.Exp), r=['at_esink'], w=['at_esink'])
            it = 0
            for n in range(NT):
                ob = n % 2
                js = [j for j in range(3) if 0 <= n + j - 1 < NT]
                c0, c1 = js[0] * P, (js[-1] + 1) * P
                for h in range(8):
                    kv = h // 4
                    r0 = (h % 2) * 64
                    sb_ = it % 2; it += 1
                    def mm(E, h=h, kv=kv, r0=r0, sb_=sb_, n=n, js=js):
                        ins = None
                        for j in js:
                            kb = n + j - 1
                            ins = E.matmul(ps_s[:, sb_, j * P:(j + 1) * P],
                                           kd[r0:r0 + 64, kv, kb * P:(kb + 1) * P],
                                           q[r0:r0 + 64, h // 2, n * P:(n + 1) * P],
                                           start=True, stop=True)
                        return ins
                    pg.op('pe', mm, r=['at_q', 'at_k'], w=[('at_ps_s', sb_)])
                    T, PT = tmp[sb_], pT[sb_]
                    pg.op('dve', lambda E, T=T, sb_=sb_, h=h, c0=c0, c1=c1: E.scalar_tensor_tensor(
                        T[:, c0:c1], ps_s[:, sb_, c0:c1], 0.125,
                        bias[:, h, :, :].rearrange("p a b -> p (a b)")[:, c0:c1], ALU.mult, ALU.add),
                        r=[('at_ps_s', sb_), 'at_bias'], w=[('at_tmp', sb_)])
                    pg.op('act', lambda E, T=T, PT=PT, c0=c0, c1=c1: E.activation(PT[:, c0:c1], T[:, c0:c1], AF.Exp),
                          r=[('at_tmp', sb_)], w=[('at_pT', sb_)])
                    def pv(E, h=h, kv=kv, PT=PT, n=n, js=js, ob=ob):
                        ins = None
                        for idx, j in enumerate(js):
                            kb = n + j - 1
                            ins = E.matmul(ps_o[:, ob, h // 4, (h % 4) * 65:(h % 4) * 65 + 65],
                                           PT[:, j * P:(j + 1) * P], va[:, kb, kv, :],
                                           start=(idx == 0), stop=(idx == len(js) - 1))
                        return ins
                    pg.op('pe', pv, r=[('at_pT', sb_), 'at_v'], w=[('at_ps_o', ob, h)])
                DEN, Y = den[ob], y[ob]
                po = ps_o[:, ob, :, 0:260].rearrange("p b (h e) -> p b h e", e=65)
                pg.op('dve', lambda E, DEN=DEN, po=po: E.tensor_tensor(
                    DEN[:].rearrange("p (b h) -> p b h", b=2), po[:, :, :, 64],
                    esink[:].rearrange("p (b h) -> p b h", b=2), ALU.add),
                    r=[('at_ps_o', ob, h) for h in range(8)] + ['at_esink'], w=[('at_den', ob)])
                pg.op('dve', lambda E, DEN=DEN: E.reciprocal(DEN[:], DEN[:]),
                      r=[('at_den', ob)], w=[('at_den', ob)])
                pg.op('dve', lambda E, DEN=DEN, Y=Y, po=po: E.tensor_tensor(
                    Y[:].rearrange("p (b h) d -> p b h d", b=2), po[:, :, :, 0:64],
                    DEN[:].rearrange("p (b h) -> p b h", b=2).unsqueeze(3).to_broadcast([P, 2, 4, 64]),
                    ALU.mult),
                    r=[('at_ps_o', ob, h) for h in range(8)] + [('at_den', ob)], w=[('at_y', ob)])
                self.transpose_out(Y[:].rearrange("p h d -> p (h d)"), ('at_y', ob), ps_t, 'at_ps_t',
                                   yst, 'at_yst', n, ob)
            self.store_yT(yst, 'at_yst', 0)

    def transpose_out(self, Yflat, ytok, ps_t, pstok, yst, ysttok, n, ob, rows=P):
        pg = self.pg
        def tr(E):
            ins = None
            for c in range(4):
                ins = E.transpose(ps_t[:, ob, c * P:(c + 1) * P], Yflat[:, c * P:(c + 1) * P], self.ident_b[:])
            return ins
        pg.op('pe', tr, r=[ytok, 'ident_b'], w=[(pstok, ob)])
        pg.op('act', lambda E: E.copy(yst[:, :, n * P:(n + 1) * P],
                                      ps_t[:, ob, :].rearrange("p (c t) -> p c t", c=4)),
              r=[(pstok, ob)], w=[(ysttok, n)])

    def store_yT(self, yst, ysttok, row0):
        dst = self.dr['yT_dram'].ap()
        for c in range(4):
            self.pg.dma('sp', dst[row0 + c * P: row0 + (c + 1) * P, :], yst[:, c, :],
                        r=[(ysttok, n) for n in range(NT)], w=[('yT_dram', row0 // P + c)])


    def load_chan(self, dst2d, src1d, wtok):
        self.pg.dma('sp', dst2d, src1d.rearrange("(c p) -> p c", p=P), w=[wtok],
                    allow_slow_non_contiguous=True)

    def conv_phase(self, l):
        nc, pg = self.nc, self.pg
        with ExitStack() as st:
            cw = self.sb(st, 'cv_w', [P, 3, 4], F32)
            for wi in range(3):
                self.load_chan(cw[:, wi, :], self.I('conv_w')[l, wi], ('cv_w', wi))
            cwt = [('cv_w', wi) for wi in range(3)]
            U = [self.sb(st, f'cv_u{i}', [P, S + 2], F32) for i in range(2)]
            A = [self.sb(st, f'cv_a{i}', [P, S], F32) for i in range(2)]
            cb = [self.sb(st, f'cv_b{i}', [P, S], BF16) for i in range(2)]
            cc = [self.sb(st, f'cv_c{i}', [P, S], BF16) for i in range(2)]
            ch = [self.sb(st, f'cv_h{i}', [P, S], BF16) for i in range(2)]
            yo = [self.sb(st, f'cv_y{i}', [P, S], BF16) for i in range(2)]
            for i in range(2):
                pg.op('pool', lambda E, i=i: E.memset(U[i][:], 0.0), w=[('cv_u', i)])
            for c in range(4):
                i = c % 2
                rows = slice(c * P, (c + 1) * P)
                pg.dma('sp', cb[i][:], self.dr['pf_cb'].ap()[rows, :], r=[('pf_cb', c)], w=[('cv_b', i)])
                pg.dma('sp', cc[i][:], self.dr['pf_cc'].ap()[rows, :], r=[('pf_cc', c)], w=[('cv_c', i)])
                pg.dma('sp', ch[i][:], self.dr['pf_ch'].ap()[rows, :], r=[('pf_ch', c)], w=[('cv_h', i)])
                pg.op('pool', lambda E, i=i: E.tensor_tensor(U[i][:, 1:S + 1], cc[i][:], ch[i][:], ALU.mult),
                      r=[('cv_c', i), ('cv_h', i)], w=[('cv_u', i)])
                pg.op('dve', lambda E, i=i, c=c: E.tensor_scalar(A[i][:], U[i][:, 1:S + 1], cw[:, 1, c:c + 1], None, ALU.mult),
                      r=[('cv_u', i)] + cwt, w=[('cv_a', i)])
                pg.op('dve', lambda E, i=i, c=c: E.scalar_tensor_tensor(A[i][:], U[i][:, 0:S], cw[:, 0, c:c + 1], A[i][:], ALU.mult, ALU.add),
                      r=[('cv_u', i), ('cv_a', i)] + cwt, w=[('cv_a', i)])
                pg.op('dve', lambda E, i=i, c=c: E.scalar_tensor_tensor(A[i][:], U[i][:, 2:S + 2], cw[:, 2, c:c + 1], A[i][:], ALU.mult, ALU.add),
                      r=[('cv_u', i), ('cv_a', i)] + cwt, w=[('cv_a', i)])
                pg.op('pool', lambda E, i=i: E.tensor_tensor(yo[i][:], A[i][:], cb[i][:], ALU.mult),
                      r=[('cv_a', i), ('cv_b', i)], w=[('cv_y', i)])
                pg.dma('sp', self.dr['yT_dram'].ap()[512 + c * P: 512 + (c + 1) * P, :], yo[i][:],
                       r=[('cv_y', i)], w=[('yT_dram', 4 + c)])

    def alloc_norm(self, st, pfx, rows, nslot=1):
        d = {}
        d['sq'] = self.sb(st, pfx + '_sq', [rows, nslot * 512], F32)
        d['on'] = self.sb(st, pfx + '_on', [rows, nslot * 512], F32)
        d['e'] = self.sb(st, pfx + '_e', [rows, nslot * 512], F32)
        d['st'] = self.sb(st, pfx + '_st', [rows, 6, nslot * 4], F32)
        d['y'] = [self.sb(st, pfx + f'_y{i}', [rows, nslot * 512], BF16) for i in range(2)]
        d['pfx'] = pfx
        return d

    def norm_gate(self, d, po, potoks, G, gtok, NG, ngtok, mode, yi, rows, nh):
        pg = self.pg
        pfx = d['pfx']
        W = nh * 128
        sq, on, e, stt = d['sq'][:, 0:W], d['on'][:, 0:W], d['e'][:, 0:W], d['st']
        Y = d['y'][yi][:, 0:W]
        v3 = lambda ap: ap.rearrange("p (h v) -> p h v", v=128)
        tk = lambda s: (pfx, s)
        ss, sm, mean, var, rstd, msq = [stt[:, i, 0:nh] for i in range(6)]
        pg.op('act', lambda E: E.activation(sq, po, AF.Square), r=potoks, w=[tk('sq')])
        pg.op('dve', lambda E: E.tensor_reduce(ss, v3(sq), AX.X, ALU.add), r=[tk('sq')], w=[tk('ss')])
        if mode == 'gn':
            pg.op('dve', lambda E: E.tensor_reduce(sm, v3(po), AX.X, ALU.add), r=potoks, w=[tk('sm')])
            pg.op('dve', lambda E: E.tensor_scalar(mean, sm, 1.0 / 128, None, ALU.mult), r=[tk('sm')], w=[tk('mean')])
            pg.op('dve', lambda E: E.tensor_tensor(msq, mean, mean, ALU.mult), r=[tk('mean')], w=[tk('msq')])
            pg.op('dve', lambda E: E.scalar_tensor_tensor(var, ss, 1.0 / 128, msq, ALU.mult, ALU.subtract),
                  r=[tk('ss'), tk('msq')], w=[tk('var')])
        else:
            pg.op('dve', lambda E: E.tensor_scalar(var, ss, 1.0 / 128, None, ALU.mult), r=[tk('ss')], w=[tk('var')])
        pg.op('act', lambda E: E.activation(rstd, var, AF.Ln, bias=HN_EPS, scale=1.0), r=[tk('var')], w=[tk('rstd')])
        pg.op('act', lambda E: E.activation(rstd, rstd, AF.Exp, scale=-0.5), r=[tk('rstd')], w=[tk('rstd')])
        bc = lambda ap: ap.unsqueeze(2).to_broadcast([rows, nh, 128])
        if mode == 'gn':
            pg.op('dve', lambda E: E.tensor_tensor(v3(on), v3(po), bc(mean), ALU.subtract),
                  r=potoks + [tk('mean')], w=[tk('on')])
            pg.op('dve', lambda E: E.tensor_tensor(v3(on), v3(on), bc(rstd), ALU.mult),
                  r=[tk('on'), tk('rstd')], w=[tk('on')])
        else:
            pg.op('dve', lambda E: E.tensor_tensor(v3(on), v3(po), bc(rstd), ALU.mult),
                  r=potoks + [tk('rstd')], w=[tk('on')])
        pg.op('pool', lambda E: E.tensor_tensor(on, on, NG, ALU.mult), r=[tk('on'), ngtok], w=[tk('on')])
        pg.op('act', lambda E: E.activation(e, G, AF.Exp, scale=-1.0), r=[gtok], w=[tk('e')])
        pg.op('pool', lambda E: E.tensor_scalar(e, e, 1.0, None, ALU.add), r=[tk('e')], w=[tk('e')])
        pg.op('dve', lambda E: E.reciprocal(e, e), r=[tk('e')], w=[tk('e')])
        pg.op('pool', lambda E: E.tensor_tensor(e, e, G, ALU.mult), r=[tk('e'), gtok], w=[tk('e')])
        pg.op('pool', lambda E: E.tensor_tensor(Y, on, e, ALU.mult), r=[tk('on'), tk('e')], w=[(pfx + '_y', yi)])
        return d['y'][yi]

    def ret_phase(self, l):
        nc, pg = self.nc, self.pg
        SC = 128.0 ** -0.5
        with ExitStack() as st:
            cst = self.sb(st, 'rt_c', [P, 5, P], F32)
            vec = self.sb(st, 'rt_vec', [P, 2, P], F32)
            pcol = self.sb(st, 'rt_pcol', [P, 2], F32)
            lg = self.sb(st, 'rt_lg', [P, 8], F32)
            GL = self.sb(st, 'rt_GL', [P, 8], F32)
            vd = self.sb(st, 'rt_vd', [P, 8], F32)
            DT = self.sb(st, 'rt_DT', [P, 4, P], F32)
            tmpD = self.sb(st, 'rt_tmpD', [P, P], F32)
            dec = self.sb(st, 'rt_dec', [P, 2, 4, P], F32)
            NG = self.sb(st, 'rt_ng', [P, 512], F32)
            prevF = self.sb(st, 'rt_prevF', [P, NT, 512], BF16)
            yst = self.sb(st, 'rt_yst', [P, 4, S], BF16)
            Fs = self.sb(st, 'rt_F', [P, 512], F32)
            Bs = self.sb(st, 'rt_B', [P, 512], F32)
            tmpS = self.sb(st, 'rt_tmpS', [P, 512], F32)
            pB = [self.sb(st, f'rt_pB{i}', [P, 512], BF16) for i in range(2)]
            Kt = [self.sb(st, f'rt_Kt{i}', [P, 512], BF16) for i in range(2)]
            Vt = [self.sb(st, f'rt_Vt{i}', [P, 512], BF16) for i in range(2)]
            Gt = [self.sb(st, f'rt_Gt{i}', [P, 512], BF16) for i in range(2)]
            Qf = [self.sb(st, f'rt_Qf{i}', [P, 4, P], BF16) for i in range(2)]
            Kf = [self.sb(st, f'rt_Kf{i}', [P, 4, P], BF16) for i in range(2)]
            vS = [self.sb(st, f'rt_vS{i}', [P, 512], BF16) for i in range(2)]
            aTm = [self.sb(st, f'rt_aTm{i}', [P, 512], BF16) for i in range(2)]
            qF = [self.sb(st, f'rt_qF{i}', [P, 4, P], BF16) for i in range(2)]
            qB = [self.sb(st, f'rt_qB{i}', [P, 4, P], BF16) for i in range(2)]
            nd = self.alloc_norm(st, 'rt_n', P)
            ps_a = self.psum(st, 'rt_ps_a', [P, 2, 512], F32)
            ps_o = self.psum(st, 'rt_ps_o', [P, 2, 512], F32)
            ps_kv = self.psum(st, 'rt_ps_kv', [P, 2, 512], F32)
            ps_t = self.psum(st, 'rt_ps_t', [P, 2, 512], BF16)
            pg.dma('sp', cst[:].rearrange("p a b -> p (a b)"), self.I('c_ret_c'), w=['rt_c'])
            pg.dma('sp', vec[:].rearrange("p a b -> p (a b)"), self.I('c_ret_vec'), w=['rt_vec'])
            pg.dma('sp', pcol[:], self.I('c_ret_pcol'), w=['rt_pcol'])
            pg.dma('sp', NG[:], self.I('ret_norm_g')[l].partition_broadcast(P), w=['rt_ng'])
            pg.dma('sp', lg[:], self.I('ret_decay_logit')[l].rearrange("a b -> (a b)").partition_broadcast(P), w=['rt_lg'])
            pg.op('act', lambda E: E.activation(lg[:], lg[:], AF.Exp, scale=-1.0), r=['rt_lg'], w=['rt_lg'])
            pg.op('dve', lambda E: E.tensor_scalar(lg[:], lg[:], 1.0, None, ALU.add), r=['rt_lg'], w=['rt_lg'])
            pg.op('act', lambda E: E.activation(lg[:], lg[:], AF.Ln), r=['rt_lg'], w=['rt_lg'])
            pg.op('dve', lambda E: E.tensor_scalar(lg[:], lg[:], -1.0, None, ALU.mult), r=['rt_lg'], w=['rt_lg'])
            pg.op('act', lambda E: E.activation(GL[:], lg[:], AF.Exp, scale=128.0), r=['rt_lg'], w=['rt_GL'])
            lnsc = float(np.log(SC))
            for h in range(4):
                pg.op('act', lambda E, h=h: E.activation(vd[:, h:h + 1], pcol[:, 0:1], AF.Exp, scale=lg[:, h:h + 1], bias=lnsc),
                      r=['rt_lg', 'rt_pcol'], w=[('rt_vd', h)])
                pg.op('act', lambda E, h=h: E.activation(vd[:, 4 + h:5 + h], pcol[:, 1:2], AF.Exp, scale=lg[:, 4 + h:5 + h], bias=lnsc),
                      r=['rt_lg', 'rt_pcol'], w=[('rt_vd', 4 + h)])
                pg.op('act', lambda E, h=h: E.activation(dec[:, 0, h, :], vec[:, 0, :], AF.Exp, scale=lg[:, h:h + 1]),
                      r=['rt_lg', 'rt_vec'], w=[('rt_dec', 0, h)])
                pg.op('act', lambda E, h=h: E.activation(dec[:, 1, h, :], vec[:, 1, :], AF.Exp, scale=lg[:, 4 + h:5 + h]),
                      r=['rt_lg', 'rt_vec'], w=[('rt_dec', 1, h)])
                pg.op('act', lambda E, h=h: E.activation(DT[:, h, :], cst[:, 0, :], AF.Exp, scale=lg[:, h:h + 1]),
                      r=['rt_lg', 'rt_c'], w=[('rt_DT', h)])
                pg.op('dve', lambda E, h=h: E.tensor_tensor(DT[:, h, :], DT[:, h, :], cst[:, 2, :], ALU.mult),
                      r=[('rt_DT', h), 'rt_c'], w=[('rt_DT', h)])
                pg.op('act', lambda E, h=h: E.activation(tmpD[:], cst[:, 1, :], AF.Exp, scale=lg[:, 4 + h:5 + h]),
                      r=['rt_lg', 'rt_c'], w=['rt_tmpD'])
                pg.op('dve', lambda E: E.tensor_tensor(tmpD[:], tmpD[:], cst[:, 3, :], ALU.mult),
                      r=['rt_tmpD', 'rt_c'], w=['rt_tmpD'])
                pg.op('dve', lambda E, h=h: E.tensor_tensor(DT[:, h, :], DT[:, h, :], tmpD[:], ALU.add),
                      r=[('rt_DT', h), 'rt_tmpD'], w=[('rt_DT', h)])
                pg.op('dve', lambda E, h=h: E.tensor_tensor(DT[:, h, :], DT[:, h, :], cst[:, 4, :], ALU.add),
                      r=[('rt_DT', h), 'rt_c'], w=[('rt_DT', h)])
                pg.op('dve', lambda E, h=h: E.tensor_scalar(DT[:, h, :], DT[:, h, :], SC, None, ALU.mult),
                      r=[('rt_DT', h)], w=[('rt_DT', h)])
            DTt = [('rt_DT', h) for h in range(4)]
            vdt = [('rt_vd', h) for h in range(8)]
            dect = [('rt_dec', a, h) for a in range(2) for h in range(4)]
            pg.op('pool', lambda E: E.memset(Fs[:], 0.0), w=['rt_F'])
            pg.op('pool', lambda E: E.memset(Bs[:], 0.0), w=['rt_B'])
            ktv = self.dr['pt_rkt'].ap().rearrange("(n p) c -> n p c", p=P)
            vtv = self.dr['pt_rv'].ap().rearrange("(n p) c -> n p c", p=P)
            gtv = self.dr['pt_rg'].ap().rearrange("(n p) c -> n p c", p=P)
            qfv = self.dr['pf_rq'].ap().rearrange("(h p) t -> p h t", p=P)
            kfv = self.dr['pf_rk'].ap().rearrange("(h p) t -> p h t", p=P)
            v3 = lambda ap: ap.rearrange("p (h v) -> p h v", v=128)
            bc4 = lambda ap: ap.unsqueeze(2).to_broadcast([P, 4, 128])
            it = 0

            def kv_step(n, i, K, V, vdcols, state, stok, GLcols):
                VS = vS[i]
                pg.op('pool', lambda E: E.tensor_tensor(v3(VS[:]), v3(V[:]), bc4(vdcols), ALU.mult),
                      r=[('rt_Vt', i)] + vdt, w=[('rt_vS', i)])
                def mm(E):
                    ins = None
                    for h in range(4):
                        ins = E.matmul(ps_kv[:, i, h * P:(h + 1) * P], K[:, h * P:(h + 1) * P], VS[:, h * P:(h + 1) * P],
                                       start=True, stop=True)
                    return ins
                pg.op('pe', mm, r=[('rt_Kt', i), ('rt_vS', i)], w=[('rt_ps_kv', i)])
                pg.op('pool', lambda E: E.tensor_tensor(v3(tmpS[:]), v3(state[:]), bc4(GLcols), ALU.mult),
                      r=[stok, 'rt_GL'], w=['rt_tmpS'])
                pg.op('dve', lambda E: E.tensor_tensor(state[:], tmpS[:], ps_kv[:, i, :], ALU.add),
                      r=['rt_tmpS', ('rt_ps_kv', i)], w=[stok])

            for n in range(NT):
                i = it % 2; it += 1
                pg.dma('sp', Kt[i][:], ktv[n], r=[('pt_rkt', n // 4)], w=[('rt_Kt', i)])
                pg.dma('sp', Vt[i][:], vtv[n], r=[('pt_rv', n // 4)], w=[('rt_Vt', i)])
                pg.op('act', lambda E, n=n: E.copy(prevF[:, n, :], Fs[:]), r=['rt_F'], w=[('rt_prevF', n)])
                kv_step(n, i, Kt[i], Vt[i], vd[:, 0:4], Fs, 'rt_F', GL[:, 0:4])
            for n in range(NT - 1, -1, -1):
                i = it % 2; it += 1
                tsl = slice(n * P, (n + 1) * P)
                pg.dma('sp', Kt[i][:], ktv[n], r=[('pt_rkt', n // 4)], w=[('rt_Kt', i)])
                pg.dma('sp', Vt[i][:], vtv[n], r=[('pt_rv', n // 4)], w=[('rt_Vt', i)])
                pg.dma('sp', Gt[i][:], gtv[n], r=[('pt_rg', n // 4)], w=[('rt_Gt', i)])
                pg.dma('sp', Qf[i][:], qfv[:, :, tsl], r=[('pf_rq', h) for h in range(4)], w=[('rt_Qf', i)])
                pg.dma('sp', Kf[i][:], kfv[:, :, tsl], r=[('pf_rk', h) for h in range(4)], w=[('rt_Kf', i)])
                def mma(E, i=i):
                    ins = None
                    for h in range(4):
                        ins = E.matmul(ps_a[:, i, h * P:(h + 1) * P], Kf[i][:, h, :], Qf[i][:, h, :], start=True, stop=True)
                    return ins
                pg.op('pe', mma, r=[('rt_Qf', i), ('rt_Kf', i)], w=[('rt_ps_a', i)])
                pg.op('dve', lambda E, i=i: E.tensor_tensor(aTm[i][:], ps_a[:, i, :], DT[:].rearrange("p h t -> p (h t)"), ALU.mult),
                      r=[('rt_ps_a', i)] + DTt, w=[('rt_aTm', i)])
                pg.op('pool', lambda E, i=i: E.tensor_tensor(qF[i][:], Qf[i][:], dec[:, 0, :, :], ALU.mult),
                      r=[('rt_Qf', i)] + dect, w=[('rt_qF', i)])
                pg.op('pool', lambda E, i=i: E.tensor_tensor(qB[i][:], Qf[i][:], dec[:, 1, :, :], ALU.mult),
                      r=[('rt_Qf', i)] + dect, w=[('rt_qB', i)])
                pg.op('act', lambda E, i=i: E.copy(pB[i][:], Bs[:]), r=['rt_B'], w=[('rt_pB', i)])
                def mmo(E, i=i, n=n):
                    ins = None
                    for h in range(4):
                        hs = slice(h * P, (h + 1) * P)
                        E.matmul(ps_o[:, i, hs], aTm[i][:, hs], Vt[i][:, hs], start=True, stop=False)
                        E.matmul(ps_o[:, i, hs], qF[i][:, h, :], prevF[:, n, hs], start=False, stop=False)
                        ins = E.matmul(ps_o[:, i, hs], qB[i][:, h, :], pB[i][:, hs], start=False, stop=True)
                    return ins
                pg.op('pe', mmo, r=[('rt_aTm', i), ('rt_Vt', i), ('rt_qF', i), ('rt_qB', i), ('rt_prevF', n), ('rt_pB', i)],
                      w=[('rt_ps_o', i)])
                kv_step(n, i, Kt[i], Vt[i], vd[:, 4:8], Bs, 'rt_B', GL[:, 4:8])
                Y = self.norm_gate(nd, ps_o[:, i, :], [('rt_ps_o', i)], Gt[i][:], ('rt_Gt', i), NG[:], 'rt_ng',
                                   'gn', i, P, 4)
                self.transpose_out(Y[:], ('rt_n_y', i), ps_t, 'rt_ps_t', yst, 'rt_yst', n, i)
            self.store_yT(yst, 'rt_yst', 1536)


    def hgrn_phase(self, l):
        nc, pg = self.nc, self.pg
        NCH = S // 64
        with ExitStack() as outer:
            DEC = self.sb(outer, 'hg_DEC', [P, 2, 3, 4, NCH], F32)
            dect = [('hg_DEC', d_, hd) for d_ in range(2) for hd in range(4)]
            with ExitStack() as st:
                lb = self.sb(st, 'hg_lb', [P, 4], F32)
                oml = self.sb(st, 'hg_oml', [P, 4], F32)
                a0 = self.sb(st, 'hg_a0', [P, 4], F32)
                cm = self.sb(st, 'hg_cm', [P, S], BF16)
                T = [self.sb(st, f'hg_T{i}', [P, S], F32) for i in range(4)]
                tmpd = self.sb(st, 'hg_tmpd', [P, NCH], F32)
                zb = [self.sb(st, f'hg_z{i}', [P, S], BF16) for i in range(2)]
                qb = [self.sb(st, f'hg_qin{i}', [P, S], BF16) for i in range(2)]
                qo = [self.sb(st, f'hg_qo{i}', [P, S], BF16) for i in range(2)]
                ko = [self.sb(st, f'hg_ko{i}', [P, S], BF16) for i in range(2)]
                pg.dma('sp', cm[:], self.I('c_hg_cm'), w=['hg_cm'])
                if l == 0:
                    pg.op('pool', lambda E: E.memset(lb[:], 0.0), w=['hg_lb'])
                else:
                    self.load_chan(a0[:], self.I('hgrn_lb')[0], 'hg_a0')
                    self.load_chan(lb[:], self.I('hgrn_lb')[1], 'hg_lb')
                    pg.op('dve', lambda E: E.tensor_tensor(lb[:], a0[:], lb[:], ALU.subtract), r=['hg_a0', 'hg_lb'], w=['hg_lb'])
                    pg.op('act', lambda E: E.activation(lb[:], lb[:], AF.Exp), r=['hg_lb'], w=['hg_lb'])
                    pg.op('dve', lambda E: E.tensor_scalar(lb[:], lb[:], 1.0, None, ALU.add), r=['hg_lb'], w=['hg_lb'])
                    pg.op('dve', lambda E: E.reciprocal(lb[:], lb[:]), r=['hg_lb'], w=['hg_lb'])
                pg.op('dve', lambda E: E.tensor_scalar(oml[:], lb[:], -1.0, 1.0, ALU.mult, ALU.add), r=['hg_lb'], w=['hg_oml'])
                it = 0
                for d_ in range(2):
                    zname = 'pf_gzf' if d_ == 0 else 'pf_gzb'
                    mid, last = (31, 63) if d_ == 0 else (32, 0)
                    for hd in range(4):
                        i = it % 2; it += 1
                        rows = slice(hd * P, (hd + 1) * P)
                        T1, T2, T3, T4 = T
                        pg.dma('sp', zb[i][:], self.dr[zname].ap()[rows, :], r=[(zname, hd)], w=[('hg_z', i)])
                        pg.dma('sp', qb[i][:], self.dr['pf_gq'].ap()[rows, :], r=[('pf_gq', hd)], w=[('hg_qin', i)])
                        pg.op('act', lambda E, i=i: E.activation(T1[:], zb[i][:], AF.Exp, scale=-1.0), r=[('hg_z', i)], w=['hg_T1'])
                        pg.op('pool', lambda E: E.tensor_scalar(T1[:], T1[:], 1.0, None, ALU.add), r=['hg_T1'], w=['hg_T1'])
                        pg.op('dve', lambda E: E.reciprocal(T1[:], T1[:]), r=['hg_T1'], w=['hg_T1'])
                        pg.op('dve', lambda E, hd=hd: E.tensor_scalar(T1[:], T1[:], oml[:, hd:hd + 1], lb[:, hd:hd + 1], ALU.mult, ALU.add),
                              r=['hg_T1', 'hg_lb', 'hg_oml'], w=['hg_T1'])
                        pg.op('act', lambda E: E.activation(T2[:], T1[:], AF.Ln), r=['hg_T1'], w=['hg_T2'])
                        pg.op('dve', lambda E: E.tensor_tensor_scan(T3[:], cm[:], T2[:], 0.0, ALU.mult, ALU.add),
                              r=['hg_cm', 'hg_T2'], w=['hg_T3'])
                        c3 = lambda ap: ap.rearrange("p (n c) -> p n c", c=64)
                        if d_ == 0:
                            Bt, Btok = T3, 'hg_T3'
                        else:
                            pg.op('dve', lambda E: E.tensor_tensor(c3(T4[:]), c3(T3[:])[:, :, 63:64].to_broadcast([P, NCH, 64]),
                                                                   c3(T3[:]), ALU.subtract), r=['hg_T3'], w=['hg_T4'])
                            pg.op('pool', lambda E: E.tensor_tensor(T4[:], T4[:], T2[:], ALU.add), r=['hg_T4', 'hg_T2'], w=['hg_T4'])
                            Bt, Btok = T4, 'hg_T4'
                        B3 = c3(Bt[:])
                        dk = ('hg_DEC', d_, hd)
                        pg.op('act', lambda E, B3=B3, d_=d_, hd=hd, last=last: E.activation(DEC[:, d_, 0, hd, :], B3[:, :, last], AF.Exp),
                              r=[Btok], w=[dk])
                        pg.op('act', lambda E, B3=B3, d_=d_, hd=hd, mid=mid: E.activation(DEC[:, d_, 1, hd, :], B3[:, :, mid], AF.Exp),
                              r=[Btok], w=[dk])
                        pg.op('dve', lambda E, B3=B3, mid=mid, last=last: E.tensor_tensor(tmpd[:], B3[:, :, last], B3[:, :, mid], ALU.subtract),
                              r=[Btok], w=['hg_tmpd'])
                        pg.op('act', lambda E, d_=d_, hd=hd: E.activation(DEC[:, d_, 2, hd, :], tmpd[:], AF.Exp),
                              r=['hg_tmpd'], w=[dk])
                        pg.op('dve', lambda E, B3=B3, mid=mid: E.tensor_tensor(c3(T2[:]), B3, B3[:, :, mid:mid + 1].to_broadcast([P, NCH, 64]),
                                                                               ALU.subtract), r=[Btok, 'hg_T2'], w=['hg_T2'])
                        EP, EPtok = (T4, 'hg_T4') if d_ == 0 else (T3, 'hg_T3')
                        pg.op('act', lambda E, EP=EP: E.activation(EP[:], T2[:], AF.Exp), r=['hg_T2', Btok], w=[EPtok])
                        pg.op('pool', lambda E, i=i, EP=EP: E.tensor_tensor(qo[i][:], qb[i][:], EP[:], ALU.mult),
                              r=[('hg_qin', i), EPtok], w=[('hg_qo', i)])
                        pg.dma('sp', self.dr[f'hg_q{d_}'].ap()[rows, :], qo[i][:], r=[('hg_qo', i)], w=[(f'hg_q{d_}', hd)])
                        EM, EMtok = (T3, 'hg_T3') if d_ == 0 else (T4, 'hg_T4')
                        pg.op('act', lambda E, EM=EM: E.activation(EM[:], T2[:], AF.Exp, scale=-1.0), r=['hg_T2', EPtok, ('hg_qo', i)], w=[EMtok])
                        pg.op('pool', lambda E: E.tensor_scalar(T1[:], T1[:], -1.0, 1.0, ALU.mult, ALU.add), r=['hg_T1'], w=['hg_T1'])
                        pg.op('dve', lambda E, i=i, EM=EM: E.tensor_tensor(ko[i][:], T1[:], EM[:], ALU.mult),
                              r=['hg_T1', EMtok], w=[('hg_ko', i)])
                        pg.dma('sp', self.dr[f'hg_k{d_}'].ap()[rows, :], ko[i][:], r=[('hg_ko', i)], w=[(f'hg_k{d_}', hd)])
            pg.barrier()
            v3 = lambda ap: ap.rearrange("p (h v) -> p h v", v=128)
            with ExitStack() as st:
                Sst = self.sb(st, 'hs_S', [P, 512], F32)
                t2 = self.sb(st, 'hs_t2', [P, 512], F32)
                Sbf = [self.sb(st, f'hs_Sbf{i}', [P, 512], BF16) for i in range(2)]
                kblk = [self.sb(st, f'hs_kb{i}', [P, 4, P], BF16) for i in range(2)]
                ktok = [self.sb(st, f'hs_kt{i}', [P, 512], BF16) for i in range(2)]
                gi = [self.sb(st, f'hs_gi{i}', [P, 512], BF16) for i in range(2)]
                ps_t = self.psum(st, 'hs_ps_t', [P, 2, 512], BF16)
                ps_kv = self.psum(st, 'hs_ps_kv', [P, 2, 512], F32)
                giv = self.dr['pt_gi'].ap().rearrange("(n p) c -> n p c", p=P)
                it = 0
                ic = 0
                for d_ in range(2):
                    kv_ = self.dr[f'hg_k{d_}'].ap().rearrange("(h p) t -> p h t", p=P)
                    Sd = self.dr[f'hg_S{d_}'].ap()
                    pg.op('pool', lambda E: E.memset(Sst[:], 0.0), r=[], w=['hs_S'])
                    order = range(NT) if d_ == 0 else range(NT - 1, -1, -1)
                    for tt in order:
                        i = it % 2; it += 1
                        pg.dma('sp', kblk[i][:], kv_[:, :, tt * P:(tt + 1) * P], r=[(f'hg_k{d_}', hd) for hd in range(4)], w=[('hs_kb', i)])
                        pg.dma('sp', gi[i][:], giv[tt], r=[('pt_gi', tt // 4)], w=[('hs_gi', i)])
                        def tr(E, i=i):
                            ins = None
                            for h in range(4):
                                ins = E.transpose(ps_t[:, i, h * P:(h + 1) * P], kblk[i][:, h, :], self.ident_b[:])
                            return ins
                        pg.op('pe', tr, r=[('hs_kb', i), 'ident_b'], w=[('hs_ps_t', i)])
                        pg.op('act', lambda E, i=i: E.copy(ktok[i][:], ps_t[:, i, :]), r=[('hs_ps_t', i)], w=[('hs_kt', i)])
                        for c in ((0, 1) if d_ == 0 else (1, 0)):
                            n = tt * 2 + c
                            j = ic % 2; ic += 1
                            r0 = c * 64
                            def mm(E, i=i, j=j, r0=r0):
                                ins = None
                                for h in range(4):
                                    hs = slice(h * P, (h + 1) * P)
                                    ins = E.matmul(ps_kv[:, j, hs], ktok[i][r0:r0 + 64, hs], gi[i][r0:r0 + 64, hs], start=True, stop=True)
                                return ins
                            pg.op('pe', mm, r=[('hs_kt', i), ('hs_gi', i)], w=[('hs_ps_kv', j)])
                            dbc = lambda kind, n=n, d_=d_: DEC[:, d_, kind, :, n].unsqueeze(2).to_broadcast([P, 4, 128])
                            pg.op('pool', lambda E, j=j, dbc=dbc: E.tensor_tensor(v3(Sbf[j][:]), v3(Sst[:]), dbc(1), ALU.mult),
                                  r=['hs_S'] + dect, w=[('hs_Sbf', j)])
                            pg.dma('sp', Sd[n], Sbf[j][:], r=[('hs_Sbf', j)], w=[(f'hg_S{d_}', n)])
                            pg.op('dve', lambda E, j=j, dbc=dbc: E.tensor_tensor(v3(t2[:]), v3(ps_kv[:, j, :]), dbc(2), ALU.mult),
                                  r=[('hs_ps_kv', j)] + dect, w=['hs_t2'])
                            pg.op('pool', lambda E, dbc=dbc: E.tensor_tensor(v3(Sst[:]), v3(Sst[:]), dbc(0), ALU.mult),
                                  r=['hs_S'] + dect, w=['hs_S'])
                            pg.op('dve', lambda E: E.tensor_tensor(Sst[:], Sst[:], t2[:], ALU.add), r=['hs_S', 'hs_t2'], w=['hs_S'])
            pg.barrier()
            with ExitStack() as st:
                H = 64
                mask = self.sb(st, 'ho_mask', [H, 2, 64], F32)
                NG = self.sb(st, 'ho_ng', [H, 2, 512], F32)
                yst = self.sb(st, 'ho_yst', [P, 4, S], BF16)
                blk = {}
                for nm in ('q0', 'k0', 'q1', 'k1'):
                    blk[nm] = [self.sb(st, f'ho_{nm}_{i}', [P, 4, P], BF16) for i in range(2)]
                gi = [self.sb(st, f'ho_gi{i}', [H, 2, 512], BF16) for i in range(2)]
                go = [self.sb(st, f'ho_go{i}', [H, 2, 512], BF16) for i in range(2)]
                Sb = [[self.sb(st, f'ho_S{d_}_{i}', [P, 2, 512], BF16) for i in range(2)] for d_ in range(2)]
                aTm = [self.sb(st, f'ho_aTm{i}', [H, 2, 2, 4, 64], BF16) for i in range(2)]
                nd = self.alloc_norm(st, 'ho_n', H, nslot=2)
                ps_a = self.psum(st, 'ho_ps_a', [H, 2, 2, 4, 64], F32)
                ps_o = self.psum(st, 'ho_ps_o', [H, 2, 2, 512], F32)
                ps_t = self.psum(st, 'ho_ps_t', [P, 2, 512], BF16)
                pg.dma('sp', mask[:].rearrange("p a b -> p (a b)"), self.I('c_hg_mask'), w=['ho_mask'])
                for c in range(2):
                    pg.dma('sp', NG[:, c, :], self.I('hgrn_norm_g')[l].partition_broadcast(H), w=[('ho_ng', c)])
                ngt = [('ho_ng', c) for c in range(2)]
                giv = self.dr['pt_gi'].ap().rearrange("(n c p) v -> n p c v", p=H, c=2)
                gov = self.dr['pt_go'].ap().rearrange("(n c p) v -> n p c v", p=H, c=2)
                fm = {nm: self.dr['hg_' + nm].ap().rearrange("(h p) t -> p h t", p=P) for nm in blk}
                Sv = [self.dr[f'hg_S{d_}'].ap().rearrange("(n c) p v -> n p c v", c=2) for d_ in range(2)]
                for tt in range(NT):
                    i = tt % 2
                    tsl = slice(tt * P, (tt + 1) * P)
                    for nm in blk:
                        pg.dma('sp', blk[nm][i][:], fm[nm][:, :, tsl], r=[('hg_' + nm, hd) for hd in range(4)], w=[('ho_' + nm, i)])
                    pg.dma('sp', gi[i][:], giv[tt], r=[('pt_gi', tt // 4)], w=[('ho_gi', i)])
                    pg.dma('sp', go[i][:], gov[tt], r=[('pt_go', tt // 4)], w=[('ho_go', i)])
                    for d_ in range(2):
                        pg.dma('sp', Sb[d_][i][:], Sv[d_][tt], r=[(f'hg_S{d_}', 2 * tt), (f'hg_S{d_}', 2 * tt + 1)], w=[('ho_S', d_, i)])
                    def mma(E, i=i):
                        ins = None
                        for c in range(2):
                            cs = slice(c * 64, (c + 1) * 64)
                            for d_ in range(2):
                                for h in range(4):
                                    ins = E.matmul(ps_a[:, c, d_, h, :], blk[f'k{d_}'][i][:, h, cs], blk[f'q{d_}'][i][:, h, cs],
                                                   start=True, stop=True)
                        return ins
                    pg.op('pe', mma, r=[('ho_' + nm, i) for nm in blk], w=['ho_ps_a'])
                    for c in range(2):
                        pg.op('dve', lambda E, i=i, c=c: E.tensor_tensor(
                            aTm[i][:, c].rearrange("p d h t -> p d h t"), ps_a[:, c],
                            mask[:].unsqueeze(2).to_broadcast([H, 2, 4, 64]), ALU.mult),
                            r=['ho_ps_a', 'ho_mask'], w=[('ho_aTm', i, c)])
                    def mmo(E, i=i):
                        ins = None
                        for c in range(2):
                            cs = slice(c * 64, (c + 1) * 64)
                            for h in range(4):
                                hs = slice(h * P, (h + 1) * P)
                                o = ps_o[:, i, c, hs]
                                E.matmul(o, aTm[i][:, c, 0, h, :], gi[i][:, c, hs], start=True, stop=False)
                                E.matmul(o, aTm[i][:, c, 1, h, :], gi[i][:, c, hs], start=False, stop=False)
                                E.matmul(o, blk['q0'][i][:, h, cs], Sb[0][i][:, c, hs], start=False, stop=False)
                                ins = E.matmul(o, blk['q1'][i][:, h, cs], Sb[1][i][:, c, hs], start=False, stop=True)
                        return ins
                    pg.op('pe', mmo, r=[('ho_aTm', i, 0), ('ho_aTm', i, 1), ('ho_gi', i), ('ho_q0', i), ('ho_q1', i),
                                        ('ho_S', 0, i), ('ho_S', 1, i)], w=[('ho_ps_o', i)])
                    Y = self.norm_gate(nd, ps_o[:, i].rearrange("p c v -> p (c v)"), [('ho_ps_o', i)],
                                       go[i][:].rearrange("p c v -> p (c v)"), ('ho_go', i),
                                       NG[:].rearrange("p c v -> p (c v)"), ngt[0], 'rms', i, H, 8)
                    def tr(E, i=i, Y=Y):
                        ins = None
                        for c in range(2):
                            for cc in range(4):
                                ins = E.transpose(ps_t[:, i, cc * P + c * 64: cc * P + (c + 1) * 64],
                                                  Y[:, c * 512 + cc * P: c * 512 + (cc + 1) * P], self.ident_b[0:H, 0:H])
                        return ins
                    pg.op('pe', tr, r=[('ho_n_y', i), 'ident_b'], w=[('ho_ps_t', i)])
                    pg.op('act', lambda E, i=i, tsl=tsl: E.copy(yst[:, :, tsl], ps_t[:, i, :].rearrange("p (c t) -> p c t", c=4)),
                          r=[('ho_ps_t', i)], w=[('ho_yst', tt)])
                self.store_yT(yst, 'ho_yst', 1024)


    def outproj_phase(self, l):
        nc, pg = self.nc, self.pg
        with ExitStack() as st:
            W = self.sb(st, 'op_w', [P, KC, D], BF16)
            wv = self.I('w_out')[l].rearrange("(k p) n -> p k n", p=P)
            for j in range(4):
                pg.dma('pool', W[:, :, j * 512:(j + 1) * 512], wv[:, :, j * 512:(j + 1) * 512], w=[('op_w', j)])
            wt = [('op_w', j) for j in range(4)]
            yT = [self.sb(st, f'op_y{i}', [P, KC, P], BF16) for i in range(2)]
            so = [self.sb(st, f'op_o{i}', [P, D], F32) for i in range(2)]
            ps = self.psum(st, 'op_ps', [P, 2, 4, 512], F32)
            yv = self.dr['yT_dram'].ap().rearrange("(k p) t -> p k t", p=P)
            rv = self.dr['res_dram'].ap().rearrange("(n p) d -> n p d", p=P)
            for t in range(NT):
                i = t % 2
                pg.dma('sp', yT[i][:], yv[:, :, t * P:(t + 1) * P], r=[('yT_dram', c) for c in range(KC)], w=[('op_y', i)])
                for j in range(4):
                    def mm(E, i=i, j=j):
                        ins = None
                        for k in range(KC):
                            ins = E.matmul(ps[:, i, j, :], yT[i][:, k, :], W[:, k, j * 512:(j + 1) * 512],
                                           start=(k == 0), stop=(k == KC - 1))
                        return ins
                    pg.op('pe', mm, r=[('op_y', i)] + wt, w=[('op_ps', i, j)])
                    o_ap = so[i][:, j * 512:(j + 1) * 512]
                    if j % 2 == 0:
                        pg.op('act', lambda E, o=o_ap, a=ps[:, i, j, :]: E.copy(o, a), r=[('op_ps', i, j)], w=[('op_o', i, j)])
                    else:
                        pg.op('dve', lambda E, o=o_ap, a=ps[:, i, j, :]: E.tensor_copy(o, a), r=[('op_ps', i, j)], w=[('op_o', i, j)])
                pg.dma('sp', rv[t], so[i][:], r=[('op_o', i, j) for j in range(4)], w=[('res_dram', t)])

    def moe_phase(self, l):
        nc, pg = self.nc, self.pg
        TBS = 1024
        NTB = S // TBS
        TPB = TBS // P
        with ExitStack() as st:
            hTb = self.sb(st, 'mo_h', [P, KC, TBS], BF16)
            acc = self.sb(st, 'mo_acc', [P, TPB, D], F32)
            hid = self.sb(st, 'mo_hid', [P, 8, TBS], BF16)
            wg = [self.sb(st, f'mo_wg{i}', [P, KC, 256], BF16) for i in range(2)]
            wu = [self.sb(st, f'mo_wu{i}', [P, KC, 256], BF16) for i in range(2)]
            wd = [self.sb(st, f'mo_wd{i}', [P, 8, 512], BF16) for i in range(2)]
            sg = [self.sb(st, f'mo_sg{i}', [P, 512], F32) for i in range(2)]
            ps_gu = self.psum(st, 'mo_ps_gu', [P, 2, 2, 512], F32)
            ps_d = self.psum(st, 'mo_ps_d', [P, 3, 512], F32)
            hTv = self.dr['hT_dram'].ap().rearrange("(k p) t -> p k t", p=P)
            rv = self.dr['res_dram'].ap().rearrange("(n j p) d -> n p j d", p=P, j=TPB)
            iw = 0; idw = 0; igu = 0; ipd = 0
            for tb in range(NTB):
                for k in range(KC):
                    pg.dma('sp', hTb[:, k, :], hTv[:, k, tb * TBS:(tb + 1) * TBS],
                           r=[('hT_dram', tb * 2), ('hT_dram', tb * 2 + 1)], w=[('mo_h', k)])
                ht = [('mo_h', k) for k in range(KC)]
                for e in range(NE):
                    wgv = self.I('w_gate')[l, e].rearrange("(k p) f -> p k f", p=P)
                    wuv = self.I('w_up')[l, e].rearrange("(k p) f -> p k f", p=P)
                    wdv = self.I('w_down')[l, e].rearrange("(c p) n -> p c n", p=P)
                    for hf in range(4):
                        wi = iw % 2; iw += 1
                        pg.dma('pool', wg[wi][:], wgv[:, :, hf * 256:(hf + 1) * 256], w=[('mo_wg', wi)])
                        pg.dma('pool', wu[wi][:], wuv[:, :, hf * 256:(hf + 1) * 256], w=[('mo_wu', wi)])
                        for f2 in range(2):
                            fc = hf * 2 + f2
                            for th in range(TBS // 512):
                                b = igu % 2; igu += 1
                                def mm(E, wi=wi, f2=f2, th=th, b=b):
                                    ins = None
                                    for k in range(KC):
                                        E.matmul(ps_gu[:, b, 0, :], wg[wi][:, k, f2 * P:(f2 + 1) * P], hTb[:, k, th * 512:(th + 1) * 512],
                                                 start=(k == 0), stop=(k == KC - 1))
                                    for k in range(KC):
                                        ins = E.matmul(ps_gu[:, b, 1, :], wu[wi][:, k, f2 * P:(f2 + 1) * P], hTb[:, k, th * 512:(th + 1) * 512],
                                                       start=(k == 0), stop=(k == KC - 1))
                                    return ins
                                pg.op('pe', mm, r=[('mo_wg', wi), ('mo_wu', wi)] + ht, w=[('mo_ps_gu', b)])
                                pg.op('act', lambda E, b=b: E.activation(sg[b][:], ps_gu[:, b, 0, :], AF.Silu),
                                      r=[('mo_ps_gu', b)], w=[('mo_sg', b)])
                                pg.op('dve', lambda E, b=b, fc=fc, th=th: E.tensor_tensor(hid[:, fc, th * 512:(th + 1) * 512], sg[b][:], ps_gu[:, b, 1, :], ALU.mult),
                                      r=[('mo_sg', b), ('mo_ps_gu', b)], w=[('mo_hid', fc, th)])
                    hidt = [('mo_hid', fc, th) for fc in range(8) for th in range(TBS // 512)]
                    for nch in range(4):
                        di = idw % 2; idw += 1
                        pg.dma('pool', wd[di][:], wdv[:, :, nch * 512:(nch + 1) * 512], w=[('mo_wd', di)])
                        for tl in range(TPB):
                            pb = ipd % 3; ipd += 1
                            tg = tb * TPB + tl
                            def mmd(E, di=di, tl=tl, pb=pb):
                                ins = None
                                for fc in range(8):
                                    ins = E.matmul(ps_d[:, pb, :], hid[:, fc, tl * P:(tl + 1) * P], wd[di][:, fc, :],
                                                   start=(fc == 0), stop=(fc == 7))
                                return ins
                            pg.op('pe', mmd, r=hidt + [('mo_wd', di)], w=[('mo_ps_d', pb)])
                            a_ap = acc[:, tl, nch * 512:(nch + 1) * 512]
                            if e == 0:
                                pg.op('dve', lambda E, a=a_ap, pb=pb, tg=tg, e=e: E.tensor_scalar(a, ps_d[:, pb, :], self.comb[:, tg, e:e + 1], None, ALU.mult),
                                      r=[('mo_ps_d', pb), ('comb', tg)], w=[('mo_acc', tl, nch)])
                            else:
                                pg.op('dve', lambda E, a=a_ap, pb=pb, tg=tg, e=e: E.scalar_tensor_tensor(a, ps_d[:, pb, :], self.comb[:, tg, e:e + 1], a, ALU.mult, ALU.add),
                                      r=[('mo_ps_d', pb), ('comb', tg)], w=[('mo_acc', tl, nch)])
                pg.dma('sp', rv[tb], acc[:], r=[('mo_acc', tl, nch) for tl in range(TPB) for nch in range(4)],
                       w=[('res_dram', tb * TPB + tl) for tl in range(TPB)])


    def route_finalize(self):
        pg = self.pg
        with ExitStack() as st:
            ones = self.sb(st, 'rf_ones', [P, P], BF16)
            ust = self.sb(st, 'rf_ust', [P, P], BF16)
            mE = self.sb(st, 'rf_mE', [P, 512], BF16)
            mS = self.sb(st, 'rf_mS', [P, 512], BF16)
            ecap = self.sb(st, 'rf_ecap', [P, 512], F32)
            thr = self.sb(st, 'rf_thr', [P, 512], F32)
            TOT = self.sb(st, 'rf_TOT', [P, NE, NT], F32)
            INC = self.sb(st, 'rf_INC', [P, NE, NT], F32)
            EXC = self.sb(st, 'rf_EXC', [P, NE, NT], F32)
            G = self.sb(st, 'rf_G', [P, NT, NE], F32)
            CE = self.sb(st, 'rf_CE', [P, NT, NE], F32)
            F1 = self.sb(st, 'rf_F1', [P, NT, NE], F32)
            F2 = self.sb(st, 'rf_F2', [P, NT, NE], F32)
            TMP = self.sb(st, 'rf_TMP', [P, NT, NE], F32)
            Df = self.sb(st, 'rf_Df', [P, 2, NT], F32)
            GT = self.sb(st, 'rf_GT', [P, NE, NT], F32)
            ntf = self.sb(st, 'rf_ntf', [P, NE], F32)
            ps = self.psum(st, 'rf_ps', [P, 2, 512], F32)
            for nm, t_ in (('ones_b', ones), ('ustrict_b', ust), ('maskE', mE), ('maskS', mS), ('ecap', ecap), ('thr', thr)):
                pg.dma('sp', t_[:], self.I('c_' + nm), w=['rf_' + nm])
            selt = [('selm', t) for t in range(NT)]
            sflat = self.selm[:].rearrange("p j e -> p (j e)")
            fl = lambda t_: t_[:].rearrange("p a b -> p (a b)")
            pg.op('pe', lambda E: E.matmul(ps[:, 0, :], ones[:], sflat, start=True, stop=True), r=selt + ['rf_ones_b'], w=['rf_ps0'])
            pg.op('pe', lambda E: E.matmul(ps[:, 1, :], ust[:], sflat, start=True, stop=True), r=selt + ['rf_ustrict_b'], w=['rf_ps1'])
            pg.op('dve', lambda E: E.tensor_copy(TOT[:], ps[:, 0, :].rearrange("p (j e) -> p e j", e=NE)), r=['rf_ps0'], w=['rf_TOT'])
            pg.op('dve', lambda E: E.tensor_tensor_scan(fl(INC), mE[:], fl(TOT), 0.0, ALU.mult, ALU.add), r=['rf_TOT', 'rf_maskE'], w=['rf_INC'])
            pg.op('dve', lambda E: E.tensor_tensor(EXC[:], INC[:], TOT[:], ALU.subtract), r=['rf_INC', 'rf_TOT'], w=['rf_EXC'])
            pg.op('dve', lambda E: E.tensor_tensor(G[:], ps[:, 1, :].rearrange("p (j e) -> p j e", e=NE),
                                                   EXC[:].rearrange("p e j -> p j e"), ALU.add), r=['rf_ps1', 'rf_EXC'], w=['rf_G'])
            pg.op('dve', lambda E: E.tensor_tensor(fl(G), fl(G), ecap[:], ALU.add), r=['rf_G', 'rf_ecap'], w=['rf_G'])
            pg.op('dve', lambda E: E.tensor_tensor_scan(fl(CE), mS[:], sflat, 0.0, ALU.mult, ALU.add), r=selt + ['rf_maskS'], w=['rf_CE'])
            for (F, val, k) in ((F1, 1.0, 0), (F2, 2.0, 1)):
                nm = f'rf_F{k}'
                pg.op('dve', lambda E, F=F, val=val: E.tensor_scalar(fl(F), fl(CE), val, None, ALU.is_equal), r=['rf_CE'], w=[nm])
                pg.op('dve', lambda E, F=F: E.tensor_tensor(fl(F), fl(F), sflat, ALU.mult), r=[nm] + selt, w=[nm])
                pg.op('dve', lambda E, F=F: E.tensor_tensor(TMP[:], F[:], G[:], ALU.mult), r=[nm, 'rf_G'], w=['rf_TMP'])
                pg.op('dve', lambda E, k=k: E.tensor_reduce(Df[:, k, :], TMP[:], AX.X, ALU.add), r=['rf_TMP'], w=[('rf_Df', k)])
                Di = self.D0i if k == 0 else self.D1i
                pg.op('dve', lambda E, k=k, Di=Di: E.tensor_copy(Di[:], Df[:, k, :]), r=[('rf_Df', k)], w=[('Di', k)])
                Wk = self.W0 if k == 0 else self.W1
                pg.op('dve', lambda E, F=F: E.tensor_tensor(TMP[:], F[:], self.comb[:], ALU.mult), r=[nm, 'rf_TMP'] + self.comb_toks, w=['rf_TMP'])
                pg.op('dve', lambda E, Wk=Wk: E.tensor_reduce(Wk[:], TMP[:], AX.X, ALU.add), r=['rf_TMP'], w=[('Wk', k)])
            pg.op('dve', lambda E: E.tensor_tensor(GT[:], INC[:, :, NT - 1:NT].to_broadcast([P, NE, NT]), thr[:].rearrange("p (e j) -> p e j", e=NE), ALU.is_gt),
                  r=['rf_INC', 'rf_thr'], w=['rf_GT'])
            pg.op('dve', lambda E: E.tensor_reduce(ntf[:], GT[:], AX.X, ALU.add), r=['rf_GT'], w=['rf_ntf'])
            pg.op('dve', lambda E: E.tensor_copy(self.nti[:], ntf[:]), r=['rf_ntf'], w=['nti'])

    def scatter_phase(self):
        pg = self.pg
        with ExitStack() as st:
            X = [self.sb(st, f'sc_x{i}', [P, D], BF16) for i in range(3)]
            hv = self.dr['hb_dram'].ap().rearrange("(n p) d -> n p d", p=P)
            bk = self.dr['bucket'].ap()
            for j in range(NT):
                i = j % 3
                pg.dma('sp', X[i][:], hv[j], r=[('hb_dram', j)], w=[('sc_x', i)])
                for Di in (self.D0i, self.D1i):
                    pg.dma('pool', None, None, r=[('sc_x', i), ('Di', 0), ('Di', 1)], w=[],
                           fn=lambda E, i=i, j=j, Di=Di: E.indirect_dma_start(
                               out=bk, out_offset=bass.IndirectOffsetOnAxis(Di[:, j:j + 1], 0), in_=X[i][:], in_offset=None))

    def expert_phase(self, l):
        pg = self.pg
        ENG = ['pe', 'act', 'dve', 'sp']
        with ExitStack() as st:
            Wg2 = [self.sb(st, f'ex_wg{i}', [P, KC, DE], BF16) for i in range(2)]
            Wu2 = [self.sb(st, f'ex_wu{i}', [P, KC, DE], BF16) for i in range(2)]
            Wd = self.sb(st, 'ex_wd', [P, 8, D], BF16)
            Xq = [self.sb(st, f'ex_x{i}', [P, D], BF16) for i in range(2)]
            xT = [self.sb(st, f'ex_xT{i}', [P, KC, P], BF16) for i in range(2)]
            sg = self.sb(st, 'ex_sg', [P, DE], F32)
            hid = [self.sb(st, f'ex_hid{i}', [P, DE], BF16) for i in range(2)]
            hidT = [self.sb(st, f'ex_hidT{i}', [P, 8, P], BF16) for i in range(2)]
            Y = [self.sb(st, f'ex_y{i}', [P, D], BF16) for i in range(2)]
            ps_xf = self.psum(st, 'ex_ps_x', [P, 2, 512], F32)
            ps_x = ps_xf[:].rearrange("p a b -> p (a b)").bitcast(BF16)
            ps_gu = self.psum(st, 'ex_ps_gu', [P, 2, 2, 512], F32)
            ps_h = self.psum(st, 'ex_ps_h', [P, 8 * P], BF16)
            ps_d1 = self.psum(st, 'ex_ps_d', [P, 512], F32)
            bk = self.dr['bucket'].ap()
            yb = self.dr['ybucket'].ap()
            it = 0
            for e in range(NE):
                wgv = self.I('w_gate')[l, e].rearrange("(k p) f -> p k f", p=P)
                wuv = self.I('w_up')[l, e].rearrange("(k p) f -> p k f", p=P)
                wdv = self.I('w_down')[l, e].rearrange("(c p) n -> p c n", p=P)
                wb = e % 2
                Wg, Wu = Wg2[wb], Wu2[wb]
                if e < self.wlim: pg.dma('pool', Wg[:], wgv, w=[('ex_wg', wb, 0), ('ex_wg', wb, 1)])
                if e < self.wlim: pg.dma('pool', Wu[:], wuv, w=[('ex_wu', wb, 0), ('ex_wu', wb, 1)])
                for n_ in range(2 if e < self.wlim else 0):
                    pg.dma('pool', Wd[:, :, n_ * 1024:(n_ + 1) * 1024], wdv[:, :, n_ * 1024:(n_ + 1) * 1024],
                           w=[('ex_wd', 2 * n_), ('ex_wd', 2 * n_ + 1)])
                for en in ENG:
                    pg.op(en, lambda E, en=en, e=e: E.reg_load(self.regs[en], self.nti[0:1, e:e + 1]), r=['nti'])
                nslots = self.qlim
                for q in range(nslots):
                    i = it % 2; it += 1
                    row0 = e * CAP + q * P
                    pg.cond_begin(self.regs, q, ENG)
                    pg.dma('sp', Xq[i][:], bk[row0:row0 + P, :], w=[('ex_x', i)])
                    def trx(E, i=i):
                        ins = None
                        for k in range(KC):
                            ins = E.transpose(ps_x[:, k * P:(k + 1) * P], Xq[i][:, k * P:(k + 1) * P], self.ident_b[:])
                        return ins
                    pg.op('pe', trx, r=[('ex_x', i), 'ident_b'], w=[('ex_psb', 0), ('ex_psb', 1)])
                    pg.op('act', lambda E, i=i: E.copy(xT[i][:, 0:8, :], ps_x[:, 0:8 * P].rearrange("p (k t) -> p k t", k=8)),
                          r=[('ex_psb', 0)], w=[('ex_xT', i, 0)])
                    pg.op('dve', lambda E, i=i: E.tensor_copy(xT[i][:, 8:16, :], ps_x[:, 8 * P:16 * P].rearrange("p (k t) -> p k t", k=8)),
                          r=[('ex_psb', 1)], w=[('ex_xT', i, 1)])
                    for fh in range(2):
                        for a, W, wn in ((0, Wg, 'ex_wg'), (1, Wu, 'ex_wu')):
                            def mgu(E, i=i, a=a, W=W, fh=fh):
                                ins = None
                                for k in range(KC):
                                    ins = E.matmul(ps_gu[:, a, fh, :], xT[i][:, k, :], W[:, k, fh * 512:(fh + 1) * 512],
                                                   start=(k == 0), stop=(k == KC - 1))
                                return ins
                            pg.op('pe', mgu, r=[('ex_xT', i, 0), ('ex_xT', i, 1), (wn, wb, fh)], w=[('ex_ps_gu', a, fh)])
                        pg.op('act', lambda E, fh=fh: E.activation(sg[:, fh * 512:(fh + 1) * 512], ps_gu[:, 0, fh, :], AF.Silu),
                              r=[('ex_ps_gu', 0, fh)], w=[('ex_sg', fh)])
                        pg.op('dve', lambda E, fh=fh, i=i: E.tensor_tensor(hid[i][:, fh * 512:(fh + 1) * 512], sg[:, fh * 512:(fh + 1) * 512],
                                                                            ps_gu[:, 1, fh, :], ALU.mult),
                              r=[('ex_sg', fh), ('ex_ps_gu', 1, fh)], w=[('ex_hid', i, fh)])
                    def trh(E, i=i):
                        ins = None
                        for fc in range(8):
                            ins = E.transpose(ps_h[:, fc * P:(fc + 1) * P], hid[i][:, fc * P:(fc + 1) * P], self.ident_b[:])
                        return ins
                    pg.op('pe', trh, r=[('ex_hid', i, 0), ('ex_hid', i, 1), 'ident_b'], w=['ex_ps_h'])
                    pg.op('act', lambda E, i=i: E.copy(hidT[i][:], ps_h[:].rearrange("p (c t) -> p c t", c=8)),
                          r=['ex_ps_h'], w=[('ex_hidT', i)])
                    for n_ in range(4):
                        if n_ % 3 == 2:
                            pd, ptok = ps_d1[:], 'ex_ps_d'
                        else:
                            pd, ptok = ps_xf[:, n_ % 3, :], ('ex_psb', n_ % 3)
                        def mmd(E, i=i, n_=n_, pd=pd):
                            ins = None
                            for fc in range(8):
                                ins = E.matmul(pd, hidT[i][:, fc, :], Wd[:, fc, n_ * 512:(n_ + 1) * 512],
                                               start=(fc == 0), stop=(fc == 7))
                            return ins
                        pg.op('pe', mmd, r=[('ex_hidT', i), ('ex_wd', n_)], w=[ptok])
                        o_ap = Y[i][:, n_ * 512:(n_ + 1) * 512]
                        if n_ % 2 == 0:
                            pg.op('act', lambda E, o=o_ap, pd=pd: E.copy(o, pd), r=[ptok], w=[('ex_y', i, n_)])
                        else:
                            pg.op('dve', lambda E, o=o_ap, pd=pd: E.tensor_copy(o, pd), r=[ptok], w=[('ex_y', i, n_)])
                    pg.dma('sp', yb[row0:row0 + P, :], Y[i][:], r=[('ex_y', i, n_) for n_ in range(4)], w=[])
                for q in range(nslots):
                    pg.cond_end()

    def gather_phase(self):
        pg = self.pg
        with ExitStack() as st:
            Y0 = [self.sb(st, f'ga_y0{i}', [P, D], BF16) for i in range(2)]
            Y1 = [self.sb(st, f'ga_y1{i}', [P, D], BF16) for i in range(2)]
            T = [self.sb(st, f'ga_t{i}', [P, D], F32) for i in range(2)]
            yb = self.dr['ybucket'].ap()
            rv = self.dr['res_dram'].ap().rearrange("(n p) d -> n p d", p=P)
            for j in range(NT):
                i = j % 2
                for (Yk, Di, nm) in ((Y0[i], self.D0i, 'ga_y0'), (Y1[i], self.D1i, 'ga_y1')):
                    pg.dma('pool', None, None, r=[('Di', 0), ('Di', 1)], w=[(nm, i)],
                           fn=lambda E, Yk=Yk, Di=Di, j=j: E.indirect_dma_start(
                               out=Yk[:], out_offset=None, in_=yb, in_offset=bass.IndirectOffsetOnAxis(Di[:, j:j + 1], 0)))
                pg.op('dve', lambda E, i=i, j=j: E.tensor_scalar(T[i][:], Y0[i][:], self.W0[:, j:j + 1], None, ALU.mult),
                      r=[('ga_y0', i), ('Wk', 0)], w=[('ga_t', i)])
                pg.op('dve', lambda E, i=i, j=j: E.scalar_tensor_tensor(T[i][:], Y1[i][:], self.W1[:, j:j + 1], T[i][:], ALU.mult, ALU.add),
                      r=[('ga_y1', i), ('Wk', 1), ('ga_t', i)], w=[('ga_t', i)])
                pg.dma('sp', rv[j], T[i][:], r=[('ga_t', i)], w=[('res_dram', j)])


_CACHE = {}


def make_in_map(inputs, core, b):
    m = {}
    for k in b.dr:
        if k.startswith('c_'):
            m[k] = b.consts[k[2:]]
        elif k in inputs:
            v = np.asarray(inputs[k])
            m[k] = np.ascontiguousarray(v[core]) if k == 'x' else np.ascontiguousarray(v)
    return m


def kernel(**inputs):
    b = Builder()
    nc = b.build()
    in_maps = [make_in_map(inputs, c, b) for c in range(8)]
    res = run_bass_kernel_spmd(nc, in_maps, core_ids=list(range(8)))
    return np.stack([np.asarray(r['out']) for r in res.results], axis=0).astype(np.float32)
```

```python
import numpy as np
import ml_dtypes
from contextlib import ExitStack
import concourse.bass as bass
import concourse.mybir as mybir
from concourse.bass_utils import run_bass_kernel_spmd

F32 = mybir.dt.float32
BF16 = mybir.dt.bfloat16
AF = mybir.ActivationFunctionType
ALU = mybir.AluOpType
AX = mybir.AxisListType

P = 128
S = 4096
D = 2048
NT = S // P
KC = D // P
DEPTH = 2
D_IN = 6912
NE = 16
DE = 1024
ALPHA = (2.0 * DEPTH) ** 0.25
LN_EPS = 1e-5
HN_EPS = 1e-6
NEG = -30000.0
CAP = 4096

C_AQ, C_AK, C_AV = 0, 512, 640
C_CB, C_CC, C_CH = 768, 1280, 1792
C_GQ, C_GZF, C_GZB, C_GI, C_GO = 2304, 2816, 3328, 3840, 4352
C_RQ, C_RK, C_RV, C_RG = 4864, 5376, 5888, 6400


class Prog:
    EPOCH = 30000

    def __init__(self, nc, es, n_dma_sems=12):
        self.nc = nc
        self.es = es
        self.E = {'pe': nc.tensor, 'act': nc.scalar, 'dve': nc.vector,
                  'pool': nc.gpsimd, 'sp': nc.sync}
        self.nsem = 0
        self.sem = {e: self._new_sem(e) for e in self.E}
        self.cnt = {e: 0 for e in self.E}
        self.known = {e: {} for e in self.E}
        self.semobj = {}
        self.tokw = {}
        self.tokr = {}
        self.dq = {}
        self._sem_owner = {}
        self._cstack = []
        for q in ('sp', 'pool', 'act'):
            self.dq[q] = {'sems': [self._new_sem('d' + q) for _ in range(n_dma_sems)],
                          'rr': 0}
            for s_ in self.dq[q]['sems']:
                self._sem_owner[id(s_)] = q
        self.dtarget = {}
        self.all_sems = {}
        self.n_ops = 0
        self.n_waits = 0

    def _new_sem(self, tag):
        self.nsem += 1
        s = self.es.enter_context(self.nc.semaphore(f"s_{tag}_{self.nsem}"))
        return s

    def _key(self, s):
        return id(s)

    def _collect(self, eng, reads, writes):
        need = {}
        def addh(h):
            k = self._key(h[0])
            if k not in need or need[k][1] < h[1]:
                need[k] = h
        for t in reads:
            for h in self.tokw.get(t, ()):
                addh(h)
        for t in writes:
            for h in self.tokw.get(t, ()):
                addh(h)
            for h in self.tokr.get(t, ()):
                addh(h)
        return need

    def _emit_waits(self, eng, need, skip_own_pe=True):
        kn = self.known[eng]
        acts = []
        for k, (s, v, src) in need.items():
            if eng == 'pe' and src == 'pe':
                continue
            if kn.get(k, 0) >= v:
                continue
            acts.append(('wait', s, v))
            kn[k] = v
            self.n_waits += 1
        return acts

    def _run(self, eng, acts):
        if self._cstack:
            assert eng in self._cstack[-1]['engines'], eng
            self._cstack[-1]['buf'][eng].extend(acts)
            return
        self._do(eng, acts)

    def _do(self, eng, acts):
        E = self.E[eng]
        for a in acts:
            if a[0] == 'wait':
                E.wait_ge(a[1], a[2])
            elif a[0] == 'ins':
                a[1](E).then_inc(a[2], a[3])
            elif a[0] == 'seminc':
                E.sem_inc(a[1], a[2])
            else:
                with E.If_cmp(a[1], a[2], "IS_GT"):
                    self._do(eng, a[3])
                with E.Else():
                    self._do(eng, a[4])

    def _update(self, h, reads, writes):
        k = self._key(h[0])
        for t in writes:
            self.tokw[t] = [h]
            self.tokr[t] = []
        for t in reads:
            if t in writes:
                continue
            lst = self.tokr.get(t)
            if lst is None:
                self.tokr[t] = [h]
            else:
                self.tokr[t] = [x for x in lst if self._key(x[0]) != k] + [h]

    def op(self, eng, fn, r=(), w=()):
        need = self._collect(eng, r, w)
        acts = self._emit_waits(eng, need)
        if self.cnt[eng] >= self.EPOCH:
            assert not self._cstack
            self.sem[eng] = self._new_sem(eng)
            self.cnt[eng] = 0
        self.cnt[eng] += 1
        acts.append(('ins', fn, self.sem[eng], 1))
        self._run(eng, acts)
        h = (self.sem[eng], self.cnt[eng], eng)
        self._update(h, r, w)
        self.n_ops += 1
        return h

    def dma(self, q, out, in_, r=(), w=(), fn=None, **kw):
        dq = self.dq[q]
        s = dq['sems'][dq['rr'] % len(dq['sems'])]
        dq['rr'] += 1
        prev = self.dtarget.get(self._key(s), 0)
        need = self._collect(q, r, w)
        if prev > 0:
            k = self._key(s)
            if k not in need or need[k][1] < prev:
                need[k] = (s, prev, 'dma')
        acts = self._emit_waits(q, need)
        tgt = prev + 16
        self.dtarget[self._key(s)] = tgt
        self.all_sems[self._key(s)] = s
        if fn is None:
            fn = lambda E, out=out, in_=in_, kw=kw: E.dma_start(out=out, in_=in_, **kw)
        acts.append(('ins', fn, s, 16))
        self._run(q, acts)
        h = (s, tgt, 'dma')
        self._update(h, r, w)
        self.n_ops += 1
        return h

    def cond_begin(self, regs, thresh, engines):
        for e in engines:
            assert self.cnt[e] < self.EPOCH - 4000 or self._cstack
        self._cstack.append({'engines': engines, 'regs': regs, 'thresh': thresh,
                             'cnt0': {e: (self.sem[e], self.cnt[e]) for e in engines},
                             'dt0': dict(self.dtarget),
                             'known0': {e: dict(self.known[e]) for e in self.E},
                             'buf': {e: [] for e in engines}})

    def cond_end(self, dma_issuers=('sp',)):
        c = self._cstack.pop()
        for e in c['engines']:
            s0, c0 = c['cnt0'][e]
            assert s0 is self.sem[e]
            n = self.cnt[e] - c0
            els = []
            if n > 0:
                if c0 > 0:
                    els.append(('wait', s0, c0))
                els.append(('seminc', s0, n))
            if e in dma_issuers:
                for k, tgt in self.dtarget.items():
                    t0 = c['dt0'].get(k, 0)
                    if tgt > t0 and self._sem_owner.get(k) == e:
                        if t0 > 0:
                            els.append(('wait', self.all_sems[k], t0))
                        els.append(('seminc', self.all_sems[k], tgt - t0))
            if not c['buf'][e] and not els:
                continue
            act = ('cond', c['regs'][e], c['thresh'], c['buf'][e], els)
            if self._cstack:
                self._cstack[-1]['buf'][e].append(act)
            else:
                self._do(e, [act])
        for e in self.E:
            self.known[e] = c['known0'][e]

    def barrier(self, engines=None, keep=()):
        engines = engines or list(self.E)
        need = {}
        for e in self.E:
            if self.cnt[e] > 0:
                need[self._key(self.sem[e])] = (self.sem[e], self.cnt[e], e)
        for k, s in self.all_sems.items():
            need[k] = (s, self.dtarget[k], 'dma')
        for e in engines:
            E = self.E[e]
            kn = self.known[e]
            for k, (s, v, src) in need.items():
                if src == e and e != 'sp':
                    pass
                if kn.get(k, 0) >= v:
                    continue
                E.wait_ge(s, v)
                kn[k] = v
        self.tokw.clear()
        self.tokr.clear()


def host_consts():
    c = {}
    c['ident_f'] = np.eye(P, dtype=np.float32)
    c['ident_b'] = np.eye(P, dtype=np.float32).astype(ml_dtypes.bfloat16)
    ab = np.zeros((P, 8, 3, P), np.float32)
    s_i = np.arange(P)[:, None]
    t_i = np.arange(P)[None, :]
    for h in range(8):
        slope = 2.0 ** (-(h + 1))
        for j in range(3):
            dist = (j - 1) * P + s_i - t_i
            ab[:, h, j, :] = np.where(np.abs(dist) <= 128, -slope * np.abs(dist), NEG)
    c['attn_bias'] = ab.reshape(P, 8 * 3 * P)
    rc = np.zeros((P, 5, P), np.float32)
    rc[:, 0] = np.maximum(t_i - s_i, 0)
    rc[:, 1] = np.maximum(s_i - t_i, 0)
    rc[:, 2] = (t_i > s_i)
    rc[:, 3] = (s_i > t_i)
    rc[:, 4] = 2.0 * np.eye(P)
    c['ret_c'] = rc.reshape(P, 5 * P)
    rv = np.zeros((P, 2, P), np.float32)
    rv[:, 0] = t_i + 1.0
    rv[:, 1] = 128.0 - t_i
    c['ret_vec'] = rv.reshape(P, 2 * P)
    cmk = np.ones((P, S), np.float32)
    cmk[:, ::64] = 0.0
    c['hg_cm'] = cmk.astype(ml_dtypes.bfloat16)
    s6 = np.arange(64)[:, None]
    t6 = np.arange(64)[None, :]
    hm = np.zeros((64, 2, 64), np.float32)
    hm[:, 0] = (s6 <= t6)
    hm[:, 1] = (s6 >= t6)
    c['hg_mask'] = hm.reshape(64, 128)
    c['ones_b'] = np.ones((P, P), np.float32).astype(ml_dtypes.bfloat16)
    c['ustrict_b'] = (s_i < t_i).astype(np.float32).astype(ml_dtypes.bfloat16)
    mE = np.ones((P, NE, NT), np.float32); mE[:, :, 0] = 0.0
    c['maskE'] = mE.reshape(P, NE * NT).astype(ml_dtypes.bfloat16)
    mS = np.ones((P, NT, NE), np.float32); mS[:, :, 0] = 0.0
    c['maskS'] = mS.reshape(P, NT * NE).astype(ml_dtypes.bfloat16)
    ec = np.zeros((P, NT, NE), np.float32); ec[:, :, :] = (np.arange(NE) * CAP)[None, None, :]
    c['ecap'] = ec.reshape(P, NT * NE)
    th_ = np.zeros((P, NE, NT), np.float32); th_[:, :, :] = (np.arange(NT) * 128.0)[None, None, :]
    c['thr'] = th_.reshape(P, NE * NT)
    c['ret_pcol'] = np.stack([127.0 - np.arange(P), np.arange(P) * 1.0], 1).astype(np.float32)
    return c


class Builder:
    def __init__(self, n_layers=DEPTH, stop=None, taps=(), skip=()):
        self.skip = set(skip)
        self.dense_moe = 'dense' in self.skip
        self.wlim = 2 if 'wlim' in self.skip else 99
        self.qlim = 8 if 'qlim' in self.skip else NT
        self.n_layers = n_layers
        self.stop = stop
        self.taps = set(taps)
        self.nc = bass.Bass("TRN2", target_bir_lowering=False)
        self.es = ExitStack()
        self.pg = Prog(self.nc, self.es)
        self.dr = {}
        self.consts = host_consts()

    def din(self, name, shape, dtype=F32):
        t = self.nc.dram_tensor(name, list(shape), dtype, kind="ExternalInput")
        self.dr[name] = t
        return t

    def dscr(self, name, shape, dtype):
        kind = "ExternalOutput" if name in self.taps else "Internal"
        t = self.nc.dram_tensor(name, list(shape), dtype, kind=kind)
        self.dr[name] = t
        return t

    def sb(self, st, name, shape, dtype):
        self._uid = getattr(self, '_uid', 0) + 1
        return st.enter_context(self.nc.sbuf_tensor(f"{name}_u{self._uid}", list(shape), dtype))

    def psum(self, st, name, shape, dtype=F32):
        self._uid = getattr(self, '_uid', 0) + 1
        return st.enter_context(self.nc.psum_tensor(f"{name}_u{self._uid}", list(shape), dtype))

    IN_SHAPES = {
        'x': [S, D], 'emb_ln_g': [D], 'emb_ln_b': [D], 'w_in': [DEPTH, D, D_IN],
        'attn_sink': [DEPTH, 8], 'conv_w': [DEPTH, 3, 512], 'hgrn_lb': [DEPTH, 512],
        'hgrn_norm_g': [DEPTH, 512], 'ret_decay_logit': [DEPTH, 2, 4], 'ret_norm_g': [DEPTH, 512],
        'w_out': [DEPTH, D, D], 'ln1_g': [DEPTH, D], 'ln1_b': [DEPTH, D],
        'router_w': [D, NE], 'router_b': [NE], 'w_gate': [DEPTH, NE, D, DE],
        'w_up': [DEPTH, NE, D, DE], 'w_down': [DEPTH, NE, DE, D],
        'ln2_g': [DEPTH, D], 'ln2_b': [DEPTH, D],
    }

    def I(self, name):
        if name not in self.dr:
            if name.startswith('c_'):
                v = self.consts[name[2:]]
                self.din(name, v.shape, BF16 if v.dtype == ml_dtypes.bfloat16 else F32)
            else:
                self.din(name, self.IN_SHAPES[name])
        return self.dr[name].ap()

    def declare(self):
        nc = self.nc
        self.out = nc.dram_tensor('out', [S, D], F32, kind="ExternalOutput")
        self.dscr('h_dram', [S, D], F32)
        self.dscr('hT_dram', [D, S], BF16)
        self.declare_proj()
        self.dscr('yT_dram', [D, S], BF16)
        self.dscr('res_dram', [S, D], F32)
        self.dscr('hb_dram', [S, D], BF16)
        self.dscr('bucket', [NE * CAP, D], BF16)
        self.dscr('ybucket', [NE * CAP, D], BF16)
        for d_ in range(2):
            self.dscr(f'hg_q{d_}', [512, S], BF16)
            self.dscr(f'hg_k{d_}', [512, S], BF16)
            self.dscr(f'hg_S{d_}', [S // 64, P, 512], BF16)

    def build(self):
        self.declare()
        nc, pg = self.nc, self.pg
        with ExitStack() as g:
            self.g = g
            self.ident_f = self.sb(g, 'ident_f', [P, P], F32)
            self.ident_b = self.sb(g, 'ident_b', [P, P], BF16)
            self.comb = self.sb(g, 'comb', [P, NT, NE], F32)
            self.comb_toks = [('comb', t) for t in range(NT)]
            I32 = mybir.dt.int32
            self.selm = self.sb(g, 'selm', [P, NT, NE], BF16)
            self.D0i = self.sb(g, 'D0i', [P, NT], I32)
            self.D1i = self.sb(g, 'D1i', [P, NT], I32)
            self.W0 = self.sb(g, 'W0', [P, NT], F32)
            self.W1 = self.sb(g, 'W1', [P, NT], F32)
            self.nti = self.sb(g, 'nti', [P, NE], I32)
            self.regs = {e: pg.E[e].alloc_register('r_nt_' + e) for e in ('pe', 'act', 'dve', 'sp')}
            pg.dma('sp', self.ident_f[:], self.I('c_ident_f'), w=['ident_f'])
            pg.dma('sp', self.ident_b[:], self.I('c_ident_b'), w=['ident_b'])
            self.ln_phase(src='x', g_ap=self.I('emb_ln_g'), b_ap=self.I('emb_ln_b'),
                          mode='x')
            pg.barrier()
            for l in range(self.n_layers):
                if self.stop == 'ln0':
                    break
                self.in_proj_phase(l)
                pg.barrier()
                if self.stop == 'inproj':
                    break
                if 'attn' not in self.skip:
                    self.attn_phase(l)
                    pg.barrier()
                if self.stop == 'attn':
                    break
                if 'conv' not in self.skip:
                    self.conv_phase(l)
                    pg.barrier()
                if 'ret' not in self.skip:
                    self.ret_phase(l)
                    pg.barrier()
                if self.stop in ('conv', 'ret'):
                    break
                if 'hgrn' not in self.skip:
                    self.hgrn_phase(l)
                    pg.barrier()
                if self.stop in ('hgrn', 'mix'):
                    break
                self.outproj_phase(l)
                pg.barrier()
                if self.stop == 'outproj':
                    break
                self.ln_phase(None, self.I('ln1_g')[l], self.I('ln1_b')[l], 'res', router=('norouter' not in self.skip))
                pg.barrier(keep=self.comb_toks)
                if self.stop == 'ln1':
                    break
                if self.dense_moe:
                    self.moe_phase(l)
                    pg.barrier()
                else:
                    self.route_finalize()
                    pg.barrier()
                    if self.stop == 'route':
                        break
                    self.scatter_phase()
                    pg.barrier()
                    if self.stop == 'scatter':
                        break
                    self.expert_phase(l)
                    pg.barrier()
                    if self.stop == 'expert':
                        break
                    self.gather_phase()
                    pg.barrier()
                    if self.stop == 'gather':
                        break
                last = (l == self.n_layers - 1)
                self.ln_phase(None, self.I('ln2_g')[l], self.I('ln2_b')[l], 'res', final=last)
                pg.barrier()
        self.es.close()
        return nc

    def ln_phase(self, src, g_ap, b_ap, mode, final=False, router=False):
        nc, pg = self.nc, self.pg
        with ExitStack() as st:
            gt = self.sb(st, 'ln_g', [P, D], F32)
            bt = self.sb(st, 'ln_b', [P, D], F32)
            pg.dma('sp', gt[:], g_ap.partition_broadcast(P), w=['ln_g'])
            pg.dma('sp', bt[:], b_ap.partition_broadcast(P), w=['ln_b'])
            NB = 3
            xt = [self.sb(st, f'ln_x{i}', [P, D], F32) for i in range(NB)]
            x2 = [self.sb(st, f'ln_r{i}', [P, D], F32) for i in range(NB)] if mode == 'res' else None
            hn = [self.sb(st, f'ln_h{i}', [P, D], F32) for i in range(NB)]
            stt = [self.sb(st, f'ln_st{i}', [P, 4, 6], F32) for i in range(NB)]
            mv = [self.sb(st, f'ln_mv{i}', [P, 4], F32) for i in range(NB)]
            hst = [self.sb(st, f'ln_hst{i}', [P, KC, 512], BF16) for i in range(2)]
            ps = self.psum(st, 'ln_ps', [P, 4 * 512], F32)
            if router:
                hTf = self.sb(st, 'ln_loT', [P, KC, P], BF16)
                Hb = self.sb(st, 'ln_Hb', [P, D], BF16)
                Lo = self.sb(st, 'ln_Lo', [P, D], F32)
                rw = self.sb(st, 'ln_rw', [P, KC, NE], F32)
                rwh = self.sb(st, 'ln_rwh', [P, KC, NE], BF16)
                rwl = self.sb(st, 'ln_rwl', [P, KC, NE], BF16)
                rb = self.sb(st, 'ln_rb', [P, NE], F32)
                rs = self.sb(st, 'ln_rs', [P, 8, NE], F32)
                ps_r = self.psum(st, 'ln_ps_r', [P, 512], F32)
                pg.dma('sp', rw[:], self.I('router_w').rearrange("(k p) e -> p k e", p=P), w=['ln_rw'])
                pg.dma('sp', rb[:], self.I('router_b').partition_broadcast(P), w=['ln_rb'])
                pg.op('act', lambda E: E.copy(rwh[:], rw[:]), r=['ln_rw'], w=['ln_rwh'])
                pg.op('dve', lambda E: E.tensor_tensor(rwl[:], rw[:], rwh[:], ALU.subtract), r=['ln_rw', 'ln_rwh'], w=['ln_rwl'])
            if mode == 'x':
                srcv = self.I(src).rearrange("(n p) d -> n p d", p=P)
            else:
                resv = self.dr['res_dram'].ap().rearrange("(n p) d -> n p d", p=P)
            hdv = self.dr['h_dram'].ap().rearrange("(n p) d -> n p d", p=P)
            outv = self.out.ap().rearrange("(n p) d -> n p d", p=P)
            hTv = self.dr['hT_dram'].ap().rearrange("(k p) t -> p k t", p=P)
            def issue_loads(t):
                i = t % NB
                if mode == 'x':
                    pg.dma('sp', xt[i][:], srcv[t], w=[f'ln_x{i}'])
                else:
                    pg.dma('sp', xt[i][:], hdv[t], r=[('h_dram', t)], w=[f'ln_x{i}'])
                    pg.dma('sp', x2[i][:], resv[t], r=[('res_dram', t)], w=[f'ln_r{i}'])
            issue_loads(0)
            for t in range(NT):
                i = t % NB
                X, H, ST, MV = xt[i], hn[i], stt[i], mv[i]
                tx, th = f'ln_x{i}', f'ln_h{i}'
                if t + 1 < NT:
                    issue_loads(t + 1)
                if mode != 'x':
                    pg.op('dve', lambda E, X=X, R=x2[i]: E.scalar_tensor_tensor(X[:], X[:], ALPHA, R[:], ALU.mult, ALU.add),
                          r=[tx, f'ln_r{i}'], w=[tx])
                self.ln_tile(X, H, ST, MV, gt, bt, tx, th, f'ln_s{i}')
                if final:
                    pg.dma('sp', outv[t], H[:], r=[th], w=[('out', t)])
                    continue
                pg.dma('sp', hdv[t], H[:], r=[th], w=[('h_dram', t)])
                slot = t % 4
                hb = (t // 4) % 2
                HS = hst[hb]
                for half in range(4):
                    def tr(E, half=half, H=H):
                        ins = None
                        for j in range(4):
                            k = half * 4 + j
                            ins = E.transpose(ps[:, half * 512 + j * P: half * 512 + (j + 1) * P],
                                              H[:, k * P:(k + 1) * P], self.ident_f[:])
                        return ins
                    pg.op('pe', tr, r=[th, 'ident_f'], w=[('ln_ps', half)])
                    o_ap = HS[:, half * 4:(half + 1) * 4, slot * P:(slot + 1) * P]
                    i_ap = ps[:, half * 512:(half + 1) * 512].rearrange("p (a b) -> p a b", a=4)
                    if half % 2 == 0:
                        pg.op('act', lambda E, o=o_ap, a=i_ap: E.copy(o, a),
                              r=[('ln_ps', half)], w=[('ln_hst', hb, half, slot)])
                    else:
                        pg.op('dve', lambda E, o=o_ap, a=i_ap: E.tensor_copy(o, a),
                              r=[('ln_ps', half)], w=[('ln_hst', hb, half, slot)])
                if slot == 3:
                    tb = t // 4
                    pg.dma('sp', hTv[:, :, tb * 512:(tb + 1) * 512], HS[:],
                           r=[('ln_hst', hb, hf, sl) for hf in range(4) for sl in range(4)],
                           w=[('hT_dram', tb)])
                if router:
                    pg.op('act', lambda E, H=H: E.copy(Hb[:], H[:]), r=[th], w=['ln_Hb'])
                    pg.dma('sp', self.dr['hb_dram'].ap().rearrange("(n p) d -> n p d", p=P)[t], Hb[:], r=['ln_Hb'], w=[('hb_dram', t)])
                    pg.op('dve', lambda E, H=H: E.tensor_tensor(Lo[:], H[:], Hb[:], ALU.subtract), r=[th, 'ln_Hb'], w=['ln_Lo'])
                    for half in range(4):
                        def tr2(E, half=half):
                            ins = None
                            for j in range(4):
                                k = half * 4 + j
                                ins = E.transpose(ps[:, half * 512 + j * P: half * 512 + (j + 1) * P],
                                                  Lo[:, k * P:(k + 1) * P], self.ident_f[:])
                            return ins
                        pg.op('pe', tr2, r=['ln_Lo', 'ident_f'], w=[('ln_ps', half)])
                        o2 = hTf[:, half * 4:(half + 1) * 4, :]
                        i_ap = ps[:, half * 512:(half + 1) * 512].rearrange("p (a b) -> p a b", a=4)
                        if half % 2 == 1:
                            pg.op('act', lambda E, o=o2, a=i_ap: E.copy(o, a), r=[('ln_ps', half)], w=[('ln_hTf', half)])
                        else:
                            pg.op('dve', lambda E, o=o2, a=i_ap: E.tensor_copy(o, a), r=[('ln_ps', half)], w=[('ln_hTf', half)])
                    hiT = HS[:, :, slot * P:(slot + 1) * P]
                    hit = [('ln_hst', hb, hf, slot) for hf in range(4)]
                    self.route_tile(t, hTf, hiT, hit, rwh, rwl, rb, rs, ps_r)

    def route_tile(self, t, loT, hiT, hit, rwh, rwl, rb, rs, ps_r):
        pg = self.pg
        def mm(E):
            ins = None
            for k in range(KC):
                E.matmul(ps_r[:, 0:NE], hiT[:, k, :], rwh[:, k, :], start=(k == 0), stop=False)
                E.matmul(ps_r[:, 0:NE], loT[:, k, :], rwh[:, k, :], start=False, stop=False)
                ins = E.matmul(ps_r[:, 0:NE], hiT[:, k, :], rwl[:, k, :], start=False, stop=(k == KC - 1))
            return ins
        pg.op('pe', mm, r=[('ln_hTf', hf) for hf in range(4)] + hit + ['ln_rwh', 'ln_rwl'], w=['ln_ps_r'])
        lg, ex, eq, ex2, sel = [rs[:, i, :] for i in range(5)]
        sm = rs[:, 5, :]
        gmk = rs[:, 6, 0:4]
        g3 = lambda ap: ap.rearrange("p (g e) -> p g e", e=4)
        bc = lambda ap: ap.unsqueeze(2).to_broadcast([P, 4, 4])
        T = 'rt'
        pg.op('dve', lambda E: E.tensor_tensor(lg, ps_r[:, 0:NE], rb[:], ALU.add), r=['ln_ps_r', 'ln_rb'], w=[T])
        pg.op('dve', lambda E: E.tensor_reduce(sm[:, 12:13], lg, AX.X, ALU.max), r=[T], w=[T])
        pg.op('dve', lambda E: E.tensor_scalar(sm[:, 12:13], sm[:, 12:13], -1.0, None, ALU.mult), r=[T], w=[T])
        pg.op('act', lambda E: E.activation(ex, lg, AF.Exp, bias=sm[:, 12:13], scale=1.0), r=[T], w=[T])
        pg.op('dve', lambda E: E.tensor_reduce(sm[:, 0:4], g3(ex), AX.X, ALU.max), r=[T], w=[T])
        pg.op('dve', lambda E: E.tensor_tensor(g3(eq), g3(ex), bc(sm[:, 0:4]), ALU.is_equal), r=[T], w=[T])
        pg.op('dve', lambda E: E.scalar_tensor_tensor(ex2, eq, -4.0, ex, ALU.mult, ALU.add), r=[T], w=[T])
        pg.op('dve', lambda E: E.tensor_reduce(sm[:, 4:8], g3(ex2), AX.X, ALU.max), r=[T], w=[T])
        pg.op('dve', lambda E: E.tensor_tensor(sm[:, 8:12], sm[:, 0:4], sm[:, 4:8], ALU.add), r=[T], w=[T])
        pg.op('dve', lambda E: E.tensor_reduce(sm[:, 13:14], sm[:, 8:12], AX.X, ALU.max), r=[T], w=[T])
        pg.op('dve', lambda E: E.tensor_scalar(gmk, sm[:, 8:12], sm[:, 13:14], None, ALU.is_equal), r=[T], w=[T])
        pg.op('dve', lambda E: E.tensor_tensor(g3(sel), g3(ex), bc(sm[:, 4:8]), ALU.is_ge), r=[T], w=[T])
        pg.op('dve', lambda E: E.tensor_tensor(g3(sel), g3(sel), bc(gmk), ALU.mult), r=[T], w=[T])
        pg.op('dve', lambda E: E.tensor_copy(self.selm[:, t, :], sel), r=[T], w=[('selm', t)])
        pg.op('dve', lambda E: E.tensor_tensor(sel, sel, ex, ALU.mult), r=[T], w=[T])
        pg.op('dve', lambda E: E.reciprocal(sm[:, 14:15], sm[:, 13:14]), r=[T], w=[T])
        pg.op('dve', lambda E: E.tensor_scalar(self.comb[:, t, :], sel, sm[:, 14:15], None, ALU.mult), r=[T], w=[('comb', t)])

    def ln_tile(self, X, H, ST, MV, gt, bt, tx, th, ts):
        pg = self.pg
        for j in range(4):
            pg.op('dve', lambda E, j=j: E.bn_stats(ST[:, j, :], X[:, j * 512:(j + 1) * 512]),
                  r=[tx], w=[(ts, 'st', j)])
        pg.op('dve', lambda E: E.bn_aggr(MV[:, 0:2], ST[:].rearrange("p a b -> p (a b)")),
              r=[(ts, 'st', j) for j in range(4)], w=[(ts, 'mv')])
        pg.op('act', lambda E: E.activation(MV[:, 2:3], MV[:, 1:2], AF.Ln, bias=LN_EPS, scale=1.0),
              r=[(ts, 'mv')], w=[(ts, 'lnv')])
        pg.op('act', lambda E: E.activation(MV[:, 3:4], MV[:, 2:3], AF.Exp, scale=-0.5),
              r=[(ts, 'lnv')], w=[(ts, 'rstd')])
        pg.op('dve', lambda E: E.tensor_scalar(H[:], X[:], MV[:, 0:1], MV[:, 3:4],
                                               ALU.subtract, ALU.mult),
              r=[tx, (ts, 'mv'), (ts, 'rstd')], w=[th])
        pg.op('pool', lambda E: E.tensor_tensor(H[:], H[:], gt[:], ALU.mult),
              r=[th, 'ln_g'], w=[th])
        pg.op('pool', lambda E: E.tensor_tensor(H[:], H[:], bt[:], ALU.add),
              r=[th, 'ln_b'], w=[th])


    PF_SPECS = [('aq', C_AQ, 512), ('akd', None, 256), ('cb', C_CB, 512), ('cc', C_CC, 512),
                ('ch', C_CH, 512), ('gq', C_GQ, 512), ('gzf', C_GZF, 512), ('gzb', C_GZB, 512),
                ('rq', C_RQ, 512), ('rk', C_RK, 512)]
    PT_SPECS = [('av', C_AV, 128), ('gi', C_GI, 512), ('go', C_GO, 512), ('rkt', C_RK, 512),
                ('rv', C_RV, 512), ('rg', C_RG, 512)]

    def declare_proj(self):
        for n, _, w in self.PF_SPECS:
            self.dscr('pf_' + n, [w, S], BF16)
        for n, _, w in self.PT_SPECS:
            self.dscr('pt_' + n, [S, w], BF16)

    def load_hT(self, st):
        hT = self.sb(st, 'hT_bf', [P, KC, S], BF16)
        hTv = self.dr['hT_dram'].ap().rearrange("(k p) t -> p k t", p=P)
        for k in range(KC):
            self.pg.dma('sp', hT[:, k, :], hTv[:, k, :], r=[('hT_dram', tb) for tb in range(8)],
                        w=[('hT_bf', k)])
        return hT

    def in_proj_phase(self, l):
        nc, pg = self.nc, self.pg
        with ExitStack() as st:
            hT = self.load_hT(st)
            hT_toks = [('hT_bf', k) for k in range(KC)]
            wt = [self.sb(st, f'ip_w{i}', [P, KC, 512], BF16) for i in range(2)]
            stF = [self.sb(st, f'ip_sf{i}', [P, S], BF16) for i in range(2)]
            stT = [self.sb(st, f'ip_st{i}', [P, 4, 512], BF16) for i in range(2)]
            ps = self.psum(st, 'ip_ps', [P, 8 * 512], F32)
            w_in = self.I('w_in')[l].rearrange("(k p) n -> p k n", p=P)
            gi = 0
            bank = 0
            ev = 0
            nsf = 0
            nst = 0
            for (name, c0, width) in self.PF_SPECS:
                W = wt[gi % 2]; wtok = f'ip_w{gi % 2}'; gi += 1
                if name == 'akd':
                    for j, cc in enumerate([C_AK, C_AK, C_AK + 64, C_AK + 64]):
                        pg.dma('pool', W[:, :, j * 64:(j + 1) * 64], w_in[:, :, cc:cc + 64],
                               w=[(wtok, j)])
                    wtoks = [(wtok, j) for j in range(4)]
                else:
                    pg.dma('pool', W[:, :, 0:width], w_in[:, :, c0:c0 + width], w=[(wtok, 0)])
                    wtoks = [(wtok, 0)]
                dst = self.dr['pf_' + name].ap()
                for j in range(width // P):
                    SF = stF[nsf % 2]; sftok = f'ip_sf{nsf % 2}'; nsf += 1
                    for tb in range(8):
                        b = bank % 8; bank += 1
                        def mm(E, W=W, j=j, tb=tb, b=b):
                            ins = None
                            for k in range(KC):
                                ins = E.matmul(ps[:, b * 512:(b + 1) * 512], W[:, k, j * P:(j + 1) * P],
                                               hT[:, k, tb * 512:(tb + 1) * 512],
                                               start=(k == 0), stop=(k == KC - 1))
                            return ins
                        pg.op('pe', mm, r=wtoks + hT_toks, w=[('ip_ps', b)])
                        o_ap = SF[:, tb * 512:(tb + 1) * 512]
                        i_ap = ps[:, b * 512:(b + 1) * 512]
                        if ev % 2 == 0:
                            pg.op('act', lambda E, o=o_ap, a=i_ap: E.copy(o, a),
                                  r=[('ip_ps', b)], w=[(sftok, tb)])
                        else:
                            pg.op('dve', lambda E, o=o_ap, a=i_ap: E.tensor_copy(o, a),
                                  r=[('ip_ps', b)], w=[(sftok, tb)])
                        ev += 1
                    pg.dma('sp', dst[j * P:(j + 1) * P, :], SF[:],
                           r=[(sftok, tb) for tb in range(8)], w=[('pf_' + name, j)])
            for (name, c0, width) in self.PT_SPECS:
                W = wt[gi % 2]; wtok = f'ip_w{gi % 2}'; gi += 1
                pg.dma('pool', W[:, :, 0:width], w_in[:, :, c0:c0 + width], w=[(wtok, 0)])
                wtoks = [(wtok, 0)]
                dst = self.dr['pt_' + name].ap().rearrange("(n j p) c -> n p j c", p=P, j=4)
                for t in range(NT):
                    b = bank % 8; bank += 1
                    slot = t % 4
                    if slot == 0:
                        ST = stT[nst % 2]; sttok = f'ip_st{nst % 2}'; nst += 1
                    def mm(E, W=W, t=t, b=b, width=width):
                        ins = None
                        for k in range(KC):
                            ins = E.matmul(ps[:, b * 512:b * 512 + width], hT[:, k, t * P:(t + 1) * P],
                                           W[:, k, 0:width], start=(k == 0), stop=(k == KC - 1))
                        return ins
                    pg.op('pe', mm, r=wtoks + hT_toks, w=[('ip_ps', b)])
                    o_ap = ST[:, slot, 0:width]
                    i_ap = ps[:, b * 512:b * 512 + width]
                    if ev % 2 == 0:
                        pg.op('act', lambda E, o=o_ap, a=i_ap: E.copy(o, a),
                              r=[('ip_ps', b)], w=[(sttok, slot)])
                    else:
                        pg.op('dve', lambda E, o=o_ap, a=i_ap: E.tensor_copy(o, a),
                              r=[('ip_ps', b)], w=[(sttok, slot)])
                    ev += 1
                    if slot == 3:
                        pg.dma('sp', dst[t // 4][:, :, 0:width], ST[:, :, 0:width],
                               r=[(sttok, s_) for s_ in range(4)],
                               w=[('pt_' + name, t // 4)])


    def attn_phase(self, l):
        nc, pg = self.nc, self.pg
        with ExitStack() as st:
            q = self.sb(st, 'at_q', [P, 4, S], BF16)
            kd = self.sb(st, 'at_k', [P, 2, S], BF16)
            va = self.sb(st, 'at_v', [P, NT, 2, 65], BF16)
            bias = self.sb(st, 'at_bias', [P, 8, 3, P], F32)
            esink = self.sb(st, 'at_esink', [P, 8], F32)
            yst = self.sb(st, 'at_yst', [P, 4, S], BF16)
            tmp = [self.sb(st, f'at_tmp{i}', [P, 3 * P], F32) for i in range(2)]
            pT = [self.sb(st, f'at_pT{i}', [P, 3 * P], BF16) for i in range(2)]
            den = [self.sb(st, f'at_den{i}', [P, 8], F32) for i in range(2)]
            y = [self.sb(st, f'at_y{i}', [P, 8, 64], BF16) for i in range(2)]
            ps_s = self.psum(st, 'at_ps_s', [P, 2, 512], F32)
            ps_o = self.psum(st, 'at_ps_o', [P, 2, 2, 512], F32)
            ps_t = self.psum(st, 'at_ps_t', [P, 2, 512], BF16)
            pg.dma('sp', q[:], self.dr['pf_aq'].ap().rearrange("(c p) t -> p c t", p=P),
                   r=[('pf_aq', j) for j in range(4)], w=['at_q'])
            pg.dma('sp', kd[:], self.dr['pf_akd'].ap().rearrange("(c p) t -> p c t", p=P),
                   r=[('pf_akd', j) for j in range(2)], w=['at_k'])
            pg.op('pool', lambda E: E.memset(va[:], 1.0), w=['at_v'])
            avv = self.dr['pt_av'].ap().rearrange("(n p) c -> p n c", p=P)
            for kv in range(2):
                pg.dma('sp', va[:, :, kv, 0:64], avv[:, :, kv * 64:(kv + 1) * 64],
                       r=[('pt_av', j) for j in range(8)], w=['at_v'])
            pg.dma('sp', bias[:].rearrange("p a b c -> p (a b c)"), self.I('c_attn_bias'), w=['at_bias'])
            pg.dma('sp', esink[:], self.I('attn_sink')[l].partition_broadcast(P), w=['at_esink'])
            pg.op('act', lambda E: E.activation(esink[:], esink[:], AF.Exp), r=['at_esink'], w=['at_esink'])
            it = 0
            for n in range(NT):
                ob = n % 2
                js = [j for j in range(3) if 0 <= n + j - 1 < NT]
                c0, c1 = js[0] * P, (js[-1] + 1) * P
                for h in range(8):
                    kv = h // 4
                    r0 = (h % 2) * 64
                    sb_ = it % 2; it += 1
                    def mm(E, h=h, kv=kv, r0=r0, sb_=sb_, n=n, js=js):
                        ins = None
                        for j in js:
                            kb = n + j - 1
                            ins = E.matmul(ps_s[:, sb_, j * P:(j + 1) * P],
                                           kd[r0:r0 + 64, kv, kb * P:(kb + 1) * P],
                                           q[r0:r0 + 64, h // 2, n * P:(n + 1) * P],
                                           start=True, stop=True)
                        return ins
                    pg.op('pe', mm, r=['at_q', 'at_k'], w=[('at_ps_s', sb_)])
                    T, PT = tmp[sb_], pT[sb_]
                    pg.op('dve', lambda E, T=T, sb_=sb_, h=h, c0=c0, c1=c1: E.scalar_tensor_tensor(
                        T[:, c0:c1], ps_s[:, sb_, c0:c1], 0.125,
                        bias[:, h, :, :].rearrange("p a b -> p (a b)")[:, c0:c1], ALU.mult, ALU.add),
                        r=[('at_ps_s', sb_), 'at_bias'], w=[('at_tmp', sb_)])
                    pg.op('act', lambda E, T=T, PT=PT, c0=c0, c1=c1: E.activation(PT[:, c0:c1], T[:, c0:c1], AF.Exp),
                          r=[('at_tmp', sb_)], w=[('at_pT', sb_)])
                    def pv(E, h=h, kv=kv, PT=PT, n=n, js=js, ob=ob):
                        ins = None
                        for idx, j in enumerate(js):
                            kb = n + j - 1
                            ins = E.matmul(ps_o[:, ob, h // 4, (h % 4) * 65:(h % 4) * 65 + 65],
                                           PT[:, j * P:(j + 1) * P], va[:, kb, kv, :],
                                           start=(idx == 0), stop=(idx == len(js) - 1))
                        return ins
                    pg.op('pe', pv, r=[('at_pT', sb_), 'at_v'], w=[('at_ps_o', ob, h)])
                DEN, Y = den[ob], y[ob]
                po = ps_o[:, ob, :, 0:260].rearrange("p b (h e) -> p b h e", e=65)
                pg.op('dve', lambda E, DEN=DEN, po=po: E.tensor_tensor(
                    DEN[:].rearrange("p (b h) -> p b h", b=2), po[:, :, :, 64],
                    esink[:].rearrange("p (b h) -> p b h", b=2), ALU.add),
                    r=[('at_ps_o', ob, h) for h in range(8)] + ['at_esink'], w=[('at_den', ob)])
                pg.op('dve', lambda E, DEN=DEN: E.reciprocal(DEN[:], DEN[:]),
                      r=[('at_den', ob)], w=[('at_den', ob)])
                pg.op('dve', lambda E, DEN=DEN, Y=Y, po=po: E.tensor_tensor(
                    Y[:].rearrange("p (b h) d -> p b h d", b=2), po[:, :, :, 0:64],
                    DEN[:].rearrange("p (b h) -> p b h", b=2).unsqueeze(3).to_broadcast([P, 2, 4, 64]),
                    ALU.mult),
                    r=[('at_ps_o', ob, h) for h in range(8)] + [('at_den', ob)], w=[('at_y', ob)])
                self.transpose_out(Y[:].rearrange("p h d -> p (h d)"), ('at_y', ob), ps_t, 'at_ps_t',
                                   yst, 'at_yst', n, ob)
            self.store_yT(yst, 'at_yst', 0)

    def transpose_out(self, Yflat, ytok, ps_t, pstok, yst, ysttok, n, ob, rows=P):
        pg = self.pg
        def tr(E):
            ins = None
            for c in range(4):
                ins = E.transpose(ps_t[:, ob, c * P:(c + 1) * P], Yflat[:, c * P:(c + 1) * P], self.ident_b[:])
            return ins
        pg.op('pe', tr, r=[ytok, 'ident_b'], w=[(pstok, ob)])
        pg.op('act', lambda E: E.copy(yst[:, :, n * P:(n + 1) * P],
                                      ps_t[:, ob, :].rearrange("p (c t) -> p c t", c=4)),
              r=[(pstok, ob)], w=[(ysttok, n)])

    def store_yT(self, yst, ysttok, row0):
        dst = self.dr['yT_dram'].ap()
        for c in range(4):
            self.pg.dma('sp', dst[row0 + c * P: row0 + (c + 1) * P, :], yst[:, c, :],
                        r=[(ysttok, n) for n in range(NT)], w=[('yT_dram', row0 // P + c)])


    def load_chan(self, dst2d, src1d, wtok):
        self.pg.dma('sp', dst2d, src1d.rearrange("(c p) -> p c", p=P), w=[wtok],
                    allow_slow_non_contiguous=True)

    def conv_phase(self, l):
        nc, pg = self.nc, self.pg
        with ExitStack() as st:
            cw = self.sb(st, 'cv_w', [P, 3, 4], F32)
            for wi in range(3):
                self.load_chan(cw[:, wi, :], self.I('conv_w')[l, wi], ('cv_w', wi))
            cwt = [('cv_w', wi) for wi in range(3)]
            U = [self.sb(st, f'cv_u{i}', [P, S + 2], F32) for i in range(2)]
            A = [self.sb(st, f'cv_a{i}', [P, S], F32) for i in range(2)]
            cb = [self.sb(st, f'cv_b{i}', [P, S], BF16) for i in range(2)]
            cc = [self.sb(st, f'cv_c{i}', [P, S], BF16) for i in range(2)]
            ch = [self.sb(st, f'cv_h{i}', [P, S], BF16) for i in range(2)]
            yo = [self.sb(st, f'cv_y{i}', [P, S], BF16) for i in range(2)]
            for i in range(2):
                pg.op('pool', lambda E, i=i: E.memset(U[i][:], 0.0), w=[('cv_u', i)])
            for c in range(4):
                i = c % 2
                rows = slice(c * P, (c + 1) * P)
                pg.dma('sp', cb[i][:], self.dr['pf_cb'].ap()[rows, :], r=[('pf_cb', c)], w=[('cv_b', i)])
                pg.dma('sp', cc[i][:], self.dr['pf_cc'].ap()[rows, :], r=[('pf_cc', c)], w=[('cv_c', i)])
                pg.dma('sp', ch[i][:], self.dr['pf_ch'].ap()[rows, :], r=[('pf_ch', c)], w=[('cv_h', i)])
                pg.op('pool', lambda E, i=i: E.tensor_tensor(U[i][:, 1:S + 1], cc[i][:], ch[i][:], ALU.mult),
                      r=[('cv_c', i), ('cv_h', i)], w=[('cv_u', i)])
                pg.op('dve', lambda E, i=i, c=c: E.tensor_scalar(A[i][:], U[i][:, 1:S + 1], cw[:, 1, c:c + 1], None, ALU.mult),
                      r=[('cv_u', i)] + cwt, w=[('cv_a', i)])
                pg.op('dve', lambda E, i=i, c=c: E.scalar_tensor_tensor(A[i][:], U[i][:, 0:S], cw[:, 0, c:c + 1], A[i][:], ALU.mult, ALU.add),
                      r=[('cv_u', i), ('cv_a', i)] + cwt, w=[('cv_a', i)])
                pg.op('dve', lambda E, i=i, c=c: E.scalar_tensor_tensor(A[i][:], U[i][:, 2:S + 2], cw[:, 2, c:c + 1], A[i][:], ALU.mult, ALU.add),
                      r=[('cv_u', i), ('cv_a', i)] + cwt, w=[('cv_a', i)])
                pg.op('pool', lambda E, i=i: E.tensor_tensor(yo[i][:], A[i][:], cb[i][:], ALU.mult),
                      r=[('cv_a', i), ('cv_b', i)], w=[('cv_y', i)])
                pg.dma('sp', self.dr['yT_dram'].ap()[512 + c * P: 512 + (c + 1) * P, :], yo[i][:],
                       r=[('cv_y', i)], w=[('yT_dram', 4 + c)])

    def alloc_norm(self, st, pfx, rows, nslot=1):
        d = {}
        d['sq'] = self.sb(st, pfx + '_sq', [rows, nslot * 512], F32)
        d['on'] = self.sb(st, pfx + '_on', [rows, nslot * 512], F32)
        d['e'] = self.sb(st, pfx + '_e', [rows, nslot * 512], F32)
        d['st'] = self.sb(st, pfx + '_st', [rows, 6, nslot * 4], F32)
        d['y'] = [self.sb(st, pfx + f'_y{i}', [rows, nslot * 512], BF16) for i in range(2)]
        d['pfx'] = pfx
        return d

    def norm_gate(self, d, po, potoks, G, gtok, NG, ngtok, mode, yi, rows, nh):
        pg = self.pg
        pfx = d['pfx']
        W = nh * 128
        sq, on, e, stt = d['sq'][:, 0:W], d['on'][:, 0:W], d['e'][:, 0:W], d['st']
        Y = d['y'][yi][:, 0:W]
        v3 = lambda ap: ap.rearrange("p (h v) -> p h v", v=128)
        tk = lambda s: (pfx, s)
        ss, sm, mean, var, rstd, msq = [stt[:, i, 0:nh] for i in range(6)]
        pg.op('act', lambda E: E.activation(sq, po, AF.Square), r=potoks, w=[tk('sq')])
        pg.op('dve', lambda E: E.tensor_reduce(ss, v3(sq), AX.X, ALU.add), r=[tk('sq')], w=[tk('ss')])
        if mode == 'gn':
            pg.op('dve', lambda E: E.tensor_reduce(sm, v3(po), AX.X, ALU.add), r=potoks, w=[tk('sm')])
            pg.op('dve', lambda E: E.tensor_scalar(mean, sm, 1.0 / 128, None, ALU.mult), r=[tk('sm')], w=[tk('mean')])
            pg.op('dve', lambda E: E.tensor_tensor(msq, mean, mean, ALU.mult), r=[tk('mean')], w=[tk('msq')])
            pg.op('dve', lambda E: E.scalar_tensor_tensor(var, ss, 1.0 / 128, msq, ALU.mult, ALU.subtract),
                  r=[tk('ss'), tk('msq')], w=[tk('var')])
        else:
            pg.op('dve', lambda E: E.tensor_scalar(var, ss, 1.0 / 128, None, ALU.mult), r=[tk('ss')], w=[tk('var')])
        pg.op('act', lambda E: E.activation(rstd, var, AF.Ln, bias=HN_EPS, scale=1.0), r=[tk('var')], w=[tk('rstd')])
        pg.op('act', lambda E: E.activation(rstd, rstd, AF.Exp, scale=-0.5), r=[tk('rstd')], w=[tk('rstd')])
        bc = lambda ap: ap.unsqueeze(2).to_broadcast([rows, nh, 128])
        if mode == 'gn':
            pg.op('dve', lambda E: E.tensor_tensor(v3(on), v3(po), bc(mean), ALU.subtract),
                  r=potoks + [tk('mean')], w=[tk('on')])
            pg.op('dve', lambda E: E.tensor_tensor(v3(on), v3(on), bc(rstd), ALU.mult),
                  r=[tk('on'), tk('rstd')], w=[tk('on')])
        else:
            pg.op('dve', lambda E: E.tensor_tensor(v3(on), v3(po), bc(rstd), ALU.mult),
                  r=potoks + [tk('rstd')], w=[tk('on')])
        pg.op('pool', lambda E: E.tensor_tensor(on, on, NG, ALU.mult), r=[tk('on'), ngtok], w=[tk('on')])
        pg.op('act', lambda E: E.activation(e, G, AF.Exp, scale=-1.0), r=[gtok], w=[tk('e')])
        pg.op('pool', lambda E: E.tensor_scalar(e, e, 1.0, None, ALU.add), r=[tk('e')], w=[tk('e')])
        pg.op('dve', lambda E: E.reciprocal(e, e), r=[tk('e')], w=[tk('e')])
        pg.op('pool', lambda E: E.tensor_tensor(e, e, G, ALU.mult), r=[tk('e'), gtok], w=[tk('e')])
        pg.op('pool', lambda E: E.tensor_tensor(Y, on, e, ALU.mult), r=[tk('on'), tk('e')], w=[(pfx + '_y', yi)])
        return d['y'][yi]

    def ret_phase(self, l):
        nc, pg = self.nc, self.pg
        SC = 128.0 ** -0.5
        with ExitStack() as st:
            cst = self.sb(st, 'rt_c', [P, 5, P], F32)
            vec = self.sb(st, 'rt_vec', [P, 2, P], F32)
            pcol = self.sb(st, 'rt_pcol', [P, 2], F32)
            lg = self.sb(st, 'rt_lg', [P, 8], F32)
            GL = self.sb(st, 'rt_GL', [P, 8], F32)
            vd = self.sb(st, 'rt_vd', [P, 8], F32)
            DT = self.sb(st, 'rt_DT', [P, 4, P], F32)
            tmpD = self.sb(st, 'rt_tmpD', [P, P], F32)
            dec = self.sb(st, 'rt_dec', [P, 2, 4, P], F32)
            NG = self.sb(st, 'rt_ng', [P, 512], F32)
            prevF = self.sb(st, 'rt_prevF', [P, NT, 512], BF16)
            yst = self.sb(st, 'rt_yst', [P, 4, S], BF16)
            Fs = self.sb(st, 'rt_F', [P, 512], F32)
            Bs = self.sb(st, 'rt_B', [P, 512], F32)
            tmpS = self.sb(st, 'rt_tmpS', [P, 512], F32)
            pB = [self.sb(st, f'rt_pB{i}', [P, 512], BF16) for i in range(2)]
            Kt = [self.sb(st, f'rt_Kt{i}', [P, 512], BF16) for i in range(2)]
            Vt = [self.sb(st, f'rt_Vt{i}', [P, 512], BF16) for i in range(2)]
            Gt = [self.sb(st, f'rt_Gt{i}', [P, 512], BF16) for i in range(2)]
            Qf = [self.sb(st, f'rt_Qf{i}', [P, 4, P], BF16) for i in range(2)]
            Kf = [self.sb(st, f'rt_Kf{i}', [P, 4, P], BF16) for i in range(2)]
            vS = [self.sb(st, f'rt_vS{i}', [P, 512], BF16) for i in range(2)]
            aTm = [self.sb(st, f'rt_aTm{i}', [P, 512], BF16) for i in range(2)]
            qF = [self.sb(st, f'rt_qF{i}', [P, 4, P], BF16) for i in range(2)]
            qB = [self.sb(st, f'rt_qB{i}', [P, 4, P], BF16) for i in range(2)]
            nd = self.alloc_norm(st, 'rt_n', P)
            ps_a = self.psum(st, 'rt_ps_a', [P, 2, 512], F32)
            ps_o = self.psum(st, 'rt_ps_o', [P, 2, 512], F32)
            ps_kv = self.psum(st, 'rt_ps_kv', [P, 2, 512], F32)
            ps_t = self.psum(st, 'rt_ps_t', [P, 2, 512], BF16)
            pg.dma('sp', cst[:].rearrange("p a b -> p (a b)"), self.I('c_ret_c'), w=['rt_c'])
            pg.dma('sp', vec[:].rearrange("p a b -> p (a b)"), self.I('c_ret_vec'), w=['rt_vec'])
            pg.dma('sp', pcol[:], self.I('c_ret_pcol'), w=['rt_pcol'])
            pg.dma('sp', NG[:], self.I('ret_norm_g')[l].partition_broadcast(P), w=['rt_ng'])
            pg.dma('sp', lg[:], self.I('ret_decay_logit')[l].rearrange("a b -> (a b)").partition_broadcast(P), w=['rt_lg'])
            pg.op('act', lambda E: E.activation(lg[:], lg[:], AF.Exp, scale=-1.0), r=['rt_lg'], w=['rt_lg'])
            pg.op('dve', lambda E: E.tensor_scalar(lg[:], lg[:], 1.0, None, ALU.add), r=['rt_lg'], w=['rt_lg'])
            pg.op('act', lambda E: E.activation(lg[:], lg[:], AF.Ln), r=['rt_lg'], w=['rt_lg'])
            pg.op('dve', lambda E: E.tensor_scalar(lg[:], lg[:], -1.0, None, ALU.mult), r=['rt_lg'], w=['rt_lg'])
            pg.op('act', lambda E: E.activation(GL[:], lg[:], AF.Exp, scale=128.0), r=['rt_lg'], w=['rt_GL'])
            lnsc = float(np.log(SC))
            for h in range(4):
                pg.op('act', lambda E, h=h: E.activation(vd[:, h:h + 1], pcol[:, 0:1], AF.Exp, scale=lg[:, h:h + 1], bias=lnsc),
                      r=['rt_lg', 'rt_pcol'], w=[('rt_vd', h)])
                pg.op('act', lambda E, h=h: E.activation(vd[:, 4 + h:5 + h], pcol[:, 1:2], AF.Exp, scale=lg[:, 4 + h:5 + h], bias=lnsc),
                      r=['rt_lg', 'rt_pcol'], w=[('rt_vd', 4 + h)])
                pg.op('act', lambda E, h=h: E.activation(dec[:, 0, h, :], vec[:, 0, :], AF.Exp, scale=lg[:, h:h + 1]),
                      r=['rt_lg', 'rt_vec'], w=[('rt_dec', 0, h)])
                pg.op('act', lambda E, h=h: E.activation(dec[:, 1, h, :], vec[:, 1, :], AF.Exp, scale=lg[:, 4 + h:5 + h]),
                      r=['rt_lg', 'rt_vec'], w=[('rt_dec', 1, h)])
                pg.op('act', lambda E, h=h: E.activation(DT[:, h, :], cst[:, 0, :], AF.Exp, scale=lg[:, h:h + 1]),
                      r=['rt_lg', 'rt_c'], w=[('rt_DT', h)])
                pg.op('dve', lambda E, h=h: E.tensor_tensor(DT[:, h, :], DT[:, h, :], cst[:, 2, :], ALU.mult),
                      r=[('rt_DT', h), 'rt_c'], w=[('rt_DT', h)])
                pg.op('act', lambda E, h=h: E.activation(tmpD[:], cst[:, 1, :], AF.Exp, scale=lg[:, 4 + h:5 + h]),
                      r=['rt_lg', 'rt_c'], w=['rt_tmpD'])
                pg.op('dve', lambda E: E.tensor_tensor(tmpD[:], tmpD[:], cst[:, 3, :], ALU.mult),
                      r=['rt_tmpD', 'rt_c'], w=['rt_tmpD'])
                pg.op('dve', lambda E, h=h: E.tensor_tensor(DT[:, h, :], DT[:, h, :], tmpD[:], ALU.add),
                      r=[('rt_DT', h), 'rt_tmpD'], w=[('rt_DT', h)])
                pg.op('dve', lambda E, h=h: E.tensor_tensor(DT[:, h, :], DT[:, h, :], cst[:, 4, :], ALU.add),
                      r=[('rt_DT', h), 'rt_c'], w=[('rt_DT', h)])
                pg.op('dve', lambda E, h=h: E.tensor_scalar(DT[:, h, :], DT[:, h, :], SC, None, ALU.mult),
                      r=[('rt_DT', h)], w=[('rt_DT', h)])
            DTt = [('rt_DT', h) for h in range(4)]
            vdt = [('rt_vd', h) for h in range(8)]
            dect = [('rt_dec', a, h) for a in range(2) for h in range(4)]
            pg.op('pool', lambda E: E.memset(Fs[:], 0.0), w=['rt_F'])
            pg.op('pool', lambda E: E.memset(Bs[:], 0.0), w=['rt_B'])
            ktv = self.dr['pt_rkt'].ap().rearrange("(n p) c -> n p c", p=P)
            vtv = self.dr['pt_rv'].ap().rearrange("(n p) c -> n p c", p=P)
            gtv = self.dr['pt_rg'].ap().rearrange("(n p) c -> n p c", p=P)
            qfv = self.dr['pf_rq'].ap().rearrange("(h p) t -> p h t", p=P)
            kfv = self.dr['pf_rk'].ap().rearrange("(h p) t -> p h t", p=P)
            v3 = lambda ap: ap.rearrange("p (h v) -> p h v", v=128)
            bc4 = lambda ap: ap.unsqueeze(2).to_broadcast([P, 4, 128])
            it = 0

            def kv_step(n, i, K, V, vdcols, state, stok, GLcols):
                VS = vS[i]
                pg.op('pool', lambda E: E.tensor_tensor(v3(VS[:]), v3(V[:]), bc4(vdcols), ALU.mult),
                      r=[('rt_Vt', i)] + vdt, w=[('rt_vS', i)])
                def mm(E):
                    ins = None
                    for h in range(4):
                        ins = E.matmul(ps_kv[:, i, h * P:(h + 1) * P], K[:, h * P:(h + 1) * P], VS[:, h * P:(h + 1) * P],
                                       start=True, stop=True)
                    return ins
                pg.op('pe', mm, r=[('rt_Kt', i), ('rt_vS', i)], w=[('rt_ps_kv', i)])
                pg.op('pool', lambda E: E.tensor_tensor(v3(tmpS[:]), v3(state[:]), bc4(GLcols), ALU.mult),
                      r=[stok, 'rt_GL'], w=['rt_tmpS'])
                pg.op('dve', lambda E: E.tensor_tensor(state[:], tmpS[:], ps_kv[:, i, :], ALU.add),
                      r=['rt_tmpS', ('rt_ps_kv', i)], w=[stok])

            for n in range(NT):
                i = it % 2; it += 1
                pg.dma('sp', Kt[i][:], ktv[n], r=[('pt_rkt', n // 4)], w=[('rt_Kt', i)])
                pg.dma('sp', Vt[i][:], vtv[n], r=[('pt_rv', n // 4)], w=[('rt_Vt', i)])
                pg.op('act', lambda E, n=n: E.copy(prevF[:, n, :], Fs[:]), r=['rt_F'], w=[('rt_prevF', n)])
                kv_step(n, i, Kt[i], Vt[i], vd[:, 0:4], Fs, 'rt_F', GL[:, 0:4])
            for n in range(NT - 1, -1, -1):
                i = it % 2; it += 1
                tsl = slice(n * P, (n + 1) * P)
                pg.dma('sp', Kt[i][:], ktv[n], r=[('pt_rkt', n // 4)], w=[('rt_Kt', i)])
                pg.dma('sp', Vt[i][:], vtv[n], r=[('pt_rv', n // 4)], w=[('rt_Vt', i)])
                pg.dma('sp', Gt[i][:], gtv[n], r=[('pt_rg', n // 4)], w=[('rt_Gt', i)])
                pg.dma('sp', Qf[i][:], qfv[:, :, tsl], r=[('pf_rq', h) for h in range(4)], w=[('rt_Qf', i)])
                pg.dma('sp', Kf[i][:], kfv[:, :, tsl], r=[('pf_rk', h) for h in range(4)], w=[('rt_Kf', i)])
                def mma(E, i=i):
                    ins = None
                    for h in range(4):
                        ins = E.matmul(ps_a[:, i, h * P:(h + 1) * P], Kf[i][:, h, :], Qf[i][:, h, :], start=True, stop=True)
                    return ins
                pg.op('pe', mma, r=[('rt_Qf', i), ('rt_Kf', i)], w=[('rt_ps_a', i)])
                pg.op('dve', lambda E, i=i: E.tensor_tensor(aTm[i][:], ps_a[:, i, :], DT[:].rearrange("p h t -> p (h t)"), ALU.mult),
                      r=[('rt_ps_a', i)] + DTt, w=[('rt_aTm', i)])
                pg.op('pool', lambda E, i=i: E.tensor_tensor(qF[i][:], Qf[i][:], dec[:, 0, :, :], ALU.mult),
                      r=[('rt_Qf', i)] + dect, w=[('rt_qF', i)])
                pg.op('pool', lambda E, i=i: E.tensor_tensor(qB[i][:], Qf[i][:], dec[:, 1, :, :], ALU.mult),
                      r=[('rt_Qf', i)] + dect, w=[('rt_qB', i)])
                pg.op('act', lambda E, i=i: E.copy(pB[i][:], Bs[:]), r=['rt_B'], w=[('rt_pB', i)])
                def mmo(E, i=i, n=n):
                    ins = None
                    for h in range(4):
                        hs = slice(h * P, (h + 1) * P)
                        E.matmul(ps_o[:, i, hs], aTm[i][:, hs], Vt[i][:, hs], start=True, stop=False)
                        E.matmul(ps_o[:, i, hs], qF[i][:, h, :], prevF[:, n, hs], start=False, stop=False)
                        ins = E.matmul(ps_o[:, i, hs], qB[i][:, h, :], pB[i][:, hs], start=False, stop=True)
                    return ins
                pg.op('pe', mmo, r=[('rt_aTm', i), ('rt_Vt', i), ('rt_qF', i), ('rt_qB', i), ('rt_prevF', n), ('rt_pB', i)],
                      w=[('rt_ps_o', i)])
                kv_step(n, i, Kt[i], Vt[i], vd[:, 4:8], Bs, 'rt_B', GL[:, 4:8])
                Y = self.norm_gate(nd, ps_o[:, i, :], [('rt_ps_o', i)], Gt[i][:], ('rt_Gt', i), NG[:], 'rt_ng',
                                   'gn', i, P, 4)
                self.transpose_out(Y[:], ('rt_n_y', i), ps_t, 'rt_ps_t', yst, 'rt_yst', n, i)
            self.store_yT(yst, 'rt_yst', 1536)


    def hgrn_phase(self, l):
        nc, pg = self.nc, self.pg
        NCH = S // 64
        with ExitStack() as outer:
            DEC = self.sb(outer, 'hg_DEC', [P, 2, 3, 4, NCH], F32)
            dect = [('hg_DEC', d_, hd) for d_ in range(2) for hd in range(4)]
            with ExitStack() as st:
                lb = self.sb(st, 'hg_lb', [P, 4], F32)
                oml = self.sb(st, 'hg_oml', [P, 4], F32)
                a0 = self.sb(st, 'hg_a0', [P, 4], F32)
                cm = self.sb(st, 'hg_cm', [P, S], BF16)
                T = [self.sb(st, f'hg_T{i}', [P, S], F32) for i in range(4)]
                tmpd = self.sb(st, 'hg_tmpd', [P, NCH], F32)
                zb = [self.sb(st, f'hg_z{i}', [P, S], BF16) for i in range(2)]
                qb = [self.sb(st, f'hg_qin{i}', [P, S], BF16) for i in range(2)]
                qo = [self.sb(st, f'hg_qo{i}', [P, S], BF16) for i in range(2)]
                ko = [self.sb(st, f'hg_ko{i}', [P, S], BF16) for i in range(2)]
                pg.dma('sp', cm[:], self.I('c_hg_cm'), w=['hg_cm'])
                if l == 0:
                    pg.op('pool', lambda E: E.memset(lb[:], 0.0), w=['hg_lb'])
                else:
                    self.load_chan(a0[:], self.I('hgrn_lb')[0], 'hg_a0')
                    self.load_chan(lb[:], self.I('hgrn_lb')[1], 'hg_lb')
                    pg.op('dve', lambda E: E.tensor_tensor(lb[:], a0[:], lb[:], ALU.subtract), r=['hg_a0', 'hg_lb'], w=['hg_lb'])
                    pg.op('act', lambda E: E.activation(lb[:], lb[:], AF.Exp), r=['hg_lb'], w=['hg_lb'])
                    pg.op('dve', lambda E: E.tensor_scalar(lb[:], lb[:], 1.0, None, ALU.add), r=['hg_lb'], w=['hg_lb'])
                    pg.op('dve', lambda E: E.reciprocal(lb[:], lb[:]), r=['hg_lb'], w=['hg_lb'])
                pg.op('dve', lambda E: E.tensor_scalar(oml[:], lb[:], -1.0, 1.0, ALU.mult, ALU.add), r=['hg_lb'], w=['hg_oml'])
                it = 0
                for d_ in range(2):
                    zname = 'pf_gzf' if d_ == 0 else 'pf_gzb'
                    mid, last = (31, 63) if d_ == 0 else (32, 0)
                    for hd in range(4):
                        i = it % 2; it += 1
                        rows = slice(hd * P, (hd + 1) * P)
                        T1, T2, T3, T4 = T
                        pg.dma('sp', zb[i][:], self.dr[zname].ap()[rows, :], r=[(zname, hd)], w=[('hg_z', i)])
                        pg.dma('sp', qb[i][:], self.dr['pf_gq'].ap()[rows, :], r=[('pf_gq', hd)], w=[('hg_qin', i)])
                        pg.op('act', lambda E, i=i: E.activation(T1[:], zb[i][:], AF.Exp, scale=-1.0), r=[('hg_z', i)], w=['hg_T1'])
                        pg.op('pool', lambda E: E.tensor_scalar(T1[:], T1[:], 1.0, None, ALU.add), r=['hg_T1'], w=['hg_T1'])
                        pg.op('dve', lambda E: E.reciprocal(T1[:], T1[:]), r=['hg_T1'], w=['hg_T1'])
                        pg.op('dve', lambda E, hd=hd: E.tensor_scalar(T1[:], T1[:], oml[:, hd:hd + 1], lb[:, hd:hd + 1], ALU.mult, ALU.add),
                              r=['hg_T1', 'hg_lb', 'hg_oml'], w=['hg_T1'])
                        pg.op('act', lambda E: E.activation(T2[:], T1[:], AF.Ln), r=['hg_T1'], w=['hg_T2'])
                        pg.op('dve', lambda E: E.tensor_tensor_scan(T3[:], cm[:], T2[:], 0.0, ALU.mult, ALU.add),
                              r=['hg_cm', 'hg_T2'], w=['hg_T3'])
                        c3 = lambda ap: ap.rearrange("p (n c) -> p n c", c=64)
                        if d_ == 0:
                            Bt, Btok = T3, 'hg_T3'
                        else:
                            pg.op('dve', lambda E: E.tensor_tensor(c3(T4[:]), c3(T3[:])[:, :, 63:64].to_broadcast([P, NCH, 64]),
                                                                   c3(T3[:]), ALU.subtract), r=['hg_T3'], w=['hg_T4'])
                            pg.op('pool', lambda E: E.tensor_tensor(T4[:], T4[:], T2[:], ALU.add), r=['hg_T4', 'hg_T2'], w=['hg_T4'])
                            Bt, Btok = T4, 'hg_T4'
                        B3 = c3(Bt[:])
                        dk = ('hg_DEC', d_, hd)
                        pg.op('act', lambda E, B3=B3, d_=d_, hd=hd, last=last: E.activation(DEC[:, d_, 0, hd, :], B3[:, :, last], AF.Exp),
                              r=[Btok], w=[dk])
                        pg.op('act', lambda E, B3=B3, d_=d_, hd=hd, mid=mid: E.activation(DEC[:, d_, 1, hd, :], B3[:, :, mid], AF.Exp),
                              r=[Btok], w=[dk])
                        pg.op('dve', lambda E, B3=B3, mid=mid, last=last: E.tensor_tensor(tmpd[:], B3[:, :, last], B3[:, :, mid], ALU.subtract),
                              r=[Btok], w=['hg_tmpd'])
                        pg.op('act', lambda E, d_=d_, hd=hd: E.activation(DEC[:, d_, 2, hd, :], tmpd[:], AF.Exp),
                              r=['hg_tmpd'], w=[dk])
                        pg.op('dve', lambda E, B3=B3, mid=mid: E.tensor_tensor(c3(T2[:]), B3, B3[:, :, mid:mid + 1].to_broadcast([P, NCH, 64]),
                                                                               ALU.subtract), r=[Btok, 'hg_T2'], w=['hg_T2'])
                        EP, EPtok = (T4, 'hg_T4') if d_ == 0 else (T3, 'hg_T3')
                        pg.op('act', lambda E, EP=EP: E.activation(EP[:], T2[:], AF.Exp), r=['hg_T2', Btok], w=[EPtok])
                        pg.op('pool', lambda E, i=i, EP=EP: E.tensor_tensor(qo[i][:], qb[i][:], EP[:], ALU.mult),
                              r=[('hg_qin', i), EPtok], w=[('hg_qo', i)])
                        pg.dma('sp', self.dr[f'hg_q{d_}'].ap()[rows, :], qo[i][:], r=[('hg_qo', i)], w=[(f'hg_q{d_}', hd)])
                        EM, EMtok = (T3, 'hg_T3') if d_ == 0 else (T4, 'hg_T4')
                        pg.op('act', lambda E, EM=EM: E.activation(EM[:], T2[:], AF.Exp, scale=-1.0), r=['hg_T2', EPtok, ('hg_qo', i)], w=[EMtok])
                        pg.op('pool', lambda E: E.tensor_scalar(T1[:], T1[:], -1.0, 1.0, ALU.mult, ALU.add), r=['hg_T1'], w=['hg_T1'])
                        pg.op('dve', lambda E, i=i, EM=EM: E.tensor_tensor(ko[i][:], T1[:], EM[:], ALU.mult),
                              r=['hg_T1', EMtok], w=[('hg_ko', i)])
                        pg.dma('sp', self.dr[f'hg_k{d_}'].ap()[rows, :], ko[i][:], r=[('hg_ko', i)], w=[(f'hg_k{d_}', hd)])
            pg.barrier()
            v3 = lambda ap: ap.rearrange("p (h v) -> p h v", v=128)
            with ExitStack() as st:
                Sst = self.sb(st, 'hs_S', [P, 512], F32)
                t2 = self.sb(st, 'hs_t2', [P, 512], F32)
                Sbf = [self.sb(st, f'hs_Sbf{i}', [P, 512], BF16) for i in range(2)]
                kblk = [self.sb(st, f'hs_kb{i}', [P, 4, P], BF16) for i in range(2)]
                ktok = [self.sb(st, f'hs_kt{i}', [P, 512], BF16) for i in range(2)]
                gi = [self.sb(st, f'hs_gi{i}', [P, 512], BF16) for i in range(2)]
                ps_t = self.psum(st, 'hs_ps_t', [P, 2, 512], BF16)
                ps_kv = self.psum(st, 'hs_ps_kv', [P, 2, 512], F32)
                giv = self.dr['pt_gi'].ap().rearrange("(n p) c -> n p c", p=P)
                it = 0
                ic = 0
                for d_ in range(2):
                    kv_ = self.dr[f'hg_k{d_}'].ap().rearrange("(h p) t -> p h t", p=P)
                    Sd = self.dr[f'hg_S{d_}'].ap()
                    pg.op('pool', lambda E: E.memset(Sst[:], 0.0), r=[], w=['hs_S'])
                    order = range(NT) if d_ == 0 else range(NT - 1, -1, -1)
                    order = list(order)
                    def hs_load(tt, i):
                        pg.dma('sp', kblk[i][:], kv_[:, :, tt * P:(tt + 1) * P], r=[(f'hg_k{d_}', hd) for hd in range(4)], w=[('hs_kb', i)])
                        pg.dma('sp', gi[i][:], giv[tt], r=[('pt_gi', tt // 4)], w=[('hs_gi', i)])
                    hs_load(order[0], it % 2)
                    for oi, tt in enumerate(order):
                        i = it % 2; it += 1
                        if oi + 1 < len(order):
                            hs_load(order[oi + 1], it % 2)
                        def tr(E, i=i):
                            ins = None
                            for h in range(4):
                                ins = E.transpose(ps_t[:, i, h * P:(h + 1) * P], kblk[i][:, h, :], self.ident_b[:])
                            return ins
                        pg.op('pe', tr, r=[('hs_kb', i), 'ident_b'], w=[('hs_ps_t', i)])
                        pg.op('act', lambda E, i=i: E.copy(ktok[i][:], ps_t[:, i, :]), r=[('hs_ps_t', i)], w=[('hs_kt', i)])
                        for c in ((0, 1) if d_ == 0 else (1, 0)):
                            n = tt * 2 + c
                            j = ic % 2; ic += 1
                            r0 = c * 64
                            def mm(E, i=i, j=j, r0=r0):
                                ins = None
                                for h in range(4):
                                    hs = slice(h * P, (h + 1) * P)
                                    ins = E.matmul(ps_kv[:, j, hs], ktok[i][r0:r0 + 64, hs], gi[i][r0:r0 + 64, hs], start=True, stop=True)
                                return ins
                            pg.op('pe', mm, r=[('hs_kt', i), ('hs_gi', i)], w=[('hs_ps_kv', j)])
                            dbc = lambda kind, n=n, d_=d_: DEC[:, d_, kind, :, n].unsqueeze(2).to_broadcast([P, 4, 128])
                            pg.op('pool', lambda E, j=j, dbc=dbc: E.tensor_tensor(v3(Sbf[j][:]), v3(Sst[:]), dbc(1), ALU.mult),
                                  r=['hs_S'] + dect, w=[('hs_Sbf', j)])
                            pg.dma('sp', Sd[n], Sbf[j][:], r=[('hs_Sbf', j)], w=[(f'hg_S{d_}', n)])
                            pg.op('dve', lambda E, j=j, dbc=dbc: E.tensor_tensor(v3(t2[:]), v3(ps_kv[:, j, :]), dbc(2), ALU.mult),
                                  r=[('hs_ps_kv', j)] + dect, w=['hs_t2'])
                            pg.op('pool', lambda E, dbc=dbc: E.tensor_tensor(v3(Sst[:]), v3(Sst[:]), dbc(0), ALU.mult),
                                  r=['hs_S'] + dect, w=['hs_S'])
                            pg.op('dve', lambda E: E.tensor_tensor(Sst[:], Sst[:], t2[:], ALU.add), r=['hs_S', 'hs_t2'], w=['hs_S'])
            pg.barrier()
            with ExitStack() as st:
                H = 64
                mask = self.sb(st, 'ho_mask', [H, 2, 64], F32)
                NG = self.sb(st, 'ho_ng', [H, 2, 512], F32)
                yst = self.sb(st, 'ho_yst', [P, 4, S], BF16)
                blk = {}
                for nm in ('q0', 'k0', 'q1', 'k1'):
                    blk[nm] = [self.sb(st, f'ho_{nm}_{i}', [P, 4, P], BF16) for i in range(2)]
                gi = [self.sb(st, f'ho_gi{i}', [H, 2, 512], BF16) for i in range(2)]
                go = [self.sb(st, f'ho_go{i}', [H, 2, 512], BF16) for i in range(2)]
                Sb = [[self.sb(st, f'ho_S{d_}_{i}', [P, 2, 512], BF16) for i in range(2)] for d_ in range(2)]
                aTm = [self.sb(st, f'ho_aTm{i}', [H, 2, 2, 4, 64], BF16) for i in range(2)]
                nd = self.alloc_norm(st, 'ho_n', H, nslot=2)
                ps_a = self.psum(st, 'ho_ps_a', [H, 2, 2, 4, 64], F32)
                ps_o = self.psum(st, 'ho_ps_o', [H, 2, 2, 512], F32)
                ps_t = self.psum(st, 'ho_ps_t', [P, 2, 512], BF16)
                pg.dma('sp', mask[:].rearrange("p a b -> p (a b)"), self.I('c_hg_mask'), w=['ho_mask'])
                for c in range(2):
                    pg.dma('sp', NG[:, c, :], self.I('hgrn_norm_g')[l].partition_broadcast(H), w=[('ho_ng', c)])
                ngt = [('ho_ng', c) for c in range(2)]
                giv = self.dr['pt_gi'].ap().rearrange("(n c p) v -> n p c v", p=H, c=2)
                gov = self.dr['pt_go'].ap().rearrange("(n c p) v -> n p c v", p=H, c=2)
                fm = {nm: self.dr['hg_' + nm].ap().rearrange("(h p) t -> p h t", p=P) for nm in blk}
                Sv = [self.dr[f'hg_S{d_}'].ap().rearrange("(n c) p v -> n p c v", c=2) for d_ in range(2)]
                for tt in range(NT):
                    i = tt % 2
                    tsl = slice(tt * P, (tt + 1) * P)
                    for nm in blk:
                        pg.dma('sp', blk[nm][i][:], fm[nm][:, :, tsl], r=[('hg_' + nm, hd) for hd in range(4)], w=[('ho_' + nm, i)])
                    pg.dma('sp', gi[i][:], giv[tt], r=[('pt_gi', tt // 4)], w=[('ho_gi', i)])
                    pg.dma('sp', go[i][:], gov[tt], r=[('pt_go', tt // 4)], w=[('ho_go', i)])
                    for d_ in range(2):
                        pg.dma('sp', Sb[d_][i][:], Sv[d_][tt], r=[(f'hg_S{d_}', 2 * tt), (f'hg_S{d_}', 2 * tt + 1)], w=[('ho_S', d_, i)])
                    def mma(E, i=i):
                        ins = None
                        for c in range(2):
                            cs = slice(c * 64, (c + 1) * 64)
                            for d_ in range(2):
                                for h in range(4):
                                    ins = E.matmul(ps_a[:, c, d_, h, :], blk[f'k{d_}'][i][:, h, cs], blk[f'q{d_}'][i][:, h, cs],
                                                   start=True, stop=True)
                        return ins
                    pg.op('pe', mma, r=[('ho_' + nm, i) for nm in blk], w=['ho_ps_a'])
                    for c in range(2):
                        pg.op('dve', lambda E, i=i, c=c: E.tensor_tensor(
                            aTm[i][:, c].rearrange("p d h t -> p d h t"), ps_a[:, c],
                            mask[:].unsqueeze(2).to_broadcast([H, 2, 4, 64]), ALU.mult),
                            r=['ho_ps_a', 'ho_mask'], w=[('ho_aTm', i, c)])
                    def mmo(E, i=i):
                        ins = None
                        for c in range(2):
                            cs = slice(c * 64, (c + 1) * 64)
                            for h in range(4):
                                hs = slice(h * P, (h + 1) * P)
                                o = ps_o[:, i, c, hs]
                                E.matmul(o, aTm[i][:, c, 0, h, :], gi[i][:, c, hs], start=True, stop=False)
                                E.matmul(o, aTm[i][:, c, 1, h, :], gi[i][:, c, hs], start=False, stop=False)
                                E.matmul(o, blk['q0'][i][:, h, cs], Sb[0][i][:, c, hs], start=False, stop=False)
                                ins = E.matmul(o, blk['q1'][i][:, h, cs], Sb[1][i][:, c, hs], start=False, stop=True)
                        return ins
                    pg.op('pe', mmo, r=[('ho_aTm', i, 0), ('ho_aTm', i, 1), ('ho_gi', i), ('ho_q0', i), ('ho_q1', i),
                                        ('ho_S', 0, i), ('ho_S', 1, i)], w=[('ho_ps_o', i)])
                    Y = self.norm_gate(nd, ps_o[:, i].rearrange("p c v -> p (c v)"), [('ho_ps_o', i)],
                                       go[i][:].rearrange("p c v -> p (c v)"), ('ho_go', i),
                                       NG[:].rearrange("p c v -> p (c v)"), ngt[0], 'rms', i, H, 8)
                    def tr(E, i=i, Y=Y):
                        ins = None
                        for c in range(2):
                            for cc in range(4):
                                ins = E.transpose(ps_t[:, i, cc * P + c * 64: cc * P + (c + 1) * 64],
                                                  Y[:, c * 512 + cc * P: c * 512 + (cc + 1) * P], self.ident_b[0:H, 0:H])
                        return ins
                    pg.op('pe', tr, r=[('ho_n_y', i), 'ident_b'], w=[('ho_ps_t', i)])
                    pg.op('act', lambda E, i=i, tsl=tsl: E.copy(yst[:, :, tsl], ps_t[:, i, :].rearrange("p (c t) -> p c t", c=4)),
                          r=[('ho_ps_t', i)], w=[('ho_yst', tt)])
                self.store_yT(yst, 'ho_yst', 1024)


    def outproj_phase(self, l):
        nc, pg = self.nc, self.pg
        with ExitStack() as st:
            W = self.sb(st, 'op_w', [P, KC, D], BF16)
            wv = self.I('w_out')[l].rearrange("(k p) n -> p k n", p=P)
            for j in range(4):
                pg.dma('pool', W[:, :, j * 512:(j + 1) * 512], wv[:, :, j * 512:(j + 1) * 512], w=[('op_w', j)])
            wt = [('op_w', j) for j in range(4)]
            yT = [self.sb(st, f'op_y{i}', [P, KC, P], BF16) for i in range(2)]
            so = [self.sb(st, f'op_o{i}', [P, D], F32) for i in range(2)]
            ps = self.psum(st, 'op_ps', [P, 2, 4, 512], F32)
            yv = self.dr['yT_dram'].ap().rearrange("(k p) t -> p k t", p=P)
            rv = self.dr['res_dram'].ap().rearrange("(n p) d -> n p d", p=P)
            pg.dma('sp', yT[0][:], yv[:, :, 0:P], r=[('yT_dram', c) for c in range(KC)], w=[('op_y', 0)])
            for t in range(NT):
                i = t % 2
                if t + 1 < NT:
                    pg.dma('sp', yT[(t + 1) % 2][:], yv[:, :, (t + 1) * P:(t + 2) * P], r=[('yT_dram', c) for c in range(KC)], w=[('op_y', (t + 1) % 2)])
                for j in range(4):
                    def mm(E, i=i, j=j):
                        ins = None
                        for k in range(KC):
                            ins = E.matmul(ps[:, i, j, :], yT[i][:, k, :], W[:, k, j * 512:(j + 1) * 512],
                                           start=(k == 0), stop=(k == KC - 1))
                        return ins
                    pg.op('pe', mm, r=[('op_y', i)] + wt, w=[('op_ps', i, j)])
                    o_ap = so[i][:, j * 512:(j + 1) * 512]
                    if j % 2 == 0:
                        pg.op('act', lambda E, o=o_ap, a=ps[:, i, j, :]: E.copy(o, a), r=[('op_ps', i, j)], w=[('op_o', i, j)])
                    else:
                        pg.op('dve', lambda E, o=o_ap, a=ps[:, i, j, :]: E.tensor_copy(o, a), r=[('op_ps', i, j)], w=[('op_o', i, j)])
                pg.dma('sp', rv[t], so[i][:], r=[('op_o', i, j) for j in range(4)], w=[('res_dram', t)])

    def moe_phase(self, l):
        nc, pg = self.nc, self.pg
        TBS = 1024
        NTB = S // TBS
        TPB = TBS // P
        with ExitStack() as st:
            hTb = self.sb(st, 'mo_h', [P, KC, TBS], BF16)
            acc = self.sb(st, 'mo_acc', [P, TPB, D], F32)
            hid = self.sb(st, 'mo_hid', [P, 8, TBS], BF16)
            wg = [self.sb(st, f'mo_wg{i}', [P, KC, 256], BF16) for i in range(2)]
            wu = [self.sb(st, f'mo_wu{i}', [P, KC, 256], BF16) for i in range(2)]
            wd = [self.sb(st, f'mo_wd{i}', [P, 8, 512], BF16) for i in range(2)]
            sg = [self.sb(st, f'mo_sg{i}', [P, 512], F32) for i in range(2)]
            ps_gu = self.psum(st, 'mo_ps_gu', [P, 2, 2, 512], F32)
            ps_d = self.psum(st, 'mo_ps_d', [P, 3, 512], F32)
            hTv = self.dr['hT_dram'].ap().rearrange("(k p) t -> p k t", p=P)
            rv = self.dr['res_dram'].ap().rearrange("(n j p) d -> n p j d", p=P, j=TPB)
            iw = 0; idw = 0; igu = 0; ipd = 0
            for tb in range(NTB):
                for k in range(KC):
                    pg.dma('sp', hTb[:, k, :], hTv[:, k, tb * TBS:(tb + 1) * TBS],
                           r=[('hT_dram', tb * 2), ('hT_dram', tb * 2 + 1)], w=[('mo_h', k)])
                ht = [('mo_h', k) for k in range(KC)]
                for e in range(NE):
                    wgv = self.I('w_gate')[l, e].rearrange("(k p) f -> p k f", p=P)
                    wuv = self.I('w_up')[l, e].rearrange("(k p) f -> p k f", p=P)
                    wdv = self.I('w_down')[l, e].rearrange("(c p) n -> p c n", p=P)
                    for hf in range(4):
                        wi = iw % 2; iw += 1
                        pg.dma('pool', wg[wi][:], wgv[:, :, hf * 256:(hf + 1) * 256], w=[('mo_wg', wi)])
                        pg.dma('pool', wu[wi][:], wuv[:, :, hf * 256:(hf + 1) * 256], w=[('mo_wu', wi)])
                        for f2 in range(2):
                            fc = hf * 2 + f2
                            for th in range(TBS // 512):
                                b = igu % 2; igu += 1
                                def mm(E, wi=wi, f2=f2, th=th, b=b):
                                    ins = None
                                    for k in range(KC):
                                        E.matmul(ps_gu[:, b, 0, :], wg[wi][:, k, f2 * P:(f2 + 1) * P], hTb[:, k, th * 512:(th + 1) * 512],
                                                 start=(k == 0), stop=(k == KC - 1))
                                    for k in range(KC):
                                        ins = E.matmul(ps_gu[:, b, 1, :], wu[wi][:, k, f2 * P:(f2 + 1) * P], hTb[:, k, th * 512:(th + 1) * 512],
                                                       start=(k == 0), stop=(k == KC - 1))
                                    return ins
                                pg.op('pe', mm, r=[('mo_wg', wi), ('mo_wu', wi)] + ht, w=[('mo_ps_gu', b)])
                                pg.op('act', lambda E, b=b: E.activation(sg[b][:], ps_gu[:, b, 0, :], AF.Silu),
                                      r=[('mo_ps_gu', b)], w=[('mo_sg', b)])
                                pg.op('dve', lambda E, b=b, fc=fc, th=th: E.tensor_tensor(hid[:, fc, th * 512:(th + 1) * 512], sg[b][:], ps_gu[:, b, 1, :], ALU.mult),
                                      r=[('mo_sg', b), ('mo_ps_gu', b)], w=[('mo_hid', fc, th)])
                    hidt = [('mo_hid', fc, th) for fc in range(8) for th in range(TBS // 512)]
                    for nch in range(4):
                        di = idw % 2; idw += 1
                        pg.dma('pool', wd[di][:], wdv[:, :, nch * 512:(nch + 1) * 512], w=[('mo_wd', di)])
                        for tl in range(TPB):
                            pb = ipd % 3; ipd += 1
                            tg = tb * TPB + tl
                            def mmd(E, di=di, tl=tl, pb=pb):
                                ins = None
                                for fc in range(8):
                                    ins = E.matmul(ps_d[:, pb, :], hid[:, fc, tl * P:(tl + 1) * P], wd[di][:, fc, :],
                                                   start=(fc == 0), stop=(fc == 7))
                                return ins
                            pg.op('pe', mmd, r=hidt + [('mo_wd', di)], w=[('mo_ps_d', pb)])
                            a_ap = acc[:, tl, nch * 512:(nch + 1) * 512]
                            if e == 0:
                                pg.op('dve', lambda E, a=a_ap, pb=pb, tg=tg, e=e: E.tensor_scalar(a, ps_d[:, pb, :], self.comb[:, tg, e:e + 1], None, ALU.mult),
                                      r=[('mo_ps_d', pb), ('comb', tg)], w=[('mo_acc', tl, nch)])
                            else:
                                pg.op('dve', lambda E, a=a_ap, pb=pb, tg=tg, e=e: E.scalar_tensor_tensor(a, ps_d[:, pb, :], self.comb[:, tg, e:e + 1], a, ALU.mult, ALU.add),
                                      r=[('mo_ps_d', pb), ('comb', tg)], w=[('mo_acc', tl, nch)])
                pg.dma('sp', rv[tb], acc[:], r=[('mo_acc', tl, nch) for tl in range(TPB) for nch in range(4)],
                       w=[('res_dram', tb * TPB + tl) for tl in range(TPB)])


    def route_finalize(self):
        pg = self.pg
        with ExitStack() as st:
            ones = self.sb(st, 'rf_ones', [P, P], BF16)
            ust = self.sb(st, 'rf_ust', [P, P], BF16)
            mE = self.sb(st, 'rf_mE', [P, 512], BF16)
            mS = self.sb(st, 'rf_mS', [P, 512], BF16)
            ecap = self.sb(st, 'rf_ecap', [P, 512], F32)
            thr = self.sb(st, 'rf_thr', [P, 512], F32)
            TOT = self.sb(st, 'rf_TOT', [P, NE, NT], F32)
            INC = self.sb(st, 'rf_INC', [P, NE, NT], F32)
            EXC = self.sb(st, 'rf_EXC', [P, NE, NT], F32)
            G = self.sb(st, 'rf_G', [P, NT, NE], F32)
            CE = self.sb(st, 'rf_CE', [P, NT, NE], F32)
            F1 = self.sb(st, 'rf_F1', [P, NT, NE], F32)
            F2 = self.sb(st, 'rf_F2', [P, NT, NE], F32)
            TMP = self.sb(st, 'rf_TMP', [P, NT, NE], F32)
            Df = self.sb(st, 'rf_Df', [P, 2, NT], F32)
            GT = self.sb(st, 'rf_GT', [P, NE, NT], F32)
            ntf = self.sb(st, 'rf_ntf', [P, NE], F32)
            ps = self.psum(st, 'rf_ps', [P, 2, 512], F32)
            for nm, t_ in (('ones_b', ones), ('ustrict_b', ust), ('maskE', mE), ('maskS', mS), ('ecap', ecap), ('thr', thr)):
                pg.dma('sp', t_[:], self.I('c_' + nm), w=['rf_' + nm])
            selt = [('selm', t) for t in range(NT)]
            sflat = self.selm[:].rearrange("p j e -> p (j e)")
            fl = lambda t_: t_[:].rearrange("p a b -> p (a b)")
            pg.op('pe', lambda E: E.matmul(ps[:, 0, :], ones[:], sflat, start=True, stop=True), r=selt + ['rf_ones_b'], w=['rf_ps0'])
            pg.op('pe', lambda E: E.matmul(ps[:, 1, :], ust[:], sflat, start=True, stop=True), r=selt + ['rf_ustrict_b'], w=['rf_ps1'])
            pg.op('dve', lambda E: E.tensor_copy(TOT[:], ps[:, 0, :].rearrange("p (j e) -> p e j", e=NE)), r=['rf_ps0'], w=['rf_TOT'])
            pg.op('dve', lambda E: E.tensor_tensor_scan(fl(INC), mE[:], fl(TOT), 0.0, ALU.mult, ALU.add), r=['rf_TOT', 'rf_maskE'], w=['rf_INC'])
            pg.op('dve', lambda E: E.tensor_tensor(EXC[:], INC[:], TOT[:], ALU.subtract), r=['rf_INC', 'rf_TOT'], w=['rf_EXC'])
            pg.op('dve', lambda E: E.tensor_tensor(G[:], ps[:, 1, :].rearrange("p (j e) -> p j e", e=NE),
                                                   EXC[:].rearrange("p e j -> p j e"), ALU.add), r=['rf_ps1', 'rf_EXC'], w=['rf_G'])
            pg.op('dve', lambda E: E.tensor_tensor(fl(G), fl(G), ecap[:], ALU.add), r=['rf_G', 'rf_ecap'], w=['rf_G'])
            pg.op('dve', lambda E: E.tensor_tensor_scan(fl(CE), mS[:], sflat, 0.0, ALU.mult, ALU.add), r=selt + ['rf_maskS'], w=['rf_CE'])
            for (F, val, k) in ((F1, 1.0, 0), (F2, 2.0, 1)):
                nm = f'rf_F{k}'
                pg.op('dve', lambda E, F=F, val=val: E.tensor_scalar(fl(F), fl(CE), val, None, ALU.is_equal), r=['rf_CE'], w=[nm])
                pg.op('dve', lambda E, F=F: E.tensor_tensor(fl(F), fl(F), sflat, ALU.mult), r=[nm] + selt, w=[nm])
                pg.op('dve', lambda E, F=F: E.tensor_tensor(TMP[:], F[:], G[:], ALU.mult), r=[nm, 'rf_G'], w=['rf_TMP'])
                pg.op('dve', lambda E, k=k: E.tensor_reduce(Df[:, k, :], TMP[:], AX.X, ALU.add), r=['rf_TMP'], w=[('rf_Df', k)])
                Di = self.D0i if k == 0 else self.D1i
                pg.op('dve', lambda E, k=k, Di=Di: E.tensor_copy(Di[:], Df[:, k, :]), r=[('rf_Df', k)], w=[('Di', k)])
                Wk = self.W0 if k == 0 else self.W1
                pg.op('dve', lambda E, F=F: E.tensor_tensor(TMP[:], F[:], self.comb[:], ALU.mult), r=[nm, 'rf_TMP'] + self.comb_toks, w=['rf_TMP'])
                pg.op('dve', lambda E, Wk=Wk: E.tensor_reduce(Wk[:], TMP[:], AX.X, ALU.add), r=['rf_TMP'], w=[('Wk', k)])
            pg.op('dve', lambda E: E.tensor_tensor(GT[:], INC[:, :, NT - 1:NT].to_broadcast([P, NE, NT]), thr[:].rearrange("p (e j) -> p e j", e=NE), ALU.is_gt),
                  r=['rf_INC', 'rf_thr'], w=['rf_GT'])
            pg.op('dve', lambda E: E.tensor_reduce(ntf[:], GT[:], AX.X, ALU.add), r=['rf_GT'], w=['rf_ntf'])
            pg.op('dve', lambda E: E.tensor_copy(self.nti[:], ntf[:]), r=['rf_ntf'], w=['nti'])

    def scatter_phase(self):
        pg = self.pg
        with ExitStack() as st:
            X = [self.sb(st, f'sc_x{i}', [P, D], BF16) for i in range(3)]
            hv = self.dr['hb_dram'].ap().rearrange("(n p) d -> n p d", p=P)
            bk = self.dr['bucket'].ap()
            for j in range(NT):
                i = j % 3
                pg.dma('sp', X[i][:], hv[j], r=[('hb_dram', j)], w=[('sc_x', i)])
                for Di in (self.D0i, self.D1i):
                    pg.dma('pool', None, None, r=[('sc_x', i), ('Di', 0), ('Di', 1)], w=[],
                           fn=lambda E, i=i, j=j, Di=Di: E.indirect_dma_start(
                               out=bk, out_offset=bass.IndirectOffsetOnAxis(Di[:, j:j + 1], 0), in_=X[i][:], in_offset=None))

    def expert_phase(self, l):
        pg = self.pg
        ENG = ['pe', 'act', 'dve', 'sp']
        with ExitStack() as st:
            Wg2 = [self.sb(st, f'ex_wg{i}', [P, KC, DE], BF16) for i in range(2)]
            Wu2 = [self.sb(st, f'ex_wu{i}', [P, KC, DE], BF16) for i in range(2)]
            Wd = self.sb(st, 'ex_wd', [P, 8, D], BF16)
            Xq = [self.sb(st, f'ex_x{i}', [P, D], BF16) for i in range(2)]
            xT = [self.sb(st, f'ex_xT{i}', [P, KC, P], BF16) for i in range(2)]
            sg = self.sb(st, 'ex_sg', [P, DE], F32)
            hid = [self.sb(st, f'ex_hid{i}', [P, DE], BF16) for i in range(2)]
            hidT = [self.sb(st, f'ex_hidT{i}', [P, 8, P], BF16) for i in range(2)]
            Y = [self.sb(st, f'ex_y{i}', [P, D], BF16) for i in range(2)]
            ps_xf = self.psum(st, 'ex_ps_x', [P, 2, 512], F32)
            ps_x = ps_xf[:].rearrange("p a b -> p (a b)").bitcast(BF16)
            ps_gu = self.psum(st, 'ex_ps_gu', [P, 2, 2, 512], F32)
            ps_h = self.psum(st, 'ex_ps_h', [P, 8 * P], BF16)
            ps_d1 = self.psum(st, 'ex_ps_d', [P, 512], F32)
            bk = self.dr['bucket'].ap()
            yb = self.dr['ybucket'].ap()
            it = 0
            for e in range(NE):
                wgv = self.I('w_gate')[l, e].rearrange("(k p) f -> p k f", p=P)
                wuv = self.I('w_up')[l, e].rearrange("(k p) f -> p k f", p=P)
                wdv = self.I('w_down')[l, e].rearrange("(c p) n -> p c n", p=P)
                wb = e % 2
                Wg, Wu = Wg2[wb], Wu2[wb]
                if e < self.wlim: pg.dma('pool', Wg[:], wgv, w=[('ex_wg', wb, 0), ('ex_wg', wb, 1)])
                if e < self.wlim: pg.dma('pool', Wu[:], wuv, w=[('ex_wu', wb, 0), ('ex_wu', wb, 1)])
                for n_ in range(2 if e < self.wlim else 0):
                    pg.dma('pool', Wd[:, :, n_ * 1024:(n_ + 1) * 1024], wdv[:, :, n_ * 1024:(n_ + 1) * 1024],
                           w=[('ex_wd', 2 * n_), ('ex_wd', 2 * n_ + 1)])
                for en in ENG:
                    pg.op(en, lambda E, en=en, e=e: E.reg_load(self.regs[en], self.nti[0:1, e:e + 1]), r=['nti'])
                nslots = self.qlim
                for q in range(nslots):
                    i = it % 2; it += 1
                    row0 = e * CAP + q * P
                    pg.cond_begin(self.regs, q, ENG)
                    pg.dma('sp', Xq[i][:], bk[row0:row0 + P, :], w=[('ex_x', i)])
                    def trx(E, i=i):
                        ins = None
                        for k in range(KC):
                            ins = E.transpose(ps_x[:, k * P:(k + 1) * P], Xq[i][:, k * P:(k + 1) * P], self.ident_b[:])
                        return ins
                    pg.op('pe', trx, r=[('ex_x', i), 'ident_b'], w=[('ex_psb', 0), ('ex_psb', 1)])
                    pg.op('act', lambda E, i=i: E.copy(xT[i][:, 0:8, :], ps_x[:, 0:8 * P].rearrange("p (k t) -> p k t", k=8)),
                          r=[('ex_psb', 0)], w=[('ex_xT', i, 0)])
                    pg.op('dve', lambda E, i=i: E.tensor_copy(xT[i][:, 8:16, :], ps_x[:, 8 * P:16 * P].rearrange("p (k t) -> p k t", k=8)),
                          r=[('ex_psb', 1)], w=[('ex_xT', i, 1)])
                    for fh in range(2):
                        for a, W, wn in ((0, Wg, 'ex_wg'), (1, Wu, 'ex_wu')):
                            def mgu(E, i=i, a=a, W=W, fh=fh):
                                ins = None
                                for k in range(KC):
                                    ins = E.matmul(ps_gu[:, a, fh, :], xT[i][:, k, :], W[:, k, fh * 512:(fh + 1) * 512],
                                                   start=(k == 0), stop=(k == KC - 1))
                                return ins
                            pg.op('pe', mgu, r=[('ex_xT', i, 0), ('ex_xT', i, 1), (wn, wb, fh)], w=[('ex_ps_gu', a, fh)])
                        pg.op('act', lambda E, fh=fh: E.activation(sg[:, fh * 512:(fh + 1) * 512], ps_gu[:, 0, fh, :], AF.Silu),
                              r=[('ex_ps_gu', 0, fh)], w=[('ex_sg', fh)])
                        pg.op('dve', lambda E, fh=fh, i=i: E.tensor_tensor(hid[i][:, fh * 512:(fh + 1) * 512], sg[:, fh * 512:(fh + 1) * 512],
                                                                            ps_gu[:, 1, fh, :], ALU.mult),
                              r=[('ex_sg', fh), ('ex_ps_gu', 1, fh)], w=[('ex_hid', i, fh)])
                    def trh(E, i=i):
                        ins = None
                        for fc in range(8):
                            ins = E.transpose(ps_h[:, fc * P:(fc + 1) * P], hid[i][:, fc * P:(fc + 1) * P], self.ident_b[:])
                        return ins
                    pg.op('pe', trh, r=[('ex_hid', i, 0), ('ex_hid', i, 1), 'ident_b'], w=['ex_ps_h'])
                    pg.op('act', lambda E, i=i: E.copy(hidT[i][:], ps_h[:].rearrange("p (c t) -> p c t", c=8)),
                          r=['ex_ps_h'], w=[('ex_hidT', i)])
                    for n_ in range(4):
                        if n_ % 3 == 2:
                            pd, ptok = ps_d1[:], 'ex_ps_d'
                        else:
                            pd, ptok = ps_xf[:, n_ % 3, :], ('ex_psb', n_ % 3)
                        def mmd(E, i=i, n_=n_, pd=pd):
                            ins = None
                            for fc in range(8):
                                ins = E.matmul(pd, hidT[i][:, fc, :], Wd[:, fc, n_ * 512:(n_ + 1) * 512],
                                               start=(fc == 0), stop=(fc == 7))
                            return ins
                        pg.op('pe', mmd, r=[('ex_hidT', i), ('ex_wd', n_)], w=[ptok])
                        o_ap = Y[i][:, n_ * 512:(n_ + 1) * 512]
                        if n_ % 2 == 0:
                            pg.op('act', lambda E, o=o_ap, pd=pd: E.copy(o, pd), r=[ptok], w=[('ex_y', i, n_)])
                        else:
                            pg.op('dve', lambda E, o=o_ap, pd=pd: E.tensor_copy(o, pd), r=[ptok], w=[('ex_y', i, n_)])
                    pg.dma('sp', yb[row0:row0 + P, :], Y[i][:], r=[('ex_y', i, n_) for n_ in range(4)], w=[])
                for q in range(nslots):
                    pg.cond_end()

    def gather_phase(self):
        pg = self.pg
        with ExitStack() as st:
            Y0 = [self.sb(st, f'ga_y0{i}', [P, D], BF16) for i in range(2)]
            Y1 = [self.sb(st, f'ga_y1{i}', [P, D], BF16) for i in range(2)]
            T = [self.sb(st, f'ga_t{i}', [P, D], F32) for i in range(2)]
            yb = self.dr['ybucket'].ap()
            rv = self.dr['res_dram'].ap().rearrange("(n p) d -> n p d", p=P)
            for j in range(NT):
                i = j % 2
                for (Yk, Di, nm) in ((Y0[i], self.D0i, 'ga_y0'), (Y1[i], self.D1i, 'ga_y1')):
                    pg.dma('pool', None, None, r=[('Di', 0), ('Di', 1)], w=[(nm, i)],
                           fn=lambda E, Yk=Yk, Di=Di, j=j: E.indirect_dma_start(
                               out=Yk[:], out_offset=None, in_=yb, in_offset=bass.IndirectOffsetOnAxis(Di[:, j:j + 1], 0)))
                pg.op('dve', lambda E, i=i, j=j: E.tensor_scalar(T[i][:], Y0[i][:], self.W0[:, j:j + 1], None, ALU.mult),
                      r=[('ga_y0', i), ('Wk', 0)], w=[('ga_t', i)])
                pg.op('dve', lambda E, i=i, j=j: E.scalar_tensor_tensor(T[i][:], Y1[i][:], self.W1[:, j:j + 1], T[i][:], ALU.mult, ALU.add),
                      r=[('ga_y1', i), ('Wk', 1), ('ga_t', i)], w=[('ga_t', i)])
                pg.dma('sp', rv[j], T[i][:], r=[('ga_t', i)], w=[('res_dram', j)])


_CACHE = {}


def make_in_map(inputs, core, b):
    m = {}
    for k in b.dr:
        if k.startswith('c_'):
            m[k] = b.consts[k[2:]]
        elif k in inputs:
            v = np.asarray(inputs[k])
            m[k] = np.ascontiguousarray(v[core]) if k == 'x' else np.ascontiguousarray(v)
    return m


def kernel(**inputs):
    b = Builder()
    nc = b.build()
    in_maps = [make_in_map(inputs, c, b) for c in range(8)]
    res = run_bass_kernel_spmd(nc, in_maps, core_ids=list(range(8)))
    return np.stack([np.asarray(r['out']) for r in res.results], axis=0).astype(np.float32)
```

```python
import numpy as np
import ml_dtypes
from contextlib import ExitStack
import concourse.bass as bass
import concourse.mybir as mybir
from concourse.bass_utils import run_bass_kernel_spmd

F32 = mybir.dt.float32
BF16 = mybir.dt.bfloat16
AF = mybir.ActivationFunctionType
ALU = mybir.AluOpType
AX = mybir.AxisListType

P = 128
S = 4096
D = 2048
NT = S // P
KC = D // P
DEPTH = 2
D_IN = 6912
NE = 16
DE = 1024
ALPHA = (2.0 * DEPTH) ** 0.25
LN_EPS = 1e-5
HN_EPS = 1e-6
NEG = -30000.0
CAP = 4096

C_AQ, C_AK, C_AV = 0, 512, 640
C_CB, C_CC, C_CH = 768, 1280, 1792
C_GQ, C_GZF, C_GZB, C_GI, C_GO = 2304, 2816, 3328, 3840, 4352
C_RQ, C_RK, C_RV, C_RG = 4864, 5376, 5888, 6400


class Prog:
    EPOCH = 30000

    def __init__(self, nc, es, n_dma_sems=12):
        self.nc = nc
        self.es = es
        self.E = {'pe': nc.tensor, 'act': nc.scalar, 'dve': nc.vector,
                  'pool': nc.gpsimd, 'sp': nc.sync}
        self.nsem = 0
        self.sem = {e: self._new_sem(e) for e in self.E}
        self.cnt = {e: 0 for e in self.E}
        self.known = {e: {} for e in self.E}
        self.semobj = {}
        self.tokw = {}
        self.tokr = {}
        self.dq = {}
        self._sem_owner = {}
        self._cstack = []
        for q in ('sp', 'pool', 'act'):
            self.dq[q] = {'sems': [self._new_sem('d' + q) for _ in range(n_dma_sems)],
                          'rr': 0}
            for s_ in self.dq[q]['sems']:
                self._sem_owner[id(s_)] = q
        self.dtarget = {}
        self.all_sems = {}
        self.n_ops = 0
        self.n_waits = 0

    def _new_sem(self, tag):
        self.nsem += 1
        s = self.es.enter_context(self.nc.semaphore(f"s_{tag}_{self.nsem}"))
        return s

    def _key(self, s):
        return id(s)

    def _collect(self, eng, reads, writes):
        need = {}
        def addh(h):
            k = self._key(h[0])
            if k not in need or need[k][1] < h[1]:
                need[k] = h
        for t in reads:
            for h in self.tokw.get(t, ()):
                addh(h)
        for t in writes:
            for h in self.tokw.get(t, ()):
                addh(h)
            for h in self.tokr.get(t, ()):
                addh(h)
        return need

    def _emit_waits(self, eng, need, skip_own_pe=True):
        kn = self.known[eng]
        acts = []
        for k, (s, v, src) in need.items():
            if eng == 'pe' and src == 'pe':
                continue
            if kn.get(k, 0) >= v:
                continue
            acts.append(('wait', s, v))
            kn[k] = v
            self.n_waits += 1
        return acts

    def _run(self, eng, acts):
        if self._cstack:
            assert eng in self._cstack[-1]['engines'], eng
            self._cstack[-1]['buf'][eng].extend(acts)
            return
        self._do(eng, acts)

    def _do(self, eng, acts):
        E = self.E[eng]
        for a in acts:
            if a[0] == 'wait':
                E.wait_ge(a[1], a[2])
            elif a[0] == 'ins':
                a[1](E).then_inc(a[2], a[3])
            elif a[0] == 'seminc':
                E.sem_inc(a[1], a[2])
            else:
                with E.If_cmp(a[1], a[2], "IS_GT"):
                    self._do(eng, a[3])
                with E.Else():
                    self._do(eng, a[4])

    def _update(self, h, reads, writes):
        k = self._key(h[0])
        for t in writes:
            self.tokw[t] = [h]
            self.tokr[t] = []
        for t in reads:
            if t in writes:
                continue
            lst = self.tokr.get(t)
            if lst is None:
                self.tokr[t] = [h]
            else:
                self.tokr[t] = [x for x in lst if self._key(x[0]) != k] + [h]

    def op(self, eng, fn, r=(), w=()):
        need = self._collect(eng, r, w)
        acts = self._emit_waits(eng, need)
        if self.cnt[eng] >= self.EPOCH:
            assert not self._cstack
            self.sem[eng] = self._new_sem(eng)
            self.cnt[eng] = 0
        self.cnt[eng] += 1
        acts.append(('ins', fn, self.sem[eng], 1))
        self._run(eng, acts)
        h = (self.sem[eng], self.cnt[eng], eng)
        self._update(h, r, w)
        self.n_ops += 1
        return h

    def dma(self, q, out, in_, r=(), w=(), fn=None, **kw):
        dq = self.dq[q]
        s = dq['sems'][dq['rr'] % len(dq['sems'])]
        dq['rr'] += 1
        prev = self.dtarget.get(self._key(s), 0)
        need = self._collect(q, r, w)
        if prev > 0:
            k = self._key(s)
            if k not in need or need[k][1] < prev:
                need[k] = (s, prev, 'dma')
        acts = self._emit_waits(q, need)
        tgt = prev + 16
        self.dtarget[self._key(s)] = tgt
        self.all_sems[self._key(s)] = s
        if fn is None:
            fn = lambda E, out=out, in_=in_, kw=kw: E.dma_start(out=out, in_=in_, **kw)
        acts.append(('ins', fn, s, 16))
        self._run(q, acts)
        h = (s, tgt, 'dma')
        self._update(h, r, w)
        self.n_ops += 1
        return h

    def cond_begin(self, regs, thresh, engines):
        for e in engines:
            assert self.cnt[e] < self.EPOCH - 4000 or self._cstack
        self._cstack.append({'engines': engines, 'regs': regs, 'thresh': thresh,
                             'cnt0': {e: (self.sem[e], self.cnt[e]) for e in engines},
                             'dt0': dict(self.dtarget),
                             'known0': {e: dict(self.known[e]) for e in self.E},
                             'buf': {e: [] for e in engines}})

    def cond_end(self, dma_issuers=('sp',)):
        c = self._cstack.pop()
        for e in c['engines']:
            s0, c0 = c['cnt0'][e]
            assert s0 is self.sem[e]
            n = self.cnt[e] - c0
            els = []
            if n > 0:
                if c0 > 0:
                    els.append(('wait', s0, c0))
                els.append(('seminc', s0, n))
            if e in dma_issuers:
                for k, tgt in self.dtarget.items():
                    t0 = c['dt0'].get(k, 0)
                    if tgt > t0 and self._sem_owner.get(k) == e:
                        if t0 > 0:
                            els.append(('wait', self.all_sems[k], t0))
                        els.append(('seminc', self.all_sems[k], tgt - t0))
            if not c['buf'][e] and not els:
                continue
            act = ('cond', c['regs'][e], c['thresh'], c['buf'][e], els)
            if self._cstack:
                self._cstack[-1]['buf'][e].append(act)
            else:
                self._do(e, [act])
        for e in self.E:
            self.known[e] = c['known0'][e]

    def barrier(self, engines=None, keep=()):
        engines = engines or list(self.E)
        need = {}
        for e in self.E:
            if self.cnt[e] > 0:
                need[self._key(self.sem[e])] = (self.sem[e], self.cnt[e], e)
        for k, s in self.all_sems.items():
            need[k] = (s, self.dtarget[k], 'dma')
        for e in engines:
            E = self.E[e]
            kn = self.known[e]
            for k, (s, v, src) in need.items():
                if src == e and e != 'sp':
                    pass
                if kn.get(k, 0) >= v:
                    continue
                E.wait_ge(s, v)
                kn[k] = v
        self.tokw.clear()
        self.tokr.clear()


def host_consts():
    c = {}
    c['ident_f'] = np.eye(P, dtype=np.float32)
    c['ident_b'] = np.eye(P, dtype=np.float32).astype(ml_dtypes.bfloat16)
    ab = np.zeros((P, 8, 3, P), np.float32)
    s_i = np.arange(P)[:, None]
    t_i = np.arange(P)[None, :]
    for h in range(8):
        slope = 2.0 ** (-(h + 1))
        for j in range(3):
            dist = (j - 1) * P + s_i - t_i
            ab[:, h, j, :] = np.where(np.abs(dist) <= 128, -slope * np.abs(dist), NEG)
    c['attn_bias'] = ab.reshape(P, 8 * 3 * P)
    rc = np.zeros((P, 5, P), np.float32)
    rc[:, 0] = np.maximum(t_i - s_i, 0)
    rc[:, 1] = np.maximum(s_i - t_i, 0)
    rc[:, 2] = (t_i > s_i)
    rc[:, 3] = (s_i > t_i)
    rc[:, 4] = 2.0 * np.eye(P)
    c['ret_c'] = rc.reshape(P, 5 * P)
    rv = np.zeros((P, 2, P), np.float32)
    rv[:, 0] = t_i + 1.0
    rv[:, 1] = 128.0 - t_i
    c['ret_vec'] = rv.reshape(P, 2 * P)
    cmk = np.ones((P, S), np.float32)
    cmk[:, ::64] = 0.0
    c['hg_cm'] = cmk.astype(ml_dtypes.bfloat16)
    s6 = np.arange(64)[:, None]
    t6 = np.arange(64)[None, :]
    hm = np.zeros((64, 2, 64), np.float32)
    hm[:, 0] = (s6 <= t6)
    hm[:, 1] = (s6 >= t6)
    c['hg_mask'] = hm.reshape(64, 128)
    c['ones_b'] = np.ones((P, P), np.float32).astype(ml_dtypes.bfloat16)
    c['ustrict_b'] = (s_i < t_i).astype(np.float32).astype(ml_dtypes.bfloat16)
    mE = np.ones((P, NE, NT), np.float32); mE[:, :, 0] = 0.0
    c['maskE'] = mE.reshape(P, NE * NT).astype(ml_dtypes.bfloat16)
    mS = np.ones((P, NT, NE), np.float32); mS[:, :, 0] = 0.0
    c['maskS'] = mS.reshape(P, NT * NE).astype(ml_dtypes.bfloat16)
    ec = np.zeros((P, NT, NE), np.float32); ec[:, :, :] = (np.arange(NE) * CAP)[None, None, :]
    c['ecap'] = ec.reshape(P, NT * NE)
    th_ = np.zeros((P, NE, NT), np.float32); th_[:, :, :] = (np.arange(NT) * 128.0)[None, None, :]
    c['thr'] = th_.reshape(P, NE * NT)
    c['ret_pcol'] = np.stack([127.0 - np.arange(P), np.arange(P) * 1.0], 1).astype(np.float32)
    return c


class Builder:
    def __init__(self, n_layers=DEPTH, stop=None, taps=(), skip=()):
        self.skip = set(skip)
        self.dense_moe = 'dense' in self.skip
        self.wlim = 2 if 'wlim' in self.skip else 99
        self.qlim = 8 if 'qlim' in self.skip else NT
        self.n_layers = n_layers
        self.stop = stop
        self.taps = set(taps)
        self.nc = bass.Bass("TRN2", target_bir_lowering=False)
        self.es = ExitStack()
        self.pg = Prog(self.nc, self.es)
        self.dr = {}
        self.consts = host_consts()

    def din(self, name, shape, dtype=F32):
        t = self.nc.dram_tensor(name, list(shape), dtype, kind="ExternalInput")
        self.dr[name] = t
        return t

    def dscr(self, name, shape, dtype):
        kind = "ExternalOutput" if name in self.taps else "Internal"
        t = self.nc.dram_tensor(name, list(shape), dtype, kind=kind)
        self.dr[name] = t
        return t

    def sb(self, st, name, shape, dtype):
        self._uid = getattr(self, '_uid', 0) + 1
        return st.enter_context(self.nc.sbuf_tensor(f"{name}_u{self._uid}", list(shape), dtype))

    def psum(self, st, name, shape, dtype=F32):
        self._uid = getattr(self, '_uid', 0) + 1
        return st.enter_context(self.nc.psum_tensor(f"{name}_u{self._uid}", list(shape), dtype))

    IN_SHAPES = {
        'x': [S, D], 'emb_ln_g': [D], 'emb_ln_b': [D], 'w_in': [DEPTH, D, D_IN],
        'attn_sink': [DEPTH, 8], 'conv_w': [DEPTH, 3, 512], 'hgrn_lb': [DEPTH, 512],
        'hgrn_norm_g': [DEPTH, 512], 'ret_decay_logit': [DEPTH, 2, 4], 'ret_norm_g': [DEPTH, 512],
        'w_out': [DEPTH, D, D], 'ln1_g': [DEPTH, D], 'ln1_b': [DEPTH, D],
        'router_w': [D, NE], 'router_b': [NE], 'w_gate': [DEPTH, NE, D, DE],
        'w_up': [DEPTH, NE, D, DE], 'w_down': [DEPTH, NE, DE, D],
        'ln2_g': [DEPTH, D], 'ln2_b': [DEPTH, D],
    }

    def I(self, name):
        if name not in self.dr:
            if name.startswith('c_'):
                v = self.consts[name[2:]]
                self.din(name, v.shape, BF16 if v.dtype == ml_dtypes.bfloat16 else F32)
            else:
                self.din(name, self.IN_SHAPES[name])
        return self.dr[name].ap()

    def declare(self):
        nc = self.nc
        self.out = nc.dram_tensor('out', [S, D], F32, kind="ExternalOutput")
        self.dscr('h_dram', [S, D], F32)
        self.dscr('hT_dram', [D, S], BF16)
        self.declare_proj()
        self.dscr('yT_dram', [D, S], BF16)
        self.dscr('res_dram', [S, D], F32)
        self.dscr('hb_dram', [S, D], BF16)
        self.dscr('bucket', [NE * CAP, D], BF16)
        self.dscr('ybucket', [NE * CAP, D], BF16)
        for d_ in range(2):
            self.dscr(f'hg_q{d_}', [512, S], BF16)
            self.dscr(f'hg_k{d_}', [512, S], BF16)
            self.dscr(f'hg_S{d_}', [S // 64, P, 512], BF16)

    def build(self):
        self.declare()
        nc, pg = self.nc, self.pg
        with ExitStack() as g:
            self.g = g
            self.ident_f = self.sb(g, 'ident_f', [P, P], F32)
            self.ident_b = self.sb(g, 'ident_b', [P, P], BF16)
            self.comb = self.sb(g, 'comb', [P, NT, NE], F32)
            self.comb_toks = [('comb', t) for t in range(NT)]
            I32 = mybir.dt.int32
            self.selm = self.sb(g, 'selm', [P, NT, NE], BF16)
            self.D0i = self.sb(g, 'D0i', [P, NT], I32)
            self.D1i = self.sb(g, 'D1i', [P, NT], I32)
            self.W0 = self.sb(g, 'W0', [P, NT], F32)
            self.W1 = self.sb(g, 'W1', [P, NT], F32)
            self.nti = self.sb(g, 'nti', [P, NE], I32)
            self.regs = {e: pg.E[e].alloc_register('r_nt_' + e) for e in ('pe', 'act', 'dve', 'sp')}
            pg.dma('sp', self.ident_f[:], self.I('c_ident_f'), w=['ident_f'])
            pg.dma('sp', self.ident_b[:], self.I('c_ident_b'), w=['ident_b'])
            self.ln_phase(src='x', g_ap=self.I('emb_ln_g'), b_ap=self.I('emb_ln_b'),
                          mode='x')
            pg.barrier()
            for l in range(self.n_layers):
                if self.stop == 'ln0':
                    break
                self.in_proj_phase(l)
                pg.barrier()
                if self.stop == 'inproj':
                    break
                if 'attn' not in self.skip:
                    self.attn_phase(l)
                    pg.barrier()
                if self.stop == 'attn':
                    break
                if 'conv' not in self.skip:
                    self.conv_phase(l)
                    pg.barrier()
                if 'ret' not in self.skip:
                    self.ret_phase(l)
                    pg.barrier()
                if self.stop in ('conv', 'ret'):
                    break
                if 'hgrn' not in self.skip:
                    self.hgrn_phase(l)
                    pg.barrier()
                if self.stop in ('hgrn', 'mix'):
                    break
                self.outproj_phase(l)
                pg.barrier()
                if self.stop == 'outproj':
                    break
                self.ln_phase(None, self.I('ln1_g')[l], self.I('ln1_b')[l], 'res', router=('norouter' not in self.skip))
                pg.barrier(keep=self.comb_toks)
                if self.stop == 'ln1':
                    break
                if self.dense_moe:
                    self.moe_phase(l)
                    pg.barrier()
                else:
                    self.route_finalize()
                    pg.barrier()
                    if self.stop == 'route':
                        break
                    self.scatter_phase()
                    pg.barrier()
                    if self.stop == 'scatter':
                        break
                    self.expert_phase(l)
                    pg.barrier()
                    if self.stop == 'expert':
                        break
                    self.gather_phase()
                    pg.barrier()
                    if self.stop == 'gather':
                        break
                last = (l == self.n_layers - 1)
                self.ln_phase(None, self.I('ln2_g')[l], self.I('ln2_b')[l], 'res', final=last)
                pg.barrier()
        self.es.close()
        return nc

    def ln_phase(self, src, g_ap, b_ap, mode, final=False, router=False):
        nc, pg = self.nc, self.pg
        with ExitStack() as st:
            gt = self.sb(st, 'ln_g', [P, D], F32)
            bt = self.sb(st, 'ln_b', [P, D], F32)
            pg.dma('sp', gt[:], g_ap.partition_broadcast(P), w=['ln_g'])
            pg.dma('sp', bt[:], b_ap.partition_broadcast(P), w=['ln_b'])
            NB = 3
            xt = [self.sb(st, f'ln_x{i}', [P, D], F32) for i in range(NB)]
            x2 = [self.sb(st, f'ln_r{i}', [P, D], F32) for i in range(NB)] if mode == 'res' else None
            hn = [self.sb(st, f'ln_h{i}', [P, D], F32) for i in range(NB)]
            stt = [self.sb(st, f'ln_st{i}', [P, 4, 6], F32) for i in range(NB)]
            mv = [self.sb(st, f'ln_mv{i}', [P, 4], F32) for i in range(NB)]
            hst = [self.sb(st, f'ln_hst{i}', [P, KC, 512], BF16) for i in range(2)]
            ps = self.psum(st, 'ln_ps', [P, 4 * 512], F32)
            if router:
                hTf = self.sb(st, 'ln_loT', [P, KC, P], BF16)
                Hb = self.sb(st, 'ln_Hb', [P, D], BF16)
                Lo = self.sb(st, 'ln_Lo', [P, D], F32)
                rw = self.sb(st, 'ln_rw', [P, KC, NE], F32)
                rwh = self.sb(st, 'ln_rwh', [P, KC, NE], BF16)
                rwl = self.sb(st, 'ln_rwl', [P, KC, NE], BF16)
                rb = self.sb(st, 'ln_rb', [P, NE], F32)
                rs = self.sb(st, 'ln_rs', [P, 8, NE], F32)
                ps_r = self.psum(st, 'ln_ps_r', [P, 512], F32)
                pg.dma('sp', rw[:], self.I('router_w').rearrange("(k p) e -> p k e", p=P), w=['ln_rw'])
                pg.dma('sp', rb[:], self.I('router_b').partition_broadcast(P), w=['ln_rb'])
                pg.op('act', lambda E: E.copy(rwh[:], rw[:]), r=['ln_rw'], w=['ln_rwh'])
                pg.op('dve', lambda E: E.tensor_tensor(rwl[:], rw[:], rwh[:], ALU.subtract), r=['ln_rw', 'ln_rwh'], w=['ln_rwl'])
            if mode == 'x':
                srcv = self.I(src).rearrange("(n p) d -> n p d", p=P)
            else:
                resv = self.dr['res_dram'].ap().rearrange("(n p) d -> n p d", p=P)
            hdv = self.dr['h_dram'].ap().rearrange("(n p) d -> n p d", p=P)
            outv = self.out.ap().rearrange("(n p) d -> n p d", p=P)
            hTv = self.dr['hT_dram'].ap().rearrange("(k p) t -> p k t", p=P)
            def issue_loads(t):
                i = t % NB
                if mode == 'x':
                    pg.dma('sp', xt[i][:], srcv[t], w=[f'ln_x{i}'])
                else:
                    pg.dma('sp', xt[i][:], hdv[t], r=[('h_dram', t)], w=[f'ln_x{i}'])
                    pg.dma('sp', x2[i][:], resv[t], r=[('res_dram', t)], w=[f'ln_r{i}'])
            issue_loads(0)
            for t in range(NT):
                i = t % NB
                X, H, ST, MV = xt[i], hn[i], stt[i], mv[i]
                tx, th = f'ln_x{i}', f'ln_h{i}'
                if t + 1 < NT:
                    issue_loads(t + 1)
                if mode != 'x':
                    pg.op('dve', lambda E, X=X, R=x2[i]: E.scalar_tensor_tensor(X[:], X[:], ALPHA, R[:], ALU.mult, ALU.add),
                          r=[tx, f'ln_r{i}'], w=[tx])
                self.ln_tile(X, H, ST, MV, gt, bt, tx, th, f'ln_s{i}')
                if final:
                    pg.dma('sp', outv[t], H[:], r=[th], w=[('out', t)])
                    continue
                pg.dma('sp', hdv[t], H[:], r=[th], w=[('h_dram', t)])
                slot = t % 4
                hb = (t // 4) % 2
                HS = hst[hb]
                for half in range(4):
                    def tr(E, half=half, H=H):
                        ins = None
                        for j in range(4):
                            k = half * 4 + j
                            ins = E.transpose(ps[:, half * 512 + j * P: half * 512 + (j + 1) * P],
                                              H[:, k * P:(k + 1) * P], self.ident_f[:])
                        return ins
                    pg.op('pe', tr, r=[th, 'ident_f'], w=[('ln_ps', half)])
                    o_ap = HS[:, half * 4:(half + 1) * 4, slot * P:(slot + 1) * P]
                    i_ap = ps[:, half * 512:(half + 1) * 512].rearrange("p (a b) -> p a b", a=4)
                    if half % 2 == 0:
                        pg.op('act', lambda E, o=o_ap, a=i_ap: E.copy(o, a),
                              r=[('ln_ps', half)], w=[('ln_hst', hb, half, slot)])
                    else:
                        pg.op('dve', lambda E, o=o_ap, a=i_ap: E.tensor_copy(o, a),
                              r=[('ln_ps', half)], w=[('ln_hst', hb, half, slot)])
                if slot == 3:
                    tb = t // 4
                    pg.dma('sp', hTv[:, :, tb * 512:(tb + 1) * 512], HS[:],
                           r=[('ln_hst', hb, hf, sl) for hf in range(4) for sl in range(4)],
                           w=[('hT_dram', tb)])
                if router:
                    pg.op('act', lambda E, H=H: E.copy(Hb[:], H[:]), r=[th], w=['ln_Hb'])
                    pg.dma('sp', self.dr['hb_dram'].ap().rearrange("(n p) d -> n p d", p=P)[t], Hb[:], r=['ln_Hb'], w=[('hb_dram', t)])
                    pg.op('dve', lambda E, H=H: E.tensor_tensor(Lo[:], H[:], Hb[:], ALU.subtract), r=[th, 'ln_Hb'], w=['ln_Lo'])
                    for half in range(4):
                        def tr2(E, half=half):
                            ins = None
                            for j in range(4):
                                k = half * 4 + j
                                ins = E.transpose(ps[:, half * 512 + j * P: half * 512 + (j + 1) * P],
                                                  Lo[:, k * P:(k + 1) * P], self.ident_f[:])
                            return ins
                        pg.op('pe', tr2, r=['ln_Lo', 'ident_f'], w=[('ln_ps', half)])
                        o2 = hTf[:, half * 4:(half + 1) * 4, :]
                        i_ap = ps[:, half * 512:(half + 1) * 512].rearrange("p (a b) -> p a b", a=4)
                        if half % 2 == 1:
                            pg.op('act', lambda E, o=o2, a=i_ap: E.copy(o, a), r=[('ln_ps', half)], w=[('ln_hTf', half)])
                        else:
                            pg.op('dve', lambda E, o=o2, a=i_ap: E.tensor_copy(o, a), r=[('ln_ps', half)], w=[('ln_hTf', half)])
                    hiT = HS[:, :, slot * P:(slot + 1) * P]
                    hit = [('ln_hst', hb, hf, slot) for hf in range(4)]
                    self.route_tile(t, hTf, hiT, hit, rwh, rwl, rb, rs, ps_r)

    def route_tile(self, t, loT, hiT, hit, rwh, rwl, rb, rs, ps_r):
        pg = self.pg
        def mm(E):
            ins = None
            for k in range(KC):
                E.matmul(ps_r[:, 0:NE], hiT[:, k, :], rwh[:, k, :], start=(k == 0), stop=False)
                E.matmul(ps_r[:, 0:NE], loT[:, k, :], rwh[:, k, :], start=False, stop=False)
                ins = E.matmul(ps_r[:, 0:NE], hiT[:, k, :], rwl[:, k, :], start=False, stop=(k == KC - 1))
            return ins
        pg.op('pe', mm, r=[('ln_hTf', hf) for hf in range(4)] + hit + ['ln_rwh', 'ln_rwl'], w=['ln_ps_r'])
        lg, ex, eq, ex2, sel = [rs[:, i, :] for i in range(5)]
        sm = rs[:, 5, :]
        gmk = rs[:, 6, 0:4]
        g3 = lambda ap: ap.rearrange("p (g e) -> p g e", e=4)
        bc = lambda ap: ap.unsqueeze(2).to_broadcast([P, 4, 4])
        T = 'rt'
        pg.op('dve', lambda E: E.tensor_tensor(lg, ps_r[:, 0:NE], rb[:], ALU.add), r=['ln_ps_r', 'ln_rb'], w=[T])
        pg.op('dve', lambda E: E.tensor_reduce(sm[:, 12:13], lg, AX.X, ALU.max), r=[T], w=[T])
        pg.op('dve', lambda E: E.tensor_scalar(sm[:, 12:13], sm[:, 12:13], -1.0, None, ALU.mult), r=[T], w=[T])
        pg.op('act', lambda E: E.activation(ex, lg, AF.Exp, bias=sm[:, 12:13], scale=1.0), r=[T], w=[T])
        pg.op('dve', lambda E: E.tensor_reduce(sm[:, 0:4], g3(ex), AX.X, ALU.max), r=[T], w=[T])
        pg.op('dve', lambda E: E.tensor_tensor(g3(eq), g3(ex), bc(sm[:, 0:4]), ALU.is_equal), r=[T], w=[T])
        pg.op('dve', lambda E: E.scalar_tensor_tensor(ex2, eq, -4.0, ex, ALU.mult, ALU.add), r=[T], w=[T])
        pg.op('dve', lambda E: E.tensor_reduce(sm[:, 4:8], g3(ex2), AX.X, ALU.max), r=[T], w=[T])
        pg.op('dve', lambda E: E.tensor_tensor(sm[:, 8:12], sm[:, 0:4], sm[:, 4:8], ALU.add), r=[T], w=[T])
        pg.op('dve', lambda E: E.tensor_reduce(sm[:, 13:14], sm[:, 8:12], AX.X, ALU.max), r=[T], w=[T])
        pg.op('dve', lambda E: E.tensor_scalar(gmk, sm[:, 8:12], sm[:, 13:14], None, ALU.is_equal), r=[T], w=[T])
        pg.op('dve', lambda E: E.tensor_tensor(g3(sel), g3(ex), bc(sm[:, 4:8]), ALU.is_ge), r=[T], w=[T])
        pg.op('dve', lambda E: E.tensor_tensor(g3(sel), g3(sel), bc(gmk), ALU.mult), r=[T], w=[T])
        pg.op('dve', lambda E: E.tensor_copy(self.selm[:, t, :], sel), r=[T], w=[('selm', t)])
        pg.op('dve', lambda E: E.tensor_tensor(sel, sel, ex, ALU.mult), r=[T], w=[T])
        pg.op('dve', lambda E: E.reciprocal(sm[:, 14:15], sm[:, 13:14]), r=[T], w=[T])
        pg.op('dve', lambda E: E.tensor_scalar(self.comb[:, t, :], sel, sm[:, 14:15], None, ALU.mult), r=[T], w=[('comb', t)])

    def ln_tile(self, X, H, ST, MV, gt, bt, tx, th, ts):
        pg = self.pg
        for j in range(4):
            pg.op('dve', lambda E, j=j: E.bn_stats(ST[:, j, :], X[:, j * 512:(j + 1) * 512]),
                  r=[tx], w=[(ts, 'st', j)])
        pg.op('dve', lambda E: E.bn_aggr(MV[:, 0:2], ST[:].rearrange("p a b -> p (a b)")),
              r=[(ts, 'st', j) for j in range(4)], w=[(ts, 'mv')])
        pg.op('act', lambda E: E.activation(MV[:, 2:3], MV[:, 1:2], AF.Ln, bias=LN_EPS, scale=1.0),
              r=[(ts, 'mv')], w=[(ts, 'lnv')])
        pg.op('act', lambda E: E.activation(MV[:, 3:4], MV[:, 2:3], AF.Exp, scale=-0.5),
              r=[(ts, 'lnv')], w=[(ts, 'rstd')])
        pg.op('dve', lambda E: E.tensor_scalar(H[:], X[:], MV[:, 0:1], MV[:, 3:4],
                                               ALU.subtract, ALU.mult),
              r=[tx, (ts, 'mv'), (ts, 'rstd')], w=[th])
        pg.op('pool', lambda E: E.tensor_tensor(H[:], H[:], gt[:], ALU.mult),
              r=[th, 'ln_g'], w=[th])
        pg.op('pool', lambda E: E.tensor_tensor(H[:], H[:], bt[:], ALU.add),
              r=[th, 'ln_b'], w=[th])


    PF_SPECS = [('aq', C_AQ, 512), ('akd', None, 256), ('cb', C_CB, 512), ('cc', C_CC, 512),
                ('ch', C_CH, 512), ('gq', C_GQ, 512), ('gzf', C_GZF, 512), ('gzb', C_GZB, 512),
                ('rq', C_RQ, 512), ('rk', C_RK, 512)]
    PT_SPECS = [('av', C_AV, 128), ('gi', C_GI, 512), ('go', C_GO, 512), ('rkt', C_RK, 512),
                ('rv', C_RV, 512), ('rg', C_RG, 512)]

    def declare_proj(self):
        for n, _, w in self.PF_SPECS:
            self.dscr('pf_' + n, [w, S], BF16)
        for n, _, w in self.PT_SPECS:
            self.dscr('pt_' + n, [S, w], BF16)

    def load_hT(self, st):
        hT = self.sb(st, 'hT_bf', [P, KC, S], BF16)
        hTv = self.dr['hT_dram'].ap().rearrange("(k p) t -> p k t", p=P)
        for k in range(KC):
            self.pg.dma('sp', hT[:, k, :], hTv[:, k, :], r=[('hT_dram', tb) for tb in range(8)],
                        w=[('hT_bf', k)])
        return hT

    def in_proj_phase(self, l):
        nc, pg = self.nc, self.pg
        with ExitStack() as st:
            hT = self.load_hT(st)
            hT_toks = [('hT_bf', k) for k in range(KC)]
            wt = [self.sb(st, f'ip_w{i}', [P, KC, 512], BF16) for i in range(2)]
            stF = [self.sb(st, f'ip_sf{i}', [P, S], BF16) for i in range(2)]
            stT = [self.sb(st, f'ip_st{i}', [P, 4, 512], BF16) for i in range(2)]
            ps = self.psum(st, 'ip_ps', [P, 8 * 512], F32)
            w_in = self.I('w_in')[l].rearrange("(k p) n -> p k n", p=P)
            gi = 0
            bank = 0
            ev = 0
            nsf = 0
            nst = 0
            for (name, c0, width) in self.PF_SPECS:
                W = wt[gi % 2]; wtok = f'ip_w{gi % 2}'; gi += 1
                if name == 'akd':
                    for j, cc in enumerate([C_AK, C_AK, C_AK + 64, C_AK + 64]):
                        pg.dma('pool', W[:, :, j * 64:(j + 1) * 64], w_in[:, :, cc:cc + 64],
                               w=[(wtok, j)])
                    wtoks = [(wtok, j) for j in range(4)]
                else:
                    pg.dma('pool', W[:, :, 0:width], w_in[:, :, c0:c0 + width], w=[(wtok, 0)])
                    wtoks = [(wtok, 0)]
                dst = self.dr['pf_' + name].ap()
                for j in range(width // P):
                    SF = stF[nsf % 2]; sftok = f'ip_sf{nsf % 2}'; nsf += 1
                    for tb in range(8):
                        b = bank % 8; bank += 1
                        def mm(E, W=W, j=j, tb=tb, b=b):
                            ins = None
                            for k in range(KC):
                                ins = E.matmul(ps[:, b * 512:(b + 1) * 512], W[:, k, j * P:(j + 1) * P],
                                               hT[:, k, tb * 512:(tb + 1) * 512],
                                               start=(k == 0), stop=(k == KC - 1))
                            return ins
                        pg.op('pe', mm, r=wtoks + hT_toks, w=[('ip_ps', b)])
                        o_ap = SF[:, tb * 512:(tb + 1) * 512]
                        i_ap = ps[:, b * 512:(b + 1) * 512]
                        if ev % 2 == 0:
                            pg.op('act', lambda E, o=o_ap, a=i_ap: E.copy(o, a),
                                  r=[('ip_ps', b)], w=[(sftok, tb)])
                        else:
                            pg.op('dve', lambda E, o=o_ap, a=i_ap: E.tensor_copy(o, a),
                                  r=[('ip_ps', b)], w=[(sftok, tb)])
                        ev += 1
                    pg.dma('sp', dst[j * P:(j + 1) * P, :], SF[:],
                           r=[(sftok, tb) for tb in range(8)], w=[('pf_' + name, j)])
            for (name, c0, width) in self.PT_SPECS:
                W = wt[gi % 2]; wtok = f'ip_w{gi % 2}'; gi += 1
                pg.dma('pool', W[:, :, 0:width], w_in[:, :, c0:c0 + width], w=[(wtok, 0)])
                wtoks = [(wtok, 0)]
                dst = self.dr['pt_' + name].ap().rearrange("(n j p) c -> n p j c", p=P, j=4)
                for t in range(NT):
                    b = bank % 8; bank += 1
                    slot = t % 4
                    if slot == 0:
                        ST = stT[nst % 2]; sttok = f'ip_st{nst % 2}'; nst += 1
                    def mm(E, W=W, t=t, b=b, width=width):
                        ins = None
                        for k in range(KC):
                            ins = E.matmul(ps[:, b * 512:b * 512 + width], hT[:, k, t * P:(t + 1) * P],
                                           W[:, k, 0:width], start=(k == 0), stop=(k == KC - 1))
                        return ins
                    pg.op('pe', mm, r=wtoks + hT_toks, w=[('ip_ps', b)])
                    o_ap = ST[:, slot, 0:width]
                    i_ap = ps[:, b * 512:b * 512 + width]
                    if ev % 2 == 0:
                        pg.op('act', lambda E, o=o_ap, a=i_ap: E.copy(o, a),
                              r=[('ip_ps', b)], w=[(sttok, slot)])
                    else:
                        pg.op('dve', lambda E, o=o_ap, a=i_ap: E.tensor_copy(o, a),
                              r=[('ip_ps', b)], w=[(sttok, slot)])
                    ev += 1
                    if slot == 3:
                        pg.dma('sp', dst[t // 4][:, :, 0:width], ST[:, :, 0:width],
                               r=[(sttok, s_) for s_ in range(4)],
                               w=[('pt_' + name, t // 4)])


    def attn_phase(self, l):
        nc, pg = self.nc, self.pg
        with ExitStack() as st:
            q = self.sb(st, 'at_q', [P, 4, S], BF16)
            kd = self.sb(st, 'at_k', [P, 2, S], BF16)
            va = self.sb(st, 'at_v', [P, NT, 2, 65], BF16)
            bias = self.sb(st, 'at_bias', [P, 8, 3, P], F32)
            esink = self.sb(st, 'at_esink', [P, 8], F32)
            yst = self.sb(st, 'at_yst', [P, 4, S], BF16)
            tmp = [self.sb(st, f'at_tmp{i}', [P, 3 * P], F32) for i in range(2)]
            pT = [self.sb(st, f'at_pT{i}', [P, 3 * P], BF16) for i in range(2)]
            den = [self.sb(st, f'at_den{i}', [P, 8], F32) for i in range(2)]
            y = [self.sb(st, f'at_y{i}', [P, 8, 64], BF16) for i in range(2)]
            ps_s = self.psum(st, 'at_ps_s', [P, 2, 512], F32)
            ps_o = self.psum(st, 'at_ps_o', [P, 2, 2, 512], F32)
            ps_t = self.psum(st, 'at_ps_t', [P, 2, 512], BF16)
            pg.dma('sp', q[:], self.dr['pf_aq'].ap().rearrange("(c p) t -> p c t", p=P),
                   r=[('pf_aq', j) for j in range(4)], w=['at_q'])
            pg.dma('sp', kd[:], self.dr['pf_akd'].ap().rearrange("(c p) t -> p c t", p=P),
                   r=[('pf_akd', j) for j in range(2)], w=['at_k'])
            pg.op('pool', lambda E: E.memset(va[:], 1.0), w=['at_v'])
            avv = self.dr['pt_av'].ap().rearrange("(n p) c -> p n c", p=P)
            for kv in range(2):
                pg.dma('sp', va[:, :, kv, 0:64], avv[:, :, kv * 64:(kv + 1) * 64],
                       r=[('pt_av', j) for j in range(8)], w=['at_v'])
            pg.dma('sp', bias[:].rearrange("p a b c -> p (a b c)"), self.I('c_attn_bias'), w=['at_bias'])
            pg.dma('sp', esink[:], self.I('attn_sink')[l].partition_broadcast(P), w=['at_esink'])
            pg.op('act', lambda E: E.activation(esink[:], esink[:], AF.Exp), r=['at_esink'], w=['at_esink'])
            it = 0
            for n in range(NT):
                ob = n % 2
                js = [j for j in range(3) if 0 <= n + j - 1 < NT]
                c0, c1 = js[0] * P, (js[-1] + 1) * P
                for h in range(8):
                    kv = h // 4
                    r0 = (h % 2) * 64
                    sb_ = it % 2; it += 1
                    def mm(E, h=h, kv=kv, r0=r0, sb_=sb_, n=n, js=js):
                        ins = None
                        for j in js:
                            kb = n + j - 1
                            ins = E.matmul(ps_s[:, sb_, j * P:(j + 1) * P],
                                           kd[r0:r0 + 64, kv, kb * P:(kb + 1) * P],
                                           q[r0:r0 + 64, h // 2, n * P:(n + 1) * P],
                                           start=True, stop=True)
                        return ins
                    pg.op('pe', mm, r=['at_q', 'at_k'], w=[('at_ps_s', sb_)])
                    T, PT = tmp[sb_], pT[sb_]
                    pg.op('dve', lambda E, T=T, sb_=sb_, h=h, c0=c0, c1=c1: E.scalar_tensor_tensor(
                        T[:, c0:c1], ps_s[:, sb_, c0:c1], 0.125,
                        bias[:, h, :, :].rearrange("p a b -> p (a b)")[:, c0:c1], ALU.mult, ALU.add),
                        r=[('at_ps_s', sb_), 'at_bias'], w=[('at_tmp', sb_)])
                    pg.op('act', lambda E, T=T, PT=PT, c0=c0, c1=c1: E.activation(PT[:, c0:c1], T[:, c0:c1], AF.Exp),
                          r=[('at_tmp', sb_)], w=[('at_pT', sb_)])
                    def pv(E, h=h, kv=kv, PT=PT, n=n, js=js, ob=ob):
                        ins = None
                        for idx, j in enumerate(js):
                            kb = n + j - 1
                            ins = E.matmul(ps_o[:, ob, h // 4, (h % 4) * 65:(h % 4) * 65 + 65],
                                           PT[:, j * P:(j + 1) * P], va[:, kb, kv, :],
                                           start=(idx == 0), stop=(idx == len(js) - 1))
                        return ins
                    pg.op('pe', pv, r=[('at_pT', sb_), 'at_v'], w=[('at_ps_o', ob, h)])
                DEN, Y = den[ob], y[ob]
                po = ps_o[:, ob, :, 0:260].rearrange("p b (h e) -> p b h e", e=65)
                pg.op('dve', lambda E, DEN=DEN, po=po: E.tensor_tensor(
                    DEN[:].rearrange("p (b h) -> p b h", b=2), po[:, :, :, 64],
                    esink[:].rearrange("p (b h) -> p b h", b=2), ALU.add),
                    r=[('at_ps_o', ob, h) for h in range(8)] + ['at_esink'], w=[('at_den', ob)])
                pg.op('dve', lambda E, DEN=DEN: E.reciprocal(DEN[:], DEN[:]),
                      r=[('at_den', ob)], w=[('at_den', ob)])
                pg.op('dve', lambda E, DEN=DEN, Y=Y, po=po: E.tensor_tensor(
                    Y[:].rearrange("p (b h) d -> p b h d", b=2), po[:, :, :, 0:64],
                    DEN[:].rearrange("p (b h) -> p b h", b=2).unsqueeze(3).to_broadcast([P, 2, 4, 64]),
                    ALU.mult),
                    r=[('at_ps_o', ob, h) for h in range(8)] + [('at_den', ob)], w=[('at_y', ob)])
                self.transpose_out(Y[:].rearrange("p h d -> p (h d)"), ('at_y', ob), ps_t, 'at_ps_t',
                                   yst, 'at_yst', n, ob)
            self.store_yT(yst, 'at_yst', 0)

    def transpose_out(self, Yflat, ytok, ps_t, pstok, yst, ysttok, n, ob, rows=P):
        pg = self.pg
        def tr(E):
            ins = None
            for c in range(4):
                ins = E.transpose(ps_t[:, ob, c * P:(c + 1) * P], Yflat[:, c * P:(c + 1) * P], self.ident_b[:])
            return ins
        pg.op('pe', tr, r=[ytok, 'ident_b'], w=[(pstok, ob)])
        pg.op('act', lambda E: E.copy(yst[:, :, n * P:(n + 1) * P],
                                      ps_t[:, ob, :].rearrange("p (c t) -> p c t", c=4)),
              r=[(pstok, ob)], w=[(ysttok, n)])

    def store_yT(self, yst, ysttok, row0):
        dst = self.dr['yT_dram'].ap()
        for c in range(4):
            self.pg.dma('sp', dst[row0 + c * P: row0 + (c + 1) * P, :], yst[:, c, :],
                        r=[(ysttok, n) for n in range(NT)], w=[('yT_dram', row0 // P + c)])


    def load_chan(self, dst2d, src1d, wtok):
        self.pg.dma('sp', dst2d, src1d.rearrange("(c p) -> p c", p=P), w=[wtok],
                    allow_slow_non_contiguous=True)

    def conv_phase(self, l):
        nc, pg = self.nc, self.pg
        with ExitStack() as st:
            cw = self.sb(st, 'cv_w', [P, 3, 4], F32)
            for wi in range(3):
                self.load_chan(cw[:, wi, :], self.I('conv_w')[l, wi], ('cv_w', wi))
            cwt = [('cv_w', wi) for wi in range(3)]
            U = [self.sb(st, f'cv_u{i}', [P, S + 2], F32) for i in range(2)]
            A = [self.sb(st, f'cv_a{i}', [P, S], F32) for i in range(2)]
            cb = [self.sb(st, f'cv_b{i}', [P, S], BF16) for i in range(2)]
            cc = [self.sb(st, f'cv_c{i}', [P, S], BF16) for i in range(2)]
            ch = [self.sb(st, f'cv_h{i}', [P, S], BF16) for i in range(2)]
            yo = [self.sb(st, f'cv_y{i}', [P, S], BF16) for i in range(2)]
            for i in range(2):
                pg.op('pool', lambda E, i=i: E.memset(U[i][:], 0.0), w=[('cv_u', i)])
            for c in range(4):
                i = c % 2
                rows = slice(c * P, (c + 1) * P)
                pg.dma('sp', cb[i][:], self.dr['pf_cb'].ap()[rows, :], r=[('pf_cb', c)], w=[('cv_b', i)])
                pg.dma('sp', cc[i][:], self.dr['pf_cc'].ap()[rows, :], r=[('pf_cc', c)], w=[('cv_c', i)])
                pg.dma('sp', ch[i][:], self.dr['pf_ch'].ap()[rows, :], r=[('pf_ch', c)], w=[('cv_h', i)])
                pg.op('pool', lambda E, i=i: E.tensor_tensor(U[i][:, 1:S + 1], cc[i][:], ch[i][:], ALU.mult),
                      r=[('cv_c', i), ('cv_h', i)], w=[('cv_u', i)])
                pg.op('dve', lambda E, i=i, c=c: E.tensor_scalar(A[i][:], U[i][:, 1:S + 1], cw[:, 1, c:c + 1], None, ALU.mult),
                      r=[('cv_u', i)] + cwt, w=[('cv_a', i)])
                pg.op('dve', lambda E, i=i, c=c: E.scalar_tensor_tensor(A[i][:], U[i][:, 0:S], cw[:, 0, c:c + 1], A[i][:], ALU.mult, ALU.add),
                      r=[('cv_u', i), ('cv_a', i)] + cwt, w=[('cv_a', i)])
                pg.op('dve', lambda E, i=i, c=c: E.scalar_tensor_tensor(A[i][:], U[i][:, 2:S + 2], cw[:, 2, c:c + 1], A[i][:], ALU.mult, ALU.add),
                      r=[('cv_u', i), ('cv_a', i)] + cwt, w=[('cv_a', i)])
                pg.op('pool', lambda E, i=i: E.tensor_tensor(yo[i][:], A[i][:], cb[i][:], ALU.mult),
                      r=[('cv_a', i), ('cv_b', i)], w=[('cv_y', i)])
                pg.dma('sp', self.dr['yT_dram'].ap()[512 + c * P: 512 + (c + 1) * P, :], yo[i][:],
                       r=[('cv_y', i)], w=[('yT_dram', 4 + c)])

    def alloc_norm(self, st, pfx, rows, nslot=1):
        d = {}
        d['sq'] = self.sb(st, pfx + '_sq', [rows, nslot * 512], F32)
        d['on'] = self.sb(st, pfx + '_on', [rows, nslot * 512], F32)
        d['e'] = self.sb(st, pfx + '_e', [rows, nslot * 512], F32)
        d['st'] = self.sb(st, pfx + '_st', [rows, 6, nslot * 4], F32)
        d['y'] = [self.sb(st, pfx + f'_y{i}', [rows, nslot * 512], BF16) for i in range(2)]
        d['pfx'] = pfx
        return d

    def norm_gate(self, d, po, potoks, G, gtok, NG, ngtok, mode, yi, rows, nh):
        pg = self.pg
        pfx = d['pfx']
        W = nh * 128
        sq, on, e, stt = d['sq'][:, 0:W], d['on'][:, 0:W], d['e'][:, 0:W], d['st']
        Y = d['y'][yi][:, 0:W]
        v3 = lambda ap: ap.rearrange("p (h v) -> p h v", v=128)
        tk = lambda s: (pfx, s)
        ss, sm, mean, var, rstd, msq = [stt[:, i, 0:nh] for i in range(6)]
        pg.op('act', lambda E: E.activation(sq, po, AF.Square), r=potoks, w=[tk('sq')])
        pg.op('dve', lambda E: E.tensor_reduce(ss, v3(sq), AX.X, ALU.add), r=[tk('sq')], w=[tk('ss')])
        if mode == 'gn':
            pg.op('dve', lambda E: E.tensor_reduce(sm, v3(po), AX.X, ALU.add), r=potoks, w=[tk('sm')])
            pg.op('dve', lambda E: E.tensor_scalar(mean, sm, 1.0 / 128, None, ALU.mult), r=[tk('sm')], w=[tk('mean')])
            pg.op('dve', lambda E: E.tensor_tensor(msq, mean, mean, ALU.mult), r=[tk('mean')], w=[tk('msq')])
            pg.op('dve', lambda E: E.scalar_tensor_tensor(var, ss, 1.0 / 128, msq, ALU.mult, ALU.subtract),
                  r=[tk('ss'), tk('msq')], w=[tk('var')])
        else:
            pg.op('dve', lambda E: E.tensor_scalar(var, ss, 1.0 / 128, None, ALU.mult), r=[tk('ss')], w=[tk('var')])
        pg.op('act', lambda E: E.activation(rstd, var, AF.Ln, bias=HN_EPS, scale=1.0), r=[tk('var')], w=[tk('rstd')])
        pg.op('act', lambda E: E.activation(rstd, rstd, AF.Exp, scale=-0.5), r=[tk('rstd')], w=[tk('rstd')])
        bc = lambda ap: ap.unsqueeze(2).to_broadcast([rows, nh, 128])
        if mode == 'gn':
            pg.op('dve', lambda E: E.tensor_tensor(v3(on), v3(po), bc(mean), ALU.subtract),
                  r=potoks + [tk('mean')], w=[tk('on')])
            pg.op('dve', lambda E: E.tensor_tensor(v3(on), v3(on), bc(rstd), ALU.mult),
                  r=[tk('on'), tk('rstd')], w=[tk('on')])
        else:
            pg.op('dve', lambda E: E.tensor_tensor(v3(on), v3(po), bc(rstd), ALU.mult),
                  r=potoks + [tk('rstd')], w=[tk('on')])
        pg.op('pool', lambda E: E.tensor_tensor(on, on, NG, ALU.mult), r=[tk('on'), ngtok], w=[tk('on')])
        pg.op('act', lambda E: E.activation(e, G, AF.Exp, scale=-1.0), r=[gtok], w=[tk('e')])
        pg.op('pool', lambda E: E.tensor_scalar(e, e, 1.0, None, ALU.add), r=[tk('e')], w=[tk('e')])
        pg.op('dve', lambda E: E.reciprocal(e, e), r=[tk('e')], w=[tk('e')])
        pg.op('pool', lambda E: E.tensor_tensor(e, e, G, ALU.mult), r=[tk('e'), gtok], w=[tk('e')])
        pg.op('pool', lambda E: E.tensor_tensor(Y, on, e, ALU.mult), r=[tk('on'), tk('e')], w=[(pfx + '_y', yi)])
        return d['y'][yi]

    def ret_phase(self, l):
        nc, pg = self.nc, self.pg
        SC = 128.0 ** -0.5
        with ExitStack() as st:
            cst = self.sb(st, 'rt_c', [P, 5, P], F32)
            vec = self.sb(st, 'rt_vec', [P, 2, P], F32)
            pcol = self.sb(st, 'rt_pcol', [P, 2], F32)
            lg = self.sb(st, 'rt_lg', [P, 8], F32)
            GL = self.sb(st, 'rt_GL', [P, 8], F32)
            vd = self.sb(st, 'rt_vd', [P, 8], F32)
            DT = self.sb(st, 'rt_DT', [P, 4, P], F32)
            tmpD = self.sb(st, 'rt_tmpD', [P, P], F32)
            dec = self.sb(st, 'rt_dec', [P, 2, 4, P], F32)
            NG = self.sb(st, 'rt_ng', [P, 512], F32)
            prevF = self.sb(st, 'rt_prevF', [P, NT, 512], BF16)
            yst = self.sb(st, 'rt_yst', [P, 4, S], BF16)
            Fs = self.sb(st, 'rt_F', [P, 512], F32)
            Bs = self.sb(st, 'rt_B', [P, 512], F32)
            tmpS = self.sb(st, 'rt_tmpS', [P, 512], F32)
            pB = [self.sb(st, f'rt_pB{i}', [P, 512], BF16) for i in range(2)]
            Kt = [self.sb(st, f'rt_Kt{i}', [P, 512], BF16) for i in range(2)]
            Vt = [self.sb(st, f'rt_Vt{i}', [P, 512], BF16) for i in range(2)]
            Gt = [self.sb(st, f'rt_Gt{i}', [P, 512], BF16) for i in range(2)]
            Qf = [self.sb(st, f'rt_Qf{i}', [P, 4, P], BF16) for i in range(2)]
            Kf = [self.sb(st, f'rt_Kf{i}', [P, 4, P], BF16) for i in range(2)]
            vS = [self.sb(st, f'rt_vS{i}', [P, 512], BF16) for i in range(2)]
            aTm = [self.sb(st, f'rt_aTm{i}', [P, 512], BF16) for i in range(2)]
            qF = [self.sb(st, f'rt_qF{i}', [P, 4, P], BF16) for i in range(2)]
            qB = [self.sb(st, f'rt_qB{i}', [P, 4, P], BF16) for i in range(2)]
            nd = self.alloc_norm(st, 'rt_n', P)
            ps_a = self.psum(st, 'rt_ps_a', [P, 2, 512], F32)
            ps_o = self.psum(st, 'rt_ps_o', [P, 2, 512], F32)
            ps_kv = self.psum(st, 'rt_ps_kv', [P, 2, 512], F32)
            ps_t = self.psum(st, 'rt_ps_t', [P, 2, 512], BF16)
            pg.dma('sp', cst[:].rearrange("p a b -> p (a b)"), self.I('c_ret_c'), w=['rt_c'])
            pg.dma('sp', vec[:].rearrange("p a b -> p (a b)"), self.I('c_ret_vec'), w=['rt_vec'])
            pg.dma('sp', pcol[:], self.I('c_ret_pcol'), w=['rt_pcol'])
            pg.dma('sp', NG[:], self.I('ret_norm_g')[l].partition_broadcast(P), w=['rt_ng'])
            pg.dma('sp', lg[:], self.I('ret_decay_logit')[l].rearrange("a b -> (a b)").partition_broadcast(P), w=['rt_lg'])
            pg.op('act', lambda E: E.activation(lg[:], lg[:], AF.Exp, scale=-1.0), r=['rt_lg'], w=['rt_lg'])
            pg.op('dve', lambda E: E.tensor_scalar(lg[:], lg[:], 1.0, None, ALU.add), r=['rt_lg'], w=['rt_lg'])
            pg.op('act', lambda E: E.activation(lg[:], lg[:], AF.Ln), r=['rt_lg'], w=['rt_lg'])
            pg.op('dve', lambda E: E.tensor_scalar(lg[:], lg[:], -1.0, None, ALU.mult), r=['rt_lg'], w=['rt_lg'])
            pg.op('act', lambda E: E.activation(GL[:], lg[:], AF.Exp, scale=128.0), r=['rt_lg'], w=['rt_GL'])
            lnsc = float(np.log(SC))
            for h in range(4):
                pg.op('act', lambda E, h=h: E.activation(vd[:, h:h + 1], pcol[:, 0:1], AF.Exp, scale=lg[:, h:h + 1], bias=lnsc),
                      r=['rt_lg', 'rt_pcol'], w=[('rt_vd', h)])
                pg.op('act', lambda E, h=h: E.activation(vd[:, 4 + h:5 + h], pcol[:, 1:2], AF.Exp, scale=lg[:, 4 + h:5 + h], bias=lnsc),
                      r=['rt_lg', 'rt_pcol'], w=[('rt_vd', 4 + h)])
                pg.op('act', lambda E, h=h: E.activation(dec[:, 0, h, :], vec[:, 0, :], AF.Exp, scale=lg[:, h:h + 1]),
                      r=['rt_lg', 'rt_vec'], w=[('rt_dec', 0, h)])
                pg.op('act', lambda E, h=h: E.activation(dec[:, 1, h, :], vec[:, 1, :], AF.Exp, scale=lg[:, 4 + h:5 + h]),
                      r=['rt_lg', 'rt_vec'], w=[('rt_dec', 1, h)])
                pg.op('act', lambda E, h=h: E.activation(DT[:, h, :], cst[:, 0, :], AF.Exp, scale=lg[:, h:h + 1]),
                      r=['rt_lg', 'rt_c'], w=[('rt_DT', h)])
                pg.op('dve', lambda E, h=h: E.tensor_tensor(DT[:, h, :], DT[:, h, :], cst[:, 2, :], ALU.mult),
                      r=[('rt_DT', h), 'rt_c'], w=[('rt_DT', h)])
                pg.op('act', lambda E, h=h: E.activation(tmpD[:], cst[:, 1, :], AF.Exp, scale=lg[:, 4 + h:5 + h]),
                      r=['rt_lg', 'rt_c'], w=['rt_tmpD'])
                pg.op('dve', lambda E: E.tensor_tensor(tmpD[:], tmpD[:], cst[:, 3, :], ALU.mult),
                      r=['rt_tmpD', 'rt_c'], w=['rt_tmpD'])
                pg.op('dve', lambda E, h=h: E.tensor_tensor(DT[:, h, :], DT[:, h, :], tmpD[:], ALU.add),
                      r=[('rt_DT', h), 'rt_tmpD'], w=[('rt_DT', h)])
                pg.op('dve', lambda E, h=h: E.tensor_tensor(DT[:, h, :], DT[:, h, :], cst[:, 4, :], ALU.add),
                      r=[('rt_DT', h), 'rt_c'], w=[('rt_DT', h)])
                pg.op('dve', lambda E, h=h: E.tensor_scalar(DT[:, h, :], DT[:, h, :], SC, None, ALU.mult),
                      r=[('rt_DT', h)], w=[('rt_DT', h)])
            DTt = [('rt_DT', h) for h in range(4)]
            vdt = [('rt_vd', h) for h in range(8)]
            dect = [('rt_dec', a, h) for a in range(2) for h in range(4)]
            pg.op('pool', lambda E: E.memset(Fs[:], 0.0), w=['rt_F'])
            pg.op('pool', lambda E: E.memset(Bs[:], 0.0), w=['rt_B'])
            ktv = self.dr['pt_rkt'].ap().rearrange("(n p) c -> n p c", p=P)
            vtv = self.dr['pt_rv'].ap().rearrange("(n p) c -> n p c", p=P)
            gtv = self.dr['pt_rg'].ap().rearrange("(n p) c -> n p c", p=P)
            qfv = self.dr['pf_rq'].ap().rearrange("(h p) t -> p h t", p=P)
            kfv = self.dr['pf_rk'].ap().rearrange("(h p) t -> p h t", p=P)
            v3 = lambda ap: ap.rearrange("p (h v) -> p h v", v=128)
            bc4 = lambda ap: ap.unsqueeze(2).to_broadcast([P, 4, 128])
            it = 0

            def kv_step(n, i, K, V, vdcols, state, stok, GLcols):
                VS = vS[i]
                pg.op('pool', lambda E: E.tensor_tensor(v3(VS[:]), v3(V[:]), bc4(vdcols), ALU.mult),
                      r=[('rt_Vt', i)] + vdt, w=[('rt_vS', i)])
                def mm(E):
                    ins = None
                    for h in range(4):
                        ins = E.matmul(ps_kv[:, i, h * P:(h + 1) * P], K[:, h * P:(h + 1) * P], VS[:, h * P:(h + 1) * P],
                                       start=True, stop=True)
                    return ins
                pg.op('pe', mm, r=[('rt_Kt', i), ('rt_vS', i)], w=[('rt_ps_kv', i)])
                pg.op('pool', lambda E: E.tensor_tensor(v3(tmpS[:]), v3(state[:]), bc4(GLcols), ALU.mult),
                      r=[stok, 'rt_GL'], w=['rt_tmpS'])
                pg.op('dve', lambda E: E.tensor_tensor(state[:], tmpS[:], ps_kv[:, i, :], ALU.add),
                      r=['rt_tmpS', ('rt_ps_kv', i)], w=[stok])

            for n in range(NT):
                i = it % 2; it += 1
                pg.dma('sp', Kt[i][:], ktv[n], r=[('pt_rkt', n // 4)], w=[('rt_Kt', i)])
                pg.dma('sp', Vt[i][:], vtv[n], r=[('pt_rv', n // 4)], w=[('rt_Vt', i)])
                pg.op('act', lambda E, n=n: E.copy(prevF[:, n, :], Fs[:]), r=['rt_F'], w=[('rt_prevF', n)])
                kv_step(n, i, Kt[i], Vt[i], vd[:, 0:4], Fs, 'rt_F', GL[:, 0:4])
            for n in range(NT - 1, -1, -1):
                i = it % 2; it += 1
                tsl = slice(n * P, (n + 1) * P)
                pg.dma('sp', Kt[i][:], ktv[n], r=[('pt_rkt', n // 4)], w=[('rt_Kt', i)])
                pg.dma('sp', Vt[i][:], vtv[n], r=[('pt_rv', n // 4)], w=[('rt_Vt', i)])
                pg.dma('sp', Gt[i][:], gtv[n], r=[('pt_rg', n // 4)], w=[('rt_Gt', i)])
                pg.dma('sp', Qf[i][:], qfv[:, :, tsl], r=[('pf_rq', h) for h in range(4)], w=[('rt_Qf', i)])
                pg.dma('sp', Kf[i][:], kfv[:, :, tsl], r=[('pf_rk', h) for h in range(4)], w=[('rt_Kf', i)])
                def mma(E, i=i):
                    ins = None
                    for h in range(4):
                        ins = E.matmul(ps_a[:, i, h * P:(h + 1) * P], Kf[i][:, h, :], Qf[i][:, h, :], start=True, stop=True)
                    return ins
                pg.op('pe', mma, r=[('rt_Qf', i), ('rt_Kf', i)], w=[('rt_ps_a', i)])
                pg.op('dve', lambda E, i=i: E.tensor_tensor(aTm[i][:], ps_a[:, i, :], DT[:].rearrange("p h t -> p (h t)"), ALU.mult),
                      r=[('rt_ps_a', i)] + DTt, w=[('rt_aTm', i)])
                pg.op('pool', lambda E, i=i: E.tensor_tensor(qF[i][:], Qf[i][:], dec[:, 0, :, :], ALU.mult),
                      r=[('rt_Qf', i)] + dect, w=[('rt_qF', i)])
                pg.op('pool', lambda E, i=i: E.tensor_tensor(qB[i][:], Qf[i][:], dec[:, 1, :, :], ALU.mult),
                      r=[('rt_Qf', i)] + dect, w=[('rt_qB', i)])
                pg.op('act', lambda E, i=i: E.copy(pB[i][:], Bs[:]), r=['rt_B'], w=[('rt_pB', i)])
                def mmo(E, i=i, n=n):
                    ins = None
                    for h in range(4):
                        hs = slice(h * P, (h + 1) * P)
                        E.matmul(ps_o[:, i, hs], aTm[i][:, hs], Vt[i][:, hs], start=True, stop=False)
                        E.matmul(ps_o[:, i, hs], qF[i][:, h, :], prevF[:, n, hs], start=False, stop=False)
                        ins = E.matmul(ps_o[:, i, hs], qB[i][:, h, :], pB[i][:, hs], start=False, stop=True)
                    return ins
                pg.op('pe', mmo, r=[('rt_aTm', i), ('rt_Vt', i), ('rt_qF', i), ('rt_qB', i), ('rt_prevF', n), ('rt_pB', i)],
                      w=[('rt_ps_o', i)])
                kv_step(n, i, Kt[i], Vt[i], vd[:, 4:8], Bs, 'rt_B', GL[:, 4:8])
                Y = self.norm_gate(nd, ps_o[:, i, :], [('rt_ps_o', i)], Gt[i][:], ('rt_Gt', i), NG[:], 'rt_ng',
                                   'gn', i, P, 4)
                self.transpose_out(Y[:], ('rt_n_y', i), ps_t, 'rt_ps_t', yst, 'rt_yst', n, i)
            self.store_yT(yst, 'rt_yst', 1536)


    def hgrn_phase(self, l):
        nc, pg = self.nc, self.pg
        NCH = S // 64
        with ExitStack() as outer:
            DEC = self.sb(outer, 'hg_DEC', [P, 2, 3, 4, NCH], F32)
            dect = [('hg_DEC', d_, hd) for d_ in range(2) for hd in range(4)]
            with ExitStack() as st:
                lb = self.sb(st, 'hg_lb', [P, 4], F32)
                oml = self.sb(st, 'hg_oml', [P, 4], F32)
                a0 = self.sb(st, 'hg_a0', [P, 4], F32)
                cm = self.sb(st, 'hg_cm', [P, S], BF16)
                T = [self.sb(st, f'hg_T{i}', [P, S], F32) for i in range(4)]
                tmpd = self.sb(st, 'hg_tmpd', [P, NCH], F32)
                zb = [self.sb(st, f'hg_z{i}', [P, S], BF16) for i in range(2)]
                qb = [self.sb(st, f'hg_qin{i}', [P, S], BF16) for i in range(2)]
                qo = [self.sb(st, f'hg_qo{i}', [P, S], BF16) for i in range(2)]
                ko = [self.sb(st, f'hg_ko{i}', [P, S], BF16) for i in range(2)]
                pg.dma('sp', cm[:], self.I('c_hg_cm'), w=['hg_cm'])
                if l == 0:
                    pg.op('pool', lambda E: E.memset(lb[:], 0.0), w=['hg_lb'])
                else:
                    self.load_chan(a0[:], self.I('hgrn_lb')[0], 'hg_a0')
                    self.load_chan(lb[:], self.I('hgrn_lb')[1], 'hg_lb')
                    pg.op('dve', lambda E: E.tensor_tensor(lb[:], a0[:], lb[:], ALU.subtract), r=['hg_a0', 'hg_lb'], w=['hg_lb'])
                    pg.op('act', lambda E: E.activation(lb[:], lb[:], AF.Exp), r=['hg_lb'], w=['hg_lb'])
                    pg.op('dve', lambda E: E.tensor_scalar(lb[:], lb[:], 1.0, None, ALU.add), r=['hg_lb'], w=['hg_lb'])
                    pg.op('dve', lambda E: E.reciprocal(lb[:], lb[:]), r=['hg_lb'], w=['hg_lb'])
                pg.op('dve', lambda E: E.tensor_scalar(oml[:], lb[:], -1.0, 1.0, ALU.mult, ALU.add), r=['hg_lb'], w=['hg_oml'])
                it = 0
                for d_ in range(2):
                    zname = 'pf_gzf' if d_ == 0 else 'pf_gzb'
                    mid, last = (31, 63) if d_ == 0 else (32, 0)
                    for hd in range(4):
                        i = it % 2; it += 1
                        rows = slice(hd * P, (hd + 1) * P)
                        T1, T2, T3, T4 = T
                        pg.dma('sp', zb[i][:], self.dr[zname].ap()[rows, :], r=[(zname, hd)], w=[('hg_z', i)])
                        pg.dma('sp', qb[i][:], self.dr['pf_gq'].ap()[rows, :], r=[('pf_gq', hd)], w=[('hg_qin', i)])
                        pg.op('act', lambda E, i=i: E.activation(T1[:], zb[i][:], AF.Exp, scale=-1.0), r=[('hg_z', i)], w=['hg_T1'])
                        pg.op('pool', lambda E: E.tensor_scalar(T1[:], T1[:], 1.0, None, ALU.add), r=['hg_T1'], w=['hg_T1'])
                        pg.op('dve', lambda E: E.reciprocal(T1[:], T1[:]), r=['hg_T1'], w=['hg_T1'])
                        pg.op('dve', lambda E, hd=hd: E.tensor_scalar(T1[:], T1[:], oml[:, hd:hd + 1], lb[:, hd:hd + 1], ALU.mult, ALU.add),
                              r=['hg_T1', 'hg_lb', 'hg_oml'], w=['hg_T1'])
                        pg.op('act', lambda E: E.activation(T2[:], T1[:], AF.Ln), r=['hg_T1'], w=['hg_T2'])
                        pg.op('dve', lambda E: E.tensor_tensor_scan(T3[:], cm[:], T2[:], 0.0, ALU.mult, ALU.add),
                              r=['hg_cm', 'hg_T2'], w=['hg_T3'])
                        c3 = lambda ap: ap.rearrange("p (n c) -> p n c", c=64)
                        if d_ == 0:
                            Bt, Btok = T3, 'hg_T3'
                        else:
                            pg.op('dve', lambda E: E.tensor_tensor(c3(T4[:]), c3(T3[:])[:, :, 63:64].to_broadcast([P, NCH, 64]),
                                                                   c3(T3[:]), ALU.subtract), r=['hg_T3'], w=['hg_T4'])
                            pg.op('pool', lambda E: E.tensor_tensor(T4[:], T4[:], T2[:], ALU.add), r=['hg_T4', 'hg_T2'], w=['hg_T4'])
                            Bt, Btok = T4, 'hg_T4'
                        B3 = c3(Bt[:])
                        dk = ('hg_DEC', d_, hd)
                        pg.op('act', lambda E, B3=B3, d_=d_, hd=hd, last=last: E.activation(DEC[:, d_, 0, hd, :], B3[:, :, last], AF.Exp),
                              r=[Btok], w=[dk])
                        pg.op('act', lambda E, B3=B3, d_=d_, hd=hd, mid=mid: E.activation(DEC[:, d_, 1, hd, :], B3[:, :, mid], AF.Exp),
                              r=[Btok], w=[dk])
                        pg.op('dve', lambda E, B3=B3, mid=mid, last=last: E.tensor_tensor(tmpd[:], B3[:, :, last], B3[:, :, mid], ALU.subtract),
                              r=[Btok], w=['hg_tmpd'])
                        pg.op('act', lambda E, d_=d_, hd=hd: E.activation(DEC[:, d_, 2, hd, :], tmpd[:], AF.Exp),
                              r=['hg_tmpd'], w=[dk])
                        pg.op('dve', lambda E, B3=B3, mid=mid: E.tensor_tensor(c3(T2[:]), B3, B3[:, :, mid:mid + 1].to_broadcast([P, NCH, 64]),
                                                                               ALU.subtract), r=[Btok, 'hg_T2'], w=['hg_T2'])
                        EP, EPtok = (T4, 'hg_T4') if d_ == 0 else (T3, 'hg_T3')
                        pg.op('act', lambda E, EP=EP: E.activation(EP[:], T2[:], AF.Exp), r=['hg_T2', Btok], w=[EPtok])
                        pg.op('pool', lambda E, i=i, EP=EP: E.tensor_tensor(qo[i][:], qb[i][:], EP[:], ALU.mult),
                              r=[('hg_qin', i), EPtok], w=[('hg_qo', i)])
                        pg.dma('sp', self.dr[f'hg_q{d_}'].ap()[rows, :], qo[i][:], r=[('hg_qo', i)], w=[(f'hg_q{d_}', hd)])
                        EM, EMtok = (T3, 'hg_T3') if d_ == 0 else (T4, 'hg_T4')
                        pg.op('act', lambda E, EM=EM: E.activation(EM[:], T2[:], AF.Exp, scale=-1.0), r=['hg_T2', EPtok, ('hg_qo', i)], w=[EMtok])
                        pg.op('pool', lambda E: E.tensor_scalar(T1[:], T1[:], -1.0, 1.0, ALU.mult, ALU.add), r=['hg_T1'], w=['hg_T1'])
                        pg.op('dve', lambda E, i=i, EM=EM: E.tensor_tensor(ko[i][:], T1[:], EM[:], ALU.mult),
                              r=['hg_T1', EMtok], w=[('hg_ko', i)])
                        pg.dma('sp', self.dr[f'hg_k{d_}'].ap()[rows, :], ko[i][:], r=[('hg_ko', i)], w=[(f'hg_k{d_}', hd)])
            pg.barrier()
            v3 = lambda ap: ap.rearrange("p (h v) -> p h v", v=128)
            with ExitStack() as st:
                Sst = self.sb(st, 'hs_S', [P, 512], F32)
                t2 = self.sb(st, 'hs_t2', [P, 512], F32)
                Sbf = [self.sb(st, f'hs_Sbf{i}', [P, 512], BF16) for i in range(2)]
                kblk = [self.sb(st, f'hs_kb{i}', [P, 4, P], BF16) for i in range(2)]
                ktok = [self.sb(st, f'hs_kt{i}', [P, 512], BF16) for i in range(2)]
                gi = [self.sb(st, f'hs_gi{i}', [P, 512], BF16) for i in range(2)]
                ps_t = self.psum(st, 'hs_ps_t', [P, 2, 512], BF16)
                ps_kv = self.psum(st, 'hs_ps_kv', [P, 2, 512], F32)
                giv = self.dr['pt_gi'].ap().rearrange("(n p) c -> n p c", p=P)
                it = 0
                ic = 0
                for d_ in range(2):
                    kv_ = self.dr[f'hg_k{d_}'].ap().rearrange("(h p) t -> p h t", p=P)
                    Sd = self.dr[f'hg_S{d_}'].ap()
                    pg.op('pool', lambda E: E.memset(Sst[:], 0.0), r=[], w=['hs_S'])
                    order = range(NT) if d_ == 0 else range(NT - 1, -1, -1)
                    order = list(order)
                    def hs_load(tt, i):
                        pg.dma('sp', kblk[i][:], kv_[:, :, tt * P:(tt + 1) * P], r=[(f'hg_k{d_}', hd) for hd in range(4)], w=[('hs_kb', i)])
                        pg.dma('sp', gi[i][:], giv[tt], r=[('pt_gi', tt // 4)], w=[('hs_gi', i)])
                    hs_load(order[0], it % 2)
                    for oi, tt in enumerate(order):
                        i = it % 2; it += 1
                        if oi + 1 < len(order):
                            hs_load(order[oi + 1], it % 2)
                        def tr(E, i=i):
                            ins = None
                            for h in range(4):
                                ins = E.transpose(ps_t[:, i, h * P:(h + 1) * P], kblk[i][:, h, :], self.ident_b[:])
                            return ins
                        pg.op('pe', tr, r=[('hs_kb', i), 'ident_b'], w=[('hs_ps_t', i)])
                        pg.op('act', lambda E, i=i: E.copy(ktok[i][:], ps_t[:, i, :]), r=[('hs_ps_t', i)], w=[('hs_kt', i)])
                        for c in ((0, 1) if d_ == 0 else (1, 0)):
                            n = tt * 2 + c
                            j = ic % 2; ic += 1
                            r0 = c * 64
                            def mm(E, i=i, j=j, r0=r0):
                                ins = None
                                for h in range(4):
                                    hs = slice(h * P, (h + 1) * P)
                                    ins = E.matmul(ps_kv[:, j, hs], ktok[i][r0:r0 + 64, hs], gi[i][r0:r0 + 64, hs], start=True, stop=True)
                                return ins
                            pg.op('pe', mm, r=[('hs_kt', i), ('hs_gi', i)], w=[('hs_ps_kv', j)])
                            dbc = lambda kind, n=n, d_=d_: DEC[:, d_, kind, :, n].unsqueeze(2).to_broadcast([P, 4, 128])
                            pg.op('pool', lambda E, j=j, dbc=dbc: E.tensor_tensor(v3(Sbf[j][:]), v3(Sst[:]), dbc(1), ALU.mult),
                                  r=['hs_S'] + dect, w=[('hs_Sbf', j)])
                            pg.dma('sp', Sd[n], Sbf[j][:], r=[('hs_Sbf', j)], w=[(f'hg_S{d_}', n)])
                            pg.op('dve', lambda E, j=j, dbc=dbc: E.tensor_tensor(v3(t2[:]), v3(ps_kv[:, j, :]), dbc(2), ALU.mult),
                                  r=[('hs_ps_kv', j)] + dect, w=['hs_t2'])
                            pg.op('pool', lambda E, dbc=dbc: E.tensor_tensor(v3(Sst[:]), v3(Sst[:]), dbc(0), ALU.mult),
                                  r=['hs_S'] + dect, w=['hs_S'])
                            pg.op('dve', lambda E: E.tensor_tensor(Sst[:], Sst[:], t2[:], ALU.add), r=['hs_S', 'hs_t2'], w=['hs_S'])
            pg.barrier()
            with ExitStack() as st:
                H = 64
                mask = self.sb(st, 'ho_mask', [H, 2, 64], F32)
                NG = self.sb(st, 'ho_ng', [H, 2, 512], F32)
                yst = self.sb(st, 'ho_yst', [P, 4, S], BF16)
                blk = {}
                for nm in ('q0', 'k0', 'q1', 'k1'):
                    blk[nm] = [self.sb(st, f'ho_{nm}_{i}', [P, 4, P], BF16) for i in range(2)]
                gi = [self.sb(st, f'ho_gi{i}', [H, 2, 512], BF16) for i in range(2)]
                go = [self.sb(st, f'ho_go{i}', [H, 2, 512], BF16) for i in range(2)]
                Sb = [[self.sb(st, f'ho_S{d_}_{i}', [P, 2, 512], BF16) for i in range(2)] for d_ in range(2)]
                aTm = [self.sb(st, f'ho_aTm{i}', [H, 2, 2, 4, 64], BF16) for i in range(2)]
                nd = self.alloc_norm(st, 'ho_n', H, nslot=2)
                ps_a = self.psum(st, 'ho_ps_a', [H, 2, 2, 4, 64], F32)
                ps_o = self.psum(st, 'ho_ps_o', [H, 2, 2, 512], F32)
                ps_t = self.psum(st, 'ho_ps_t', [P, 2, 512], BF16)
                pg.dma('sp', mask[:].rearrange("p a b -> p (a b)"), self.I('c_hg_mask'), w=['ho_mask'])
                for c in range(2):
                    pg.dma('sp', NG[:, c, :], self.I('hgrn_norm_g')[l].partition_broadcast(H), w=[('ho_ng', c)])
                ngt = [('ho_ng', c) for c in range(2)]
                giv = self.dr['pt_gi'].ap().rearrange("(n c p) v -> n p c v", p=H, c=2)
                gov = self.dr['pt_go'].ap().rearrange("(n c p) v -> n p c v", p=H, c=2)
                fm = {nm: self.dr['hg_' + nm].ap().rearrange("(h p) t -> p h t", p=P) for nm in blk}
                Sv = [self.dr[f'hg_S{d_}'].ap().rearrange("(n c) p v -> n p c v", c=2) for d_ in range(2)]
                for tt in range(NT):
                    i = tt % 2
                    tsl = slice(tt * P, (tt + 1) * P)
                    for nm in blk:
                        pg.dma('sp', blk[nm][i][:], fm[nm][:, :, tsl], r=[('hg_' + nm, hd) for hd in range(4)], w=[('ho_' + nm, i)])
                    pg.dma('sp', gi[i][:], giv[tt], r=[('pt_gi', tt // 4)], w=[('ho_gi', i)])
                    pg.dma('sp', go[i][:], gov[tt], r=[('pt_go', tt // 4)], w=[('ho_go', i)])
                    for d_ in range(2):
                        pg.dma('sp', Sb[d_][i][:], Sv[d_][tt], r=[(f'hg_S{d_}', 2 * tt), (f'hg_S{d_}', 2 * tt + 1)], w=[('ho_S', d_, i)])
                    def mma(E, i=i):
                        ins = None
                        for c in range(2):
                            cs = slice(c * 64, (c + 1) * 64)
                            for d_ in range(2):
                                for h in range(4):
                                    ins = E.matmul(ps_a[:, c, d_, h, :], blk[f'k{d_}'][i][:, h, cs], blk[f'q{d_}'][i][:, h, cs],
                                                   start=True, stop=True)
                        return ins
                    pg.op('pe', mma, r=[('ho_' + nm, i) for nm in blk], w=['ho_ps_a'])
                    for c in range(2):
                        pg.op('dve', lambda E, i=i, c=c: E.tensor_tensor(
                            aTm[i][:, c].rearrange("p d h t -> p d h t"), ps_a[:, c],
                            mask[:].unsqueeze(2).to_broadcast([H, 2, 4, 64]), ALU.mult),
                            r=['ho_ps_a', 'ho_mask'], w=[('ho_aTm', i, c)])
                    def mmo(E, i=i):
                        ins = None
                        for c in range(2):
                            cs = slice(c * 64, (c + 1) * 64)
                            for h in range(4):
                                hs = slice(h * P, (h + 1) * P)
                                o = ps_o[:, i, c, hs]
                                E.matmul(o, aTm[i][:, c, 0, h, :], gi[i][:, c, hs], start=True, stop=False)
                                E.matmul(o, aTm[i][:, c, 1, h, :], gi[i][:, c, hs], start=False, stop=False)
                                E.matmul(o, blk['q0'][i][:, h, cs], Sb[0][i][:, c, hs], start=False, stop=False)
                                ins = E.matmul(o, blk['q1'][i][:, h, cs], Sb[1][i][:, c, hs], start=False, stop=True)
                        return ins
                    pg.op('pe', mmo, r=[('ho_aTm', i, 0), ('ho_aTm', i, 1), ('ho_gi', i), ('ho_q0', i), ('ho_q1', i),
                                        ('ho_S', 0, i), ('ho_S', 1, i)], w=[('ho_ps_o', i)])
                    Y = self.norm_gate(nd, ps_o[:, i].rearrange("p c v -> p (c v)"), [('ho_ps_o', i)],
                                       go[i][:].rearrange("p c v -> p (c v)"), ('ho_go', i),
                                       NG[:].rearrange("p c v -> p (c v)"), ngt[0], 'rms', i, H, 8)
                    def tr(E, i=i, Y=Y):
                        ins = None
                        for c in range(2):
                            for cc in range(4):
                                ins = E.transpose(ps_t[:, i, cc * P + c * 64: cc * P + (c + 1) * 64],
                                                  Y[:, c * 512 + cc * P: c * 512 + (cc + 1) * P], self.ident_b[0:H, 0:H])
                        return ins
                    pg.op('pe', tr, r=[('ho_n_y', i), 'ident_b'], w=[('ho_ps_t', i)])
                    pg.op('act', lambda E, i=i, tsl=tsl: E.copy(yst[:, :, tsl], ps_t[:, i, :].rearrange("p (c t) -> p c t", c=4)),
                          r=[('ho_ps_t', i)], w=[('ho_yst', tt)])
                self.store_yT(yst, 'ho_yst', 1024)


    def outproj_phase(self, l):
        nc, pg = self.nc, self.pg
        with ExitStack() as st:
            W = self.sb(st, 'op_w', [P, KC, D], BF16)
            wv = self.I('w_out')[l].rearrange("(k p) n -> p k n", p=P)
            for j in range(4):
                pg.dma('pool', W[:, :, j * 512:(j + 1) * 512], wv[:, :, j * 512:(j + 1) * 512], w=[('op_w', j)])
            wt = [('op_w', j) for j in range(4)]
            yT = [self.sb(st, f'op_y{i}', [P, KC, P], BF16) for i in range(2)]
            so = [self.sb(st, f'op_o{i}', [P, D], F32) for i in range(2)]
            ps = self.psum(st, 'op_ps', [P, 2, 4, 512], F32)
            yv = self.dr['yT_dram'].ap().rearrange("(k p) t -> p k t", p=P)
            rv = self.dr['res_dram'].ap().rearrange("(n p) d -> n p d", p=P)
            pg.dma('sp', yT[0][:], yv[:, :, 0:P], r=[('yT_dram', c) for c in range(KC)], w=[('op_y', 0)])
            for t in range(NT):
                i = t % 2
                if t + 1 < NT:
                    pg.dma('sp', yT[(t + 1) % 2][:], yv[:, :, (t + 1) * P:(t + 2) * P], r=[('yT_dram', c) for c in range(KC)], w=[('op_y', (t + 1) % 2)])
                for j in range(4):
                    def mm(E, i=i, j=j):
                        ins = None
                        for k in range(KC):
                            ins = E.matmul(ps[:, i, j, :], yT[i][:, k, :], W[:, k, j * 512:(j + 1) * 512],
                                           start=(k == 0), stop=(k == KC - 1))
                        return ins
                    pg.op('pe', mm, r=[('op_y', i)] + wt, w=[('op_ps', i, j)])
                    o_ap = so[i][:, j * 512:(j + 1) * 512]
                    if j % 2 == 0:
                        pg.op('act', lambda E, o=o_ap, a=ps[:, i, j, :]: E.copy(o, a), r=[('op_ps', i, j)], w=[('op_o', i, j)])
                    else:
                        pg.op('dve', lambda E, o=o_ap, a=ps[:, i, j, :]: E.tensor_copy(o, a), r=[('op_ps', i, j)], w=[('op_o', i, j)])
                pg.dma('sp', rv[t], so[i][:], r=[('op_o', i, j) for j in range(4)], w=[('res_dram', t)])

    def moe_phase(self, l):
        nc, pg = self.nc, self.pg
        TBS = 1024
        NTB = S // TBS
        TPB = TBS // P
        with ExitStack() as st:
            hTb = self.sb(st, 'mo_h', [P, KC, TBS], BF16)
            acc = self.sb(st, 'mo_acc', [P, TPB, D], F32)
            hid = self.sb(st, 'mo_hid', [P, 8, TBS], BF16)
            wg = [self.sb(st, f'mo_wg{i}', [P, KC, 256], BF16) for i in range(2)]
            wu = [self.sb(st, f'mo_wu{i}', [P, KC, 256], BF16) for i in range(2)]
            wd = [self.sb(st, f'mo_wd{i}', [P, 8, 512], BF16) for i in range(2)]
            sg = [self.sb(st, f'mo_sg{i}', [P, 512], F32) for i in range(2)]
            ps_gu = self.psum(st, 'mo_ps_gu', [P, 2, 2, 512], F32)
            ps_d = self.psum(st, 'mo_ps_d', [P, 3, 512], F32)
            hTv = self.dr['hT_dram'].ap().rearrange("(k p) t -> p k t", p=P)
            rv = self.dr['res_dram'].ap().rearrange("(n j p) d -> n p j d", p=P, j=TPB)
            iw = 0; idw = 0; igu = 0; ipd = 0
            for tb in range(NTB):
                for k in range(KC):
                    pg.dma('sp', hTb[:, k, :], hTv[:, k, tb * TBS:(tb + 1) * TBS],
                           r=[('hT_dram', tb * 2), ('hT_dram', tb * 2 + 1)], w=[('mo_h', k)])
                ht = [('mo_h', k) for k in range(KC)]
                for e in range(NE):
                    wgv = self.I('w_gate')[l, e].rearrange("(k p) f -> p k f", p=P)
                    wuv = self.I('w_up')[l, e].rearrange("(k p) f -> p k f", p=P)
                    wdv = self.I('w_down')[l, e].rearrange("(c p) n -> p c n", p=P)
                    for hf in range(4):
                        wi = iw % 2; iw += 1
                        pg.dma('pool', wg[wi][:], wgv[:, :, hf * 256:(hf + 1) * 256], w=[('mo_wg', wi)])
                        pg.dma('pool', wu[wi][:], wuv[:, :, hf * 256:(hf + 1) * 256], w=[('mo_wu', wi)])
                        for f2 in range(2):
                            fc = hf * 2 + f2
                            for th in range(TBS // 512):
                                b = igu % 2; igu += 1
                                def mm(E, wi=wi, f2=f2, th=th, b=b):
                                    ins = None
                                    for k in range(KC):
                                        E.matmul(ps_gu[:, b, 0, :], wg[wi][:, k, f2 * P:(f2 + 1) * P], hTb[:, k, th * 512:(th + 1) * 512],
                                                 start=(k == 0), stop=(k == KC - 1))
                                    for k in range(KC):
                                        ins = E.matmul(ps_gu[:, b, 1, :], wu[wi][:, k, f2 * P:(f2 + 1) * P], hTb[:, k, th * 512:(th + 1) * 512],
                                                       start=(k == 0), stop=(k == KC - 1))
                                    return ins
                                pg.op('pe', mm, r=[('mo_wg', wi), ('mo_wu', wi)] + ht, w=[('mo_ps_gu', b)])
                                pg.op('act', lambda E, b=b: E.activation(sg[b][:], ps_gu[:, b, 0, :], AF.Silu),
                                      r=[('mo_ps_gu', b)], w=[('mo_sg', b)])
                                pg.op('dve', lambda E, b=b, fc=fc, th=th: E.tensor_tensor(hid[:, fc, th * 512:(th + 1) * 512], sg[b][:], ps_gu[:, b, 1, :], ALU.mult),
                                      r=[('mo_sg', b), ('mo_ps_gu', b)], w=[('mo_hid', fc, th)])
                    hidt = [('mo_hid', fc, th) for fc in range(8) for th in range(TBS // 512)]
                    for nch in range(4):
                        di = idw % 2; idw += 1
                        pg.dma('pool', wd[di][:], wdv[:, :, nch * 512:(nch + 1) * 512], w=[('mo_wd', di)])
                        for tl in range(TPB):
                            pb = ipd % 3; ipd += 1
                            tg = tb * TPB + tl
                            def mmd(E, di=di, tl=tl, pb=pb):
                                ins = None
                                for fc in range(8):
                                    ins = E.matmul(ps_d[:, pb, :], hid[:, fc, tl * P:(tl + 1) * P], wd[di][:, fc, :],
                                                   start=(fc == 0), stop=(fc == 7))
                                return ins
                            pg.op('pe', mmd, r=hidt + [('mo_wd', di)], w=[('mo_ps_d', pb)])
                            a_ap = acc[:, tl, nch * 512:(nch + 1) * 512]
                            if e == 0:
                                pg.op('dve', lambda E, a=a_ap, pb=pb, tg=tg, e=e: E.tensor_scalar(a, ps_d[:, pb, :], self.comb[:, tg, e:e + 1], None, ALU.mult),
                                      r=[('mo_ps_d', pb), ('comb', tg)], w=[('mo_acc', tl, nch)])
                            else:
                                pg.op('dve', lambda E, a=a_ap, pb=pb, tg=tg, e=e: E.scalar_tensor_tensor(a, ps_d[:, pb, :], self.comb[:, tg, e:e + 1], a, ALU.mult, ALU.add),
                                      r=[('mo_ps_d', pb), ('comb', tg)], w=[('mo_acc', tl, nch)])
                pg.dma('sp', rv[tb], acc[:], r=[('mo_acc', tl, nch) for tl in range(TPB) for nch in range(4)],
                       w=[('res_dram', tb * TPB + tl) for tl in range(TPB)])


    def route_finalize(self):
        pg = self.pg
        with ExitStack() as st:
            ones = self.sb(st, 'rf_ones', [P, P], BF16)
            ust = self.sb(st, 'rf_ust', [P, P], BF16)
            mE = self.sb(st, 'rf_mE', [P, 512], BF16)
            mS = self.sb(st, 'rf_mS', [P, 512], BF16)
            ecap = self.sb(st, 'rf_ecap', [P, 512], F32)
            thr = self.sb(st, 'rf_thr', [P, 512], F32)
            TOT = self.sb(st, 'rf_TOT', [P, NE, NT], F32)
            INC = self.sb(st, 'rf_INC', [P, NE, NT], F32)
            EXC = self.sb(st, 'rf_EXC', [P, NE, NT], F32)
            G = self.sb(st, 'rf_G', [P, NT, NE], F32)
            CE = self.sb(st, 'rf_CE', [P, NT, NE], F32)
            F1 = self.sb(st, 'rf_F1', [P, NT, NE], F32)
            F2 = self.sb(st, 'rf_F2', [P, NT, NE], F32)
            TMP = self.sb(st, 'rf_TMP', [P, NT, NE], F32)
            Df = self.sb(st, 'rf_Df', [P, 2, NT], F32)
            GT = self.sb(st, 'rf_GT', [P, NE, NT], F32)
            ntf = self.sb(st, 'rf_ntf', [P, NE], F32)
            ps = self.psum(st, 'rf_ps', [P, 2, 512], F32)
            for nm, t_ in (('ones_b', ones), ('ustrict_b', ust), ('maskE', mE), ('maskS', mS), ('ecap', ecap), ('thr', thr)):
                pg.dma('sp', t_[:], self.I('c_' + nm), w=['rf_' + nm])
            selt = [('selm', t) for t in range(NT)]
            sflat = self.selm[:].rearrange("p j e -> p (j e)")
            fl = lambda t_: t_[:].rearrange("p a b -> p (a b)")
            pg.op('pe', lambda E: E.matmul(ps[:, 0, :], ones[:], sflat, start=True, stop=True), r=selt + ['rf_ones_b'], w=['rf_ps0'])
            pg.op('pe', lambda E: E.matmul(ps[:, 1, :], ust[:], sflat, start=True, stop=True), r=selt + ['rf_ustrict_b'], w=['rf_ps1'])
            pg.op('dve', lambda E: E.tensor_copy(TOT[:], ps[:, 0, :].rearrange("p (j e) -> p e j", e=NE)), r=['rf_ps0'], w=['rf_TOT'])
            pg.op('dve', lambda E: E.tensor_tensor_scan(fl(INC), mE[:], fl(TOT), 0.0, ALU.mult, ALU.add), r=['rf_TOT', 'rf_maskE'], w=['rf_INC'])
            pg.op('dve', lambda E: E.tensor_tensor(EXC[:], INC[:], TOT[:], ALU.subtract), r=['rf_INC', 'rf_TOT'], w=['rf_EXC'])
            pg.op('dve', lambda E: E.tensor_tensor(G[:], ps[:, 1, :].rearrange("p (j e) -> p j e", e=NE),
                                                   EXC[:].rearrange("p e j -> p j e"), ALU.add), r=['rf_ps1', 'rf_EXC'], w=['rf_G'])
            pg.op('dve', lambda E: E.tensor_tensor(fl(G), fl(G), ecap[:], ALU.add), r=['rf_G', 'rf_ecap'], w=['rf_G'])
            pg.op('dve', lambda E: E.tensor_tensor_scan(fl(CE), mS[:], sflat, 0.0, ALU.mult, ALU.add), r=selt + ['rf_maskS'], w=['rf_CE'])
            for (F, val, k) in ((F1, 1.0, 0), (F2, 2.0, 1)):
                nm = f'rf_F{k}'
                pg.op('dve', lambda E, F=F, val=val: E.tensor_scalar(fl(F), fl(CE), val, None, ALU.is_equal), r=['rf_CE'], w=[nm])
                pg.op('dve', lambda E, F=F: E.tensor_tensor(fl(F), fl(F), sflat, ALU.mult), r=[nm] + selt, w=[nm])
                pg.op('dve', lambda E, F=F: E.tensor_tensor(TMP[:], F[:], G[:], ALU.mult), r=[nm, 'rf_G'], w=['rf_TMP'])
                pg.op('dve', lambda E, k=k: E.tensor_reduce(Df[:, k, :], TMP[:], AX.X, ALU.add), r=['rf_TMP'], w=[('rf_Df', k)])
                Di = self.D0i if k == 0 else self.D1i
                pg.op('dve', lambda E, k=k, Di=Di: E.tensor_copy(Di[:], Df[:, k, :]), r=[('rf_Df', k)], w=[('Di', k)])
                Wk = self.W0 if k == 0 else self.W1
                pg.op('dve', lambda E, F=F: E.tensor_tensor(TMP[:], F[:], self.comb[:], ALU.mult), r=[nm, 'rf_TMP'] + self.comb_toks, w=['rf_TMP'])
                pg.op('dve', lambda E, Wk=Wk: E.tensor_reduce(Wk[:], TMP[:], AX.X, ALU.add), r=['rf_TMP'], w=[('Wk', k)])
            pg.op('dve', lambda E: E.tensor_tensor(GT[:], INC[:, :, NT - 1:NT].to_broadcast([P, NE, NT]), thr[:].rearrange("p (e j) -> p e j", e=NE), ALU.is_gt),
                  r=['rf_INC', 'rf_thr'], w=['rf_GT'])
            pg.op('dve', lambda E: E.tensor_reduce(ntf[:], GT[:], AX.X, ALU.add), r=['rf_GT'], w=['rf_ntf'])
            pg.op('dve', lambda E: E.tensor_copy(self.nti[:], ntf[:]), r=['rf_ntf'], w=['nti'])

    def scatter_phase(self):
        pg = self.pg
        with ExitStack() as st:
            X = [self.sb(st, f'sc_x{i}', [P, D], BF16) for i in range(3)]
            hv = self.dr['hb_dram'].ap().rearrange("(n p) d -> n p d", p=P)
            bk = self.dr['bucket'].ap()
            for j in range(NT):
                i = j % 3
                pg.dma('sp', X[i][:], hv[j], r=[('hb_dram', j)], w=[('sc_x', i)])
                for Di in (self.D0i, self.D1i):
                    pg.dma('pool', None, None, r=[('sc_x', i), ('Di', 0), ('Di', 1)], w=[],
                           fn=lambda E, i=i, j=j, Di=Di: E.indirect_dma_start(
                               out=bk, out_offset=bass.IndirectOffsetOnAxis(Di[:, j:j + 1], 0), in_=X[i][:], in_offset=None))

    def expert_phase(self, l):
        pg = self.pg
        ENG = ['pe', 'act', 'dve', 'sp']
        with ExitStack() as st:
            Wg2 = [self.sb(st, f'ex_wg{i}', [P, KC, DE], BF16) for i in range(2)]
            Wu2 = [self.sb(st, f'ex_wu{i}', [P, KC, DE], BF16) for i in range(2)]
            Wd = self.sb(st, 'ex_wd', [P, 8, D], BF16)
            Xq = [self.sb(st, f'ex_x{i}', [P, D], BF16) for i in range(2)]
            xT = [self.sb(st, f'ex_xT{i}', [P, KC, P], BF16) for i in range(2)]
            sg = self.sb(st, 'ex_sg', [P, DE], F32)
            hid = [self.sb(st, f'ex_hid{i}', [P, DE], BF16) for i in range(2)]
            hidT = [self.sb(st, f'ex_hidT{i}', [P, 8, P], BF16) for i in range(2)]
            Y = [self.sb(st, f'ex_y{i}', [P, D], BF16) for i in range(2)]
            ps_xf = self.psum(st, 'ex_ps_x', [P, 2, 512], F32)
            ps_x = ps_xf[:].rearrange("p a b -> p (a b)").bitcast(BF16)
            ps_gu = self.psum(st, 'ex_ps_gu', [P, 2, 2, 512], F32)
            ps_h = self.psum(st, 'ex_ps_h', [P, 8 * P], BF16)
            ps_d1 = self.psum(st, 'ex_ps_d', [P, 512], F32)
            bk = self.dr['bucket'].ap()
            yb = self.dr['ybucket'].ap()
            it = 0
            for e in range(NE):
                wgv = self.I('w_gate')[l, e].rearrange("(k p) f -> p k f", p=P)
                wuv = self.I('w_up')[l, e].rearrange("(k p) f -> p k f", p=P)
                wdv = self.I('w_down')[l, e].rearrange("(c p) n -> p c n", p=P)
                wb = e % 2
                Wg, Wu = Wg2[wb], Wu2[wb]
                if e < self.wlim: pg.dma('pool', Wg[:], wgv, w=[('ex_wg', wb, 0), ('ex_wg', wb, 1)])
                if e < self.wlim: pg.dma('pool', Wu[:], wuv, w=[('ex_wu', wb, 0), ('ex_wu', wb, 1)])
                for n_ in range(2 if e < self.wlim else 0):
                    pg.dma('pool', Wd[:, :, n_ * 1024:(n_ + 1) * 1024], wdv[:, :, n_ * 1024:(n_ + 1) * 1024],
                           w=[('ex_wd', 2 * n_), ('ex_wd', 2 * n_ + 1)])
                for en in ENG:
                    pg.op(en, lambda E, en=en, e=e: E.reg_load(self.regs[en], self.nti[0:1, e:e + 1]), r=['nti'])
                nslots = self.qlim
                for q in range(nslots):
                    i = it % 2; it += 1
                    row0 = e * CAP + q * P
                    pg.cond_begin(self.regs, q, ENG)
                    if q == 0:
                        pg.dma('sp', Xq[i][:], bk[row0:row0 + P, :], w=[('ex_x', i)])
                    if q + 1 < nslots:
                        pg.dma('sp', Xq[1 - i][:], bk[row0 + P:row0 + 2 * P, :], w=[('ex_x', 1 - i)])
                    def trx(E, i=i):
                        ins = None
                        for k in range(KC):
                            ins = E.transpose(ps_x[:, k * P:(k + 1) * P], Xq[i][:, k * P:(k + 1) * P], self.ident_b[:])
                        return ins
                    pg.op('pe', trx, r=[('ex_x', i), 'ident_b'], w=[('ex_psb', 0), ('ex_psb', 1)])
                    pg.op('act', lambda E, i=i: E.copy(xT[i][:, 0:8, :], ps_x[:, 0:8 * P].rearrange("p (k t) -> p k t", k=8)),
                          r=[('ex_psb', 0)], w=[('ex_xT', i, 0)])
                    pg.op('dve', lambda E, i=i: E.tensor_copy(xT[i][:, 8:16, :], ps_x[:, 8 * P:16 * P].rearrange("p (k t) -> p k t", k=8)),
                          r=[('ex_psb', 1)], w=[('ex_xT', i, 1)])
                    for fh in range(2):
                        for a, W, wn in ((0, Wg, 'ex_wg'), (1, Wu, 'ex_wu')):
                            def mgu(E, i=i, a=a, W=W, fh=fh):
                                ins = None
                                for k in range(KC):
                                    ins = E.matmul(ps_gu[:, a, fh, :], xT[i][:, k, :], W[:, k, fh * 512:(fh + 1) * 512],
                                                   start=(k == 0), stop=(k == KC - 1))
                                return ins
                            pg.op('pe', mgu, r=[('ex_xT', i, 0), ('ex_xT', i, 1), (wn, wb, fh)], w=[('ex_ps_gu', a, fh)])
                        pg.op('act', lambda E, fh=fh: E.activation(sg[:, fh * 512:(fh + 1) * 512], ps_gu[:, 0, fh, :], AF.Silu),
                              r=[('ex_ps_gu', 0, fh)], w=[('ex_sg', fh)])
                        pg.op('dve', lambda E, fh=fh, i=i: E.tensor_tensor(hid[i][:, fh * 512:(fh + 1) * 512], sg[:, fh * 512:(fh + 1) * 512],
                                                                            ps_gu[:, 1, fh, :], ALU.mult),
                              r=[('ex_sg', fh), ('ex_ps_gu', 1, fh)], w=[('ex_hid', i, fh)])
                    def trh(E, i=i):
                        ins = None
                        for fc in range(8):
                            ins = E.transpose(ps_h[:, fc * P:(fc + 1) * P], hid[i][:, fc * P:(fc + 1) * P], self.ident_b[:])
                        return ins
                    pg.op('pe', trh, r=[('ex_hid', i, 0), ('ex_hid', i, 1), 'ident_b'], w=['ex_ps_h'])
                    pg.op('act', lambda E, i=i: E.copy(hidT[i][:], ps_h[:].rearrange("p (c t) -> p c t", c=8)),
                          r=['ex_ps_h'], w=[('ex_hidT', i)])
                    for n_ in range(4):
                        if n_ % 3 == 2:
                            pd, ptok = ps_d1[:], 'ex_ps_d'
                        else:
                            pd, ptok = ps_xf[:, n_ % 3, :], ('ex_psb', n_ % 3)
                        def mmd(E, i=i, n_=n_, pd=pd):
                            ins = None
                            for fc in range(8):
                                ins = E.matmul(pd, hidT[i][:, fc, :], Wd[:, fc, n_ * 512:(n_ + 1) * 512],
                                               start=(fc == 0), stop=(fc == 7))
                            return ins
                        pg.op('pe', mmd, r=[('ex_hidT', i), ('ex_wd', n_)], w=[ptok])
                        o_ap = Y[i][:, n_ * 512:(n_ + 1) * 512]
                        if n_ % 2 == 0:
                            pg.op('act', lambda E, o=o_ap, pd=pd: E.copy(o, pd), r=[ptok], w=[('ex_y', i, n_)])
                        else:
                            pg.op('dve', lambda E, o=o_ap, pd=pd: E.tensor_copy(o, pd), r=[ptok], w=[('ex_y', i, n_)])
                    pg.dma('sp', yb[row0:row0 + P, :], Y[i][:], r=[('ex_y', i, n_) for n_ in range(4)], w=[])
                for q in range(nslots):
                    pg.cond_end()

    def gather_phase(self):
        pg = self.pg
        with ExitStack() as st:
            Y0 = [self.sb(st, f'ga_y0{i}', [P, D], BF16) for i in range(2)]
            Y1 = [self.sb(st, f'ga_y1{i}', [P, D], BF16) for i in range(2)]
            T = [self.sb(st, f'ga_t{i}', [P, D], F32) for i in range(2)]
            yb = self.dr['ybucket'].ap()
            rv = self.dr['res_dram'].ap().rearrange("(n p) d -> n p d", p=P)
            for j in range(NT):
                i = j % 2
                for (Yk, Di, nm) in ((Y0[i], self.D0i, 'ga_y0'), (Y1[i], self.D1i, 'ga_y1')):
                    pg.dma('pool', None, None, r=[('Di', 0), ('Di', 1)], w=[(nm, i)],
                           fn=lambda E, Yk=Yk, Di=Di, j=j: E.indirect_dma_start(
                               out=Yk[:], out_offset=None, in_=yb, in_offset=bass.IndirectOffsetOnAxis(Di[:, j:j + 1], 0)))
                pg.op('dve', lambda E, i=i, j=j: E.tensor_scalar(T[i][:], Y0[i][:], self.W0[:, j:j + 1], None, ALU.mult),
                      r=[('ga_y0', i), ('Wk', 0)], w=[('ga_t', i)])
                pg.op('dve', lambda E, i=i, j=j: E.scalar_tensor_tensor(T[i][:], Y1[i][:], self.W1[:, j:j + 1], T[i][:], ALU.mult, ALU.add),
                      r=[('ga_y1', i), ('Wk', 1), ('ga_t', i)], w=[('ga_t', i)])
                pg.dma('sp', rv[j], T[i][:], r=[('ga_t', i)], w=[('res_dram', j)])


_CACHE = {}


def make_in_map(inputs, core, b):
    m = {}
    for k in b.dr:
        if k.startswith('c_'):
            m[k] = b.consts[k[2:]]
        elif k in inputs:
            v = np.asarray(inputs[k])
            m[k] = np.ascontiguousarray(v[core]) if k == 'x' else np.ascontiguousarray(v)
    return m


def kernel(**inputs):
    b = Builder()
    nc = b.build()
    in_maps = [make_in_map(inputs, c, b) for c in range(8)]
    res = run_bass_kernel_spmd(nc, in_maps, core_ids=list(range(8)))
    return np.stack([np.asarray(r['out']) for r in res.results], axis=0).astype(np.float32)
```

```python
import numpy as np
import ml_dtypes
from contextlib import ExitStack
import concourse.bass as bass
import concourse.mybir as mybir
from concourse.bass_utils import run_bass_kernel_spmd

F32 = mybir.dt.float32
BF16 = mybir.dt.bfloat16
AF = mybir.ActivationFunctionType
ALU = mybir.AluOpType
AX = mybir.AxisListType

P = 128
S = 4096
D = 2048
NT = S // P
KC = D // P
DEPTH = 2
D_IN = 6912
NE = 16
DE = 1024
ALPHA = (2.0 * DEPTH) ** 0.25
LN_EPS = 1e-5
HN_EPS = 1e-6
NEG = -30000.0
CAP = 4096

C_AQ, C_AK, C_AV = 0, 512, 640
C_CB, C_CC, C_CH = 768, 1280, 1792
C_GQ, C_GZF, C_GZB, C_GI, C_GO = 2304, 2816, 3328, 3840, 4352
C_RQ, C_RK, C_RV, C_RG = 4864, 5376, 5888, 6400


class Prog:
    EPOCH = 30000

    def __init__(self, nc, es, n_dma_sems=12):
        self.nc = nc
        self.es = es
        self.E = {'pe': nc.tensor, 'act': nc.scalar, 'dve': nc.vector,
                  'pool': nc.gpsimd, 'sp': nc.sync}
        self.nsem = 0
        self.sem = {e: self._new_sem(e) for e in self.E}
        self.cnt = {e: 0 for e in self.E}
        self.known = {e: {} for e in self.E}
        self.semobj = {}
        self.tokw = {}
        self.tokr = {}
        self.dq = {}
        self._sem_owner = {}
        self._cstack = []
        for q in ('sp', 'pool', 'act'):
            self.dq[q] = {'sems': [self._new_sem('d' + q) for _ in range(n_dma_sems)],
                          'rr': 0}
            for s_ in self.dq[q]['sems']:
                self._sem_owner[id(s_)] = q
        self.dtarget = {}
        self.all_sems = {}
        self.n_ops = 0
        self.n_waits = 0

    def _new_sem(self, tag):
        self.nsem += 1
        s = self.es.enter_context(self.nc.semaphore(f"s_{tag}_{self.nsem}"))
        return s

    def _key(self, s):
        return id(s)

    def _collect(self, eng, reads, writes):
        need = {}
        def addh(h):
            k = self._key(h[0])
            if k not in need or need[k][1] < h[1]:
                need[k] = h
        for t in reads:
            for h in self.tokw.get(t, ()):
                addh(h)
        for t in writes:
            for h in self.tokw.get(t, ()):
                addh(h)
            for h in self.tokr.get(t, ()):
                addh(h)
        return need

    def _emit_waits(self, eng, need, skip_own_pe=True):
        kn = self.known[eng]
        acts = []
        for k, (s, v, src) in need.items():
            if eng == 'pe' and src == 'pe':
                continue
            if kn.get(k, 0) >= v:
                continue
            acts.append(('wait', s, v))
            kn[k] = v
            self.n_waits += 1
        return acts

    def _run(self, eng, acts):
        if self._cstack:
            assert eng in self._cstack[-1]['engines'], eng
            self._cstack[-1]['buf'][eng].extend(acts)
            return
        self._do(eng, acts)

    def _do(self, eng, acts):
        E = self.E[eng]
        for a in acts:
            if a[0] == 'wait':
                E.wait_ge(a[1], a[2])
            elif a[0] == 'ins':
                a[1](E).then_inc(a[2], a[3])
            elif a[0] == 'seminc':
                E.sem_inc(a[1], a[2])
            else:
                with E.If_cmp(a[1], a[2], "IS_GT"):
                    self._do(eng, a[3])
                with E.Else():
                    self._do(eng, a[4])

    def _update(self, h, reads, writes):
        k = self._key(h[0])
        for t in writes:
            self.tokw[t] = [h]
            self.tokr[t] = []
        for t in reads:
            if t in writes:
                continue
            lst = self.tokr.get(t)
            if lst is None:
                self.tokr[t] = [h]
            else:
                self.tokr[t] = [x for x in lst if self._key(x[0]) != k] + [h]

    def op(self, eng, fn, r=(), w=()):
        need = self._collect(eng, r, w)
        acts = self._emit_waits(eng, need)
        if self.cnt[eng] >= self.EPOCH:
            assert not self._cstack
            self.sem[eng] = self._new_sem(eng)
            self.cnt[eng] = 0
        self.cnt[eng] += 1
        acts.append(('ins', fn, self.sem[eng], 1))
        self._run(eng, acts)
        h = (self.sem[eng], self.cnt[eng], eng)
        self._update(h, r, w)
        self.n_ops += 1
        return h

    def dma(self, q, out, in_, r=(), w=(), fn=None, **kw):
        dq = self.dq[q]
        s = dq['sems'][dq['rr'] % len(dq['sems'])]
        dq['rr'] += 1
        prev = self.dtarget.get(self._key(s), 0)
        need = self._collect(q, r, w)
        if prev > 0:
            k = self._key(s)
            if k not in need or need[k][1] < prev:
                need[k] = (s, prev, 'dma')
        acts = self._emit_waits(q, need)
        tgt = prev + 16
        self.dtarget[self._key(s)] = tgt
        self.all_sems[self._key(s)] = s
        if fn is None:
            fn = lambda E, out=out, in_=in_, kw=kw: E.dma_start(out=out, in_=in_, **kw)
        acts.append(('ins', fn, s, 16))
        self._run(q, acts)
        h = (s, tgt, 'dma')
        self._update(h, r, w)
        self.n_ops += 1
        return h

    def cond_begin(self, regs, thresh, engines):
        for e in engines:
            assert self.cnt[e] < self.EPOCH - 4000 or self._cstack
        self._cstack.append({'engines': engines, 'regs': regs, 'thresh': thresh,
                             'cnt0': {e: (self.sem[e], self.cnt[e]) for e in engines},
                             'dt0': dict(self.dtarget),
                             'known0': {e: dict(self.known[e]) for e in self.E},
                             'buf': {e: [] for e in engines}})

    def cond_end(self, dma_issuers=('sp',)):
        c = self._cstack.pop()
        for e in c['engines']:
            s0, c0 = c['cnt0'][e]
            assert s0 is self.sem[e]
            n = self.cnt[e] - c0
            els = []
            if n > 0:
                if c0 > 0:
                    els.append(('wait', s0, c0))
                els.append(('seminc', s0, n))
            if e in dma_issuers:
                for k, tgt in self.dtarget.items():
                    t0 = c['dt0'].get(k, 0)
                    if tgt > t0 and self._sem_owner.get(k) == e:
                        if t0 > 0:
                            els.append(('wait', self.all_sems[k], t0))
                        els.append(('seminc', self.all_sems[k], tgt - t0))
            if not c['buf'][e] and not els:
                continue
            act = ('cond', c['regs'][e], c['thresh'], c['buf'][e], els)
            if self._cstack:
                self._cstack[-1]['buf'][e].append(act)
            else:
                self._do(e, [act])
        for e in self.E:
            self.known[e] = c['known0'][e]

    def barrier(self, engines=None, keep=()):
        engines = engines or list(self.E)
        need = {}
        for e in self.E:
            if self.cnt[e] > 0:
                need[self._key(self.sem[e])] = (self.sem[e], self.cnt[e], e)
        for k, s in self.all_sems.items():
            need[k] = (s, self.dtarget[k], 'dma')
        for e in engines:
            E = self.E[e]
            kn = self.known[e]
            for k, (s, v, src) in need.items():
                if src == e and e != 'sp':
                    pass
                if kn.get(k, 0) >= v:
                    continue
                E.wait_ge(s, v)
                kn[k] = v
        self.tokw.clear()
        self.tokr.clear()


def host_consts():
    c = {}
    c['ident_f'] = np.eye(P, dtype=np.float32)
    c['ident_b'] = np.eye(P, dtype=np.float32).astype(ml_dtypes.bfloat16)
    ab = np.zeros((P, 8, 3, P), np.float32)
    s_i = np.arange(P)[:, None]
    t_i = np.arange(P)[None, :]
    for h in range(8):
        slope = 2.0 ** (-(h + 1))
        for j in range(3):
            dist = (j - 1) * P + s_i - t_i
            ab[:, h, j, :] = np.where(np.abs(dist) <= 128, -slope * np.abs(dist), NEG)
    c['attn_bias'] = ab.reshape(P, 8 * 3 * P)
    rc = np.zeros((P, 5, P), np.float32)
    rc[:, 0] = np.maximum(t_i - s_i, 0)
    rc[:, 1] = np.maximum(s_i - t_i, 0)
    rc[:, 2] = (t_i > s_i)
    rc[:, 3] = (s_i > t_i)
    rc[:, 4] = 2.0 * np.eye(P)
    c['ret_c'] = rc.reshape(P, 5 * P)
    rv = np.zeros((P, 2, P), np.float32)
    rv[:, 0] = t_i + 1.0
    rv[:, 1] = 128.0 - t_i
    c['ret_vec'] = rv.reshape(P, 2 * P)
    cmk = np.ones((P, S), np.float32)
    cmk[:, ::64] = 0.0
    c['hg_cm'] = cmk.astype(ml_dtypes.bfloat16)
    s6 = np.arange(64)[:, None]
    t6 = np.arange(64)[None, :]
    hm = np.zeros((64, 2, 64), np.float32)
    hm[:, 0] = (s6 <= t6)
    hm[:, 1] = (s6 >= t6)
    c['hg_mask'] = hm.reshape(64, 128)
    c['ones_b'] = np.ones((P, P), np.float32).astype(ml_dtypes.bfloat16)
    c['ustrict_b'] = (s_i < t_i).astype(np.float32).astype(ml_dtypes.bfloat16)
    mE = np.ones((P, NE, NT), np.float32); mE[:, :, 0] = 0.0
    c['maskE'] = mE.reshape(P, NE * NT).astype(ml_dtypes.bfloat16)
    mS = np.ones((P, NT, NE), np.float32); mS[:, :, 0] = 0.0
    c['maskS'] = mS.reshape(P, NT * NE).astype(ml_dtypes.bfloat16)
    ec = np.zeros((P, NT, NE), np.float32); ec[:, :, :] = (np.arange(NE) * CAP)[None, None, :]
    c['ecap'] = ec.reshape(P, NT * NE)
    th_ = np.zeros((P, NE, NT), np.float32); th_[:, :, :] = (np.arange(NT) * 128.0)[None, None, :]
    c['thr'] = th_.reshape(P, NE * NT)
    c['ret_pcol'] = np.stack([127.0 - np.arange(P), np.arange(P) * 1.0], 1).astype(np.float32)
    return c


class Builder:
    def __init__(self, n_layers=DEPTH, stop=None, taps=(), skip=()):
        self.skip = set(skip)
        self.dense_moe = 'dense' in self.skip
        self.wlim = 2 if 'wlim' in self.skip else 99
        self.qlim = 8 if 'qlim' in self.skip else NT
        self.n_layers = n_layers
        self.stop = stop
        self.taps = set(taps)
        self.nc = bass.Bass("TRN2", target_bir_lowering=False)
        self.es = ExitStack()
        self.pg = Prog(self.nc, self.es)
        self.dr = {}
        self.consts = host_consts()

    def din(self, name, shape, dtype=F32):
        t = self.nc.dram_tensor(name, list(shape), dtype, kind="ExternalInput")
        self.dr[name] = t
        return t

    def dscr(self, name, shape, dtype):
        kind = "ExternalOutput" if name in self.taps else "Internal"
        t = self.nc.dram_tensor(name, list(shape), dtype, kind=kind)
        self.dr[name] = t
        return t

    def sb(self, st, name, shape, dtype):
        self._uid = getattr(self, '_uid', 0) + 1
        return st.enter_context(self.nc.sbuf_tensor(f"{name}_u{self._uid}", list(shape), dtype))

    def psum(self, st, name, shape, dtype=F32):
        self._uid = getattr(self, '_uid', 0) + 1
        return st.enter_context(self.nc.psum_tensor(f"{name}_u{self._uid}", list(shape), dtype))

    IN_SHAPES = {
        'x': [S, D], 'emb_ln_g': [D], 'emb_ln_b': [D], 'w_in': [DEPTH, D, D_IN],
        'attn_sink': [DEPTH, 8], 'conv_w': [DEPTH, 3, 512], 'hgrn_lb': [DEPTH, 512],
        'hgrn_norm_g': [DEPTH, 512], 'ret_decay_logit': [DEPTH, 2, 4], 'ret_norm_g': [DEPTH, 512],
        'w_out': [DEPTH, D, D], 'ln1_g': [DEPTH, D], 'ln1_b': [DEPTH, D],
        'router_w': [D, NE], 'router_b': [NE], 'w_gate': [DEPTH, NE, D, DE],
        'w_up': [DEPTH, NE, D, DE], 'w_down': [DEPTH, NE, DE, D],
        'ln2_g': [DEPTH, D], 'ln2_b': [DEPTH, D],
    }

    def I(self, name):
        if name not in self.dr:
            if name.startswith('c_'):
                v = self.consts[name[2:]]
                self.din(name, v.shape, BF16 if v.dtype == ml_dtypes.bfloat16 else F32)
            else:
                self.din(name, self.IN_SHAPES[name])
        return self.dr[name].ap()

    def declare(self):
        nc = self.nc
        self.out = nc.dram_tensor('out', [S, D], F32, kind="ExternalOutput")
        self.dscr('h_dram', [S, D], F32)
        self.dscr('hT_dram', [D, S], BF16)
        self.declare_proj()
        self.dscr('yT_dram', [D, S], BF16)
        self.dscr('res_dram', [S, D], F32)
        self.dscr('hb_dram', [S, D], BF16)
        self.dscr('bucket', [NE * CAP, D], BF16)
        self.dscr('ybucket', [NE * CAP, D], BF16)
        for d_ in range(2):
            self.dscr(f'hg_q{d_}', [512, S], BF16)
            self.dscr(f'hg_k{d_}', [512, S], BF16)
            self.dscr(f'hg_S{d_}', [S // 64, P, 512], BF16)

    def build(self):
        self.declare()
        nc, pg = self.nc, self.pg
        with ExitStack() as g:
            self.g = g
            self.ident_f = self.sb(g, 'ident_f', [P, P], F32)
            self.ident_b = self.sb(g, 'ident_b', [P, P], BF16)
            self.comb = self.sb(g, 'comb', [P, NT, NE], F32)
            self.comb_toks = [('comb', t) for t in range(NT)]
            I32 = mybir.dt.int32
            self.selm = self.sb(g, 'selm', [P, NT, NE], BF16)
            self.D0i = self.sb(g, 'D0i', [P, NT], I32)
            self.D1i = self.sb(g, 'D1i', [P, NT], I32)
            self.W0 = self.sb(g, 'W0', [P, NT], F32)
            self.W1 = self.sb(g, 'W1', [P, NT], F32)
            self.nti = self.sb(g, 'nti', [P, NE], I32)
            self.regs = {e: pg.E[e].alloc_register('r_nt_' + e) for e in ('pe', 'act', 'dve', 'sp')}
            pg.dma('sp', self.ident_f[:], self.I('c_ident_f'), w=['ident_f'])
            pg.dma('sp', self.ident_b[:], self.I('c_ident_b'), w=['ident_b'])
            self.ln_phase(src='x', g_ap=self.I('emb_ln_g'), b_ap=self.I('emb_ln_b'),
                          mode='x')
            pg.barrier()
            for l in range(self.n_layers):
                if self.stop == 'ln0':
                    break
                self.in_proj_phase(l)
                pg.barrier()
                if self.stop == 'inproj':
                    break
                if 'attn' not in self.skip:
                    self.attn_phase(l)
                    pg.barrier()
                if self.stop == 'attn':
                    break
                if 'conv' not in self.skip:
                    self.conv_phase(l)
                    pg.barrier()
                if 'ret' not in self.skip:
                    self.ret_phase(l)
                    pg.barrier()
                if self.stop in ('conv', 'ret'):
                    break
                if 'hgrn' not in self.skip:
                    self.hgrn_phase(l)
                    pg.barrier()
                if self.stop in ('hgrn', 'mix'):
                    break
                self.outproj_phase(l)
                pg.barrier()
                if self.stop == 'outproj':
                    break
                self.ln_phase(None, self.I('ln1_g')[l], self.I('ln1_b')[l], 'res', router=('norouter' not in self.skip))
                pg.barrier(keep=self.comb_toks)
                if self.stop == 'ln1':
                    break
                if self.dense_moe:
                    self.moe_phase(l)
                    pg.barrier()
                else:
                    self.route_finalize()
                    pg.barrier()
                    if self.stop == 'route':
                        break
                    self.scatter_phase()
                    pg.barrier()
                    if self.stop == 'scatter':
                        break
                    self.expert_phase(l)
                    pg.barrier()
                    if self.stop == 'expert':
                        break
                    self.gather_phase()
                    pg.barrier()
                    if self.stop == 'gather':
                        break
                last = (l == self.n_layers - 1)
                self.ln_phase(None, self.I('ln2_g')[l], self.I('ln2_b')[l], 'res', final=last)
                pg.barrier()
        self.es.close()
        return nc

    def ln_phase(self, src, g_ap, b_ap, mode, final=False, router=False):
        nc, pg = self.nc, self.pg
        with ExitStack() as st:
            gt = self.sb(st, 'ln_g', [P, D], F32)
            bt = self.sb(st, 'ln_b', [P, D], F32)
            pg.dma('sp', gt[:], g_ap.partition_broadcast(P), w=['ln_g'])
            pg.dma('sp', bt[:], b_ap.partition_broadcast(P), w=['ln_b'])
            NB = 3
            xt = [self.sb(st, f'ln_x{i}', [P, D], F32) for i in range(NB)]
            x2 = [self.sb(st, f'ln_r{i}', [P, D], F32) for i in range(NB)] if mode == 'res' else None
            hn = [self.sb(st, f'ln_h{i}', [P, D], F32) for i in range(NB)]
            stt = [self.sb(st, f'ln_st{i}', [P, 4, 6], F32) for i in range(NB)]
            mv = [self.sb(st, f'ln_mv{i}', [P, 4], F32) for i in range(NB)]
            hst = [self.sb(st, f'ln_hst{i}', [P, KC, 512], BF16) for i in range(2)]
            ps = self.psum(st, 'ln_ps', [P, 4 * 512], F32)
            if router:
                hTf = self.sb(st, 'ln_loT', [P, KC, P], BF16)
                Hb = self.sb(st, 'ln_Hb', [P, D], BF16)
                Lo = self.sb(st, 'ln_Lo', [P, D], F32)
                rw = self.sb(st, 'ln_rw', [P, KC, NE], F32)
                rwh = self.sb(st, 'ln_rwh', [P, KC, NE], BF16)
                rwl = self.sb(st, 'ln_rwl', [P, KC, NE], BF16)
                rb = self.sb(st, 'ln_rb', [P, NE], F32)
                rs = self.sb(st, 'ln_rs', [P, 8, NE], F32)
                ps_r = self.psum(st, 'ln_ps_r', [P, 512], F32)
                pg.dma('sp', rw[:], self.I('router_w').rearrange("(k p) e -> p k e", p=P), w=['ln_rw'])
                pg.dma('sp', rb[:], self.I('router_b').partition_broadcast(P), w=['ln_rb'])
                pg.op('act', lambda E: E.copy(rwh[:], rw[:]), r=['ln_rw'], w=['ln_rwh'])
                pg.op('dve', lambda E: E.tensor_tensor(rwl[:], rw[:], rwh[:], ALU.subtract), r=['ln_rw', 'ln_rwh'], w=['ln_rwl'])
            if mode == 'x':
                srcv = self.I(src).rearrange("(n p) d -> n p d", p=P)
            else:
                resv = self.dr['res_dram'].ap().rearrange("(n p) d -> n p d", p=P)
            hdv = self.dr['h_dram'].ap().rearrange("(n p) d -> n p d", p=P)
            outv = self.out.ap().rearrange("(n p) d -> n p d", p=P)
            hTv = self.dr['hT_dram'].ap().rearrange("(k p) t -> p k t", p=P)
            def issue_loads(t):
                i = t % NB
                if mode == 'x':
                    pg.dma('sp', xt[i][:], srcv[t], w=[f'ln_x{i}'])
                else:
                    pg.dma('sp', xt[i][:], hdv[t], r=[('h_dram', t)], w=[f'ln_x{i}'])
                    pg.dma('sp', x2[i][:], resv[t], r=[('res_dram', t)], w=[f'ln_r{i}'])
            issue_loads(0)
            for t in range(NT):
                i = t % NB
                X, H, ST, MV = xt[i], hn[i], stt[i], mv[i]
                tx, th = f'ln_x{i}', f'ln_h{i}'
                if t + 1 < NT:
                    issue_loads(t + 1)
                if mode != 'x':
                    pg.op('dve', lambda E, X=X, R=x2[i]: E.scalar_tensor_tensor(X[:], X[:], ALPHA, R[:], ALU.mult, ALU.add),
                          r=[tx, f'ln_r{i}'], w=[tx])
                self.ln_tile(X, H, ST, MV, gt, bt, tx, th, f'ln_s{i}')
                if final:
                    pg.dma('sp', outv[t], H[:], r=[th], w=[('out', t)])
                    continue
                pg.dma('sp', hdv[t], H[:], r=[th], w=[('h_dram', t)])
                slot = t % 4
                hb = (t // 4) % 2
                HS = hst[hb]
                for half in range(4):
                    def tr(E, half=half, H=H):
                        ins = None
                        for j in range(4):
                            k = half * 4 + j
                            ins = E.transpose(ps[:, half * 512 + j * P: half * 512 + (j + 1) * P],
                                              H[:, k * P:(k + 1) * P], self.ident_f[:])
                        return ins
                    pg.op('pe', tr, r=[th, 'ident_f'], w=[('ln_ps', half)])
                    o_ap = HS[:, half * 4:(half + 1) * 4, slot * P:(slot + 1) * P]
                    i_ap = ps[:, half * 512:(half + 1) * 512].rearrange("p (a b) -> p a b", a=4)
                    if half % 2 == 0:
                        pg.op('act', lambda E, o=o_ap, a=i_ap: E.copy(o, a),
                              r=[('ln_ps', half)], w=[('ln_hst', hb, half, slot)])
                    else:
                        pg.op('dve', lambda E, o=o_ap, a=i_ap: E.tensor_copy(o, a),
                              r=[('ln_ps', half)], w=[('ln_hst', hb, half, slot)])
                if slot == 3:
                    tb = t // 4
                    pg.dma('sp', hTv[:, :, tb * 512:(tb + 1) * 512], HS[:],
                           r=[('ln_hst', hb, hf, sl) for hf in range(4) for sl in range(4)],
                           w=[('hT_dram', tb)])
                if router:
                    pg.op('act', lambda E, H=H: E.copy(Hb[:], H[:]), r=[th], w=['ln_Hb'])
                    pg.dma('sp', self.dr['hb_dram'].ap().rearrange("(n p) d -> n p d", p=P)[t], Hb[:], r=['ln_Hb'], w=[('hb_dram', t)])
                    pg.op('dve', lambda E, H=H: E.tensor_tensor(Lo[:], H[:], Hb[:], ALU.subtract), r=[th, 'ln_Hb'], w=['ln_Lo'])
                    for half in range(4):
                        def tr2(E, half=half):
                            ins = None
                            for j in range(4):
                                k = half * 4 + j
                                ins = E.transpose(ps[:, half * 512 + j * P: half * 512 + (j + 1) * P],
                                                  Lo[:, k * P:(k + 1) * P], self.ident_f[:])
                            return ins
                        pg.op('pe', tr2, r=['ln_Lo', 'ident_f'], w=[('ln_ps', half)])
                        o2 = hTf[:, half * 4:(half + 1) * 4, :]
                        i_ap = ps[:, half * 512:(half + 1) * 512].rearrange("p (a b) -> p a b", a=4)
                        if half % 2 == 1:
                            pg.op('act', lambda E, o=o2, a=i_ap: E.copy(o, a), r=[('ln_ps', half)], w=[('ln_hTf', half)])
                        else:
                            pg.op('dve', lambda E, o=o2, a=i_ap: E.tensor_copy(o, a), r=[('ln_ps', half)], w=[('ln_hTf', half)])
                    hiT = HS[:, :, slot * P:(slot + 1) * P]
                    hit = [('ln_hst', hb, hf, slot) for hf in range(4)]
                    self.route_tile(t, hTf, hiT, hit, rwh, rwl, rb, rs, ps_r)

    def route_tile(self, t, loT, hiT, hit, rwh, rwl, rb, rs, ps_r):
        pg = self.pg
        def mm(E):
            ins = None
            for k in range(KC):
                E.matmul(ps_r[:, 0:NE], hiT[:, k, :], rwh[:, k, :], start=(k == 0), stop=False)
                E.matmul(ps_r[:, 0:NE], loT[:, k, :], rwh[:, k, :], start=False, stop=False)
                ins = E.matmul(ps_r[:, 0:NE], hiT[:, k, :], rwl[:, k, :], start=False, stop=(k == KC - 1))
            return ins
        pg.op('pe', mm, r=[('ln_hTf', hf) for hf in range(4)] + hit + ['ln_rwh', 'ln_rwl'], w=['ln_ps_r'])
        lg, ex, eq, ex2, sel = [rs[:, i, :] for i in range(5)]
        sm = rs[:, 5, :]
        gmk = rs[:, 6, 0:4]
        g3 = lambda ap: ap.rearrange("p (g e) -> p g e", e=4)
        bc = lambda ap: ap.unsqueeze(2).to_broadcast([P, 4, 4])
        T = 'rt'
        pg.op('dve', lambda E: E.tensor_tensor(lg, ps_r[:, 0:NE], rb[:], ALU.add), r=['ln_ps_r', 'ln_rb'], w=[T])
        pg.op('dve', lambda E: E.tensor_reduce(sm[:, 12:13], lg, AX.X, ALU.max), r=[T], w=[T])
        pg.op('dve', lambda E: E.tensor_scalar(sm[:, 12:13], sm[:, 12:13], -1.0, None, ALU.mult), r=[T], w=[T])
        pg.op('act', lambda E: E.activation(ex, lg, AF.Exp, bias=sm[:, 12:13], scale=1.0), r=[T], w=[T])
        pg.op('dve', lambda E: E.tensor_reduce(sm[:, 0:4], g3(ex), AX.X, ALU.max), r=[T], w=[T])
        pg.op('dve', lambda E: E.tensor_tensor(g3(eq), g3(ex), bc(sm[:, 0:4]), ALU.is_equal), r=[T], w=[T])
        pg.op('dve', lambda E: E.scalar_tensor_tensor(ex2, eq, -4.0, ex, ALU.mult, ALU.add), r=[T], w=[T])
        pg.op('dve', lambda E: E.tensor_reduce(sm[:, 4:8], g3(ex2), AX.X, ALU.max), r=[T], w=[T])
        pg.op('dve', lambda E: E.tensor_tensor(sm[:, 8:12], sm[:, 0:4], sm[:, 4:8], ALU.add), r=[T], w=[T])
        pg.op('dve', lambda E: E.tensor_reduce(sm[:, 13:14], sm[:, 8:12], AX.X, ALU.max), r=[T], w=[T])
        pg.op('dve', lambda E: E.tensor_scalar(gmk, sm[:, 8:12], sm[:, 13:14], None, ALU.is_equal), r=[T], w=[T])
        pg.op('dve', lambda E: E.tensor_tensor(g3(sel), g3(ex), bc(sm[:, 4:8]), ALU.is_ge), r=[T], w=[T])
        pg.op('dve', lambda E: E.tensor_tensor(g3(sel), g3(sel), bc(gmk), ALU.mult), r=[T], w=[T])
        pg.op('dve', lambda E: E.tensor_copy(self.selm[:, t, :], sel), r=[T], w=[('selm', t)])
        pg.op('dve', lambda E: E.tensor_tensor(sel, sel, ex, ALU.mult), r=[T], w=[T])
        pg.op('dve', lambda E: E.reciprocal(sm[:, 14:15], sm[:, 13:14]), r=[T], w=[T])
        pg.op('dve', lambda E: E.tensor_scalar(self.comb[:, t, :], sel, sm[:, 14:15], None, ALU.mult), r=[T], w=[('comb', t)])

    def ln_tile(self, X, H, ST, MV, gt, bt, tx, th, ts):
        pg = self.pg
        for j in range(4):
            pg.op('dve', lambda E, j=j: E.bn_stats(ST[:, j, :], X[:, j * 512:(j + 1) * 512]),
                  r=[tx], w=[(ts, 'st', j)])
        pg.op('dve', lambda E: E.bn_aggr(MV[:, 0:2], ST[:].rearrange("p a b -> p (a b)")),
              r=[(ts, 'st', j) for j in range(4)], w=[(ts, 'mv')])
        pg.op('act', lambda E: E.activation(MV[:, 2:3], MV[:, 1:2], AF.Ln, bias=LN_EPS, scale=1.0),
              r=[(ts, 'mv')], w=[(ts, 'lnv')])
        pg.op('act', lambda E: E.activation(MV[:, 3:4], MV[:, 2:3], AF.Exp, scale=-0.5),
              r=[(ts, 'lnv')], w=[(ts, 'rstd')])
        pg.op('dve', lambda E: E.scalar_tensor_tensor(H[:], X[:], MV[:, 0:1], gt[:], ALU.subtract, ALU.mult),
              r=[tx, (ts, 'mv'), 'ln_g'], w=[th])
        pg.op('dve', lambda E: E.scalar_tensor_tensor(H[:], H[:], MV[:, 3:4], bt[:], ALU.mult, ALU.add),
              r=[th, (ts, 'rstd'), 'ln_b'], w=[th])


    PF_SPECS = [('aq', C_AQ, 512), ('akd', None, 256), ('cb', C_CB, 512), ('cc', C_CC, 512),
                ('ch', C_CH, 512), ('gq', C_GQ, 512), ('gzf', C_GZF, 512), ('gzb', C_GZB, 512),
                ('rq', C_RQ, 512), ('rk', C_RK, 512)]
    PT_SPECS = [('av', C_AV, 128), ('gi', C_GI, 512), ('go', C_GO, 512), ('rkt', C_RK, 512),
                ('rv', C_RV, 512), ('rg', C_RG, 512)]

    def declare_proj(self):
        for n, _, w in self.PF_SPECS:
            self.dscr('pf_' + n, [w, S], BF16)
        for n, _, w in self.PT_SPECS:
            self.dscr('pt_' + n, [S, w], BF16)

    def load_hT(self, st):
        hT = self.sb(st, 'hT_bf', [P, KC, S], BF16)
        hTv = self.dr['hT_dram'].ap().rearrange("(k p) t -> p k t", p=P)
        for k in range(KC):
            self.pg.dma('sp', hT[:, k, :], hTv[:, k, :], r=[('hT_dram', tb) for tb in range(8)],
                        w=[('hT_bf', k)])
        return hT

    def in_proj_phase(self, l):
        nc, pg = self.nc, self.pg
        with ExitStack() as st:
            hT = self.load_hT(st)
            hT_toks = [('hT_bf', k) for k in range(KC)]
            wt = [self.sb(st, f'ip_w{i}', [P, KC, 512], BF16) for i in range(2)]
            stF = [self.sb(st, f'ip_sf{i}', [P, S], BF16) for i in range(2)]
            stT = [self.sb(st, f'ip_st{i}', [P, 4, 512], BF16) for i in range(2)]
            ps = self.psum(st, 'ip_ps', [P, 8 * 512], F32)
            w_in = self.I('w_in')[l].rearrange("(k p) n -> p k n", p=P)
            gi = 0
            bank = 0
            ev = 0
            nsf = 0
            nst = 0
            for (name, c0, width) in self.PF_SPECS:
                W = wt[gi % 2]; wtok = f'ip_w{gi % 2}'; gi += 1
                if name == 'akd':
                    for j, cc in enumerate([C_AK, C_AK, C_AK + 64, C_AK + 64]):
                        pg.dma('pool', W[:, :, j * 64:(j + 1) * 64], w_in[:, :, cc:cc + 64],
                               w=[(wtok, j)])
                    wtoks = [(wtok, j) for j in range(4)]
                else:
                    pg.dma('pool', W[:, :, 0:width], w_in[:, :, c0:c0 + width], w=[(wtok, 0)])
                    wtoks = [(wtok, 0)]
                dst = self.dr['pf_' + name].ap()
                for j in range(width // P):
                    SF = stF[nsf % 2]; sftok = f'ip_sf{nsf % 2}'; nsf += 1
                    for tb in range(8):
                        b = bank % 8; bank += 1
                        def mm(E, W=W, j=j, tb=tb, b=b):
                            ins = None
                            for k in range(KC):
                                ins = E.matmul(ps[:, b * 512:(b + 1) * 512], W[:, k, j * P:(j + 1) * P],
                                               hT[:, k, tb * 512:(tb + 1) * 512],
                                               start=(k == 0), stop=(k == KC - 1))
                            return ins
                        pg.op('pe', mm, r=wtoks + hT_toks, w=[('ip_ps', b)])
                        o_ap = SF[:, tb * 512:(tb + 1) * 512]
                        i_ap = ps[:, b * 512:(b + 1) * 512]
                        if ev % 2 == 0:
                            pg.op('act', lambda E, o=o_ap, a=i_ap: E.copy(o, a),
                                  r=[('ip_ps', b)], w=[(sftok, tb)])
                        else:
                            pg.op('dve', lambda E, o=o_ap, a=i_ap: E.tensor_copy(o, a),
                                  r=[('ip_ps', b)], w=[(sftok, tb)])
                        ev += 1
                    pg.dma('sp', dst[j * P:(j + 1) * P, :], SF[:],
                           r=[(sftok, tb) for tb in range(8)], w=[('pf_' + name, j)])
            for (name, c0, width) in self.PT_SPECS:
                W = wt[gi % 2]; wtok = f'ip_w{gi % 2}'; gi += 1
                pg.dma('pool', W[:, :, 0:width], w_in[:, :, c0:c0 + width], w=[(wtok, 0)])
                wtoks = [(wtok, 0)]
                dst = self.dr['pt_' + name].ap().rearrange("(n j p) c -> n p j c", p=P, j=4)
                for t in range(NT):
                    b = bank % 8; bank += 1
                    slot = t % 4
                    if slot == 0:
                        ST = stT[nst % 2]; sttok = f'ip_st{nst % 2}'; nst += 1
                    def mm(E, W=W, t=t, b=b, width=width):
                        ins = None
                        for k in range(KC):
                            ins = E.matmul(ps[:, b * 512:b * 512 + width], hT[:, k, t * P:(t + 1) * P],
                                           W[:, k, 0:width], start=(k == 0), stop=(k == KC - 1))
                        return ins
                    pg.op('pe', mm, r=wtoks + hT_toks, w=[('ip_ps', b)])
                    o_ap = ST[:, slot, 0:width]
                    i_ap = ps[:, b * 512:b * 512 + width]
                    if ev % 2 == 0:
                        pg.op('act', lambda E, o=o_ap, a=i_ap: E.copy(o, a),
                              r=[('ip_ps', b)], w=[(sttok, slot)])
                    else:
                        pg.op('dve', lambda E, o=o_ap, a=i_ap: E.tensor_copy(o, a),
                              r=[('ip_ps', b)], w=[(sttok, slot)])
                    ev += 1
                    if slot == 3:
                        pg.dma('sp', dst[t // 4][:, :, 0:width], ST[:, :, 0:width],
                               r=[(sttok, s_) for s_ in range(4)],
                               w=[('pt_' + name, t // 4)])


    def attn_phase(self, l):
        nc, pg = self.nc, self.pg
        with ExitStack() as st:
            q = self.sb(st, 'at_q', [P, 4, S], BF16)
            kd = self.sb(st, 'at_k', [P, 2, S], BF16)
            va = self.sb(st, 'at_v', [P, NT, 2, 65], BF16)
            bias = self.sb(st, 'at_bias', [P, 8, 3, P], F32)
            esink = self.sb(st, 'at_esink', [P, 8], F32)
            yst = self.sb(st, 'at_yst', [P, 4, S], BF16)
            tmp = [self.sb(st, f'at_tmp{i}', [P, 3 * P], F32) for i in range(2)]
            pT = [self.sb(st, f'at_pT{i}', [P, 3 * P], BF16) for i in range(2)]
            den = [self.sb(st, f'at_den{i}', [P, 8], F32) for i in range(2)]
            y = [self.sb(st, f'at_y{i}', [P, 8, 64], BF16) for i in range(2)]
            ps_s = self.psum(st, 'at_ps_s', [P, 2, 512], F32)
            ps_o = self.psum(st, 'at_ps_o', [P, 2, 2, 512], F32)
            ps_t = self.psum(st, 'at_ps_t', [P, 2, 512], BF16)
            pg.dma('sp', q[:], self.dr['pf_aq'].ap().rearrange("(c p) t -> p c t", p=P),
                   r=[('pf_aq', j) for j in range(4)], w=['at_q'])
            pg.dma('sp', kd[:], self.dr['pf_akd'].ap().rearrange("(c p) t -> p c t", p=P),
                   r=[('pf_akd', j) for j in range(2)], w=['at_k'])
            pg.op('pool', lambda E: E.memset(va[:], 1.0), w=['at_v'])
            avv = self.dr['pt_av'].ap().rearrange("(n p) c -> p n c", p=P)
            for kv in range(2):
                pg.dma('sp', va[:, :, kv, 0:64], avv[:, :, kv * 64:(kv + 1) * 64],
                       r=[('pt_av', j) for j in range(8)], w=['at_v'])
            pg.dma('sp', bias[:].rearrange("p a b c -> p (a b c)"), self.I('c_attn_bias'), w=['at_bias'])
            pg.dma('sp', esink[:], self.I('attn_sink')[l].partition_broadcast(P), w=['at_esink'])
            pg.op('act', lambda E: E.activation(esink[:], esink[:], AF.Exp), r=['at_esink'], w=['at_esink'])
            it = 0
            for n in range(NT):
                ob = n % 2
                js = [j for j in range(3) if 0 <= n + j - 1 < NT]
                c0, c1 = js[0] * P, (js[-1] + 1) * P
                for h in range(8):
                    kv = h // 4
                    r0 = (h % 2) * 64
                    sb_ = it % 2; it += 1
                    def mm(E, h=h, kv=kv, r0=r0, sb_=sb_, n=n, js=js):
                        ins = None
                        for j in js:
                            kb = n + j - 1
                            ins = E.matmul(ps_s[:, sb_, j * P:(j + 1) * P],
                                           kd[r0:r0 + 64, kv, kb * P:(kb + 1) * P],
                                           q[r0:r0 + 64, h // 2, n * P:(n + 1) * P],
                                           start=True, stop=True)
                        return ins
                    pg.op('pe', mm, r=['at_q', 'at_k'], w=[('at_ps_s', sb_)])
                    T, PT = tmp[sb_], pT[sb_]
                    pg.op('dve', lambda E, T=T, sb_=sb_, h=h, c0=c0, c1=c1: E.scalar_tensor_tensor(
                        T[:, c0:c1], ps_s[:, sb_, c0:c1], 0.125,
                        bias[:, h, :, :].rearrange("p a b -> p (a b)")[:, c0:c1], ALU.mult, ALU.add),
                        r=[('at_ps_s', sb_), 'at_bias'], w=[('at_tmp', sb_)])
                    pg.op('act', lambda E, T=T, PT=PT, c0=c0, c1=c1: E.activation(PT[:, c0:c1], T[:, c0:c1], AF.Exp),
                          r=[('at_tmp', sb_)], w=[('at_pT', sb_)])
                    def pv(E, h=h, kv=kv, PT=PT, n=n, js=js, ob=ob):
                        ins = None
                        for idx, j in enumerate(js):
                            kb = n + j - 1
                            ins = E.matmul(ps_o[:, ob, h // 4, (h % 4) * 65:(h % 4) * 65 + 65],
                                           PT[:, j * P:(j + 1) * P], va[:, kb, kv, :],
                                           start=(idx == 0), stop=(idx == len(js) - 1))
                        return ins
                    pg.op('pe', pv, r=[('at_pT', sb_), 'at_v'], w=[('at_ps_o', ob, h)])
                DEN, Y = den[ob], y[ob]
                po = ps_o[:, ob, :, 0:260].rearrange("p b (h e) -> p b h e", e=65)
                pg.op('dve', lambda E, DEN=DEN, po=po: E.tensor_tensor(
                    DEN[:].rearrange("p (b h) -> p b h", b=2), po[:, :, :, 64],
                    esink[:].rearrange("p (b h) -> p b h", b=2), ALU.add),
                    r=[('at_ps_o', ob, h) for h in range(8)] + ['at_esink'], w=[('at_den', ob)])
                pg.op('dve', lambda E, DEN=DEN: E.reciprocal(DEN[:], DEN[:]),
                      r=[('at_den', ob)], w=[('at_den', ob)])
                pg.op('dve', lambda E, DEN=DEN, Y=Y, po=po: E.tensor_tensor(
                    Y[:].rearrange("p (b h) d -> p b h d", b=2), po[:, :, :, 0:64],
                    DEN[:].rearrange("p (b h) -> p b h", b=2).unsqueeze(3).to_broadcast([P, 2, 4, 64]),
                    ALU.mult),
                    r=[('at_ps_o', ob, h) for h in range(8)] + [('at_den', ob)], w=[('at_y', ob)])
                self.transpose_out(Y[:].rearrange("p h d -> p (h d)"), ('at_y', ob), ps_t, 'at_ps_t',
                                   yst, 'at_yst', n, ob)
            self.store_yT(yst, 'at_yst', 0)

    def transpose_out(self, Yflat, ytok, ps_t, pstok, yst, ysttok, n, ob, rows=P):
        pg = self.pg
        def tr(E):
            ins = None
            for c in range(4):
                ins = E.transpose(ps_t[:, ob, c * P:(c + 1) * P], Yflat[:, c * P:(c + 1) * P], self.ident_b[:])
            return ins
        pg.op('pe', tr, r=[ytok, 'ident_b'], w=[(pstok, ob)])
        pg.op('act', lambda E: E.copy(yst[:, :, n * P:(n + 1) * P],
                                      ps_t[:, ob, :].rearrange("p (c t) -> p c t", c=4)),
              r=[(pstok, ob)], w=[(ysttok, n)])

    def store_yT(self, yst, ysttok, row0):
        dst = self.dr['yT_dram'].ap()
        for c in range(4):
            self.pg.dma('sp', dst[row0 + c * P: row0 + (c + 1) * P, :], yst[:, c, :],
                        r=[(ysttok, n) for n in range(NT)], w=[('yT_dram', row0 // P + c)])


    def load_chan(self, dst2d, src1d, wtok):
        self.pg.dma('sp', dst2d, src1d.rearrange("(c p) -> p c", p=P), w=[wtok],
                    allow_slow_non_contiguous=True)

    def conv_phase(self, l):
        nc, pg = self.nc, self.pg
        with ExitStack() as st:
            cw = self.sb(st, 'cv_w', [P, 3, 4], F32)
            for wi in range(3):
                self.load_chan(cw[:, wi, :], self.I('conv_w')[l, wi], ('cv_w', wi))
            cwt = [('cv_w', wi) for wi in range(3)]
            U = [self.sb(st, f'cv_u{i}', [P, S + 2], F32) for i in range(2)]
            A = [self.sb(st, f'cv_a{i}', [P, S], F32) for i in range(2)]
            cb = [self.sb(st, f'cv_b{i}', [P, S], BF16) for i in range(2)]
            cc = [self.sb(st, f'cv_c{i}', [P, S], BF16) for i in range(2)]
            ch = [self.sb(st, f'cv_h{i}', [P, S], BF16) for i in range(2)]
            yo = [self.sb(st, f'cv_y{i}', [P, S], BF16) for i in range(2)]
            for i in range(2):
                pg.op('pool', lambda E, i=i: E.memset(U[i][:], 0.0), w=[('cv_u', i)])
            for c in range(4):
                i = c % 2
                rows = slice(c * P, (c + 1) * P)
                pg.dma('sp', cb[i][:], self.dr['pf_cb'].ap()[rows, :], r=[('pf_cb', c)], w=[('cv_b', i)])
                pg.dma('sp', cc[i][:], self.dr['pf_cc'].ap()[rows, :], r=[('pf_cc', c)], w=[('cv_c', i)])
                pg.dma('sp', ch[i][:], self.dr['pf_ch'].ap()[rows, :], r=[('pf_ch', c)], w=[('cv_h', i)])
                pg.op('pool', lambda E, i=i: E.tensor_tensor(U[i][:, 1:S + 1], cc[i][:], ch[i][:], ALU.mult),
                      r=[('cv_c', i), ('cv_h', i)], w=[('cv_u', i)])
                pg.op('dve', lambda E, i=i, c=c: E.tensor_scalar(A[i][:], U[i][:, 1:S + 1], cw[:, 1, c:c + 1], None, ALU.mult),
                      r=[('cv_u', i)] + cwt, w=[('cv_a', i)])
                pg.op('dve', lambda E, i=i, c=c: E.scalar_tensor_tensor(A[i][:], U[i][:, 0:S], cw[:, 0, c:c + 1], A[i][:], ALU.mult, ALU.add),
                      r=[('cv_u', i), ('cv_a', i)] + cwt, w=[('cv_a', i)])
                pg.op('dve', lambda E, i=i, c=c: E.scalar_tensor_tensor(A[i][:], U[i][:, 2:S + 2], cw[:, 2, c:c + 1], A[i][:], ALU.mult, ALU.add),
                      r=[('cv_u', i), ('cv_a', i)] + cwt, w=[('cv_a', i)])
                pg.op('pool', lambda E, i=i: E.tensor_tensor(yo[i][:], A[i][:], cb[i][:], ALU.mult),
                      r=[('cv_a', i), ('cv_b', i)], w=[('cv_y', i)])
                pg.dma('sp', self.dr['yT_dram'].ap()[512 + c * P: 512 + (c + 1) * P, :], yo[i][:],
                       r=[('cv_y', i)], w=[('yT_dram', 4 + c)])

    def alloc_norm(self, st, pfx, rows, nslot=1):
        d = {}
        d['sq'] = self.sb(st, pfx + '_sq', [rows, nslot * 512], F32)
        d['on'] = self.sb(st, pfx + '_on', [rows, nslot * 512], F32)
        d['e'] = self.sb(st, pfx + '_e', [rows, nslot * 512], F32)
        d['st'] = self.sb(st, pfx + '_st', [rows, 6, nslot * 4], F32)
        d['y'] = [self.sb(st, pfx + f'_y{i}', [rows, nslot * 512], BF16) for i in range(2)]
        d['pfx'] = pfx
        return d

    def norm_gate(self, d, po, potoks, G, gtok, NG, ngtok, mode, yi, rows, nh):
        pg = self.pg
        pfx = d['pfx']
        W = nh * 128
        sq, on, e, stt = d['sq'][:, 0:W], d['on'][:, 0:W], d['e'][:, 0:W], d['st']
        Y = d['y'][yi][:, 0:W]
        v3 = lambda ap: ap.rearrange("p (h v) -> p h v", v=128)
        tk = lambda s: (pfx, s)
        ss, sm, mean, var, rstd, msq = [stt[:, i, 0:nh] for i in range(6)]
        pg.op('act', lambda E: E.activation(sq, po, AF.Square), r=potoks, w=[tk('sq')])
        pg.op('dve', lambda E: E.tensor_reduce(ss, v3(sq), AX.X, ALU.add), r=[tk('sq')], w=[tk('ss')])
        if mode == 'gn':
            pg.op('dve', lambda E: E.tensor_reduce(sm, v3(po), AX.X, ALU.add), r=potoks, w=[tk('sm')])
            pg.op('dve', lambda E: E.tensor_scalar(mean, sm, 1.0 / 128, None, ALU.mult), r=[tk('sm')], w=[tk('mean')])
            pg.op('dve', lambda E: E.tensor_tensor(msq, mean, mean, ALU.mult), r=[tk('mean')], w=[tk('msq')])
            pg.op('dve', lambda E: E.scalar_tensor_tensor(var, ss, 1.0 / 128, msq, ALU.mult, ALU.subtract),
                  r=[tk('ss'), tk('msq')], w=[tk('var')])
        else:
            pg.op('dve', lambda E: E.tensor_scalar(var, ss, 1.0 / 128, None, ALU.mult), r=[tk('ss')], w=[tk('var')])
        pg.op('act', lambda E: E.activation(rstd, var, AF.Ln, bias=HN_EPS, scale=1.0), r=[tk('var')], w=[tk('rstd')])
        pg.op('act', lambda E: E.activation(rstd, rstd, AF.Exp, scale=-0.5), r=[tk('rstd')], w=[tk('rstd')])
        bc = lambda ap: ap.unsqueeze(2).to_broadcast([rows, nh, 128])
        if mode == 'gn':
            pg.op('dve', lambda E: E.tensor_tensor(v3(on), v3(po), bc(mean), ALU.subtract),
                  r=potoks + [tk('mean')], w=[tk('on')])
            pg.op('dve', lambda E: E.tensor_tensor(v3(on), v3(on), bc(rstd), ALU.mult),
                  r=[tk('on'), tk('rstd')], w=[tk('on')])
        else:
            pg.op('dve', lambda E: E.tensor_tensor(v3(on), v3(po), bc(rstd), ALU.mult),
                  r=potoks + [tk('rstd')], w=[tk('on')])
        pg.op('pool', lambda E: E.tensor_tensor(on, on, NG, ALU.mult), r=[tk('on'), ngtok], w=[tk('on')])
        pg.op('act', lambda E: E.activation(e, G, AF.Exp, scale=-1.0), r=[gtok], w=[tk('e')])
        pg.op('pool', lambda E: E.tensor_scalar(e, e, 1.0, None, ALU.add), r=[tk('e')], w=[tk('e')])
        pg.op('dve', lambda E: E.reciprocal(e, e), r=[tk('e')], w=[tk('e')])
        pg.op('pool', lambda E: E.tensor_tensor(e, e, G, ALU.mult), r=[tk('e'), gtok], w=[tk('e')])
        pg.op('pool', lambda E: E.tensor_tensor(Y, on, e, ALU.mult), r=[tk('on'), tk('e')], w=[(pfx + '_y', yi)])
        return d['y'][yi]

    def ret_phase(self, l):
        nc, pg = self.nc, self.pg
        SC = 128.0 ** -0.5
        with ExitStack() as st:
            cst = self.sb(st, 'rt_c', [P, 5, P], F32)
            vec = self.sb(st, 'rt_vec', [P, 2, P], F32)
            pcol = self.sb(st, 'rt_pcol', [P, 2], F32)
            lg = self.sb(st, 'rt_lg', [P, 8], F32)
            GL = self.sb(st, 'rt_GL', [P, 8], F32)
            vd = self.sb(st, 'rt_vd', [P, 8], F32)
            DT = self.sb(st, 'rt_DT', [P, 4, P], F32)
            tmpD = self.sb(st, 'rt_tmpD', [P, P], F32)
            dec = self.sb(st, 'rt_dec', [P, 2, 4, P], F32)
            NG = self.sb(st, 'rt_ng', [P, 512], F32)
            prevF = self.sb(st, 'rt_prevF', [P, NT, 512], BF16)
            yst = self.sb(st, 'rt_yst', [P, 4, S], BF16)
            Fs = self.sb(st, 'rt_F', [P, 512], F32)
            Bs = self.sb(st, 'rt_B', [P, 512], F32)
            tmpS = self.sb(st, 'rt_tmpS', [P, 512], F32)
            pB = [self.sb(st, f'rt_pB{i}', [P, 512], BF16) for i in range(2)]
            Kt = [self.sb(st, f'rt_Kt{i}', [P, 512], BF16) for i in range(2)]
            Vt = [self.sb(st, f'rt_Vt{i}', [P, 512], BF16) for i in range(2)]
            Gt = [self.sb(st, f'rt_Gt{i}', [P, 512], BF16) for i in range(2)]
            Qf = [self.sb(st, f'rt_Qf{i}', [P, 4, P], BF16) for i in range(2)]
            Kf = [self.sb(st, f'rt_Kf{i}', [P, 4, P], BF16) for i in range(2)]
            vS = [self.sb(st, f'rt_vS{i}', [P, 512], BF16) for i in range(2)]
            aTm = [self.sb(st, f'rt_aTm{i}', [P, 512], BF16) for i in range(2)]
            qF = [self.sb(st, f'rt_qF{i}', [P, 4, P], BF16) for i in range(2)]
            qB = [self.sb(st, f'rt_qB{i}', [P, 4, P], BF16) for i in range(2)]
            nd = self.alloc_norm(st, 'rt_n', P)
            ps_a = self.psum(st, 'rt_ps_a', [P, 2, 512], F32)
            ps_o = self.psum(st, 'rt_ps_o', [P, 2, 512], F32)
            ps_kv = self.psum(st, 'rt_ps_kv', [P, 2, 512], F32)
            ps_t = self.psum(st, 'rt_ps_t', [P, 2, 512], BF16)
            pg.dma('sp', cst[:].rearrange("p a b -> p (a b)"), self.I('c_ret_c'), w=['rt_c'])
            pg.dma('sp', vec[:].rearrange("p a b -> p (a b)"), self.I('c_ret_vec'), w=['rt_vec'])
            pg.dma('sp', pcol[:], self.I('c_ret_pcol'), w=['rt_pcol'])
            pg.dma('sp', NG[:], self.I('ret_norm_g')[l].partition_broadcast(P), w=['rt_ng'])
            pg.dma('sp', lg[:], self.I('ret_decay_logit')[l].rearrange("a b -> (a b)").partition_broadcast(P), w=['rt_lg'])
            pg.op('act', lambda E: E.activation(lg[:], lg[:], AF.Exp, scale=-1.0), r=['rt_lg'], w=['rt_lg'])
            pg.op('dve', lambda E: E.tensor_scalar(lg[:], lg[:], 1.0, None, ALU.add), r=['rt_lg'], w=['rt_lg'])
            pg.op('act', lambda E: E.activation(lg[:], lg[:], AF.Ln), r=['rt_lg'], w=['rt_lg'])
            pg.op('dve', lambda E: E.tensor_scalar(lg[:], lg[:], -1.0, None, ALU.mult), r=['rt_lg'], w=['rt_lg'])
            pg.op('act', lambda E: E.activation(GL[:], lg[:], AF.Exp, scale=128.0), r=['rt_lg'], w=['rt_GL'])
            lnsc = float(np.log(SC))
            for h in range(4):
                pg.op('act', lambda E, h=h: E.activation(vd[:, h:h + 1], pcol[:, 0:1], AF.Exp, scale=lg[:, h:h + 1], bias=lnsc),
                      r=['rt_lg', 'rt_pcol'], w=[('rt_vd', h)])
                pg.op('act', lambda E, h=h: E.activation(vd[:, 4 + h:5 + h], pcol[:, 1:2], AF.Exp, scale=lg[:, 4 + h:5 + h], bias=lnsc),
                      r=['rt_lg', 'rt_pcol'], w=[('rt_vd', 4 + h)])
                pg.op('act', lambda E, h=h: E.activation(dec[:, 0, h, :], vec[:, 0, :], AF.Exp, scale=lg[:, h:h + 1]),
                      r=['rt_lg', 'rt_vec'], w=[('rt_dec', 0, h)])
                pg.op('act', lambda E, h=h: E.activation(dec[:, 1, h, :], vec[:, 1, :], AF.Exp, scale=lg[:, 4 + h:5 + h]),
                      r=['rt_lg', 'rt_vec'], w=[('rt_dec', 1, h)])
                pg.op('act', lambda E, h=h: E.activation(DT[:, h, :], cst[:, 0, :], AF.Exp, scale=lg[:, h:h + 1]),
                      r=['rt_lg', 'rt_c'], w=[('rt_DT', h)])
                pg.op('dve', lambda E, h=h: E.tensor_tensor(DT[:, h, :], DT[:, h, :], cst[:, 2, :], ALU.mult),
                      r=[('rt_DT', h), 'rt_c'], w=[('rt_DT', h)])
                pg.op('act', lambda E, h=h: E.activation(tmpD[:], cst[:, 1, :], AF.Exp, scale=lg[:, 4 + h:5 + h]),
                      r=['rt_lg', 'rt_c'], w=['rt_tmpD'])
                pg.op('dve', lambda E: E.tensor_tensor(tmpD[:], tmpD[:], cst[:, 3, :], ALU.mult),
                      r=['rt_tmpD', 'rt_c'], w=['rt_tmpD'])
                pg.op('dve', lambda E, h=h: E.tensor_tensor(DT[:, h, :], DT[:, h, :], tmpD[:], ALU.add),
                      r=[('rt_DT', h), 'rt_tmpD'], w=[('rt_DT', h)])
                pg.op('dve', lambda E, h=h: E.tensor_tensor(DT[:, h, :], DT[:, h, :], cst[:, 4, :], ALU.add),
                      r=[('rt_DT', h), 'rt_c'], w=[('rt_DT', h)])
                pg.op('dve', lambda E, h=h: E.tensor_scalar(DT[:, h, :], DT[:, h, :], SC, None, ALU.mult),
                      r=[('rt_DT', h)], w=[('rt_DT', h)])
            DTt = [('rt_DT', h) for h in range(4)]
            vdt = [('rt_vd', h) for h in range(8)]
            dect = [('rt_dec', a, h) for a in range(2) for h in range(4)]
            pg.op('pool', lambda E: E.memset(Fs[:], 0.0), w=['rt_F'])
            pg.op('pool', lambda E: E.memset(Bs[:], 0.0), w=['rt_B'])
            ktv = self.dr['pt_rkt'].ap().rearrange("(n p) c -> n p c", p=P)
            vtv = self.dr['pt_rv'].ap().rearrange("(n p) c -> n p c", p=P)
            gtv = self.dr['pt_rg'].ap().rearrange("(n p) c -> n p c", p=P)
            qfv = self.dr['pf_rq'].ap().rearrange("(h p) t -> p h t", p=P)
            kfv = self.dr['pf_rk'].ap().rearrange("(h p) t -> p h t", p=P)
            v3 = lambda ap: ap.rearrange("p (h v) -> p h v", v=128)
            bc4 = lambda ap: ap.unsqueeze(2).to_broadcast([P, 4, 128])
            it = 0

            def kv_step(n, i, K, V, vdcols, state, stok, GLcols):
                VS = vS[i]
                pg.op('pool', lambda E: E.tensor_tensor(v3(VS[:]), v3(V[:]), bc4(vdcols), ALU.mult),
                      r=[('rt_Vt', i)] + vdt, w=[('rt_vS', i)])
                def mm(E):
                    ins = None
                    for h in range(4):
                        ins = E.matmul(ps_kv[:, i, h * P:(h + 1) * P], K[:, h * P:(h + 1) * P], VS[:, h * P:(h + 1) * P],
                                       start=True, stop=True)
                    return ins
                pg.op('pe', mm, r=[('rt_Kt', i), ('rt_vS', i)], w=[('rt_ps_kv', i)])
                pg.op('pool', lambda E: E.tensor_tensor(v3(tmpS[:]), v3(state[:]), bc4(GLcols), ALU.mult),
                      r=[stok, 'rt_GL'], w=['rt_tmpS'])
                pg.op('dve', lambda E: E.tensor_tensor(state[:], tmpS[:], ps_kv[:, i, :], ALU.add),
                      r=['rt_tmpS', ('rt_ps_kv', i)], w=[stok])

            for n in range(NT):
                i = it % 2; it += 1
                pg.dma('sp', Kt[i][:], ktv[n], r=[('pt_rkt', n // 4)], w=[('rt_Kt', i)])
                pg.dma('sp', Vt[i][:], vtv[n], r=[('pt_rv', n // 4)], w=[('rt_Vt', i)])
                pg.op('act', lambda E, n=n: E.copy(prevF[:, n, :], Fs[:]), r=['rt_F'], w=[('rt_prevF', n)])
                kv_step(n, i, Kt[i], Vt[i], vd[:, 0:4], Fs, 'rt_F', GL[:, 0:4])
            for n in range(NT - 1, -1, -1):
                i = it % 2; it += 1
                tsl = slice(n * P, (n + 1) * P)
                pg.dma('sp', Kt[i][:], ktv[n], r=[('pt_rkt', n // 4)], w=[('rt_Kt', i)])
                pg.dma('sp', Vt[i][:], vtv[n], r=[('pt_rv', n // 4)], w=[('rt_Vt', i)])
                pg.dma('sp', Gt[i][:], gtv[n], r=[('pt_rg', n // 4)], w=[('rt_Gt', i)])
                pg.dma('sp', Qf[i][:], qfv[:, :, tsl], r=[('pf_rq', h) for h in range(4)], w=[('rt_Qf', i)])
                pg.dma('sp', Kf[i][:], kfv[:, :, tsl], r=[('pf_rk', h) for h in range(4)], w=[('rt_Kf', i)])
                def mma(E, i=i):
                    ins = None
                    for h in range(4):
                        ins = E.matmul(ps_a[:, i, h * P:(h + 1) * P], Kf[i][:, h, :], Qf[i][:, h, :], start=True, stop=True)
                    return ins
                pg.op('pe', mma, r=[('rt_Qf', i), ('rt_Kf', i)], w=[('rt_ps_a', i)])
                pg.op('dve', lambda E, i=i: E.tensor_tensor(aTm[i][:], ps_a[:, i, :], DT[:].rearrange("p h t -> p (h t)"), ALU.mult),
                      r=[('rt_ps_a', i)] + DTt, w=[('rt_aTm', i)])
                pg.op('pool', lambda E, i=i: E.tensor_tensor(qF[i][:], Qf[i][:], dec[:, 0, :, :], ALU.mult),
                      r=[('rt_Qf', i)] + dect, w=[('rt_qF', i)])
                pg.op('pool', lambda E, i=i: E.tensor_tensor(qB[i][:], Qf[i][:], dec[:, 1, :, :], ALU.mult),
                      r=[('rt_Qf', i)] + dect, w=[('rt_qB', i)])
                pg.op('act', lambda E, i=i: E.copy(pB[i][:], Bs[:]), r=['rt_B'], w=[('rt_pB', i)])
                def mmo(E, i=i, n=n):
                    ins = None
                    for h in range(4):
                        hs = slice(h * P, (h + 1) * P)
                        E.matmul(ps_o[:, i, hs], aTm[i][:, hs], Vt[i][:, hs], start=True, stop=False)
                        E.matmul(ps_o[:, i, hs], qF[i][:, h, :], prevF[:, n, hs], start=False, stop=False)
                        ins = E.matmul(ps_o[:, i, hs], qB[i][:, h, :], pB[i][:, hs], start=False, stop=True)
                    return ins
                pg.op('pe', mmo, r=[('rt_aTm', i), ('rt_Vt', i), ('rt_qF', i), ('rt_qB', i), ('rt_prevF', n), ('rt_pB', i)],
                      w=[('rt_ps_o', i)])
                kv_step(n, i, Kt[i], Vt[i], vd[:, 4:8], Bs, 'rt_B', GL[:, 4:8])
                Y = self.norm_gate(nd, ps_o[:, i, :], [('rt_ps_o', i)], Gt[i][:], ('rt_Gt', i), NG[:], 'rt_ng',
                                   'gn', i, P, 4)
                self.transpose_out(Y[:], ('rt_n_y', i), ps_t, 'rt_ps_t', yst, 'rt_yst', n, i)
            self.store_yT(yst, 'rt_yst', 1536)


    def hgrn_phase(self, l):
        nc, pg = self.nc, self.pg
        NCH = S // 64
        with ExitStack() as outer:
            DEC = self.sb(outer, 'hg_DEC', [P, 2, 3, 4, NCH], F32)
            dect = [('hg_DEC', d_, hd) for d_ in range(2) for hd in range(4)]
            with ExitStack() as st:
                lb = self.sb(st, 'hg_lb', [P, 4], F32)
                oml = self.sb(st, 'hg_oml', [P, 4], F32)
                a0 = self.sb(st, 'hg_a0', [P, 4], F32)
                cm = self.sb(st, 'hg_cm', [P, S], BF16)
                T = [self.sb(st, f'hg_T{i}', [P, S], F32) for i in range(4)]
                tmpd = self.sb(st, 'hg_tmpd', [P, NCH], F32)
                zb = [self.sb(st, f'hg_z{i}', [P, S], BF16) for i in range(2)]
                qb = [self.sb(st, f'hg_qin{i}', [P, S], BF16) for i in range(2)]
                qo = [self.sb(st, f'hg_qo{i}', [P, S], BF16) for i in range(2)]
                ko = [self.sb(st, f'hg_ko{i}', [P, S], BF16) for i in range(2)]
                pg.dma('sp', cm[:], self.I('c_hg_cm'), w=['hg_cm'])
                if l == 0:
                    pg.op('pool', lambda E: E.memset(lb[:], 0.0), w=['hg_lb'])
                else:
                    self.load_chan(a0[:], self.I('hgrn_lb')[0], 'hg_a0')
                    self.load_chan(lb[:], self.I('hgrn_lb')[1], 'hg_lb')
                    pg.op('dve', lambda E: E.tensor_tensor(lb[:], a0[:], lb[:], ALU.subtract), r=['hg_a0', 'hg_lb'], w=['hg_lb'])
                    pg.op('act', lambda E: E.activation(lb[:], lb[:], AF.Exp), r=['hg_lb'], w=['hg_lb'])
                    pg.op('dve', lambda E: E.tensor_scalar(lb[:], lb[:], 1.0, None, ALU.add), r=['hg_lb'], w=['hg_lb'])
                    pg.op('dve', lambda E: E.reciprocal(lb[:], lb[:]), r=['hg_lb'], w=['hg_lb'])
                pg.op('dve', lambda E: E.tensor_scalar(oml[:], lb[:], -1.0, 1.0, ALU.mult, ALU.add), r=['hg_lb'], w=['hg_oml'])
                it = 0
                for d_ in range(2):
                    zname = 'pf_gzf' if d_ == 0 else 'pf_gzb'
                    mid, last = (31, 63) if d_ == 0 else (32, 0)
                    for hd in range(4):
                        i = it % 2; it += 1
                        rows = slice(hd * P, (hd + 1) * P)
                        T1, T2, T3, T4 = T
                        pg.dma('sp', zb[i][:], self.dr[zname].ap()[rows, :], r=[(zname, hd)], w=[('hg_z', i)])
                        pg.dma('sp', qb[i][:], self.dr['pf_gq'].ap()[rows, :], r=[('pf_gq', hd)], w=[('hg_qin', i)])
                        pg.op('act', lambda E, i=i: E.activation(T1[:], zb[i][:], AF.Exp, scale=-1.0), r=[('hg_z', i)], w=['hg_T1'])
                        pg.op('pool', lambda E: E.tensor_scalar(T1[:], T1[:], 1.0, None, ALU.add), r=['hg_T1'], w=['hg_T1'])
                        pg.op('dve', lambda E: E.reciprocal(T1[:], T1[:]), r=['hg_T1'], w=['hg_T1'])
                        pg.op('dve', lambda E, hd=hd: E.tensor_scalar(T1[:], T1[:], oml[:, hd:hd + 1], lb[:, hd:hd + 1], ALU.mult, ALU.add),
                              r=['hg_T1', 'hg_lb', 'hg_oml'], w=['hg_T1'])
                        pg.op('act', lambda E: E.activation(T2[:], T1[:], AF.Ln), r=['hg_T1'], w=['hg_T2'])
                        pg.op('dve', lambda E: E.tensor_tensor_scan(T3[:], cm[:], T2[:], 0.0, ALU.mult, ALU.add),
                              r=['hg_cm', 'hg_T2'], w=['hg_T3'])
                        c3 = lambda ap: ap.rearrange("p (n c) -> p n c", c=64)
                        if d_ == 0:
                            Bt, Btok = T3, 'hg_T3'
                        else:
                            pg.op('dve', lambda E: E.tensor_tensor(c3(T4[:]), c3(T3[:])[:, :, 63:64].to_broadcast([P, NCH, 64]),
                                                                   c3(T3[:]), ALU.subtract), r=['hg_T3'], w=['hg_T4'])
                            pg.op('pool', lambda E: E.tensor_tensor(T4[:], T4[:], T2[:], ALU.add), r=['hg_T4', 'hg_T2'], w=['hg_T4'])
                            Bt, Btok = T4, 'hg_T4'
                        B3 = c3(Bt[:])
                        dk = ('hg_DEC', d_, hd)
                        pg.op('act', lambda E, B3=B3, d_=d_, hd=hd, last=last: E.activation(DEC[:, d_, 0, hd, :], B3[:, :, last], AF.Exp),
                              r=[Btok], w=[dk])
                        pg.op('act', lambda E, B3=B3, d_=d_, hd=hd, mid=mid: E.activation(DEC[:, d_, 1, hd, :], B3[:, :, mid], AF.Exp),
                              r=[Btok], w=[dk])
                        pg.op('dve', lambda E, B3=B3, mid=mid, last=last: E.tensor_tensor(tmpd[:], B3[:, :, last], B3[:, :, mid], ALU.subtract),
                              r=[Btok], w=['hg_tmpd'])
                        pg.op('act', lambda E, d_=d_, hd=hd: E.activation(DEC[:, d_, 2, hd, :], tmpd[:], AF.Exp),
                              r=['hg_tmpd'], w=[dk])
                        pg.op('dve', lambda E, B3=B3, mid=mid: E.tensor_tensor(c3(T2[:]), B3, B3[:, :, mid:mid + 1].to_broadcast([P, NCH, 64]),
                                                                               ALU.subtract), r=[Btok, 'hg_T2'], w=['hg_T2'])
                        EP, EPtok = (T4, 'hg_T4') if d_ == 0 else (T3, 'hg_T3')
                        pg.op('act', lambda E, EP=EP: E.activation(EP[:], T2[:], AF.Exp), r=['hg_T2', Btok], w=[EPtok])
                        pg.op('pool', lambda E, i=i, EP=EP: E.tensor_tensor(qo[i][:], qb[i][:], EP[:], ALU.mult),
                              r=[('hg_qin', i), EPtok], w=[('hg_qo', i)])
                        pg.dma('sp', self.dr[f'hg_q{d_}'].ap()[rows, :], qo[i][:], r=[('hg_qo', i)], w=[(f'hg_q{d_}', hd)])
                        EM, EMtok = (T3, 'hg_T3') if d_ == 0 else (T4, 'hg_T4')
                        pg.op('act', lambda E, EM=EM: E.activation(EM[:], T2[:], AF.Exp, scale=-1.0), r=['hg_T2', EPtok, ('hg_qo', i)], w=[EMtok])
                        pg.op('pool', lambda E: E.tensor_scalar(T1[:], T1[:], -1.0, 1.0, ALU.mult, ALU.add), r=['hg_T1'], w=['hg_T1'])
                        pg.op('dve', lambda E, i=i, EM=EM: E.tensor_tensor(ko[i][:], T1[:], EM[:], ALU.mult),
                              r=['hg_T1', EMtok], w=[('hg_ko', i)])
                        pg.dma('sp', self.dr[f'hg_k{d_}'].ap()[rows, :], ko[i][:], r=[('hg_ko', i)], w=[(f'hg_k{d_}', hd)])
            pg.barrier()
            v3 = lambda ap: ap.rearrange("p (h v) -> p h v", v=128)
            with ExitStack() as st:
                Sst = self.sb(st, 'hs_S', [P, 512], F32)
                t2 = self.sb(st, 'hs_t2', [P, 512], F32)
                Sbf = [self.sb(st, f'hs_Sbf{i}', [P, 512], BF16) for i in range(2)]
                kblk = [self.sb(st, f'hs_kb{i}', [P, 4, P], BF16) for i in range(2)]
                ktok = [self.sb(st, f'hs_kt{i}', [P, 512], BF16) for i in range(2)]
                gi = [self.sb(st, f'hs_gi{i}', [P, 512], BF16) for i in range(2)]
                ps_t = self.psum(st, 'hs_ps_t', [P, 2, 512], BF16)
                ps_kv = self.psum(st, 'hs_ps_kv', [P, 2, 512], F32)
                giv = self.dr['pt_gi'].ap().rearrange("(n p) c -> n p c", p=P)
                it = 0
                ic = 0
                for d_ in range(2):
                    kv_ = self.dr[f'hg_k{d_}'].ap().rearrange("(h p) t -> p h t", p=P)
                    Sd = self.dr[f'hg_S{d_}'].ap()
                    pg.op('pool', lambda E: E.memset(Sst[:], 0.0), r=[], w=['hs_S'])
                    order = range(NT) if d_ == 0 else range(NT - 1, -1, -1)
                    order = list(order)
                    def hs_load(tt, i):
                        pg.dma('sp', kblk[i][:], kv_[:, :, tt * P:(tt + 1) * P], r=[(f'hg_k{d_}', hd) for hd in range(4)], w=[('hs_kb', i)])
                        pg.dma('sp', gi[i][:], giv[tt], r=[('pt_gi', tt // 4)], w=[('hs_gi', i)])
                    hs_load(order[0], it % 2)
                    for oi, tt in enumerate(order):
                        i = it % 2; it += 1
                        if oi + 1 < len(order):
                            hs_load(order[oi + 1], it % 2)
                        def tr(E, i=i):
                            ins = None
                            for h in range(4):
                                ins = E.transpose(ps_t[:, i, h * P:(h + 1) * P], kblk[i][:, h, :], self.ident_b[:])
                            return ins
                        pg.op('pe', tr, r=[('hs_kb', i), 'ident_b'], w=[('hs_ps_t', i)])
                        pg.op('act', lambda E, i=i: E.copy(ktok[i][:], ps_t[:, i, :]), r=[('hs_ps_t', i)], w=[('hs_kt', i)])
                        for c in ((0, 1) if d_ == 0 else (1, 0)):
                            n = tt * 2 + c
                            j = ic % 2; ic += 1
                            r0 = c * 64
                            def mm(E, i=i, j=j, r0=r0):
                                ins = None
                                for h in range(4):
                                    hs = slice(h * P, (h + 1) * P)
                                    ins = E.matmul(ps_kv[:, j, hs], ktok[i][r0:r0 + 64, hs], gi[i][r0:r0 + 64, hs], start=True, stop=True)
                                return ins
                            pg.op('pe', mm, r=[('hs_kt', i), ('hs_gi', i)], w=[('hs_ps_kv', j)])
                            dbc = lambda kind, n=n, d_=d_: DEC[:, d_, kind, :, n].unsqueeze(2).to_broadcast([P, 4, 128])
                            pg.op('pool', lambda E, j=j, dbc=dbc: E.tensor_tensor(v3(Sbf[j][:]), v3(Sst[:]), dbc(1), ALU.mult),
                                  r=['hs_S'] + dect, w=[('hs_Sbf', j)])
                            pg.dma('sp', Sd[n], Sbf[j][:], r=[('hs_Sbf', j)], w=[(f'hg_S{d_}', n)])
                            pg.op('dve', lambda E, j=j, dbc=dbc: E.tensor_tensor(v3(t2[:]), v3(ps_kv[:, j, :]), dbc(2), ALU.mult),
                                  r=[('hs_ps_kv', j)] + dect, w=['hs_t2'])
                            pg.op('pool', lambda E, dbc=dbc: E.tensor_tensor(v3(Sst[:]), v3(Sst[:]), dbc(0), ALU.mult),
                                  r=['hs_S'] + dect, w=['hs_S'])
                            pg.op('dve', lambda E: E.tensor_tensor(Sst[:], Sst[:], t2[:], ALU.add), r=['hs_S', 'hs_t2'], w=['hs_S'])
            pg.barrier()
            with ExitStack() as st:
                H = 64
                mask = self.sb(st, 'ho_mask', [H, 2, 64], F32)
                NG = self.sb(st, 'ho_ng', [H, 2, 512], F32)
                yst = self.sb(st, 'ho_yst', [P, 4, S], BF16)
                blk = {}
                for nm in ('q0', 'k0', 'q1', 'k1'):
                    blk[nm] = [self.sb(st, f'ho_{nm}_{i}', [P, 4, P], BF16) for i in range(2)]
                gi = [self.sb(st, f'ho_gi{i}', [H, 2, 512], BF16) for i in range(2)]
                go = [self.sb(st, f'ho_go{i}', [H, 2, 512], BF16) for i in range(2)]
                Sb = [[self.sb(st, f'ho_S{d_}_{i}', [P, 2, 512], BF16) for i in range(2)] for d_ in range(2)]
                aTm = [self.sb(st, f'ho_aTm{i}', [H, 2, 2, 4, 64], BF16) for i in range(2)]
                nd = self.alloc_norm(st, 'ho_n', H, nslot=2)
                ps_a = self.psum(st, 'ho_ps_a', [H, 2, 2, 4, 64], F32)
                ps_o = self.psum(st, 'ho_ps_o', [H, 2, 2, 512], F32)
                ps_t = self.psum(st, 'ho_ps_t', [P, 2, 512], BF16)
                pg.dma('sp', mask[:].rearrange("p a b -> p (a b)"), self.I('c_hg_mask'), w=['ho_mask'])
                for c in range(2):
                    pg.dma('sp', NG[:, c, :], self.I('hgrn_norm_g')[l].partition_broadcast(H), w=[('ho_ng', c)])
                ngt = [('ho_ng', c) for c in range(2)]
                giv = self.dr['pt_gi'].ap().rearrange("(n c p) v -> n p c v", p=H, c=2)
                gov = self.dr['pt_go'].ap().rearrange("(n c p) v -> n p c v", p=H, c=2)
                fm = {nm: self.dr['hg_' + nm].ap().rearrange("(h p) t -> p h t", p=P) for nm in blk}
                Sv = [self.dr[f'hg_S{d_}'].ap().rearrange("(n c) p v -> n p c v", c=2) for d_ in range(2)]
                for tt in range(NT):
                    i = tt % 2
                    tsl = slice(tt * P, (tt + 1) * P)
                    for nm in blk:
                        pg.dma('sp', blk[nm][i][:], fm[nm][:, :, tsl], r=[('hg_' + nm, hd) for hd in range(4)], w=[('ho_' + nm, i)])
                    pg.dma('sp', gi[i][:], giv[tt], r=[('pt_gi', tt // 4)], w=[('ho_gi', i)])
                    pg.dma('sp', go[i][:], gov[tt], r=[('pt_go', tt // 4)], w=[('ho_go', i)])
                    for d_ in range(2):
                        pg.dma('sp', Sb[d_][i][:], Sv[d_][tt], r=[(f'hg_S{d_}', 2 * tt), (f'hg_S{d_}', 2 * tt + 1)], w=[('ho_S', d_, i)])
                    def mma(E, i=i):
                        ins = None
                        for c in range(2):
                            cs = slice(c * 64, (c + 1) * 64)
                            for d_ in range(2):
                                for h in range(4):
                                    ins = E.matmul(ps_a[:, c, d_, h, :], blk[f'k{d_}'][i][:, h, cs], blk[f'q{d_}'][i][:, h, cs],
                                                   start=True, stop=True)
                        return ins
                    pg.op('pe', mma, r=[('ho_' + nm, i) for nm in blk], w=['ho_ps_a'])
                    for c in range(2):
                        pg.op('dve', lambda E, i=i, c=c: E.tensor_tensor(
                            aTm[i][:, c].rearrange("p d h t -> p d h t"), ps_a[:, c],
                            mask[:].unsqueeze(2).to_broadcast([H, 2, 4, 64]), ALU.mult),
                            r=['ho_ps_a', 'ho_mask'], w=[('ho_aTm', i, c)])
                    def mmo(E, i=i):
                        ins = None
                        for c in range(2):
                            cs = slice(c * 64, (c + 1) * 64)
                            for h in range(4):
                                hs = slice(h * P, (h + 1) * P)
                                o = ps_o[:, i, c, hs]
                                E.matmul(o, aTm[i][:, c, 0, h, :], gi[i][:, c, hs], start=True, stop=False)
                                E.matmul(o, aTm[i][:, c, 1, h, :], gi[i][:, c, hs], start=False, stop=False)
                                E.matmul(o, blk['q0'][i][:, h, cs], Sb[0][i][:, c, hs], start=False, stop=False)
                                ins = E.matmul(o, blk['q1'][i][:, h, cs], Sb[1][i][:, c, hs], start=False, stop=True)
                        return ins
                    pg.op('pe', mmo, r=[('ho_aTm', i, 0), ('ho_aTm', i, 1), ('ho_gi', i), ('ho_q0', i), ('ho_q1', i),
                                        ('ho_S', 0, i), ('ho_S', 1, i)], w=[('ho_ps_o', i)])
                    Y = self.norm_gate(nd, ps_o[:, i].rearrange("p c v -> p (c v)"), [('ho_ps_o', i)],
                                       go[i][:].rearrange("p c v -> p (c v)"), ('ho_go', i),
                                       NG[:].rearrange("p c v -> p (c v)"), ngt[0], 'rms', i, H, 8)
                    def tr(E, i=i, Y=Y):
                        ins = None
                        for c in range(2):
                            for cc in range(4):
                                ins = E.transpose(ps_t[:, i, cc * P + c * 64: cc * P + (c + 1) * 64],
                                                  Y[:, c * 512 + cc * P: c * 512 + (cc + 1) * P], self.ident_b[0:H, 0:H])
                        return ins
                    pg.op('pe', tr, r=[('ho_n_y', i), 'ident_b'], w=[('ho_ps_t', i)])
                    pg.op('act', lambda E, i=i, tsl=tsl: E.copy(yst[:, :, tsl], ps_t[:, i, :].rearrange("p (c t) -> p c t", c=4)),
                          r=[('ho_ps_t', i)], w=[('ho_yst', tt)])
                self.store_yT(yst, 'ho_yst', 1024)


    def outproj_phase(self, l):
        nc, pg = self.nc, self.pg
        with ExitStack() as st:
            W = self.sb(st, 'op_w', [P, KC, D], BF16)
            wv = self.I('w_out')[l].rearrange("(k p) n -> p k n", p=P)
            for j in range(4):
                pg.dma('pool', W[:, :, j * 512:(j + 1) * 512], wv[:, :, j * 512:(j + 1) * 512], w=[('op_w', j)])
            wt = [('op_w', j) for j in range(4)]
            yT = [self.sb(st, f'op_y{i}', [P, KC, P], BF16) for i in range(2)]
            so = [self.sb(st, f'op_o{i}', [P, D], F32) for i in range(2)]
            ps = self.psum(st, 'op_ps', [P, 2, 4, 512], F32)
            yv = self.dr['yT_dram'].ap().rearrange("(k p) t -> p k t", p=P)
            rv = self.dr['res_dram'].ap().rearrange("(n p) d -> n p d", p=P)
            pg.dma('sp', yT[0][:], yv[:, :, 0:P], r=[('yT_dram', c) for c in range(KC)], w=[('op_y', 0)])
            for t in range(NT):
                i = t % 2
                if t + 1 < NT:
                    pg.dma('sp', yT[(t + 1) % 2][:], yv[:, :, (t + 1) * P:(t + 2) * P], r=[('yT_dram', c) for c in range(KC)], w=[('op_y', (t + 1) % 2)])
                for j in range(4):
                    def mm(E, i=i, j=j):
                        ins = None
                        for k in range(KC):
                            ins = E.matmul(ps[:, i, j, :], yT[i][:, k, :], W[:, k, j * 512:(j + 1) * 512],
                                           start=(k == 0), stop=(k == KC - 1))
                        return ins
                    pg.op('pe', mm, r=[('op_y', i)] + wt, w=[('op_ps', i, j)])
                    o_ap = so[i][:, j * 512:(j + 1) * 512]
                    if j % 2 == 0:
                        pg.op('act', lambda E, o=o_ap, a=ps[:, i, j, :]: E.copy(o, a), r=[('op_ps', i, j)], w=[('op_o', i, j)])
                    else:
                        pg.op('dve', lambda E, o=o_ap, a=ps[:, i, j, :]: E.tensor_copy(o, a), r=[('op_ps', i, j)], w=[('op_o', i, j)])
                pg.dma('sp', rv[t], so[i][:], r=[('op_o', i, j) for j in range(4)], w=[('res_dram', t)])

    def moe_phase(self, l):
        nc, pg = self.nc, self.pg
        TBS = 1024
        NTB = S // TBS
        TPB = TBS // P
        with ExitStack() as st:
            hTb = self.sb(st, 'mo_h', [P, KC, TBS], BF16)
            acc = self.sb(st, 'mo_acc', [P, TPB, D], F32)
            hid = self.sb(st, 'mo_hid', [P, 8, TBS], BF16)
            wg = [self.sb(st, f'mo_wg{i}', [P, KC, 256], BF16) for i in range(2)]
            wu = [self.sb(st, f'mo_wu{i}', [P, KC, 256], BF16) for i in range(2)]
            wd = [self.sb(st, f'mo_wd{i}', [P, 8, 512], BF16) for i in range(2)]
            sg = [self.sb(st, f'mo_sg{i}', [P, 512], F32) for i in range(2)]
            ps_gu = self.psum(st, 'mo_ps_gu', [P, 2, 2, 512], F32)
            ps_d = self.psum(st, 'mo_ps_d', [P, 3, 512], F32)
            hTv = self.dr['hT_dram'].ap().rearrange("(k p) t -> p k t", p=P)
            rv = self.dr['res_dram'].ap().rearrange("(n j p) d -> n p j d", p=P, j=TPB)
            iw = 0; idw = 0; igu = 0; ipd = 0
            for tb in range(NTB):
                for k in range(KC):
                    pg.dma('sp', hTb[:, k, :], hTv[:, k, tb * TBS:(tb + 1) * TBS],
                           r=[('hT_dram', tb * 2), ('hT_dram', tb * 2 + 1)], w=[('mo_h', k)])
                ht = [('mo_h', k) for k in range(KC)]
                for e in range(NE):
                    wgv = self.I('w_gate')[l, e].rearrange("(k p) f -> p k f", p=P)
                    wuv = self.I('w_up')[l, e].rearrange("(k p) f -> p k f", p=P)
                    wdv = self.I('w_down')[l, e].rearrange("(c p) n -> p c n", p=P)
                    for hf in range(4):
                        wi = iw % 2; iw += 1
                        pg.dma('pool', wg[wi][:], wgv[:, :, hf * 256:(hf + 1) * 256], w=[('mo_wg', wi)])
                        pg.dma('pool', wu[wi][:], wuv[:, :, hf * 256:(hf + 1) * 256], w=[('mo_wu', wi)])
                        for f2 in range(2):
                            fc = hf * 2 + f2
                            for th in range(TBS // 512):
                                b = igu % 2; igu += 1
                                def mm(E, wi=wi, f2=f2, th=th, b=b):
                                    ins = None
                                    for k in range(KC):
                                        E.matmul(ps_gu[:, b, 0, :], wg[wi][:, k, f2 * P:(f2 + 1) * P], hTb[:, k, th * 512:(th + 1) * 512],
                                                 start=(k == 0), stop=(k == KC - 1))
                                    for k in range(KC):
                                        ins = E.matmul(ps_gu[:, b, 1, :], wu[wi][:, k, f2 * P:(f2 + 1) * P], hTb[:, k, th * 512:(th + 1) * 512],
                                                       start=(k == 0), stop=(k == KC - 1))
                                    return ins
                                pg.op('pe', mm, r=[('mo_wg', wi), ('mo_wu', wi)] + ht, w=[('mo_ps_gu', b)])
                                pg.op('act', lambda E, b=b: E.activation(sg[b][:], ps_gu[:, b, 0, :], AF.Silu),
                                      r=[('mo_ps_gu', b)], w=[('mo_sg', b)])
                                pg.op('dve', lambda E, b=b, fc=fc, th=th: E.tensor_tensor(hid[:, fc, th * 512:(th + 1) * 512], sg[b][:], ps_gu[:, b, 1, :], ALU.mult),
                                      r=[('mo_sg', b), ('mo_ps_gu', b)], w=[('mo_hid', fc, th)])
                    hidt = [('mo_hid', fc, th) for fc in range(8) for th in range(TBS // 512)]
                    for nch in range(4):
                        di = idw % 2; idw += 1
                        pg.dma('pool', wd[di][:], wdv[:, :, nch * 512:(nch + 1) * 512], w=[('mo_wd', di)])
                        for tl in range(TPB):
                            pb = ipd % 3; ipd += 1
                            tg = tb * TPB + tl
                            def mmd(E, di=di, tl=tl, pb=pb):
                                ins = None
                                for fc in range(8):
                                    ins = E.matmul(ps_d[:, pb, :], hid[:, fc, tl * P:(tl + 1) * P], wd[di][:, fc, :],
                                                   start=(fc == 0), stop=(fc == 7))
                                return ins
                            pg.op('pe', mmd, r=hidt + [('mo_wd', di)], w=[('mo_ps_d', pb)])
                            a_ap = acc[:, tl, nch * 512:(nch + 1) * 512]
                            if e == 0:
                                pg.op('dve', lambda E, a=a_ap, pb=pb, tg=tg, e=e: E.tensor_scalar(a, ps_d[:, pb, :], self.comb[:, tg, e:e + 1], None, ALU.mult),
                                      r=[('mo_ps_d', pb), ('comb', tg)], w=[('mo_acc', tl, nch)])
                            else:
                                pg.op('dve', lambda E, a=a_ap, pb=pb, tg=tg, e=e: E.scalar_tensor_tensor(a, ps_d[:, pb, :], self.comb[:, tg, e:e + 1], a, ALU.mult, ALU.add),
                                      r=[('mo_ps_d', pb), ('comb', tg)], w=[('mo_acc', tl, nch)])
                pg.dma('sp', rv[tb], acc[:], r=[('mo_acc', tl, nch) for tl in range(TPB) for nch in range(4)],
                       w=[('res_dram', tb * TPB + tl) for tl in range(TPB)])


    def route_finalize(self):
        pg = self.pg
        with ExitStack() as st:
            ones = self.sb(st, 'rf_ones', [P, P], BF16)
            ust = self.sb(st, 'rf_ust', [P, P], BF16)
            mE = self.sb(st, 'rf_mE', [P, 512], BF16)
            mS = self.sb(st, 'rf_mS', [P, 512], BF16)
            ecap = self.sb(st, 'rf_ecap', [P, 512], F32)
            thr = self.sb(st, 'rf_thr', [P, 512], F32)
            TOT = self.sb(st, 'rf_TOT', [P, NE, NT], F32)
            INC = self.sb(st, 'rf_INC', [P, NE, NT], F32)
            EXC = self.sb(st, 'rf_EXC', [P, NE, NT], F32)
            G = self.sb(st, 'rf_G', [P, NT, NE], F32)
            CE = self.sb(st, 'rf_CE', [P, NT, NE], F32)
            F1 = self.sb(st, 'rf_F1', [P, NT, NE], F32)
            F2 = self.sb(st, 'rf_F2', [P, NT, NE], F32)
            TMP = self.sb(st, 'rf_TMP', [P, NT, NE], F32)
            Df = self.sb(st, 'rf_Df', [P, 2, NT], F32)
            GT = self.sb(st, 'rf_GT', [P, NE, NT], F32)
            ntf = self.sb(st, 'rf_ntf', [P, NE], F32)
            ps = self.psum(st, 'rf_ps', [P, 2, 512], F32)
            for nm, t_ in (('ones_b', ones), ('ustrict_b', ust), ('maskE', mE), ('maskS', mS), ('ecap', ecap), ('thr', thr)):
                pg.dma('sp', t_[:], self.I('c_' + nm), w=['rf_' + nm])
            selt = [('selm', t) for t in range(NT)]
            sflat = self.selm[:].rearrange("p j e -> p (j e)")
            fl = lambda t_: t_[:].rearrange("p a b -> p (a b)")
            pg.op('pe', lambda E: E.matmul(ps[:, 0, :], ones[:], sflat, start=True, stop=True), r=selt + ['rf_ones_b'], w=['rf_ps0'])
            pg.op('pe', lambda E: E.matmul(ps[:, 1, :], ust[:], sflat, start=True, stop=True), r=selt + ['rf_ustrict_b'], w=['rf_ps1'])
            pg.op('dve', lambda E: E.tensor_copy(TOT[:], ps[:, 0, :].rearrange("p (j e) -> p e j", e=NE)), r=['rf_ps0'], w=['rf_TOT'])
            pg.op('dve', lambda E: E.tensor_tensor_scan(fl(INC), mE[:], fl(TOT), 0.0, ALU.mult, ALU.add), r=['rf_TOT', 'rf_maskE'], w=['rf_INC'])
            pg.op('dve', lambda E: E.tensor_tensor(EXC[:], INC[:], TOT[:], ALU.subtract), r=['rf_INC', 'rf_TOT'], w=['rf_EXC'])
            pg.op('dve', lambda E: E.tensor_tensor(G[:], ps[:, 1, :].rearrange("p (j e) -> p j e", e=NE),
                                                   EXC[:].rearrange("p e j -> p j e"), ALU.add), r=['rf_ps1', 'rf_EXC'], w=['rf_G'])
            pg.op('dve', lambda E: E.tensor_tensor(fl(G), fl(G), ecap[:], ALU.add), r=['rf_G', 'rf_ecap'], w=['rf_G'])
            pg.op('dve', lambda E: E.tensor_tensor_scan(fl(CE), mS[:], sflat, 0.0, ALU.mult, ALU.add), r=selt + ['rf_maskS'], w=['rf_CE'])
            for (F, val, k) in ((F1, 1.0, 0), (F2, 2.0, 1)):
                nm = f'rf_F{k}'
                pg.op('dve', lambda E, F=F, val=val: E.tensor_scalar(fl(F), fl(CE), val, None, ALU.is_equal), r=['rf_CE'], w=[nm])
                pg.op('dve', lambda E, F=F: E.tensor_tensor(fl(F), fl(F), sflat, ALU.mult), r=[nm] + selt, w=[nm])
                pg.op('dve', lambda E, F=F: E.tensor_tensor(TMP[:], F[:], G[:], ALU.mult), r=[nm, 'rf_G'], w=['rf_TMP'])
                pg.op('dve', lambda E, k=k: E.tensor_reduce(Df[:, k, :], TMP[:], AX.X, ALU.add), r=['rf_TMP'], w=[('rf_Df', k)])
                Di = self.D0i if k == 0 else self.D1i
                pg.op('dve', lambda E, k=k, Di=Di: E.tensor_copy(Di[:], Df[:, k, :]), r=[('rf_Df', k)], w=[('Di', k)])
                Wk = self.W0 if k == 0 else self.W1
                pg.op('dve', lambda E, F=F: E.tensor_tensor(TMP[:], F[:], self.comb[:], ALU.mult), r=[nm, 'rf_TMP'] + self.comb_toks, w=['rf_TMP'])
                pg.op('dve', lambda E, Wk=Wk: E.tensor_reduce(Wk[:], TMP[:], AX.X, ALU.add), r=['rf_TMP'], w=[('Wk', k)])
            pg.op('dve', lambda E: E.tensor_tensor(GT[:], INC[:, :, NT - 1:NT].to_broadcast([P, NE, NT]), thr[:].rearrange("p (e j) -> p e j", e=NE), ALU.is_gt),
                  r=['rf_INC', 'rf_thr'], w=['rf_GT'])
            pg.op('dve', lambda E: E.tensor_reduce(ntf[:], GT[:], AX.X, ALU.add), r=['rf_GT'], w=['rf_ntf'])
            pg.op('dve', lambda E: E.tensor_copy(self.nti[:], ntf[:]), r=['rf_ntf'], w=['nti'])

    def scatter_phase(self):
        pg = self.pg
        with ExitStack() as st:
            X = [self.sb(st, f'sc_x{i}', [P, D], BF16) for i in range(3)]
            hv = self.dr['hb_dram'].ap().rearrange("(n p) d -> n p d", p=P)
            bk = self.dr['bucket'].ap()
            for j in range(NT):
                i = j % 3
                pg.dma('sp', X[i][:], hv[j], r=[('hb_dram', j)], w=[('sc_x', i)])
                for Di in (self.D0i, self.D1i):
                    pg.dma('pool', None, None, r=[('sc_x', i), ('Di', 0), ('Di', 1)], w=[],
                           fn=lambda E, i=i, j=j, Di=Di: E.indirect_dma_start(
                               out=bk, out_offset=bass.IndirectOffsetOnAxis(Di[:, j:j + 1], 0), in_=X[i][:], in_offset=None))

    def expert_phase(self, l):
        pg = self.pg
        ENG = ['pe', 'act', 'dve', 'sp']
        with ExitStack() as st:
            Wg2 = [self.sb(st, f'ex_wg{i}', [P, KC, DE], BF16) for i in range(2)]
            Wu2 = [self.sb(st, f'ex_wu{i}', [P, KC, DE], BF16) for i in range(2)]
            Wd = self.sb(st, 'ex_wd', [P, 8, D], BF16)
            Xq = [self.sb(st, f'ex_x{i}', [P, D], BF16) for i in range(2)]
            xT = [self.sb(st, f'ex_xT{i}', [P, KC, P], BF16) for i in range(2)]
            sg = self.sb(st, 'ex_sg', [P, DE], F32)
            hid = [self.sb(st, f'ex_hid{i}', [P, DE], BF16) for i in range(2)]
            hidT = [self.sb(st, f'ex_hidT{i}', [P, 8, P], BF16) for i in range(2)]
            Y = [self.sb(st, f'ex_y{i}', [P, D], BF16) for i in range(2)]
            ps_xf = self.psum(st, 'ex_ps_x', [P, 2, 512], F32)
            ps_x = ps_xf[:].rearrange("p a b -> p (a b)").bitcast(BF16)
            ps_gu = self.psum(st, 'ex_ps_gu', [P, 2, 2, 512], F32)
            ps_h = self.psum(st, 'ex_ps_h', [P, 8 * P], BF16)
            ps_d1 = self.psum(st, 'ex_ps_d', [P, 512], F32)
            bk = self.dr['bucket'].ap()
            yb = self.dr['ybucket'].ap()
            it = 0
            for e in range(NE):
                wgv = self.I('w_gate')[l, e].rearrange("(k p) f -> p k f", p=P)
                wuv = self.I('w_up')[l, e].rearrange("(k p) f -> p k f", p=P)
                wdv = self.I('w_down')[l, e].rearrange("(c p) n -> p c n", p=P)
                wb = e % 2
                Wg, Wu = Wg2[wb], Wu2[wb]
                if e < self.wlim: pg.dma('pool', Wg[:], wgv, w=[('ex_wg', wb, 0), ('ex_wg', wb, 1)])
                if e < self.wlim: pg.dma('pool', Wu[:], wuv, w=[('ex_wu', wb, 0), ('ex_wu', wb, 1)])
                for n_ in range(2 if e < self.wlim else 0):
                    pg.dma('pool', Wd[:, :, n_ * 1024:(n_ + 1) * 1024], wdv[:, :, n_ * 1024:(n_ + 1) * 1024],
                           w=[('ex_wd', 2 * n_), ('ex_wd', 2 * n_ + 1)])
                for en in ENG:
                    pg.op(en, lambda E, en=en, e=e: E.reg_load(self.regs[en], self.nti[0:1, e:e + 1]), r=['nti'])
                nslots = self.qlim
                for q in range(nslots):
                    i = it % 2; it += 1
                    row0 = e * CAP + q * P
                    pg.cond_begin(self.regs, q, ENG)
                    if q == 0:
                        pg.dma('sp', Xq[i][:], bk[row0:row0 + P, :], w=[('ex_x', i)])
                    if q + 1 < nslots:
                        pg.dma('sp', Xq[1 - i][:], bk[row0 + P:row0 + 2 * P, :], w=[('ex_x', 1 - i)])
                    def trx(E, i=i):
                        ins = None
                        for k in range(KC):
                            ins = E.transpose(ps_x[:, k * P:(k + 1) * P], Xq[i][:, k * P:(k + 1) * P], self.ident_b[:])
                        return ins
                    pg.op('pe', trx, r=[('ex_x', i), 'ident_b'], w=[('ex_psb', 0), ('ex_psb', 1)])
                    pg.op('act', lambda E, i=i: E.copy(xT[i][:, 0:8, :], ps_x[:, 0:8 * P].rearrange("p (k t) -> p k t", k=8)),
                          r=[('ex_psb', 0)], w=[('ex_xT', i, 0)])
                    pg.op('dve', lambda E, i=i: E.tensor_copy(xT[i][:, 8:16, :], ps_x[:, 8 * P:16 * P].rearrange("p (k t) -> p k t", k=8)),
                          r=[('ex_psb', 1)], w=[('ex_xT', i, 1)])
                    for fh in range(2):
                        for a, W, wn in ((0, Wg, 'ex_wg'), (1, Wu, 'ex_wu')):
                            def mgu(E, i=i, a=a, W=W, fh=fh):
                                ins = None
                                for k in range(KC):
                                    ins = E.matmul(ps_gu[:, a, fh, :], xT[i][:, k, :], W[:, k, fh * 512:(fh + 1) * 512],
                                                   start=(k == 0), stop=(k == KC - 1))
                                return ins
                            pg.op('pe', mgu, r=[('ex_xT', i, 0), ('ex_xT', i, 1), (wn, wb, fh)], w=[('ex_ps_gu', a, fh)])
                        pg.op('act', lambda E, fh=fh: E.activation(sg[:, fh * 512:(fh + 1) * 512], ps_gu[:, 0, fh, :], AF.Silu),
                              r=[('ex_ps_gu', 0, fh)], w=[('ex_sg', fh)])
                        pg.op('dve', lambda E, fh=fh, i=i: E.tensor_tensor(hid[i][:, fh * 512:(fh + 1) * 512], sg[:, fh * 512:(fh + 1) * 512],
                                                                            ps_gu[:, 1, fh, :], ALU.mult),
                              r=[('ex_sg', fh), ('ex_ps_gu', 1, fh)], w=[('ex_hid', i, fh)])
                    def trh(E, i=i):
                        ins = None
                        for fc in range(8):
                            ins = E.transpose(ps_h[:, fc * P:(fc + 1) * P], hid[i][:, fc * P:(fc + 1) * P], self.ident_b[:])
                        return ins
                    pg.op('pe', trh, r=[('ex_hid', i, 0), ('ex_hid', i, 1), 'ident_b'], w=['ex_ps_h'])
                    pg.op('act', lambda E, i=i: E.copy(hidT[i][:], ps_h[:].rearrange("p (c t) -> p c t", c=8)),
                          r=['ex_ps_h'], w=[('ex_hidT', i)])
                    for n_ in range(4):
                        if n_ % 3 == 2:
                            pd, ptok = ps_d1[:], 'ex_ps_d'
                        else:
                            pd, ptok = ps_xf[:, n_ % 3, :], ('ex_psb', n_ % 3)
                        def mmd(E, i=i, n_=n_, pd=pd):
                            ins = None
                            for fc in range(8):
                                ins = E.matmul(pd, hidT[i][:, fc, :], Wd[:, fc, n_ * 512:(n_ + 1) * 512],
                                               start=(fc == 0), stop=(fc == 7))
                            return ins
                        pg.op('pe', mmd, r=[('ex_hidT', i), ('ex_wd', n_)], w=[ptok])
                        o_ap = Y[i][:, n_ * 512:(n_ + 1) * 512]
                        if n_ % 2 == 0:
                            pg.op('act', lambda E, o=o_ap, pd=pd: E.copy(o, pd), r=[ptok], w=[('ex_y', i, n_)])
                        else:
                            pg.op('dve', lambda E, o=o_ap, pd=pd: E.tensor_copy(o, pd), r=[ptok], w=[('ex_y', i, n_)])
                    pg.dma('sp', yb[row0:row0 + P, :], Y[i][:], r=[('ex_y', i, n_) for n_ in range(4)], w=[])
                for q in range(nslots):
                    pg.cond_end()

    def gather_phase(self):
        pg = self.pg
        with ExitStack() as st:
            Y0 = [self.sb(st, f'ga_y0{i}', [P, D], BF16) for i in range(2)]
            Y1 = [self.sb(st, f'ga_y1{i}', [P, D], BF16) for i in range(2)]
            T = [self.sb(st, f'ga_t{i}', [P, D], F32) for i in range(2)]
            yb = self.dr['ybucket'].ap()
            rv = self.dr['res_dram'].ap().rearrange("(n p) d -> n p d", p=P)
            for j in range(NT):
                i = j % 2
                for (Yk, Di, nm) in ((Y0[i], self.D0i, 'ga_y0'), (Y1[i], self.D1i, 'ga_y1')):
                    pg.dma('pool', None, None, r=[('Di', 0), ('Di', 1)], w=[(nm, i)],
                           fn=lambda E, Yk=Yk, Di=Di, j=j: E.indirect_dma_start(
                               out=Yk[:], out_offset=None, in_=yb, in_offset=bass.IndirectOffsetOnAxis(Di[:, j:j + 1], 0)))
                pg.op('dve', lambda E, i=i, j=j: E.tensor_scalar(T[i][:], Y0[i][:], self.W0[:, j:j + 1], None, ALU.mult),
                      r=[('ga_y0', i), ('Wk', 0)], w=[('ga_t', i)])
                pg.op('dve', lambda E, i=i, j=j: E.scalar_tensor_tensor(T[i][:], Y1[i][:], self.W1[:, j:j + 1], T[i][:], ALU.mult, ALU.add),
                      r=[('ga_y1', i), ('Wk', 1), ('ga_t', i)], w=[('ga_t', i)])
                pg.dma('sp', rv[j], T[i][:], r=[('ga_t', i)], w=[('res_dram', j)])


_CACHE = {}


def make_in_map(inputs, core, b):
    m = {}
    for k in b.dr:
        if k.startswith('c_'):
            m[k] = b.consts[k[2:]]
        elif k in inputs:
            v = np.asarray(inputs[k])
            m[k] = np.ascontiguousarray(v[core]) if k == 'x' else np.ascontiguousarray(v)
    return m


def kernel(**inputs):
    b = Builder()
    nc = b.build()
    in_maps = [make_in_map(inputs, c, b) for c in range(8)]
    res = run_bass_kernel_spmd(nc, in_maps, core_ids=list(range(8)))
    return np.stack([np.asarray(r['out']) for r in res.results], axis=0).astype(np.float32)
```
